# Optimizing a Trainium2 kernel written in Bass

```python
import jax
import jax.numpy as jnp
from jax import lax
import numpy as np

D_MODEL = 1024
BATCH = 16
SEQ = 2048
DEPTH = 4

CTX_LEN = 256
GRID_W = 64
N_MOD = 6
EPS = 1e-6
MLA_W = D_MODEL // 2
V_HEAD = 64
N_MLA_HEADS = MLA_W // V_HEAD
QK_NOPE = 64
QK_ROPE = 32
QK_DIM = QK_NOPE + QK_ROPE
Q_LORA = 3 * D_MODEL // 8
KV_LORA = D_MODEL // 4
ROPE_BASE = 10000.0
Q_BLOCK = 128
HGRN_W = D_MODEL // 2
HGRN_K = 128
N_HGRN_HEADS = HGRN_W // HGRN_K
HGRN_V = HGRN_W // N_HGRN_HEADS
CHUNK = 64
IN_SPLITS = (Q_LORA, KV_LORA, QK_ROPE, HGRN_W, HGRN_W, HGRN_W, HGRN_W, HGRN_W)
IN_W = Q_LORA + KV_LORA + QK_ROPE + 5 * HGRN_W
MIX_W = MLA_W + HGRN_W
POOL_WINDOWS = (2, 4, 8, 16)
N_POOL = 4
POOL_C = D_MODEL // N_POOL
D_FF = ((8 * D_MODEL // 3 + 255) // 256) * 256
N_EXPERTS = 8
TOP_K = 2
D_FF_EXPERT = 7 * D_MODEL // 2
N_AB = (DEPTH + 1) // 2
N_C = DEPTH // 2

kernel_name = "hybrid_mla_hgrn2_pool_moe_dit"

F32 = jnp.float32


def rmsnorm(x, g):
    xf = x.astype(F32)
    y = xf * lax.rsqrt(jnp.mean(xf * xf, axis=-1, keepdims=True) + EPS) * g.astype(F32)
    return y.astype(x.dtype)


def modulate(x, shift, scale):
    return x * (1.0 + scale) + shift


def split_cols(p):
    out, start = [], 0
    for w in IN_SPLITS:
        out.append(p[..., start:start + w])
        start += w
    return out


def rope_tables(n_tok):
    rows = n_tok // GRID_W
    row = jnp.repeat(jnp.arange(rows, dtype=F32), GRID_W)
    col = jnp.tile(jnp.arange(GRID_W, dtype=F32), rows)
    half = QK_ROPE // 2
    inv = 1.0 / (ROPE_BASE ** (jnp.arange(0, half, 2, dtype=F32) / half))
    ar = row[:, None] * inv[None, :]
    ac = col[:, None] * inv[None, :]
    ang = jnp.concatenate([ar, ar, ac, ac], axis=-1)
    return jnp.cos(ang)[:, None, :], jnp.sin(ang)[:, None, :]


def rope2d(x, cos, sin):
    xs = x.reshape(x.shape[:-1] + (2, 2, QK_ROPE // 4))
    rot = jnp.stack([-xs[..., 1, :], xs[..., 0, :]], axis=-2).reshape(x.shape)
    return x * cos.astype(x.dtype) + rot * sin.astype(x.dtype)


def mla_q(cq, g_cq, w_uq, cos, sin):
    b, t, _ = cq.shape
    q = (rmsnorm(cq, g_cq) @ w_uq).reshape(b, t, N_MLA_HEADS, QK_DIM)
    q_nope, q_rope = q[..., :QK_NOPE], q[..., QK_NOPE:]
    if cos is not None:
        q_rope = rope2d(q_rope, cos, sin)
    return jnp.concatenate([q_nope, q_rope], axis=-1)


def mla_kv(ckv, kr, g_ckv, w_ukv, cos, sin):
    b, t, _ = ckv.shape
    kv = (rmsnorm(ckv, g_ckv) @ w_ukv).reshape(b, t, N_MLA_HEADS, QK_NOPE + V_HEAD)
    k_nope, v = kv[..., :QK_NOPE], kv[..., QK_NOPE:]
    kr = kr[:, :, None, :]
    if cos is not None:
        kr = rope2d(kr, cos, sin)
    k = jnp.concatenate([k_nope, jnp.broadcast_to(kr, (b, t, N_MLA_HEADS, QK_ROPE))], axis=-1)
    return k, v


def softmax_attend(q, k, v):
    s = jnp.einsum('bqhd,bkhd->bhqk', q, k).astype(F32) * (QK_DIM ** -0.5)
    p = jax.nn.softmax(s, axis=-1).astype(v.dtype)
    return jnp.einsum('bhqk,bkhd->bqhd', p, v)


def blocked_attend(q, k, v):
    b, t, h, dk = q.shape
    nb = t // Q_BLOCK
    qb = q.reshape(b, nb, Q_BLOCK, h, dk).transpose(1, 0, 2, 3, 4)
    o = lax.map(lambda qi: softmax_attend(qi, k, v), qb)
    return o.transpose(1, 0, 2, 3, 4).reshape(b, t, h, v.shape[-1])


def hgrn_heads(a):
    return a.reshape(a.shape[0], a.shape[1], N_HGRN_HEADS, -1)


def hgrn_gates(hf, lb):
    hf = hf.astype(F32)
    k_in = (1.0 - lb) * jax.nn.sigmoid(-hf)
    log_f = jnp.log1p(-k_in)
    return k_in, log_f


def gla_chunked(q, k, v, log_f, s0):
    b, t, h, dk = q.shape
    dv = v.shape[-1]
    n = t // CHUNK

    def to_chunks(a):
        return a.astype(F32).reshape(b, n, CHUNK, h, a.shape[-1]).transpose(1, 0, 3, 2, 4)

    qc, kc, vc, gc = to_chunks(q), to_chunks(k), to_chunks(v), to_chunks(log_f)
    lower = jnp.tril(jnp.ones((CHUNK, CHUNK), dtype=bool))[:, :, None]

    def step(state, inp):
        qi, ki, vi, gi = inp
        g_cum = jnp.cumsum(gi, axis=2)
        o_inter = jnp.einsum('bhlk,bhkv->bhlv', qi * jnp.exp(g_cum), state)
        diff = g_cum[:, :, :, None, :] - g_cum[:, :, None, :, :]
        decay = jnp.where(lower, jnp.exp(jnp.where(lower, diff, 0.0)), 0.0)
        attn = jnp.einsum('bhtk,bhsk,bhtsk->bhts', qi, ki, decay)
        o = o_inter + jnp.einsum('bhts,bhsv->bhtv', attn, vi)
        g_last = g_cum[:, :, -1:, :]
        new_state = (jnp.exp(g_last[:, :, 0, :])[..., None] * state
                     + jnp.einsum('bhlk,bhlv->bhkv', ki * jnp.exp(g_last - g_cum), vi))
        return new_state, o

    s_final, o = lax.scan(step, s0, (qc, kc, vc, gc))
    return o.transpose(1, 0, 3, 2, 4).reshape(b, t, h, dv), s_final


def final_state(k, v, log_f):
    g_cum = jnp.cumsum(log_f, axis=1)
    dec = jnp.exp(g_cum[:, -1:] - g_cum)
    return jnp.einsum('bthk,bthv->bhkv', k * dec, v.astype(F32))


def flip(a):
    return jnp.flip(a, axis=1)


def hgrn_readout(o, hg, g_norm):
    b, t = o.shape[0], o.shape[1]
    y = rmsnorm(o, g_norm) * jax.nn.silu(hgrn_heads(hg).astype(F32))
    return y.reshape(b, t, HGRN_W).astype(hg.dtype)


def mixer_ab(u, uc, w_in, g_cq, g_ckv, w_uq, w_ukv, lb, g_norm, w_out, cos, sin, need_ctx_out):
    b, s, _ = u.shape
    cq, ckv, kr, hq, hff, hfb, hi, hg = split_cols(u @ w_in)
    cq_c, ckv_c, kr_c, hq_c, hff_c, hfb_c, hi_c, hg_c = split_cols(uc @ w_in)

    q = mla_q(cq, g_cq, w_uq, cos, sin)
    k, v = mla_kv(ckv, kr, g_ckv, w_ukv, cos, sin)
    k_c, v_c = mla_kv(ckv_c, kr_c, g_ckv, w_ukv, None, None)
    a_lat = blocked_attend(q, jnp.concatenate([k, k_c], axis=1),
                           jnp.concatenate([v, v_c], axis=1)).reshape(b, s, MLA_W)

    qh = jax.nn.silu(hgrn_heads(hq).astype(F32))
    vh = hgrn_heads(hi).astype(F32)
    kf, lgf = hgrn_gates(hgrn_heads(hff), lb[0])
    kb, lgb = hgrn_gates(hgrn_heads(hfb), lb[1])
    qh_c = jax.nn.silu(hgrn_heads(hq_c).astype(F32))
    vh_c = hgrn_heads(hi_c).astype(F32)
    kf_c, lgf_c = hgrn_gates(hgrn_heads(hff_c), lb[0])
    kb_c, lgb_c = hgrn_gates(hgrn_heads(hfb_c), lb[1])
    zero = jnp.zeros((b, N_HGRN_HEADS, HGRN_K, HGRN_V), F32)
    if need_ctx_out:
        oc_f, sc_f = gla_chunked(qh_c, kf_c, vh_c, lgf_c, zero)
        oc_b, sc_b = gla_chunked(flip(qh_c), flip(kb_c), flip(vh_c), flip(lgb_c), zero)
        oc = oc_f + flip(oc_b)
    else:
        sc_f = final_state(kf_c, vh_c, lgf_c)
        sc_b = final_state(flip(kb_c), flip(vh_c), flip(lgb_c))
    ol_f, _ = gla_chunked(qh, kf, vh, lgf, sc_f)
    ol_b, _ = gla_chunked(flip(qh), flip(kb), flip(vh), flip(lgb), sc_b)
    b_lat = hgrn_readout(ol_f + flip(ol_b), hg, g_norm)

    y = jnp.concatenate([a_lat, b_lat], axis=-1) @ w_out
    yc = None
    if need_ctx_out:
        qc = mla_q(cq_c, g_cq, w_uq, None, None)
        a_ctx = softmax_attend(qc, k_c, v_c).reshape(uc.shape[0], uc.shape[1], MLA_W)
        b_ctx = hgrn_readout(oc, hg_c, g_norm)
        yc = jnp.concatenate([a_ctx, b_ctx], axis=-1) @ w_out
    return y, yc


def pool_mix(u, w, bias, scale):
    b, t, _ = u.shape
    uf = u.astype(F32)
    cs = jnp.pad(jnp.cumsum(uf, axis=1), ((0, 0), (1, 0), (0, 0))).reshape(b, t + 1, N_POOL, POOL_C)
    pos = jnp.arange(t)[:, None]
    win = jnp.array(POOL_WINDOWS, dtype=jnp.int32)[None, :]
    lo = jnp.clip(pos - win // 2, 0, t)
    hi = jnp.clip(pos - win // 2 + win, 0, t)
    grp = jnp.arange(N_POOL)[None, :]
    win_sum = cs[:, hi, grp] - cs[:, lo, grp]
    cnt = (hi - lo).astype(F32)[None, :, :, None]
    pooled = (win_sum / cnt - uf.reshape(b, t, N_POOL, POOL_C)).astype(u.dtype)
    y = jnp.einsum('btgc,gcd->btgd', pooled, w) + bias
    return y.reshape(b, t, D_MODEL) * scale


def swiglu(h, wg, wu, wd):
    return (jax.nn.silu(h @ wg) * (h @ wu)) @ wd


def moe(h, router, wg, wu, wd):
    logits = (h @ router).astype(F32)
    top_v, top_i = lax.top_k(logits, TOP_K)
    top_w = jax.nn.softmax(top_v, axis=-1)
    gates = jnp.sum(jax.nn.one_hot(top_i, N_EXPERTS, dtype=F32) * top_w[..., None], axis=-2)
    y = jnp.zeros_like(h)
    for e in range(N_EXPERTS):
        y = y + gates[..., e:e + 1].astype(h.dtype) * swiglu(h, wg[e], wu[e], wd[e])
    return y


def setup_inputs(seed: int = 0) -> dict:
    key = jax.random.key(seed)
    keys = iter(jax.random.split(key, 40))

    def nrm(shape, scale):
        return jax.random.normal(next(keys), shape, F32) * scale

    d = D_MODEL
    return {
        "x": nrm((BATCH, SEQ, d), 1.0),
        "c": nrm((BATCH, d), 1.0),
        "ctx": nrm((BATCH, CTX_LEN, d), 1.0),
        "c_ctx": nrm((d,), 1.0),
        "w_mod": nrm((DEPTH, d, N_MOD * d), 0.5 * d ** -0.5),
        "b_mod": nrm((DEPTH, N_MOD * d), 0.02),
        "norm_g": 1.0 + nrm((DEPTH, 2, d), 0.1),
        "final_g": 1.0 + nrm((d,), 0.1),
        "ab_w_in": nrm((N_AB, d, IN_W), d ** -0.5),
        "ab_g_cq": 1.0 + nrm((N_AB, Q_LORA), 0.1),
        "ab_g_ckv": 1.0 + nrm((N_AB, KV_LORA), 0.1),
        "ab_w_uq": nrm((N_AB, Q_LORA, N_MLA_HEADS * QK_DIM), Q_LORA ** -0.5),
        "ab_w_ukv": nrm((N_AB, KV_LORA, N_MLA_HEADS * (QK_NOPE + V_HEAD)), KV_LORA ** -0.5),
        "hgrn_lb_logits": nrm((N_AB, 2, HGRN_W), 0.5),
        "hgrn_g_norm": 1.0 + nrm((N_AB, HGRN_V), 0.1),
        "ab_w_out": nrm((N_AB, MIX_W, d), MIX_W ** -0.5),
        "ffn_w_gate": nrm((N_AB, d, D_FF), d ** -0.5),
        "ffn_w_up": nrm((N_AB, d, D_FF), d ** -0.5),
        "ffn_w_down": nrm((N_AB, D_FF, d), D_FF ** -0.5),
        "pool_w": nrm((N_C, N_POOL, POOL_C, POOL_C), POOL_C ** -0.5),
        "pool_b": nrm((N_C, N_POOL, POOL_C), 0.02),
        "pool_scale": 1.0 + nrm((N_C, d), 0.1),
        "moe_router": nrm((N_C, d, N_EXPERTS), d ** -0.5),
        "moe_w_gate": nrm((N_C, N_EXPERTS, d, D_FF_EXPERT), d ** -0.5),
        "moe_w_up": nrm((N_C, N_EXPERTS, d, D_FF_EXPERT), d ** -0.5),
        "moe_w_down": nrm((N_C, N_EXPERTS, D_FF_EXPERT, d), D_FF_EXPERT ** -0.5),
    }


def reference(x, c, ctx, c_ctx, w_mod, b_mod, norm_g, final_g, ab_w_in, ab_g_cq, ab_g_ckv,
              ab_w_uq, ab_w_ukv, hgrn_lb_logits, hgrn_g_norm, ab_w_out, ffn_w_gate, ffn_w_up,
              ffn_w_down, pool_w, pool_b, pool_scale, moe_router, moe_w_gate, moe_w_up, moe_w_down):
    seq = x.shape[1]
    cos, sin = rope_tables(seq)
    lb_p = jax.nn.softmax(hgrn_lb_logits.astype(F32), axis=0)
    lb_all = (jnp.cumsum(lb_p, axis=0) - lb_p[:1]).reshape(N_AB, 2, N_HGRN_HEADS, HGRN_K)
    silu_c = jax.nn.silu(c)
    silu_cc = jax.nn.silu(c_ctx)
    h, hc = x, ctx
    for l in range(DEPTH):
        j = l // 2
        even = l % 2 == 0
        ctx_later = any(m % 2 == 0 for m in range(l + 1, DEPTH))
        sh1, sc1, g1, sh2, sc2, g2 = jnp.split((silu_c @ w_mod[l] + b_mod[l])[:, None, :], N_MOD, axis=-1)
        u = modulate(rmsnorm(h, norm_g[l, 0]), sh1, sc1)
        uc = None
        if even or ctx_later:
            csh1, csc1, cg1, csh2, csc2, cg2 = jnp.split(
                (silu_cc @ w_mod[l] + b_mod[l])[None, None, :], N_MOD, axis=-1)
            uc = modulate(rmsnorm(hc, norm_g[l, 0]), csh1, csc1)
        if even:
            y, yc = mixer_ab(u, uc, ab_w_in[j], ab_g_cq[j], ab_g_ckv[j], ab_w_uq[j], ab_w_ukv[j],
                             lb_all[j], hgrn_g_norm[j], ab_w_out[j], cos, sin, ctx_later)
        else:
            y = pool_mix(u, pool_w[j], pool_b[j], pool_scale[j])
            yc = pool_mix(uc, pool_w[j], pool_b[j], pool_scale[j]) if ctx_later else None
        h = h + g1 * y
        v = modulate(rmsnorm(h, norm_g[l, 1]), sh2, sc2)
        if even:
            h = h + g2 * swiglu(v, ffn_w_gate[j], ffn_w_up[j], ffn_w_down[j])
        else:
            h = h + g2 * moe(v, moe_router[j], moe_w_gate[j], moe_w_up[j], moe_w_down[j])
        if ctx_later:
            hc = hc + cg1 * yc
            vc = modulate(rmsnorm(hc, norm_g[l, 1]), csh2, csc2)
            if even:
                hc = hc + cg2 * swiglu(vc, ffn_w_gate[j], ffn_w_up[j], ffn_w_down[j])
            else:
                hc = hc + cg2 * moe(vc, moe_router[j], moe_w_gate[j], moe_w_up[j], moe_w_down[j])
    return rmsnorm(h, final_g)
```

```python
import contextlib
import numpy as np
import concourse.bass as bass
import concourse.mybir as mybir
from concourse.bass_utils import run_bass_kernel_spmd

F32 = mybir.dt.float32
BF16 = mybir.dt.bfloat16
AF = mybir.ActivationFunctionType
ALU = mybir.AluOpType
AX = mybir.AxisListType

EPS = 1e-6
POOL_WINDOWS = (2, 4, 8, 16)


class Buf:
    def __init__(self, name, t):
        self.name = name
        self.t = t
        self.w = []
        self.r = []
        self.dkey = None

    def __getitem__(self, idx):
        return self.t[idx]


class Prog:
    ENG = ("pe", "dve", "act", "pool", "sp")

    def __init__(self, nc, n_dsem=40):
        self.nc = nc
        self.e = {"pe": nc.tensor, "dve": nc.vector, "act": nc.scalar, "pool": nc.gpsimd, "sp": nc.sync}
        self.sems = {}
        self.cnt = {}
        for k in self.ENG:
            self.sems[k] = nc.alloc_semaphore("s_" + k)
            self.cnt[k] = 0
        self.waited = {k: {} for k in self.ENG}
        self.dfree = []
        for i in range(n_dsem):
            key = ("d", i)
            self.sems[key] = nc.alloc_semaphore("d_%d" % i)
            self.cnt[key] = 0
            self.dfree.append(key)
        self.stage_bufs = []
        self.uid = 0
        self.in_names = []

    def _name(self, name):
        self.uid += 1
        return "%s_%d" % (name, self.uid)

    def sb(self, stack, name, shape, dt):
        t = stack.enter_context(self.nc.sbuf_tensor(self._name(name), list(shape), dt))
        b = Buf(name, t)
        self.stage_bufs.append(b)
        return b

    def ps(self, stack, name, shape, dt=F32):
        t = stack.enter_context(self.nc.psum_tensor(self._name(name), list(shape), dt))
        b = Buf(name, t)
        b.excl = True
        self.stage_bufs.append(b)
        return b

    def dram(self, name, shape, dt, kind="Internal"):
        if kind == "ExternalInput":
            self.in_names.append(name)
        t = self.nc.dram_tensor(name, list(shape), dt, kind=kind)
        b = Buf(name, t)
        b.persistent = True
        return b

    def _dkey(self, b):
        if b.dkey is None:
            b.dkey = self.dfree.pop()
            if not getattr(b, "persistent", False):
                pass
            self.stage_bufs.append(b) if b not in self.stage_bufs else None
        return b.dkey

    def _wait(self, eng, events):
        wd = self.waited[eng]
        need = {}
        for (k, v) in events:
            if need.get(k, 0) < v:
                need[k] = v
        for k, v in need.items():
            if k == "pe" and eng == "pe":
                continue
            if wd.get(k, 0) >= v:
                continue
            self.e[eng].wait_ge(self.sems[k], v)
            wd[k] = v

    @staticmethod
    def _compact(evs):
        m = {}
        for k, v in evs:
            if m.get(k, 0) < v:
                m[k] = v
        return list(m.items())

    def op(self, eng, fn, reads=(), writes=()):
        reads = [b for b in reads if b is not None]
        ev = []
        for b in reads:
            ev += b.w
            if getattr(b, "excl", False):
                ev += [e for e in b.r if e[0] != eng]
        for b in writes:
            ev += b.w
            ev += b.r
        self._wait(eng, ev)
        inst = fn()
        self.cnt[eng] += 1
        inst.then_inc(self.sems[eng], 1)
        me = (eng, self.cnt[eng])
        for b in writes:
            b.w = [me]
            b.r = []
        for b in reads:
            if b not in writes:
                b.r.append(me)
                if len(b.r) > 16:
                    b.r = self._compact(b.r)
        return inst

    def dma(self, q, out_ap, in_ap, src, dst, **kw):
        key = self._dkey(dst)
        ev = list(src.w) + list(dst.r) + [e for e in dst.w if e[0] != key]
        self._wait(q, ev)
        inst = self.e[q].dma_start(out=out_ap, in_=in_ap, **kw)
        self.cnt[key] += 16
        inst.then_inc(self.sems[key], 16)
        me = (key, self.cnt[key])
        dst.w = [me]
        dst.r = []
        src.r.append(me)
        if len(src.r) > 16:
            src.r = self._compact(src.r)
        return inst

    def barrier(self):
        allev = [(k, v) for k, v in self.cnt.items() if v > 0]
        for eng in self.ENG:
            self._wait(eng, allev)
        for b in self.stage_bufs:
            if b.dkey is not None:
                self.dfree.append(b.dkey)
                b.dkey = None
            b.w = []
            b.r = []
        self.stage_bufs = []

    def mm(self, ps, out_ap, lhsT, rhs, rd, start=True, stop=True, **kw):
        return self.op("pe", lambda: self.nc.tensor.matmul(out_ap, lhsT, rhs, start=start, stop=stop, **kw),
                       reads=rd, writes=[ps])

    def act(self, out_ap, in_ap, func, rd, wr, **kw):
        return self.op("act", lambda: self.nc.scalar.activation(out=out_ap, in_=in_ap, func=func, **kw),
                       reads=rd, writes=wr)

    def tt(self, eng, out_ap, a, b, op, rd, wr):
        return self.op(eng, lambda: self.e[eng].tensor_tensor(out=out_ap, in0=a, in1=b, op=op), reads=rd, writes=wr)

    def ts(self, eng, out_ap, a, s1, s2, op0, op1, rd, wr):
        if s2 is None:
            return self.op(eng, lambda: self.e[eng].tensor_scalar(out=out_ap, in0=a, scalar1=s1, scalar2=None, op0=op0),
                           reads=rd, writes=wr)
        return self.op(eng, lambda: self.e[eng].tensor_scalar(out=out_ap, in0=a, scalar1=s1, scalar2=s2, op0=op0, op1=op1),
                       reads=rd, writes=wr)

    def stt(self, out_ap, a, s, b, op0, op1, rd, wr):
        return self.op("dve", lambda: self.nc.vector.scalar_tensor_tensor(out=out_ap, in0=a, scalar=s, in1=b, op0=op0, op1=op1),
                       reads=rd, writes=wr)

    def copy(self, eng, out_ap, in_ap, rd, wr):
        if eng == "act":
            return self.op("act", lambda: self.nc.scalar.copy(out_ap, in_ap), reads=rd, writes=wr)
        return self.op(eng, lambda: self.e[eng].tensor_copy(out_ap, in_ap), reads=rd, writes=wr)

    def memset(self, eng, ap, val, wr):
        return self.op(eng, lambda: self.e[eng].memset(ap, val), writes=wr)


class Cfg:
    def __init__(self, T=2048, C=256, BL=2, layers=(0, 1, 2, 3), grid_w=64):
        self.D = 1024
        self.KC = 8
        self.T = T
        self.C = C
        self.TT = T + C
        self.BL = BL
        self.NB = BL + 1
        self.layers = tuple(layers)
        self.grid_w = grid_w
        self.DFF = 2816
        self.DFFE = 3584
        self.NE = 8
        self.blocks = []
        for s in range(0, T, 512):
            self.blocks.append((s, min(512, T - s), False))
        for s in range(0, C, 512):
            self.blocks.append((T + s, min(512, C - s), True))
        self.lat_blocks = [b for b in self.blocks if not b[2]]
        self.ctx_blocks = [b for b in self.blocks if b[2]]


def ctx_later(l):
    return any(m % 2 == 0 for m in range(l + 1, 4))


def vec_layout():
    off = {}
    n = 0

    def add(name, k):
        nonlocal n
        off[name] = (n, k)
        n += k

    for l in range(4):
        add("b_mod%d" % l, 48)
        add("ng%d_0" % l, 8)
        add("ng%d_1" % l, 8)
    add("final_g", 8)
    for j in range(2):
        add("g_cq%d" % j, 3)
        add("g_ckv%d" % j, 2)
        add("lbl%d_0" % j, 4)
        add("lbl%d_1" % j, 4)
        add("pool_b%d" % j, 8)
        add("pool_s%d" % j, 8)
    return off, n


def build_vecs(inp):
    off, n = vec_layout()
    v = np.zeros((128, n), np.float32)

    def put(name, arr):
        o, k = off[name]
        v[:, o:o + k] = np.asarray(arr, np.float32).reshape(k, 128).T

    for l in range(4):
        put("b_mod%d" % l, inp["b_mod"][l])
        put("ng%d_0" % l, inp["norm_g"][l, 0])
        put("ng%d_1" % l, inp["norm_g"][l, 1])
    put("final_g", inp["final_g"])
    for j in range(2):
        put("g_cq%d" % j, inp["ab_g_cq"][j])
        put("g_ckv%d" % j, inp["ab_g_ckv"][j])
        put("lbl%d_0" % j, inp["hgrn_lb_logits"][j, 0])
        put("lbl%d_1" % j, inp["hgrn_lb_logits"][j, 1])
        put("pool_b%d" % j, inp["pool_b"][j].reshape(-1))
        put("pool_s%d" % j, inp["pool_scale"][j])
    return v


class Builder:
    def __init__(self, cfg):
        self.cfg = cfg
        nc = bass.Bass("TRN2", target_bir_lowering=False)
        self.nc = nc
        self.P = Prog(nc)
        P = self.P
        c = cfg
        self.xT = P.dram("xT", [c.BL, c.D, c.TT], F32, kind="ExternalInput")
        self.cond = P.dram("cond", [128, c.KC, c.NB], F32, kind="ExternalInput")
        self.voff, nv = vec_layout()
        self.vecs_d = P.dram("vecs", [128, nv], F32, kind="ExternalInput")
        self.w_mod = P.dram("w_mod", [4, c.D, 6 * c.D], F32, kind="ExternalInput")
        self.ffn_wg = P.dram("ffn_wg", [2, c.D, c.DFF], F32, kind="ExternalInput")
        self.ffn_wu = P.dram("ffn_wu", [2, c.D, c.DFF], F32, kind="ExternalInput")
        self.ffn_wd = P.dram("ffn_wd", [2, c.DFF, c.D], F32, kind="ExternalInput")
        if getattr(c, "no_moe", False):
            self.moe_wg = P.dram("moe_wg", [2, 1, 8, 8], F32, kind="ExternalInput")
            self.moe_wu = P.dram("moe_wu", [2, 1, 8, 8], F32, kind="ExternalInput")
            self.moe_wd = P.dram("moe_wd", [2, 1, 8, 8], F32, kind="ExternalInput")
        else:
            self.moe_wg = P.dram("moe_wg", [2, c.NE, c.D, c.DFFE], F32, kind="ExternalInput")
            self.moe_wu = P.dram("moe_wu", [2, c.NE, c.D, c.DFFE], F32, kind="ExternalInput")
            self.moe_wd = P.dram("moe_wd", [2, c.NE, c.DFFE, c.D], F32, kind="ExternalInput")
        self.router = P.dram("router", [2, 128, c.KC, c.NE], F32, kind="ExternalInput")
        self.pool_w = P.dram("pool_w", [2, 4, 256, 256], F32, kind="ExternalInput")
        self.invcnt = P.dram("invcnt", [2, 4, max(c.T, c.C)], F32, kind="ExternalInput")
        self.w_in = P.dram("w_in", [2, c.D, 3264], F32, kind="ExternalInput")
        self.w_uq = P.dram("w_uq", [2, 384, 1024], F32, kind="ExternalInput")
        self.w_uk = P.dram("w_uk", [2, 256, 1024], F32, kind="ExternalInput")
        self.w_uv = P.dram("w_uv", [2, 256, 512], F32, kind="ExternalInput")
        self.w_out = P.dram("w_out", [2, 1024, c.D], F32, kind="ExternalInput")
        self.gnorm = P.dram("gnorm", [2, 128], F32, kind="ExternalInput")
        self.rope = P.dram("rope", [2, 32, c.TT], F32, kind="ExternalInput")
        self.trimask = P.dram("trimask", [2, 128, 128], F32, kind="ExternalInput")
        self.outT = P.dram("outT", [c.BL, c.D, c.T], F32, kind="ExternalOutput")
        self.cqn_d = P.dram("cqn_d", [384, c.TT], BF16)
        self.ckvn_d = P.dram("ckvn_d", [256, c.TT], BF16)
        self.krr_d = P.dram("krr_d", [32, c.TT], BF16)
        self.qh_d = P.dram("qh_d", [512, c.TT], F32)
        self.hf_d = P.dram("hf_d", [2, 512, c.TT], F32)
        self.vg_d = P.dram("vg_d", [c.TT, 512], BF16)
        self.sg_d = P.dram("sg_d", [c.TT, 512], F32)
        self.mix_d = P.dram("mix_d", [1024, c.TT], BF16)
        self.hbuf = [P.dram("hA", [c.BL, c.D, c.TT], F32), P.dram("hB", [c.BL, c.D, c.TT], F32)]
        self.cur = self.xT

        self._in_names = P.in_names
        self.glob = contextlib.ExitStack()
        g = self.glob
        self.vecs = P.sb(g, "vecs", [128, nv], F32)
        self.modT = P.sb(g, "modT", [128, 4, 48, c.NB], F32)
        self.ones_bf = P.sb(g, "ones_bf", [128, 128], BF16)
        self.eps_t = P.sb(g, "eps", [128, 1], F32)
        self.ident = P.sb(g, "ident", [128, 128], F32)
        self.ident_d = P.dram("ident", [128, 128], F32, kind="ExternalInput")
        self.psb = [P.ps(g, "ps%d" % i, [128, 512], F32) for i in range(8)]

    def input_names(self):
        return [k for k, v in self.__dict__.items() if False] or self._in_names

    def vec(self, name):
        o, k = self.voff[name]
        return self.vecs[:, o:o + k]

    def next_h(self):
        return self.hbuf[0] if self.cur is not self.hbuf[0] else self.hbuf[1]

    def setup(self):
        P, nc, c = self.P, self.nc, self.cfg
        P.dma("sp", self.vecs[:], self.vecs_d[:], self.vecs_d, self.vecs)
        P.memset("dve", self.ones_bf[:], 1.0, [self.ones_bf])
        P.dma("sp", self.ident[:], self.ident_d[:], self.ident_d, self.ident)
        P.memset("dve", self.eps_t[:], EPS, [self.eps_t])
        with contextlib.ExitStack() as st:
            cf = P.sb(st, "cond_f", [128, c.KC, c.NB], F32)
            sg = P.sb(st, "cond_sg", [128, c.KC, c.NB], F32)
            cb = P.sb(st, "cond_b", [128, c.KC, c.NB], BF16)
            wsl = [P.sb(st, "wmod_sl%d" % i, [128, c.KC, 1024], BF16) for i in range(2)]
            P.dma("sp", cf[:], self.cond[:], self.cond, cf)
            P.act(sg[:], cf[:], AF.Sigmoid, [cf], [sg])
            P.tt("dve", cb[:], cf[:], sg[:], ALU.mult, [cf, sg], [cb])
            it = 0
            for l in c.layers:
                for s in range(6):
                    w = wsl[it % 2]
                    it += 1
                    src = self.w_mod[l].rearrange("(k p) n -> p k n", p=128)[:, :, s * 1024:(s + 1) * 1024]
                    P.dma("pool", w[:], src, self.w_mod, w)
                    ps = self.psb[it % 2]
                    for jj in range(8):
                        for k in range(c.KC):
                            P.mm(ps, ps[:, jj * c.NB:(jj + 1) * c.NB], w[:, k, jj * 128:(jj + 1) * 128], cb[:, k, :],
                                 [w, cb], start=(k == 0), stop=(k == c.KC - 1))
                    o, _ = self.voff["b_mod%d" % l]
                    bsl = self.vecs[:, o + s * 8:o + s * 8 + 8]
                    P.tt("dve", self.modT[:, l, s * 8:(s + 1) * 8, :],
                         ps[:, 0:8 * c.NB].rearrange("p (j n) -> p j n", n=c.NB),
                         bsl.unsqueeze(2).to_broadcast([128, 8, c.NB]), ALU.add, [ps, self.vecs], [self.modT])
            P.barrier()

    def mod(self, l, which, n):
        return self.modT[:, l, which * 8:(which + 1) * 8, n]

    def make_gp(self, st, l, which_norm, n, name):
        P = self.P
        gp = P.sb(st, name, [128, 8], F32)
        sc = self.mod(l, 1 + 3 * which_norm, n)
        P.stt(gp[:], sc, 1.0, self.vec("ng%d_%d" % (l, which_norm)), ALU.add, ALU.mult, [self.modT, self.vecs], [gp])
        return gp

    def rstd_block(self, hsrc, hap_fn, n, sq, rstd, ps, nk=8, inv_n=1.0 / 1024):
        P = self.P
        for k in range(nk):
            P.act(sq[:, k, :n], hap_fn(k), AF.Square, [hsrc], [sq])
        on = self.ones_bf
        for k in range(nk):
            P.mm(ps, ps[:, :n], on[:], sq[:, k, :n], [on, sq], start=(k == 0), stop=(k == nk - 1))
        P.act(rstd[:, :n], ps[:, :n], AF.Sqrt, [ps, self.eps_t], [rstd], bias=self.eps_t[:], scale=inv_n)
        P.op("dve", lambda: self.nc.vector.reciprocal(rstd[:, :n], rstd[:, :n]), reads=[rstd], writes=[rstd])

    def ffn_stage(self, l, b, moe):
        P, nc, c = self.P, self.nc, self.cfg
        j = l // 2
        with_ctx = ctx_later(l)
        blocks = c.blocks if with_ctx else c.lat_blocks
        ntok = c.TT if with_ctx else c.T
        last = (l == c.layers[-1])
        src = self.cur
        dst = self.next_h()
        nf = (c.DFFE if moe else c.DFF) // 128
        groups = []
        f0 = 0
        while f0 < nf:
            groups.append((f0, min(4, nf - f0)))
            f0 += 4
        ne = c.NE if moe else 1
        if not getattr(self, 'do_ffn', True):
            ne = 0
            moe = False
        with contextlib.ExitStack() as st:
            hT = P.sb(st, "hT", [128, 8, ntok], F32)
            vT = P.sb(st, "vT", [128, 8, ntok], BF16)
            sq = P.sb(st, "sq", [128, 8, 256], BF16)
            rstd = P.sb(st, "rstd", [128, 256], F32)
            tmp = P.sb(st, "tmp", [128, 256], F32)
            nblocks = []
            for (s0_, n_, isc_) in blocks:
                for q0 in range(0, n_, 256):
                    nblocks.append((s0_ + q0, min(256, n_ - q0), isc_))
            wg = [P.sb(st, "wg%d" % i, [128, 8, 512], BF16) for i in range(2)]
            wu = [P.sb(st, "wu%d" % i, [128, 8, 512], BF16) for i in range(2)]
            wd = [P.sb(st, "wd%d" % i, [128, 4, 1024], BF16) for i in range(2)]
            hid = [P.sb(st, "hid%d" % i, [128, 4, 512], BF16) for i in range(2)]
            sl = [P.sb(st, "sl%d" % i, [128, 512], F32) for i in range(2)]
            sgb = [P.sb(st, "sgb%d" % i, [128, 512], F32) for i in range(2)]
            gp = {}
            for n in ([b, c.BL] if with_ctx else [b]):
                gp[n] = self.make_gp(st, l, 1, n, "gp%d" % n)
            for k in range(8):
                P.dma("sp", hT[:, k, :], src[b, k * 128:(k + 1) * 128, 0:ntok], src, hT)
            psn = self.psb[6]
            for (s0, n, isc) in nblocks:
                cn = c.BL if isc else b
                self.rstd_block(hT, lambda k: hT[:, k, s0:s0 + n], n, sq, rstd, psn)
                for k in range(8):
                    P.tt("dve", tmp[:, :n], hT[:, k, s0:s0 + n], rstd[:, :n], ALU.mult, [hT, rstd], [tmp])
                    P.act(vT[:, k, s0:s0 + n], tmp[:, :n], AF.Identity, [tmp, gp[cn], self.modT], [vT],
                          scale=gp[cn][:, k:k + 1], bias=self.mod(l, 3, cn)[:, k:k + 1])
            gates = None
            dbg = getattr(c, "dbg", ())
            if moe:
                if "nogates" not in dbg:
                    gates = self.moe_gates(st, l, vT, blocks, ntok)
                gbc = P.sb(st, "gbc", [128, ntok], F32)
                if "nogates" in dbg or "nogbc" in dbg:
                    P.memset("dve", gbc[:], 0.125, [gbc])
            it = 0
            pi = 0
            for e in range(ne):
                if moe:
                    sel, gT = gates if gates is not None else (None, None)
                    for (s0, n, isc) in (blocks if ("nogates" not in dbg and "nogbc" not in dbg) else []):
                        ps = self.psb[6]
                        for t0 in range(0, n, 128):
                            P.mm(ps, ps[:, t0:t0 + 128], sel[:, e, :], gT[:, s0 + t0:s0 + t0 + 128], [sel, gT])
                        P.copy("act", gbc[:, s0:s0 + n], ps[:, :n], [ps], [gbc])
                    Wg, Wu, Wd = self.moe_wg[j, e], self.moe_wu[j, e], self.moe_wd[j, e]
                else:
                    Wg, Wu, Wd = self.ffn_wg[j], self.ffn_wu[j], self.ffn_wd[j]
                for (f0, fg) in groups:
                    a = it % 2
                    it += 1
                    P.dma("pool", wg[a][:, :, :fg * 128],
                          Wg.rearrange("(k p) f -> p k f", p=128)[:, :, f0 * 128:(f0 + fg) * 128], self.moe_wg if moe else self.ffn_wg, wg[a])
                    P.dma("pool", wu[a][:, :, :fg * 128],
                          Wu.rearrange("(k p) f -> p k f", p=128)[:, :, f0 * 128:(f0 + fg) * 128], self.moe_wu if moe else self.ffn_wu, wu[a])
                    P.dma("pool", wd[a][:, :fg, :],
                          Wd[f0 * 128:(f0 + fg) * 128, :].rearrange("(f p) d -> p f d", p=128), self.moe_wd if moe else self.ffn_wd, wd[a])
                    for (s0, n, isc) in blocks:
                        cn = c.BL if isc else b
                        hb = hid[pi % 2]
                        for jj in range(fg):
                            pg = self.psb[(2 * pi + jj) % 2]
                            pu = self.psb[2 + (2 * pi + jj) % 2]
                            for k in range(8):
                                P.mm(pg, pg[:, :n], wg[a][:, k, jj * 128:(jj + 1) * 128], vT[:, k, s0:s0 + n], [wg[a], vT],
                                     start=(k == 0), stop=(k == 7))
                            for k in range(8):
                                P.mm(pu, pu[:, :n], wu[a][:, k, jj * 128:(jj + 1) * 128], vT[:, k, s0:s0 + n], [wu[a], vT],
                                     start=(k == 0), stop=(k == 7))
                            s_ = sl[jj % 2]
                            P.act(s_[:, :n], pg[:, :n], AF.Silu, [pg], [s_])
                            if moe:
                                g_ = sgb[jj % 2]
                                P.tt("pool", g_[:, :n], s_[:, :n], gbc[:, s0:s0 + n], ALU.mult, [s_, gbc], [g_])
                                s_ = g_
                            P.tt("dve", hb[:, jj, :n], pu[:, :n], s_[:, :n], ALU.mult, [pu, s_], [hb])
                        for dch in range(8):
                            py = self.psb[4 + dch % 2]
                            for jj in range(fg):
                                P.mm(py, py[:, :n], wd[a][:, jj, dch * 128:(dch + 1) * 128], hb[:, jj, :n], [wd[a], hb],
                                     start=(jj == 0), stop=(jj == fg - 1))
                            P.stt(hT[:, dch, s0:s0 + n], py[:, :n], self.mod(l, 5, cn)[:, dch:dch + 1], hT[:, dch, s0:s0 + n],
                                  ALU.mult, ALU.add, [py, self.modT, hT], [hT])
                        pi += 1
            if last:
                for (s0, n, isc) in [nb for nb in nblocks if not nb[2]]:
                    self.rstd_block(hT, lambda k: hT[:, k, s0:s0 + n], n, sq, rstd, psn)
                    for k in range(8):
                        P.tt("dve", tmp[:, :n], hT[:, k, s0:s0 + n], rstd[:, :n], ALU.mult, [hT, rstd], [tmp])
                        o_ = sl[k % 2]
                        P.ts("dve", o_[:, :n], tmp[:, :n], self.vec("final_g")[:, k:k + 1], None, ALU.mult, None,
                             [tmp, self.vecs], [o_])
                        P.dma("sp", self.outT[b, k * 128:(k + 1) * 128, s0:s0 + n], o_[:, :n], o_, self.outT)
            else:
                for k in range(8):
                    P.dma("sp", dst[b, k * 128:(k + 1) * 128, 0:ntok], hT[:, k, :], hT, dst)
            P.barrier()

    def moe_gates(self, st, l, vT, blocks, ntok):
        P, nc, c = self.P, self.nc, self.cfg
        j = l // 2
        rt = P.sb(st, "router", [128, 8, c.NE], BF16)
        P.dma("pool", rt[:], self.router[j], self.router, rt)
        sel = P.sb(st, "sel", [8, c.NE, 128], F32)
        gT = P.sb(st, "gT", [8, ntok], F32)
        lg = P.sb(st, "lg", [128, 8], F32)
        top = P.sb(st, "top", [128, 8], F32)
        ex = P.sb(st, "ex", [128, 8], F32)
        msk = P.sb(st, "msk", [128, 8], F32)
        den = P.sb(st, "den", [128, 1], F32)
        nmx = P.sb(st, "nmx", [128, 1], F32)
        gt = P.sb(st, "gt", [128, 8], F32)
        P.copy("dve", sel[:], self.ident[0:8, 0:8].unsqueeze(2).to_broadcast([8, 8, 128]), [self.ident], [sel])
        ps = self.psb[7]
        pt = self.psb[6]
        for (s0, n, isc) in blocks:
            for t0 in range(0, n, 128):
                a0 = s0 + t0
                for k in range(8):
                    P.mm(ps, ps[:, 0:8], vT[:, k, a0:a0 + 128], rt[:, k, :], [vT, rt], start=(k == 0), stop=(k == 7))
                P.copy("dve", lg[:], ps[:, 0:8], [ps], [lg])
                P.op("dve", lambda: nc.vector.max(out=top[:], in_=lg[:]), reads=[lg], writes=[top])
                P.ts("dve", nmx[:], top[:, 0:1], -1.0, None, ALU.mult, None, [top], [nmx])
                P.act(ex[:], lg[:], AF.Exp, [lg, nmx], [ex], bias=nmx[:], scale=1.0)
                P.ts("dve", msk[:], lg[:], top[:, 1:2], None, ALU.is_ge, None, [lg, top], [msk])
                P.tt("dve", gt[:], ex[:], msk[:], ALU.mult, [ex, msk], [gt])
                P.op("dve", lambda: nc.vector.reduce_sum(out=den[:], in_=gt[:], axis=AX.X), reads=[gt], writes=[den])
                P.op("dve", lambda: nc.vector.reciprocal(den[:], den[:]), reads=[den], writes=[den])
                P.ts("dve", gt[:], gt[:], den[:, 0:1], None, ALU.mult, None, [gt, den], [gt])
                P.op("pe", lambda: nc.tensor.transpose(pt[0:8, 0:128], gt[:], self.ident[:]), reads=[gt, self.ident], writes=[pt])
                P.copy("act", gT[:, a0:a0 + 128], pt[0:8, 0:128], [pt], [gT])
        return sel, gT

    def ensure_ident(self):
        if getattr(self, "_ident_done", False):
            return
        P, nc = self.P, self.nc
        P.memset("pool", self.ident[:], 0.0, [self.ident])
        P.op("pool", lambda: nc.gpsimd.affine_select(out=self.ident[:], in_=self.ident[:], pattern=[[-1, 128]],
                                                      compare_op=ALU.not_equal, fill=1.0, base=0, channel_multiplier=1),
             reads=[self.ident], writes=[self.ident])
        self._ident_done = True

    def pool_stage(self, l, b):
        P, nc, c = self.P, self.nc, self.cfg
        j = l // 2
        with_ctx = ctx_later(l)
        src = self.cur
        dst = self.next_h()
        streams = [(0, c.T, b, 0)] + ([(c.T, c.C, c.BL, 1)] if with_ctx else [])
        nmax = max(c.T, c.C)
        with contextlib.ExitStack() as st:
            hT = P.sb(st, "hT", [128, 8, nmax], F32)
            sq = P.sb(st, "sq", [128, 8, 512], BF16)
            rstd = P.sb(st, "rstd", [128, nmax], F32)
            upad = P.sb(st, "upad", [128, nmax + 16], F32)
            a1 = P.sb(st, "a1", [128, nmax + 16], F32)
            a2 = P.sb(st, "a2", [128, nmax + 16], F32)
            icn = P.sb(st, "icn", [128, 4, nmax], F32)
            pooled = P.sb(st, "pooled", [128, 8, nmax], BF16)
            pw = P.sb(st, "pw", [128, 4, 2, 256], BF16)
            A = P.sb(st, "A", [128, 8], F32)
            Bc = P.sb(st, "Bc", [128, 8], F32)
            tmp = P.sb(st, "tmp", [128, 512], F32)
            P.dma("pool", pw[:], self.pool_w[j].rearrange("g (k p) d -> p g k d", p=128), self.pool_w, pw)
            for (t0, ns, cn, si) in streams:
                P.dma("sp", icn[:].rearrange("p g n -> p (g n)"),
                      self.invcnt[si].rearrange("g n -> (g n)").partition_broadcast(128), self.invcnt, icn)
                gp = self.make_gp(st, l, 0, cn, "gp%d" % si)
                P.tt("dve", A[:], self.mod(l, 2, cn), self.vec("pool_s%d" % j), ALU.mult, [self.modT, self.vecs], [A])
                P.tt("dve", Bc[:], A[:], self.vec("pool_b%d" % j), ALU.mult, [A, self.vecs], [Bc])
                for k in range(8):
                    P.dma("sp", hT[:, k, :ns], src[b, k * 128:(k + 1) * 128, t0:t0 + ns], src, hT)
                for s0 in range(0, ns, 512):
                    n = min(512, ns - s0)
                    self.rstd_block(hT, lambda k: hT[:, k, s0:s0 + n], n, sq, tmp, self.psb[6])
                    P.copy("dve", rstd[:, s0:s0 + n], tmp[:, :n], [tmp], [rstd])
                P.memset("pool", upad[:], 0.0, [upad])
                for k in range(8):
                    g = k // 2
                    w = POOL_WINDOWS[g]
                    m = g + 1
                    u = upad[:, 8:8 + ns]
                    P.tt("dve", u, hT[:, k, :ns], rstd[:, :ns], ALU.mult, [hT, rstd], [upad])
                    P.act(u, u, AF.Identity, [upad, gp, self.modT], [upad],
                          scale=gp[:, k:k + 1], bias=self.mod(l, 0, cn)[:, k:k + 1])
                    W = ns + 16
                    cur_, cb_ = upad, upad
                    bufs = [a1, a2]
                    for mm_ in range(m):
                        sh = 1 << mm_
                        nb = bufs[mm_ % 2]
                        ln = W - 2 * sh + 1 if mm_ == 0 else W - (2 << mm_) + 1
                        ln = W - ((2 << mm_) - 1)
                        P.tt("pool", nb[:, :ln], cur_[:, 0:ln], cur_[:, sh:sh + ln], ALU.add, [cb_], [nb])
                        cur_, cb_ = nb, nb
                    o0 = 8 - w // 2
                    P.tt("dve", a1[:, :ns] if cur_ is a2 else a2[:, :ns], cur_[:, o0:o0 + ns], icn[:, g, :ns], ALU.mult,
                         [cb_, icn], [a1 if cur_ is a2 else a2])
                    oth = a1 if cur_ is a2 else a2
                    P.tt("dve", pooled[:, k, :ns], oth[:, :ns], u, ALU.subtract, [oth, upad], [pooled])
                for s0 in range(0, ns, 512):
                    n = min(512, ns - s0)
                    for g in range(4):
                        for dd in range(2):
                            dch = 2 * g + dd
                            ps = self.psb[dch % 2]
                            for kk in range(2):
                                P.mm(ps, ps[:, :n], pw[:, g, kk, dd * 128:(dd + 1) * 128], pooled[:, 2 * g + kk, s0:s0 + n],
                                     [pw, pooled], start=(kk == 0), stop=(kk == 1))
                            P.stt(hT[:, dch, s0:s0 + n], ps[:, :n], A[:, dch:dch + 1], hT[:, dch, s0:s0 + n],
                                  ALU.mult, ALU.add, [ps, A, hT], [hT])
                            P.ts("dve", hT[:, dch, s0:s0 + n], hT[:, dch, s0:s0 + n], Bc[:, dch:dch + 1], None, ALU.add, None,
                                 [hT, Bc], [hT])
                for k in range(8):
                    P.dma("sp", dst[b, k * 128:(k + 1) * 128, t0:t0 + ns], hT[:, k, :ns], hT, dst)
            P.barrier()

    def build(self, do_mixer=True, do_ffn=True):
        c = self.cfg
        self.do_ffn = do_ffn
        self.setup()
        for l in c.layers:
            even = (l % 2 == 0)
            for b in range(c.BL):
                save = self.cur
                if do_mixer:
                    if even:
                        self.ab_stage(l, b)
                    else:
                        self.pool_stage(l, b)
                    self.cur = self.next_h()
                self.ffn_stage(l, b, moe=not even)
                self.cur = save
            if do_mixer:
                self.cur = self.next_h()
            self.cur = self.next_h()
        self.P.barrier()
        self.glob.close()
        return self.nc

    def ab_stage(self, l, b):
        dbg = getattr(self.cfg, "dbg", ())
        if "no1" not in dbg:
            self.ab_inproj(l, b)
        if "no2" not in dbg:
            self.ab_mla(l, b)
        if "no3" not in dbg:
            self.ab_gla(l, b)
        if "no4" not in dbg:
            self.ab_outproj(l, b)

    def ab_inproj(self, l, b):
        P, nc, c = self.P, self.nc, self.cfg
        j = l // 2
        src = self.cur
        OQ, OF, OI, OG = 704, 1216, 2240, 2752
        with contextlib.ExitStack() as st:
            win = P.sb(st, "win", [128, 8, 3264], BF16)
            hblk = [P.sb(st, "hblk%d" % i, [128, 8, 512], F32) for i in range(2)]
            uT = [P.sb(st, "uT%d" % i, [128, 8, 512], BF16) for i in range(2)]
            sq = P.sb(st, "sq", [128, 8, 512], BF16)
            rstd = P.sb(st, "rstd", [128, 512], F32)
            tmp = P.sb(st, "tmp", [128, 512], F32)
            cf = P.sb(st, "cf", [128, 3, 512], F32)
            cn = P.sb(st, "cn", [128, 3, 512], BF16)
            rs2 = P.sb(st, "rs2", [128, 512], F32)
            kro = P.sb(st, "kro", [32, 512], BF16)
            t1 = P.sb(st, "t1", [32, 512], F32)
            t2 = P.sb(st, "t2", [32, 512], F32)
            rp = P.sb(st, "rp", [32, 2, c.TT], F32)
            fo = [P.sb(st, "fo%d" % i, [128, 512], F32) for i in range(3)]
            tk = [P.sb(st, "tk%d" % i, [128, 512], BF16) for i in range(2)]
            tg = [P.sb(st, "tg%d" % i, [128, 512], F32) for i in range(2)]
            gps = {}
            for n_ in (b, c.BL):
                gps[n_] = self.make_gp(st, l, 0, n_, "gp%d" % n_)
            for s in range(0, 3264, 1088):
                P.dma("pool", win[:, :, s:s + 1088], self.w_in[j].rearrange("(k p) n -> p k n", p=128)[:, :, s:s + 1088], self.w_in, win)
            P.dma("sp", rp[:], self.rope[:].rearrange("a d t -> d a t"), self.rope, rp)
            ic = 0
            for bi, (s0, n, isc) in enumerate(c.blocks):
                cnd = c.BL if isc else b
                hb, u = hblk[bi % 2], uT[bi % 2]
                for k in range(8):
                    P.dma("sp", hb[:, k, :n], src[b, k * 128:(k + 1) * 128, s0:s0 + n], src, hb)
                self.rstd_block(hb, lambda k: hb[:, k, :n], n, sq, rstd, self.psb[7])
                for k in range(8):
                    P.tt("dve", tmp[:, :n], hb[:, k, :n], rstd[:, :n], ALU.mult, [hb, rstd], [tmp])
                    P.act(u[:, k, :n], tmp[:, :n], AF.Identity, [tmp, gps[cnd], self.modT], [u],
                          scale=gps[cnd][:, k:k + 1], bias=self.mod(l, 0, cnd)[:, k:k + 1])

                def proj(ps, col0, m):
                    for k in range(8):
                        P.mm(ps, ps[:m, :n], win[:, k, col0:col0 + m], u[:, k, :n], [win, u], start=(k == 0), stop=(k == 7))

                for (col0, nch, gname, dd) in ((0, 3, "g_cq%d" % j, self.cqn_d), (384, 2, "g_ckv%d" % j, self.ckvn_d)):
                    for q_ in range(nch):
                        ps = self.psb[ic % 4]
                        ic += 1
                        proj(ps, col0 + q_ * 128, 128)
                        P.copy("act", cf[:, q_, :n], ps[:, :n], [ps], [cf])
                    self.rstd_block(cf, lambda k: cf[:, k, :n], n, sq, rs2, self.psb[7], nk=nch, inv_n=1.0 / (nch * 128))
                    for q_ in range(nch):
                        P.tt("dve", tmp[:, :n], cf[:, q_, :n], rs2[:, :n], ALU.mult, [cf, rs2], [tmp])
                        P.ts("dve", cn[:, q_, :n], tmp[:, :n], self.vec(gname)[:, q_:q_ + 1], None, ALU.mult, None,
                             [tmp, self.vecs], [cn])
                    P.dma("sp", dd[:, s0:s0 + n].rearrange("(q p) t -> p q t", p=128), cn[:, :nch, :n], cn, dd)
                pa, pb = self.psb[ic % 4], self.psb[(ic + 1) % 4]
                ic += 2
                proj(pa, 640, 32)
                proj(pb, 672, 32)
                P.tt("dve", t1[:, :n], pa[:32, :n], rp[:, 0, s0:s0 + n], ALU.mult, [pa, rp], [t1])
                P.tt("dve", t2[:, :n], pb[:32, :n], rp[:, 1, s0:s0 + n], ALU.mult, [pb, rp], [t2])
                P.tt("pool", kro[:, :n], t1[:, :n], t2[:, :n], ALU.add, [t1, t2], [kro])
                P.dma("sp", self.krr_d[:, s0:s0 + n], kro[:, :n], kro, self.krr_d)
                for q_ in range(12):
                    ps = self.psb[ic % 4]
                    ic += 1
                    proj(ps, OQ + q_ * 128, 128)
                    o_ = fo[q_ % 3]
                    if q_ < 4:
                        P.act(o_[:, :n], ps[:, :n], AF.Silu, [ps], [o_])
                        P.dma("sp", self.qh_d[q_ * 128:(q_ + 1) * 128, s0:s0 + n], o_[:, :n], o_, self.qh_d)
                    else:
                        P.copy("act", o_[:, :n], ps[:, :n], [ps], [o_])
                        d_, h_ = (q_ - 4) // 4, (q_ - 4) % 4
                        P.dma("sp", self.hf_d[d_, h_ * 128:(h_ + 1) * 128, s0:s0 + n], o_[:, :n], o_, self.hf_d)
                for t0 in range(0, n, 128):
                    a0 = s0 + t0
                    for which, col0 in ((0, OI), (1, OG)):
                        ps = self.psb[ic % 4]
                        ic += 1
                        for k in range(8):
                            P.mm(ps, ps[:, :512], u[:, k, t0:t0 + 128], win[:, k, col0:col0 + 512], [u, win],
                                 start=(k == 0), stop=(k == 7))
                        if which == 0:
                            o_ = tk[(t0 // 128) % 2]
                            P.copy("act", o_[:], ps[:, :512], [ps], [o_])
                            P.dma("sp", self.vg_d[a0:a0 + 128, :], o_[:], o_, self.vg_d)
                        else:
                            o_ = tg[(t0 // 128) % 2]
                            P.act(o_[:], ps[:, :512], AF.Silu, [ps], [o_])
                            P.dma("sp", self.sg_d[a0:a0 + 128, :], o_[:], o_, self.sg_d)
            P.barrier()

    def ab_mla(self, l, b):
        P, nc, c = self.P, self.nc, self.cfg
        j = l // 2
        need_ctx = ctx_later(l)
        NT = c.TT // 128
        scale = 96.0 ** -0.5
        with contextlib.ExitStack() as st:
            cqn = P.sb(st, "cqn", [128, 3, c.TT], BF16)
            ckvn = P.sb(st, "ckvn", [128, 2, c.TT], BF16)
            krr = P.sb(st, "krr", [32, c.TT], BF16)
            rp = P.sb(st, "rp", [32, 2, c.TT], F32)
            wq = P.sb(st, "wq", [128, 3, 1024], BF16)
            wk = P.sb(st, "wk", [128, 2, 1024], BF16)
            wv = P.sb(st, "wv", [128, 2, 512], BF16)
            qT = P.sb(st, "qT", [96, 8, c.TT], BF16)
            kT = P.sb(st, "kT", [96, 8, c.TT], BF16)
            va = P.sb(st, "va", [128, NT, 8, 66], BF16)
            PT = [P.sb(st, "PT%d" % i, [128, NT, 512], BF16) for i in range(2)]
            t1 = P.sb(st, "t1", [32, 512], F32)
            t2 = P.sb(st, "t2", [32, 512], F32)
            alat = P.sb(st, "alat", [128, 4, 512], BF16)
            rden = P.sb(st, "rden", [128, 1], F32)
            mo = [P.sb(st, "mo%d" % i, [128, 4, 512], BF16) for i in range(2)]
            idb = P.sb(st, "idb", [128, 128], BF16)
            P.copy("dve", idb[:], self.ident[:], [self.ident], [idb])
            P.dma("sp", cqn[:], self.cqn_d[:].rearrange("(q p) t -> p q t", p=128), self.cqn_d, cqn)
            P.dma("sp", ckvn[:], self.ckvn_d[:].rearrange("(q p) t -> p q t", p=128), self.ckvn_d, ckvn)
            P.dma("sp", krr[:], self.krr_d[:], self.krr_d, krr)
            P.dma("sp", rp[:], self.rope[:].rearrange("a d t -> d a t"), self.rope, rp)
            P.dma("pool", wq[:], self.w_uq[j].rearrange("(k p) n -> p k n", p=128), self.w_uq, wq)
            P.dma("pool", wk[:], self.w_uk[j].rearrange("(k p) n -> p k n", p=128), self.w_uk, wk)
            P.dma("pool", wv[:], self.w_uv[j].rearrange("(k p) n -> p k n", p=128), self.w_uv, wv)
            P.memset("pool", va[:], 1.0, [va])
            ic = 0
            dbg = getattr(c, "dbg", ())
            for (s0, n, isc) in (c.blocks if "mla_noproj" not in dbg else []):
                for h in range(8):
                    pa, pb = self.psb[ic % 4], self.psb[(ic + 1) % 4]
                    pk = self.psb[(ic + 2) % 4]
                    ic += 3
                    if "skq" in dbg:
                        continue
                    for k in range(3):
                        P.mm(pa, pa[:96, :n], wq[:, k, h * 128:h * 128 + 96], cqn[:, k, s0:s0 + n], [wq, cqn], start=(k == 0), stop=(k == 2))
                    for k in range(3):
                        P.mm(pb, pb[:32, :n], wq[:, k, h * 128 + 96:h * 128 + 128], cqn[:, k, s0:s0 + n], [wq, cqn], start=(k == 0), stop=(k == 2))
                    P.copy("act", qT[:, h, s0:s0 + n], pa[:96, :n], [pa], [qT])
                    P.tt("dve", t1[:, :n], pa[:32, :n], rp[:, 0, s0:s0 + n], ALU.mult, [pa, rp], [t1])
                    P.tt("dve", t2[:, :n], pb[:32, :n], rp[:, 1, s0:s0 + n], ALU.mult, [pb, rp], [t2])
                    P.tt("dve" if "mla_dve" in dbg else "pool", qT[0:32, h, s0:s0 + n], t1[:, :n], t2[:, :n], ALU.add, [t1, t2], [qT])
                    if "skk" in dbg:
                        continue
                    for k in range(2):
                        P.mm(pk, pk[:96, :n], wk[:, k, h * 128:h * 128 + 96], ckvn[:, k, s0:s0 + n], [wk, ckvn], start=(k == 0), stop=(k == 1))
                    P.copy("act", kT[:, h, s0:s0 + n], pk[:96, :n], [pk], [kT])
                    P.copy("dve" if "mla_dve" in dbg else "pool", kT[0:32, h, s0:s0 + n], krr[:, s0:s0 + n], [krr], [kT])
                for t0 in (range(0, n, 128) if "skv" not in dbg else []):
                    a0 = s0 + t0
                    ps = self.psb[ic % 4]
                    ic += 1
                    for k in range(2):
                        P.mm(ps, ps[:, :512], ckvn[:, k, a0:a0 + 128], wv[:, k, :], [ckvn, wv], start=(k == 0), stop=(k == 1))
                    P.copy("act", va[:, a0 // 128, :, 0:64], ps[:, :512].rearrange("p (h d) -> p h d", d=64), [ps], [va])
            qblocks = c.blocks if need_ctx else c.lat_blocks
            if "mla_noattn" in dbg:
                qblocks = []
            ib = 0
            for (s0, n, isc) in qblocks:
                kts = list(range(c.T // 128, NT)) if isc else list(range(NT))
                for h in range(8):
                    pt_ = PT[ib % 2]
                    ib += 1
                    for ki, kt in enumerate(kts):
                        ps = self.psb[ki % 2]
                        P.mm(ps, ps[:, :n], kT[:, h, kt * 128:(kt + 1) * 128], qT[:, h, s0:s0 + n], [kT, qT])
                        P.act(pt_[:, ki, :n], ps[:, :n], AF.Exp, [ps], [pt_], scale=scale)
                    for qs in range(n // 128):
                        po = self.psb[2 + qs % 2]
                        for ki, kt in enumerate(kts):
                            P.mm(po, po[:, 0:65], pt_[:, ki, qs * 128:(qs + 1) * 128], va[:, kt, h, 0:65], [pt_, va],
                                 start=(ki == 0), stop=(ki == len(kts) - 1))
                        P.op("dve", lambda: nc.vector.reciprocal(rden[:], po[:, 64:65]), reads=[po], writes=[rden])
                        P.ts("dve", alat[:, qs, h * 64:(h + 1) * 64], po[:, 0:64], rden[:, 0:1], None, ALU.mult, None,
                             [po, rden], [alat])
                m_ = mo[(s0 // 512) % 2]
                for qs in range(n // 128):
                    for fc in range(4):
                        ptb = self.psb[4 + (qs * 4 + fc) % 2]
                        pv = ptb[:].bitcast(BF16)
                        P.op("pe", lambda: nc.tensor.transpose(pv[:, 0:128], alat[:, qs, fc * 128:(fc + 1) * 128], idb[:]),
                             reads=[alat, idb], writes=[ptb])
                        P.copy("act", m_[:, fc, qs * 128:(qs + 1) * 128], pv[:, 0:128], [ptb], [m_])
                P.dma("sp", self.mix_d[0:512, s0:s0 + n].rearrange("(q p) t -> p q t", p=128), m_[:, :, :n], m_, self.mix_d)
            P.barrier()

    def ab_gla(self, l, b):
        P, nc, c = self.P, self.nc, self.cfg
        j = l // 2
        need_ctx = ctx_later(l)
        NT = c.TT // 128
        NCH = c.TT // 32
        lat_tiles = list(range(c.T // 128))
        ctx_tiles = list(range(c.T // 128, NT))
        with contextlib.ExitStack() as st:
            vg = P.sb(st, "vg", [128, NT, 512], BF16)
            oacc = P.sb(st, "oacc", [128, NT, 512], F32)
            smask = P.sb(st, "smask", [128, c.TT], F32)
            tri = P.sb(st, "tri", [128, 2, 128], F32)
            oml = P.sb(st, "oml", [128, 2, 4], F32)
            qh = P.sb(st, "qh", [128, c.TT], F32)
            hf = P.sb(st, "hf", [128, c.TT], F32)
            kin = P.sb(st, "kin", [128, c.TT], F32)
            G = P.sb(st, "G", [128, c.TT], F32)
            eG = P.sb(st, "eG", [128, c.TT], F32)
            w1 = P.sb(st, "w1", [128, c.TT], F32)
            qe = P.sb(st, "qe", [128, c.TT], BF16)
            ke = P.sb(st, "ke", [128, c.TT], BF16)
            kd = P.sb(st, "kd", [128, c.TT], BF16)
            kdt = [P.sb(st, "kdt%d" % i, [128, 4, 128], BF16) for i in range(2)]
            qeb = [P.sb(st, "qeb%d" % i, [128, 4, 128], BF16) for i in range(2)]
            bm = P.sb(st, "bm", [128, 4], F32)
            cm = P.sb(st, "cm", [128, 4, 128], BF16)
            atm = [P.sb(st, "atm%d" % i, [128, 128], BF16) for i in range(2)]
            S = P.sb(st, "S", [128, 128], F32)
            Sb = [P.sb(st, "Sb%d" % i, [128, 128], BF16) for i in range(2)]
            idb = P.sb(st, "idb", [128, 128], BF16)
            gnb = P.sb(st, "gnb", [128, 128], F32)
            sgt = [P.sb(st, "sgt%d" % i, [128, 512], F32) for i in range(2)]
            ssq = P.sb(st, "ssq", [128, 4], F32)
            junk = P.sb(st, "junk", [128, 128], F32)
            yt = P.sb(st, "yt", [128, 512], F32)
            blat = P.sb(st, "blat", [128, 512], BF16)
            mo = [P.sb(st, "mo%d" % i, [128, 4, 128], BF16) for i in range(2)]
            P.copy("dve", idb[:], self.ident[:], [self.ident], [idb])
            P.op("dve", lambda: nc.vector.reduce_sum(out=bm[:], in_=self.ident[:].rearrange("p (c l) -> p c l", l=32), axis=AX.X),
                 reads=[self.ident], writes=[bm])
            P.memset("pool", cm[:], 0.0, [cm])
            for cc in range(4):
                P.memset("pool", cm[:, cc, cc * 32:(cc + 1) * 32], 1.0, [cm])
            P.dma("sp", vg[:], self.vg_d[:].rearrange("(t p) f -> p t f", p=128), self.vg_d, vg)
            P.dma("sp", tri[:], self.trimask[:].rearrange("a s t -> s a t"), self.trimask, tri)
            P.dma("sp", gnb[:], self.gnorm[j].partition_broadcast(128), self.gnorm, gnb)
            P.memset("pool", smask[:], 1.0, [smask])
            P.memset("pool", smask[:].rearrange("p (c l) -> p c l", l=32)[:, :, 0:1], 0.0, [smask])
            if j == 0:
                P.memset("dve", oml[:], 1.0, [oml])
            else:
                for d in range(2):
                    P.tt("dve", oml[:, d, :], self.vec("lbl0_%d" % d), self.vec("lbl1_%d" % d), ALU.subtract, [self.vecs], [oml])
                P.act(oml[:], oml[:], AF.Sigmoid, [oml], [oml])
            ich = 0
            for h in range(4):
                P.dma("sp", qh[:], self.qh_d[h * 128:(h + 1) * 128, :], self.qh_d, qh)
                for d in range(2):
                    P.dma("sp", hf[:], self.hf_d[d, h * 128:(h + 1) * 128, :], self.hf_d, hf)
                    P.act(kin[:], hf[:], AF.Sigmoid, [hf], [kin], scale=-1.0)
                    P.ts("dve", kin[:], kin[:], oml[:, d, h:h + 1], None, ALU.mult, None, [kin, oml], [kin])
                    P.act(w1[:], kin[:], AF.Ln, [kin], [w1], scale=-1.0, bias=1.0)
                    if d == 0:
                        P.op("dve", lambda: nc.vector.tensor_tensor_scan(out=G[:], data0=smask[:], data1=w1[:], initial=0.0,
                                                                          op0=ALU.mult, op1=ALU.add), reads=[smask, w1], writes=[G])
                    else:
                        P.op("dve", lambda: nc.vector.tensor_tensor_scan(out=G[:, ::-1], data0=smask[:], data1=w1[:, ::-1], initial=0.0,
                                                                          op0=ALU.mult, op1=ALU.add), reads=[smask, w1], writes=[G])
                    G3 = G[:].rearrange("p (c l) -> p c l", l=32)
                    gend = G3[:, :, 31:32] if d == 0 else G3[:, :, 0:1]
                    P.act(eG[:], G[:], AF.Exp, [G], [eG])
                    P.tt("pool", qe[:], qh[:], eG[:], ALU.mult, [qh, eG], [qe])
                    P.tt("dve", w1[:].rearrange("p (c l) -> p c l", l=32), gend.to_broadcast([128, NCH, 32]), G3, ALU.subtract, [G], [w1])
                    P.act(w1[:], w1[:], AF.Exp, [w1], [w1])
                    P.tt("pool", kd[:], kin[:], w1[:], ALU.mult, [kin, w1], [kd])
                    P.act(w1[:], G[:], AF.Exp, [G], [w1], scale=-1.0)
                    P.tt("dve", ke[:], kin[:], w1[:], ALU.mult, [kin, w1], [ke])
                    eG3 = eG[:].rearrange("p (c l) -> p c l", l=32)
                    order = (ctx_tiles + lat_tiles) if d == 0 else (ctx_tiles[::-1] + lat_tiles[::-1])
                    if "gla_noscan" in getattr(c, "dbg", ()):
                        order = []
                    P.memset("dve", S[:], 0.0, [S])
                    cur_sb = 0
                    P.memset("pool", Sb[0][:], 0.0, [Sb[0]])
                    for ti, tl in enumerate(order):
                        a0 = tl * 128
                        skip_out = (tl in ctx_tiles) and not need_ctx
                        kt_, am_ = kdt[ti % 2], atm[ti % 2]
                        pk = self.psb[ti % 2]
                        pkv = pk[:].bitcast(BF16)
                        P.op("pe", lambda: nc.tensor.transpose(pkv[:, 0:128], kd[:, a0:a0 + 128], idb[:]), reads=[kd, idb], writes=[pk])
                        P.tt("dve", kt_[:], pkv[:, 0:128].unsqueeze(1).to_broadcast([128, 4, 128]),
                             bm[:].unsqueeze(2).to_broadcast([128, 4, 128]), ALU.mult, [pk, bm], [kt_])
                        qb_ = qeb[ti % 2]
                        if not skip_out:
                            P.tt("pool", qb_[:], qe[:, a0:a0 + 128].unsqueeze(1).to_broadcast([128, 4, 128]), cm[:], ALU.mult, [qe, cm], [qb_])
                        po = self.psb[2 + ti % 2]
                        if not skip_out:
                            pa = self.psb[4 + ti % 2]
                            P.mm(pa, pa[:, 0:128], ke[:, a0:a0 + 128], qe[:, a0:a0 + 128], [ke, qe])
                            P.tt("dve", am_[:], pa[:, 0:128], tri[:, d, :], ALU.mult, [pa, tri], [am_])
                            P.mm(po, po[:, 0:128], am_[:], vg[:, tl, h * 128:(h + 1) * 128], [am_, vg], start=True, stop=False)
                        pd = self.psb[6 + ti % 2]
                        crange = range(4) if d == 0 else range(3, -1, -1)
                        for cc in crange:
                            sl_ = slice(cc * 32, (cc + 1) * 32)
                            P.mm(pd, pd[:, cc * 128:(cc + 1) * 128], kt_[:, cc, :], vg[:, tl, h * 128:(h + 1) * 128], [kt_, vg])
                        for ci, cc in enumerate(crange):
                            sl_ = slice(cc * 32, (cc + 1) * 32)
                            if not skip_out:
                                P.mm(po, po[:, 0:128], qb_[:, cc, :], Sb[cur_sb][:], [qb_, Sb[cur_sb]],
                                     start=False, stop=(ci == 3))
                            chn = tl * 4 + cc
                            eg = eG3[:, chn, 31:32] if d == 0 else eG3[:, chn, 0:1]
                            P.stt(S[:], S[:], eg, pd[:, cc * 128:(cc + 1) * 128], ALU.mult, ALU.add, [S, eG, pd], [S])
                            cur_sb ^= 1
                            P.copy("act", Sb[cur_sb][:], S[:], [S], [Sb[cur_sb]])
                        if not skip_out:
                            if d == 0:
                                P.copy("act", oacc[:, tl, h * 128:(h + 1) * 128], po[:, 0:128], [po], [oacc])
                            else:
                                P.tt("dve", oacc[:, tl, h * 128:(h + 1) * 128], po[:, 0:128], oacc[:, tl, h * 128:(h + 1) * 128],
                                     ALU.add, [po, oacc], [oacc])
            tiles = (lat_tiles + ctx_tiles) if need_ctx else lat_tiles
            if "gla_noread" in getattr(c, "dbg", ()):
                tiles = []
            for ti, tl in enumerate(tiles):
                a0 = tl * 128
                sg_ = sgt[ti % 2]
                P.dma("sp", sg_[:], self.sg_d[a0:a0 + 128, :], self.sg_d, sg_)
                for h in range(4):
                    P.act(junk[:], oacc[:, tl, h * 128:(h + 1) * 128], AF.Square, [oacc], [junk, ssq], accum_out=ssq[:, h:h + 1])
                P.act(ssq[:], ssq[:], AF.Sqrt, [ssq, self.eps_t], [ssq], bias=self.eps_t[:], scale=1.0 / 128)
                P.op("dve", lambda: nc.vector.reciprocal(ssq[:], ssq[:]), reads=[ssq], writes=[ssq])
                for h in range(4):
                    P.stt(yt[:, h * 128:(h + 1) * 128], oacc[:, tl, h * 128:(h + 1) * 128], ssq[:, h:h + 1], gnb[:], ALU.mult, ALU.mult,
                          [oacc, ssq, gnb], [yt])
                P.tt("dve", blat[:], yt[:], sg_[:], ALU.mult, [yt, sg_], [blat])
                m_ = mo[ti % 2]
                for fc in range(4):
                    ptb = self.psb[fc % 2]
                    pv = ptb[:].bitcast(BF16)
                    P.op("pe", lambda: nc.tensor.transpose(pv[:, 0:128], blat[:, fc * 128:(fc + 1) * 128], idb[:]), reads=[blat, idb], writes=[ptb])
                    P.copy("act", m_[:, fc, :], pv[:, 0:128], [ptb], [m_])
                P.dma("sp", self.mix_d[512:1024, a0:a0 + 128].rearrange("(q p) t -> p q t", p=128), m_[:], m_, self.mix_d)
            P.barrier()

    def ab_outproj(self, l, b):
        P, nc, c = self.P, self.nc, self.cfg
        j = l // 2
        need_ctx = ctx_later(l)
        src = self.cur
        dst = self.next_h()
        blocks = c.blocks if need_ctx else c.lat_blocks
        with contextlib.ExitStack() as st:
            wo = P.sb(st, "wo", [128, 8, 1024], BF16)
            mx = [P.sb(st, "mx%d" % i, [128, 8, 512], BF16) for i in range(2)]
            hb = [P.sb(st, "hb%d" % i, [128, 8, 512], F32) for i in range(2)]
            P.dma("pool", wo[:], self.w_out[j].rearrange("(k p) n -> p k n", p=128), self.w_out, wo)
            for bi, (s0, n, isc) in enumerate(blocks):
                cnd = c.BL if isc else b
                m_, h_ = mx[bi % 2], hb[bi % 2]
                P.dma("sp", m_[:, :, :n], self.mix_d[:, s0:s0 + n].rearrange("(q p) t -> p q t", p=128), self.mix_d, m_)
                for k in range(8):
                    P.dma("sp", h_[:, k, :n], src[b, k * 128:(k + 1) * 128, s0:s0 + n], src, h_)
                for dch in range(8):
                    ps = self.psb[dch % 4]
                    for k in range(8):
                        P.mm(ps, ps[:, :n], wo[:, k, dch * 128:(dch + 1) * 128], m_[:, k, :n], [wo, m_], start=(k == 0), stop=(k == 7))
                    P.stt(h_[:, dch, :n], ps[:, :n], self.mod(l, 2, cnd)[:, dch:dch + 1], h_[:, dch, :n], ALU.mult, ALU.add,
                          [ps, self.modT, h_], [h_])
                for k in range(8):
                    P.dma("sp", dst[b, k * 128:(k + 1) * 128, s0:s0 + n], h_[:, k, :n], h_, dst)
            P.barrier()


def shared_inputs(inp, cfg):
    c = cfg
    sh = {}
    sh["vecs"] = build_vecs(inp)
    sh["w_mod"] = np.ascontiguousarray(inp["w_mod"], np.float32)
    sh["ffn_wg"] = np.ascontiguousarray(inp["ffn_w_gate"], np.float32)
    sh["ffn_wu"] = np.ascontiguousarray(inp["ffn_w_up"], np.float32)
    sh["ffn_wd"] = np.ascontiguousarray(inp["ffn_w_down"], np.float32)
    sh["moe_wg"] = np.ascontiguousarray(inp["moe_w_gate"], np.float32)
    sh["moe_wu"] = np.ascontiguousarray(inp["moe_w_up"], np.float32)
    sh["moe_wd"] = np.ascontiguousarray(inp["moe_w_down"], np.float32)
    sh["router"] = np.ascontiguousarray(
        np.asarray(inp["moe_router"], np.float32).reshape(2, c.KC, 128, c.NE).transpose(0, 2, 1, 3))
    sh["pool_w"] = np.ascontiguousarray(inp["pool_w"], np.float32)
    nmax = max(c.T, c.C)
    ic = np.ones((2, 4, nmax), np.float32)
    for si, n in enumerate((c.T, c.C)):
        pos = np.arange(n)
        for g, w in enumerate(POOL_WINDOWS):
            lo = np.clip(pos - w // 2, 0, n)
            hi = np.clip(pos - w // 2 + w, 0, n)
            ic[si, g, :n] = 1.0 / (hi - lo).astype(np.float32)
    sh["invcnt"] = ic
    sh["ident"] = np.eye(128, dtype=np.float32)
    w_in = np.asarray(inp["ab_w_in"], np.float32)
    swp = np.arange(32).reshape(2, 2, 8)[:, ::-1, :].reshape(32)
    o_kr = 384 + 256
    kr = w_in[:, :, o_kr:o_kr + 32]
    sh["w_in"] = np.ascontiguousarray(np.concatenate(
        [w_in[:, :, :o_kr + 32], kr[:, :, swp], w_in[:, :, o_kr + 32:]], axis=2))
    w_uq = np.asarray(inp["ab_w_uq"], np.float32).reshape(2, 384, 8, 96)
    nope, ropq = w_uq[..., :64], w_uq[..., 64:]
    sh["w_uq"] = np.ascontiguousarray(np.concatenate([ropq, nope, ropq[..., swp]], axis=3).reshape(2, 384, 1024))
    w_ukv = np.asarray(inp["ab_w_ukv"], np.float32).reshape(2, 256, 8, 128)
    z = np.zeros((2, 256, 8, 32), np.float32)
    sh["w_uk"] = np.ascontiguousarray(np.concatenate([z, w_ukv[..., :64], z], axis=3).reshape(2, 256, 1024))
    sh["w_uv"] = np.ascontiguousarray(w_ukv[..., 64:].reshape(2, 256, 512))
    sh["w_out"] = np.ascontiguousarray(inp["ab_w_out"], np.float32)
    sh["gnorm"] = np.ascontiguousarray(inp["hgrn_g_norm"], np.float32)
    tpos = np.arange(c.T)
    row = (tpos // c.grid_w).astype(np.float32)
    col = (tpos % c.grid_w).astype(np.float32)
    inv = (1.0 / (np.float32(10000.0) ** (np.arange(0, 16, 2, dtype=np.float32) / np.float32(16)))).astype(np.float32)
    ar = row[:, None] * inv[None, :]
    ac = col[:, None] * inv[None, :]
    ang = np.concatenate([ar, ar, ac, ac], axis=1).astype(np.float32)
    sign = np.tile(np.concatenate([-np.ones(8, np.float32), np.ones(8, np.float32)]), 2)
    rope = np.zeros((2, 32, c.TT), np.float32)
    rope[0, :, :c.T] = np.cos(ang).T
    rope[1, :, :c.T] = (np.sin(ang) * sign[None, :]).T
    rope[0, :, c.T:] = 1.0
    sh["rope"] = rope
    ii = np.arange(128)
    same = (ii[:, None] // 32) == (ii[None, :] // 32)
    tm = np.zeros((2, 128, 128), np.float32)
    tm[0] = (same & (ii[:, None] <= ii[None, :]))
    tm[1] = (same & (ii[:, None] >= ii[None, :]))
    sh["trimask"] = tm
    return sh


def core_inputs(inp, cfg, b0):
    c = cfg
    x = np.asarray(inp["x"], np.float32)[b0:b0 + c.BL]
    cx = np.asarray(inp["ctx"], np.float32)[b0:b0 + c.BL]
    xT = np.ascontiguousarray(np.concatenate([x.transpose(0, 2, 1), cx.transpose(0, 2, 1)], axis=2))
    cv = np.concatenate([np.asarray(inp["c"], np.float32)[b0:b0 + c.BL], np.asarray(inp["c_ctx"], np.float32)[None, :]], axis=0)
    cond = np.ascontiguousarray(cv.reshape(c.NB, c.KC, 128).transpose(2, 1, 0))
    return {"xT": xT, "cond": cond}


def kernel(**inp):
    cfg = Cfg()
    bld = Builder(cfg)
    nc = bld.build()
    sh = shared_inputs(inp, cfg)
    names = bld.input_names()
    in_maps = []
    for core in range(8):
        m = dict(sh)
        m.update(core_inputs(inp, cfg, core * cfg.BL))
        in_maps.append({k: m[k] for k in names})
    res = run_bass_kernel_spmd(nc, in_maps, core_ids=list(range(8)))
    outs = []
    for core in range(8):
        oT = np.asarray(res.results[core]["outT"])
        outs.append(oT.transpose(0, 2, 1))
    return np.ascontiguousarray(np.concatenate(outs, axis=0).astype(np.float32))
```

```python
import contextlib
import numpy as np
import concourse.bass as bass
import concourse.mybir as mybir
from concourse.bass_utils import run_bass_kernel_spmd

F32 = mybir.dt.float32
BF16 = mybir.dt.bfloat16
AF = mybir.ActivationFunctionType
ALU = mybir.AluOpType
AX = mybir.AxisListType

EPS = 1e-6
POOL_WINDOWS = (2, 4, 8, 16)


class Buf:
    def __init__(self, name, t):
        self.name = name
        self.t = t
        self.w = []
        self.r = []
        self.dkey = None

    def __getitem__(self, idx):
        return self.t[idx]


class Prog:
    ENG = ("pe", "dve", "act", "pool", "sp")

    def __init__(self, nc, n_dsem=40):
        self.nc = nc
        self.e = {"pe": nc.tensor, "dve": nc.vector, "act": nc.scalar, "pool": nc.gpsimd, "sp": nc.sync}
        self.sems = {}
        self.cnt = {}
        for k in self.ENG:
            self.sems[k] = nc.alloc_semaphore("s_" + k)
            self.cnt[k] = 0
        self.waited = {k: {} for k in self.ENG}
        self.dfree = {"sw": [], "hw": []}
        for i in range(n_dsem):
            key = ("d", i)
            self.sems[key] = nc.alloc_semaphore("d_%d" % i)
            self.cnt[key] = 0
            self.dfree["sw" if i < n_dsem // 3 else "hw"].append(key)
        self.stage_bufs = []
        self.uid = 0
        self.in_names = []

    def _name(self, name):
        self.uid += 1
        return "%s_%d" % (name, self.uid)

    def sb(self, stack, name, shape, dt):
        t = stack.enter_context(self.nc.sbuf_tensor(self._name(name), list(shape), dt))
        b = Buf(name, t)
        self.stage_bufs.append(b)
        return b

    def ps(self, stack, name, shape, dt=F32):
        t = stack.enter_context(self.nc.psum_tensor(self._name(name), list(shape), dt))
        b = Buf(name, t)
        b.excl = True
        self.stage_bufs.append(b)
        return b

    def dram(self, name, shape, dt, kind="Internal"):
        if kind == "ExternalInput":
            self.in_names.append(name)
        t = self.nc.dram_tensor(name, list(shape), dt, kind=kind)
        b = Buf(name, t)
        b.persistent = True
        return b

    def _dkey(self, b, q):
        kind = "sw" if q == "pool" else "hw"
        if b.dkey is None:
            b.dkey = self.dfree[kind].pop()
            b.dkind = kind
            self.stage_bufs.append(b) if b not in self.stage_bufs else None
        assert b.dkind == kind, "buffer %s gets DMAs from both SW and HW DGE" % b.name
        return b.dkey

    def _wait(self, eng, events):
        wd = self.waited[eng]
        need = {}
        for (k, v) in events:
            if need.get(k, 0) < v:
                need[k] = v
        for k, v in need.items():
            if k == "pe" and eng == "pe":
                continue
            if wd.get(k, 0) >= v:
                continue
            self.e[eng].wait_ge(self.sems[k], v)
            wd[k] = v

    @staticmethod
    def _compact(evs):
        m = {}
        for k, v in evs:
            if m.get(k, 0) < v:
                m[k] = v
        return list(m.items())

    def op(self, eng, fn, reads=(), writes=()):
        reads = [b for b in reads if b is not None]
        ev = []
        for b in reads:
            ev += b.w
            if getattr(b, "excl", False):
                ev += [e for e in b.r if e[0] != eng]
        for b in writes:
            ev += b.w
            ev += b.r
        self._wait(eng, ev)
        inst = fn()
        self.cnt[eng] += 1
        inst.then_inc(self.sems[eng], 1)
        me = (eng, self.cnt[eng])
        for b in writes:
            b.w = [me]
            b.r = []
        for b in reads:
            if b not in writes:
                b.r.append(me)
                if len(b.r) > 16:
                    b.r = self._compact(b.r)
        return inst

    def dma(self, q, out_ap, in_ap, src, dst, **kw):
        key = self._dkey(dst, q)
        ev = list(src.w) + list(dst.r) + [e for e in dst.w if e[0] != key]
        self._wait(q, ev)
        inst = self.e[q].dma_start(out=out_ap, in_=in_ap, **kw)
        self.cnt[key] += 16
        inst.then_inc(self.sems[key], 16)
        me = (key, self.cnt[key])
        dst.w = [me]
        dst.r = []
        src.r.append(me)
        if len(src.r) > 16:
            src.r = self._compact(src.r)
        return inst

    def barrier(self):
        allev = [(k, v) for k, v in self.cnt.items() if v > 0]
        for eng in self.ENG:
            self._wait(eng, allev)
        for b in self.stage_bufs:
            if b.dkey is not None:
                self.dfree[b.dkind].append(b.dkey)
                b.dkey = None
            b.w = []
            b.r = []
        self.stage_bufs = []

    def mm(self, ps, out_ap, lhsT, rhs, rd, start=True, stop=True, **kw):
        return self.op("pe", lambda: self.nc.tensor.matmul(out_ap, lhsT, rhs, start=start, stop=stop, **kw),
                       reads=rd, writes=[ps])

    def act(self, out_ap, in_ap, func, rd, wr, **kw):
        return self.op("act", lambda: self.nc.scalar.activation(out=out_ap, in_=in_ap, func=func, **kw),
                       reads=rd, writes=wr)

    def tt(self, eng, out_ap, a, b, op, rd, wr):
        return self.op(eng, lambda: self.e[eng].tensor_tensor(out=out_ap, in0=a, in1=b, op=op), reads=rd, writes=wr)

    def ts(self, eng, out_ap, a, s1, s2, op0, op1, rd, wr):
        if s2 is None:
            return self.op(eng, lambda: self.e[eng].tensor_scalar(out=out_ap, in0=a, scalar1=s1, scalar2=None, op0=op0),
                           reads=rd, writes=wr)
        return self.op(eng, lambda: self.e[eng].tensor_scalar(out=out_ap, in0=a, scalar1=s1, scalar2=s2, op0=op0, op1=op1),
                       reads=rd, writes=wr)

    def stt(self, out_ap, a, s, b, op0, op1, rd, wr):
        return self.op("dve", lambda: self.nc.vector.scalar_tensor_tensor(out=out_ap, in0=a, scalar=s, in1=b, op0=op0, op1=op1),
                       reads=rd, writes=wr)

    def copy(self, eng, out_ap, in_ap, rd, wr):
        if eng == "act":
            return self.op("act", lambda: self.nc.scalar.copy(out_ap, in_ap), reads=rd, writes=wr)
        return self.op(eng, lambda: self.e[eng].tensor_copy(out_ap, in_ap), reads=rd, writes=wr)

    def memset(self, eng, ap, val, wr):
        return self.op(eng, lambda: self.e[eng].memset(ap, val), writes=wr)


class Cfg:
    def __init__(self, T=2048, C=256, BL=2, layers=(0, 1, 2, 3), grid_w=64):
        self.D = 1024
        self.KC = 8
        self.T = T
        self.C = C
        self.TT = T + C
        self.BL = BL
        self.NB = BL + 1
        self.layers = tuple(layers)
        self.grid_w = grid_w
        self.DFF = 2816
        self.DFFE = 3584
        self.NE = 8
        self.blocks = []
        for s in range(0, T, 512):
            self.blocks.append((s, min(512, T - s), False))
        for s in range(0, C, 512):
            self.blocks.append((T + s, min(512, C - s), True))
        self.lat_blocks = [b for b in self.blocks if not b[2]]
        self.ctx_blocks = [b for b in self.blocks if b[2]]


def ctx_later(l):
    return any(m % 2 == 0 for m in range(l + 1, 4))


def vec_layout():
    off = {}
    n = 0

    def add(name, k):
        nonlocal n
        off[name] = (n, k)
        n += k

    for l in range(4):
        add("b_mod%d" % l, 48)
        add("ng%d_0" % l, 8)
        add("ng%d_1" % l, 8)
    add("final_g", 8)
    for j in range(2):
        add("g_cq%d" % j, 3)
        add("g_ckv%d" % j, 2)
        add("lbl%d_0" % j, 4)
        add("lbl%d_1" % j, 4)
        add("pool_b%d" % j, 8)
        add("pool_s%d" % j, 8)
    return off, n


def build_vecs(inp):
    off, n = vec_layout()
    v = np.zeros((128, n), np.float32)

    def put(name, arr):
        o, k = off[name]
        v[:, o:o + k] = np.asarray(arr, np.float32).reshape(k, 128).T

    for l in range(4):
        put("b_mod%d" % l, inp["b_mod"][l])
        put("ng%d_0" % l, inp["norm_g"][l, 0])
        put("ng%d_1" % l, inp["norm_g"][l, 1])
    put("final_g", inp["final_g"])
    for j in range(2):
        put("g_cq%d" % j, inp["ab_g_cq"][j])
        put("g_ckv%d" % j, inp["ab_g_ckv"][j])
        put("lbl%d_0" % j, inp["hgrn_lb_logits"][j, 0])
        put("lbl%d_1" % j, inp["hgrn_lb_logits"][j, 1])
        put("pool_b%d" % j, inp["pool_b"][j].reshape(-1))
        put("pool_s%d" % j, inp["pool_scale"][j])
    return v


class Builder:
    def __init__(self, cfg):
        self.cfg = cfg
        nc = bass.Bass("TRN2", target_bir_lowering=False)
        self.nc = nc
        self.P = Prog(nc)
        P = self.P
        c = cfg
        self.xT = P.dram("xT", [c.BL, c.D, c.TT], F32, kind="ExternalInput")
        self.cond = P.dram("cond", [128, c.KC, c.NB], F32, kind="ExternalInput")
        self.voff, nv = vec_layout()
        self.vecs_d = P.dram("vecs", [128, nv], F32, kind="ExternalInput")
        self.w_mod = P.dram("w_mod", [4, c.D, 6 * c.D], F32, kind="ExternalInput")
        self.ffn_wg = P.dram("ffn_wg", [2, c.D, c.DFF], F32, kind="ExternalInput")
        self.ffn_wu = P.dram("ffn_wu", [2, c.D, c.DFF], F32, kind="ExternalInput")
        self.ffn_wd = P.dram("ffn_wd", [2, c.DFF, c.D], F32, kind="ExternalInput")
        if getattr(c, "no_moe", False):
            self.moe_wg = P.dram("moe_wg", [2, 1, 8, 8], F32, kind="ExternalInput")
            self.moe_wu = P.dram("moe_wu", [2, 1, 8, 8], F32, kind="ExternalInput")
            self.moe_wd = P.dram("moe_wd", [2, 1, 8, 8], F32, kind="ExternalInput")
        else:
            self.moe_wg = P.dram("moe_wg", [2, c.NE, c.D, c.DFFE], F32, kind="ExternalInput")
            self.moe_wu = P.dram("moe_wu", [2, c.NE, c.D, c.DFFE], F32, kind="ExternalInput")
            self.moe_wd = P.dram("moe_wd", [2, c.NE, c.DFFE, c.D], F32, kind="ExternalInput")
        self.router = P.dram("router", [2, 128, c.KC, c.NE], F32, kind="ExternalInput")
        self.pool_w = P.dram("pool_w", [2, 4, 256, 256], F32, kind="ExternalInput")
        self.invcnt = P.dram("invcnt", [2, 4, max(c.T, c.C)], F32, kind="ExternalInput")
        self.w_in = P.dram("w_in", [2, c.D, 3264], F32, kind="ExternalInput")
        self.w_uq = P.dram("w_uq", [2, 384, 1024], F32, kind="ExternalInput")
        self.w_uk = P.dram("w_uk", [2, 256, 1024], F32, kind="ExternalInput")
        self.w_uv = P.dram("w_uv", [2, 256, 512], F32, kind="ExternalInput")
        self.w_out = P.dram("w_out", [2, 1024, c.D], F32, kind="ExternalInput")
        self.gnorm = P.dram("gnorm", [2, 128], F32, kind="ExternalInput")
        self.rope = P.dram("rope", [2, 32, c.TT], F32, kind="ExternalInput")
        self.trimask = P.dram("trimask", [2, 128, 128], F32, kind="ExternalInput")
        self.outT = P.dram("outT", [c.BL, c.D, c.T], F32, kind="ExternalOutput")
        self.cqn_d = P.dram("cqn_d", [384, c.TT], BF16)
        self.ckvn_d = P.dram("ckvn_d", [256, c.TT], BF16)
        self.krr_d = P.dram("krr_d", [32, c.TT], BF16)
        self.qh_d = P.dram("qh_d", [512, c.TT], F32)
        self.hf_d = P.dram("hf_d", [2, 512, c.TT], F32)
        self.vg_d = P.dram("vg_d", [c.TT, 512], BF16)
        self.sg_d = P.dram("sg_d", [c.TT, 512], F32)
        self.mix_d = P.dram("mix_d", [1024, c.TT], BF16)
        self.hbuf = [P.dram("hA", [c.BL, c.D, c.TT], F32), P.dram("hB", [c.BL, c.D, c.TT], F32)]
        self.cur = self.xT

        self._in_names = P.in_names
        self.glob = contextlib.ExitStack()
        g = self.glob
        self.vecs = P.sb(g, "vecs", [128, nv], F32)
        self.modT = P.sb(g, "modT", [128, 4, 48, c.NB], F32)
        self.ones_bf = P.sb(g, "ones_bf", [128, 128], BF16)
        self.eps_t = P.sb(g, "eps", [128, 1], F32)
        self.ident = P.sb(g, "ident", [128, 128], F32)
        self.ident_d = P.dram("ident", [128, 128], F32, kind="ExternalInput")
        self.psb = [P.ps(g, "ps%d" % i, [128, 512], F32) for i in range(8)]

    def input_names(self):
        return [k for k, v in self.__dict__.items() if False] or self._in_names

    def vec(self, name):
        o, k = self.voff[name]
        return self.vecs[:, o:o + k]

    def next_h(self):
        return self.hbuf[0] if self.cur is not self.hbuf[0] else self.hbuf[1]

    def setup(self):
        P, nc, c = self.P, self.nc, self.cfg
        P.dma("sp", self.vecs[:], self.vecs_d[:], self.vecs_d, self.vecs)
        P.memset("dve", self.ones_bf[:], 1.0, [self.ones_bf])
        P.dma("sp", self.ident[:], self.ident_d[:], self.ident_d, self.ident)
        P.memset("dve", self.eps_t[:], EPS, [self.eps_t])
        with contextlib.ExitStack() as st:
            cf = P.sb(st, "cond_f", [128, c.KC, c.NB], F32)
            sg = P.sb(st, "cond_sg", [128, c.KC, c.NB], F32)
            cb = P.sb(st, "cond_b", [128, c.KC, c.NB], BF16)
            wsl = [P.sb(st, "wmod_sl%d" % i, [128, c.KC, 1024], BF16) for i in range(2)]
            P.dma("sp", cf[:], self.cond[:], self.cond, cf)
            P.act(sg[:], cf[:], AF.Sigmoid, [cf], [sg])
            P.tt("dve", cb[:], cf[:], sg[:], ALU.mult, [cf, sg], [cb])
            it = 0
            for l in c.layers:
                for s in range(6):
                    w = wsl[it % 2]
                    it += 1
                    src = self.w_mod[l].rearrange("(k p) n -> p k n", p=128)[:, :, s * 1024:(s + 1) * 1024]
                    P.dma("pool", w[:], src, self.w_mod, w)
                    ps = self.psb[it % 2]
                    for jj in range(8):
                        for k in range(c.KC):
                            P.mm(ps, ps[:, jj * c.NB:(jj + 1) * c.NB], w[:, k, jj * 128:(jj + 1) * 128], cb[:, k, :],
                                 [w, cb], start=(k == 0), stop=(k == c.KC - 1))
                    o, _ = self.voff["b_mod%d" % l]
                    bsl = self.vecs[:, o + s * 8:o + s * 8 + 8]
                    P.tt("dve", self.modT[:, l, s * 8:(s + 1) * 8, :],
                         ps[:, 0:8 * c.NB].rearrange("p (j n) -> p j n", n=c.NB),
                         bsl.unsqueeze(2).to_broadcast([128, 8, c.NB]), ALU.add, [ps, self.vecs], [self.modT])
            P.barrier()

    def mod(self, l, which, n):
        return self.modT[:, l, which * 8:(which + 1) * 8, n]

    def make_gp(self, st, l, which_norm, n, name):
        P = self.P
        gp = P.sb(st, name, [128, 8], F32)
        sc = self.mod(l, 1 + 3 * which_norm, n)
        P.stt(gp[:], sc, 1.0, self.vec("ng%d_%d" % (l, which_norm)), ALU.add, ALU.mult, [self.modT, self.vecs], [gp])
        return gp

    def rstd_block(self, hsrc, hap_fn, n, sq, rstd, ps, nk=8, inv_n=1.0 / 1024):
        P = self.P
        for k in range(nk):
            P.act(sq[:, k, :n], hap_fn(k), AF.Square, [hsrc], [sq])
        on = self.ones_bf
        for k in range(nk):
            P.mm(ps, ps[:, :n], on[:], sq[:, k, :n], [on, sq], start=(k == 0), stop=(k == nk - 1))
        P.act(rstd[:, :n], ps[:, :n], AF.Sqrt, [ps, self.eps_t], [rstd], bias=self.eps_t[:], scale=inv_n)
        P.op("dve", lambda: self.nc.vector.reciprocal(rstd[:, :n], rstd[:, :n]), reads=[rstd], writes=[rstd])

    def ffn_stage(self, l, b, moe):
        P, nc, c = self.P, self.nc, self.cfg
        j = l // 2
        with_ctx = ctx_later(l)
        blocks = c.blocks if with_ctx else c.lat_blocks
        ntok = c.TT if with_ctx else c.T
        last = (l == c.layers[-1])
        src = self.cur
        dst = self.next_h()
        nf = (c.DFFE if moe else c.DFF) // 128
        groups = []
        f0 = 0
        while f0 < nf:
            groups.append((f0, min(4, nf - f0)))
            f0 += 4
        ne = c.NE if moe else 1
        if not getattr(self, 'do_ffn', True):
            ne = 0
            moe = False
        with contextlib.ExitStack() as st:
            hT = P.sb(st, "hT", [128, 8, ntok], F32)
            vT = P.sb(st, "vT", [128, 8, ntok], BF16)
            sq = P.sb(st, "sq", [128, 8, 256], BF16)
            rstd = P.sb(st, "rstd", [128, 256], F32)
            tmp = P.sb(st, "tmp", [128, 256], F32)
            nblocks = []
            for (s0_, n_, isc_) in blocks:
                for q0 in range(0, n_, 256):
                    nblocks.append((s0_ + q0, min(256, n_ - q0), isc_))
            wg = [P.sb(st, "wg%d" % i, [128, 8, 512], BF16) for i in range(2)]
            wu = [P.sb(st, "wu%d" % i, [128, 8, 512], BF16) for i in range(2)]
            wd = [P.sb(st, "wd%d" % i, [128, 4, 1024], BF16) for i in range(2)]
            hid = [P.sb(st, "hid%d" % i, [128, 4, 512], BF16) for i in range(2)]
            sl = [P.sb(st, "sl%d" % i, [128, 512], F32) for i in range(2)]
            sgb = [P.sb(st, "sgb%d" % i, [128, 512], F32) for i in range(2)]
            gp = {}
            for n in ([b, c.BL] if with_ctx else [b]):
                gp[n] = self.make_gp(st, l, 1, n, "gp%d" % n)
            for k in range(8):
                P.dma("sp", hT[:, k, :], src[b, k * 128:(k + 1) * 128, 0:ntok], src, hT)
            psn = self.psb[6]
            for (s0, n, isc) in nblocks:
                cn = c.BL if isc else b
                self.rstd_block(hT, lambda k: hT[:, k, s0:s0 + n], n, sq, rstd, psn)
                for k in range(8):
                    P.tt("dve", tmp[:, :n], hT[:, k, s0:s0 + n], rstd[:, :n], ALU.mult, [hT, rstd], [tmp])
                    P.act(vT[:, k, s0:s0 + n], tmp[:, :n], AF.Identity, [tmp, gp[cn], self.modT], [vT],
                          scale=gp[cn][:, k:k + 1], bias=self.mod(l, 3, cn)[:, k:k + 1])
            gates = None
            dbg = getattr(c, "dbg", ())
            if moe:
                if "nogates" not in dbg:
                    gates = self.moe_gates(st, l, vT, blocks, ntok)
                gbc = P.sb(st, "gbc", [128, ntok], F32)
                if "nogates" in dbg or "nogbc" in dbg:
                    P.memset("dve", gbc[:], 0.125, [gbc])
            items = [(e, f0, fg) for e in range(ne) for (f0, fg) in groups]
            Wsrc = (self.moe_wg, self.moe_wu, self.moe_wd) if moe else (self.ffn_wg, self.ffn_wu, self.ffn_wd)

            def emit_dma(i):
                e, f0, fg = items[i]
                a = i % 2
                if moe:
                    Wg, Wu, Wd = self.moe_wg[j, e], self.moe_wu[j, e], self.moe_wd[j, e]
                else:
                    Wg, Wu, Wd = self.ffn_wg[j], self.ffn_wu[j], self.ffn_wd[j]
                P.dma("pool", wg[a][:, :, :fg * 128],
                      Wg.rearrange("(k p) f -> p k f", p=128)[:, :, f0 * 128:(f0 + fg) * 128], Wsrc[0], wg[a])
                P.dma("pool", wu[a][:, :, :fg * 128],
                      Wu.rearrange("(k p) f -> p k f", p=128)[:, :, f0 * 128:(f0 + fg) * 128], Wsrc[1], wu[a])
                P.dma("pool", wd[a][:, :fg, :],
                      Wd[f0 * 128:(f0 + fg) * 128, :].rearrange("(f p) d -> p f d", p=128), Wsrc[2], wd[a])

            def emit_gbc(e):
                sel, gT = gates
                for (s0, n, isc) in blocks:
                    ps = self.psb[6]
                    for t0 in range(0, n, 128):
                        P.mm(ps, ps[:, t0:t0 + 128], sel[:, e, :], gT[:, s0 + t0:s0 + t0 + 128], [sel, gT])
                    P.copy("act", gbc[:, s0:s0 + n], ps[:, :n], [ps], [gbc])

            cnt_ = [0]

            def GU(i, blk, hb):
                e, f0, fg = items[i]
                a = i % 2
                (s0, n, isc) = blk
                for jj in range(fg):
                    q_ = cnt_[0]
                    cnt_[0] += 1
                    pg = self.psb[q_ % 2]
                    pu = self.psb[2 + q_ % 2]
                    for k in range(8):
                        P.mm(pg, pg[:, :n], wg[a][:, k, jj * 128:(jj + 1) * 128], vT[:, k, s0:s0 + n], [wg[a], vT],
                             start=(k == 0), stop=(k == 7))
                    for k in range(8):
                        P.mm(pu, pu[:, :n], wu[a][:, k, jj * 128:(jj + 1) * 128], vT[:, k, s0:s0 + n], [wu[a], vT],
                             start=(k == 0), stop=(k == 7))
                    s_ = sl[q_ % 2]
                    P.act(s_[:, :n], pg[:, :n], AF.Silu, [pg], [s_])
                    if moe:
                        g_ = sgb[q_ % 2]
                        P.tt("dve", g_[:, :n], s_[:, :n], gbc[:, s0:s0 + n], ALU.mult, [s_, gbc], [g_])
                        s_ = g_
                    P.tt("dve", hb[:, jj, :n], pu[:, :n], s_[:, :n], ALU.mult, [pu, s_], [hb])

            dcnt = [0]

            def DD(i, blk, hb):
                e, f0, fg = items[i]
                a = i % 2
                (s0, n, isc) = blk
                cn = c.BL if isc else b
                for dch in range(8):
                    py = self.psb[4 + dcnt[0] % 2]
                    dcnt[0] += 1
                    for jj in range(fg):
                        P.mm(py, py[:, :n], wd[a][:, jj, dch * 128:(dch + 1) * 128], hb[:, jj, :n], [wd[a], hb],
                             start=(jj == 0), stop=(jj == fg - 1))
                    P.stt(hT[:, dch, s0:s0 + n], py[:, :n], self.mod(l, 5, cn)[:, dch:dch + 1], hT[:, dch, s0:s0 + n],
                          ALU.mult, ALU.add, [py, self.modT, hT], [hT])

            if items:
                emit_dma(0)
            prev = None
            wi = 0
            cur_e = -1
            for i in range(len(items)):
                for bi, blk in enumerate(blocks):
                    if moe and items[i][0] != cur_e:
                        cur_e = items[i][0]
                        emit_gbc(cur_e)
                    hb = hid[wi % 2]
                    wi += 1
                    GU(i, blk, hb)
                    if prev is not None:
                        DD(*prev)
                    prev = (i, blk, hb)
                    if bi == 0 and i + 1 < len(items):
                        emit_dma(i + 1)
            if prev is not None:
                DD(*prev)
            if last:
                for (s0, n, isc) in [nb for nb in nblocks if not nb[2]]:
                    self.rstd_block(hT, lambda k: hT[:, k, s0:s0 + n], n, sq, rstd, psn)
                    for k in range(8):
                        P.tt("dve", tmp[:, :n], hT[:, k, s0:s0 + n], rstd[:, :n], ALU.mult, [hT, rstd], [tmp])
                        o_ = sl[k % 2]
                        P.ts("dve", o_[:, :n], tmp[:, :n], self.vec("final_g")[:, k:k + 1], None, ALU.mult, None,
                             [tmp, self.vecs], [o_])
                        P.dma("sp", self.outT[b, k * 128:(k + 1) * 128, s0:s0 + n], o_[:, :n], o_, self.outT)
            else:
                for k in range(8):
                    P.dma("sp", dst[b, k * 128:(k + 1) * 128, 0:ntok], hT[:, k, :], hT, dst)
            P.barrier()

    def moe_gates(self, st, l, vT, blocks, ntok):
        P, nc, c = self.P, self.nc, self.cfg
        j = l // 2
        rt = P.sb(st, "router", [128, 8, c.NE], BF16)
        P.dma("pool", rt[:], self.router[j], self.router, rt)
        sel = P.sb(st, "sel", [8, c.NE, 128], F32)
        gT = P.sb(st, "gT", [8, ntok], F32)
        lg = P.sb(st, "lg", [128, 8], F32)
        top = P.sb(st, "top", [128, 8], F32)
        ex = P.sb(st, "ex", [128, 8], F32)
        msk = P.sb(st, "msk", [128, 8], F32)
        den = P.sb(st, "den", [128, 1], F32)
        nmx = P.sb(st, "nmx", [128, 1], F32)
        gt = P.sb(st, "gt", [128, 8], F32)
        P.copy("dve", sel[:], self.ident[0:8, 0:8].unsqueeze(2).to_broadcast([8, 8, 128]), [self.ident], [sel])
        ps = self.psb[7]
        pt = self.psb[6]
        for (s0, n, isc) in blocks:
            for t0 in range(0, n, 128):
                a0 = s0 + t0
                for k in range(8):
                    P.mm(ps, ps[:, 0:8], vT[:, k, a0:a0 + 128], rt[:, k, :], [vT, rt], start=(k == 0), stop=(k == 7))
                P.copy("dve", lg[:], ps[:, 0:8], [ps], [lg])
                P.op("dve", lambda: nc.vector.max(out=top[:], in_=lg[:]), reads=[lg], writes=[top])
                P.ts("dve", nmx[:], top[:, 0:1], -1.0, None, ALU.mult, None, [top], [nmx])
                P.act(ex[:], lg[:], AF.Exp, [lg, nmx], [ex], bias=nmx[:], scale=1.0)
                P.ts("dve", msk[:], lg[:], top[:, 1:2], None, ALU.is_ge, None, [lg, top], [msk])
                P.tt("dve", gt[:], ex[:], msk[:], ALU.mult, [ex, msk], [gt])
                P.op("dve", lambda: nc.vector.reduce_sum(out=den[:], in_=gt[:], axis=AX.X), reads=[gt], writes=[den])
                P.op("dve", lambda: nc.vector.reciprocal(den[:], den[:]), reads=[den], writes=[den])
                P.ts("dve", gt[:], gt[:], den[:, 0:1], None, ALU.mult, None, [gt, den], [gt])
                P.op("pe", lambda: nc.tensor.transpose(pt[0:8, 0:128], gt[:], self.ident[:]), reads=[gt, self.ident], writes=[pt])
                P.copy("act", gT[:, a0:a0 + 128], pt[0:8, 0:128], [pt], [gT])
        return sel, gT

    def ensure_ident(self):
        if getattr(self, "_ident_done", False):
            return
        P, nc = self.P, self.nc
        P.memset("pool", self.ident[:], 0.0, [self.ident])
        P.op("pool", lambda: nc.gpsimd.affine_select(out=self.ident[:], in_=self.ident[:], pattern=[[-1, 128]],
                                                      compare_op=ALU.not_equal, fill=1.0, base=0, channel_multiplier=1),
             reads=[self.ident], writes=[self.ident])
        self._ident_done = True

    def pool_stage(self, l, b):
        P, nc, c = self.P, self.nc, self.cfg
        j = l // 2
        with_ctx = ctx_later(l)
        src = self.cur
        dst = self.next_h()
        streams = [(0, c.T, b, 0)] + ([(c.T, c.C, c.BL, 1)] if with_ctx else [])
        nmax = max(c.T, c.C)
        with contextlib.ExitStack() as st:
            hT = P.sb(st, "hT", [128, 8, nmax], F32)
            sq = P.sb(st, "sq", [128, 8, 512], BF16)
            rstd = P.sb(st, "rstd", [128, nmax], F32)
            upad = P.sb(st, "upad", [128, nmax + 16], F32)
            a1 = P.sb(st, "a1", [128, nmax + 16], F32)
            a2 = P.sb(st, "a2", [128, nmax + 16], F32)
            icn = P.sb(st, "icn", [128, 4, nmax], F32)
            pooled = P.sb(st, "pooled", [128, 8, nmax], BF16)
            pw = P.sb(st, "pw", [128, 4, 2, 256], BF16)
            A = P.sb(st, "A", [128, 8], F32)
            Bc = P.sb(st, "Bc", [128, 8], F32)
            tmp = P.sb(st, "tmp", [128, 512], F32)
            P.dma("pool", pw[:], self.pool_w[j].rearrange("g (k p) d -> p g k d", p=128), self.pool_w, pw)
            for (t0, ns, cn, si) in streams:
                P.dma("sp", icn[:].rearrange("p g n -> p (g n)"),
                      self.invcnt[si].rearrange("g n -> (g n)").partition_broadcast(128), self.invcnt, icn)
                gp = self.make_gp(st, l, 0, cn, "gp%d" % si)
                P.tt("dve", A[:], self.mod(l, 2, cn), self.vec("pool_s%d" % j), ALU.mult, [self.modT, self.vecs], [A])
                P.tt("dve", Bc[:], A[:], self.vec("pool_b%d" % j), ALU.mult, [A, self.vecs], [Bc])
                for k in range(8):
                    P.dma("sp", hT[:, k, :ns], src[b, k * 128:(k + 1) * 128, t0:t0 + ns], src, hT)
                for s0 in range(0, ns, 512):
                    n = min(512, ns - s0)
                    self.rstd_block(hT, lambda k: hT[:, k, s0:s0 + n], n, sq, tmp, self.psb[6])
                    P.copy("dve", rstd[:, s0:s0 + n], tmp[:, :n], [tmp], [rstd])
                P.memset("pool", upad[:], 0.0, [upad])
                for k in range(8):
                    g = k // 2
                    w = POOL_WINDOWS[g]
                    m = g + 1
                    u = upad[:, 8:8 + ns]
                    P.tt("dve", u, hT[:, k, :ns], rstd[:, :ns], ALU.mult, [hT, rstd], [upad])
                    P.act(u, u, AF.Identity, [upad, gp, self.modT], [upad],
                          scale=gp[:, k:k + 1], bias=self.mod(l, 0, cn)[:, k:k + 1])
                    W = ns + 16
                    cur_, cb_ = upad, upad
                    bufs = [a1, a2]
                    for mm_ in range(m):
                        sh = 1 << mm_
                        nb = bufs[mm_ % 2]
                        ln = W - 2 * sh + 1 if mm_ == 0 else W - (2 << mm_) + 1
                        ln = W - ((2 << mm_) - 1)
                        P.tt("pool", nb[:, :ln], cur_[:, 0:ln], cur_[:, sh:sh + ln], ALU.add, [cb_], [nb])
                        cur_, cb_ = nb, nb
                    o0 = 8 - w // 2
                    P.tt("dve", a1[:, :ns] if cur_ is a2 else a2[:, :ns], cur_[:, o0:o0 + ns], icn[:, g, :ns], ALU.mult,
                         [cb_, icn], [a1 if cur_ is a2 else a2])
                    oth = a1 if cur_ is a2 else a2
                    P.tt("dve", pooled[:, k, :ns], oth[:, :ns], u, ALU.subtract, [oth, upad], [pooled])
                for s0 in range(0, ns, 512):
                    n = min(512, ns - s0)
                    for g in range(4):
                        for dd in range(2):
                            dch = 2 * g + dd
                            ps = self.psb[dch % 2]
                            for kk in range(2):
                                P.mm(ps, ps[:, :n], pw[:, g, kk, dd * 128:(dd + 1) * 128], pooled[:, 2 * g + kk, s0:s0 + n],
                                     [pw, pooled], start=(kk == 0), stop=(kk == 1))
                            P.stt(hT[:, dch, s0:s0 + n], ps[:, :n], A[:, dch:dch + 1], hT[:, dch, s0:s0 + n],
                                  ALU.mult, ALU.add, [ps, A, hT], [hT])
                            P.ts("dve", hT[:, dch, s0:s0 + n], hT[:, dch, s0:s0 + n], Bc[:, dch:dch + 1], None, ALU.add, None,
                                 [hT, Bc], [hT])
                for k in range(8):
                    P.dma("sp", dst[b, k * 128:(k + 1) * 128, t0:t0 + ns], hT[:, k, :ns], hT, dst)
            P.barrier()

    def build(self, do_mixer=True, do_ffn=True):
        c = self.cfg
        self.do_ffn = do_ffn
        self.setup()
        for l in c.layers:
            even = (l % 2 == 0)
            for b in range(c.BL):
                save = self.cur
                if do_mixer:
                    if even:
                        self.ab_stage(l, b)
                    else:
                        self.pool_stage(l, b)
                    self.cur = self.next_h()
                self.ffn_stage(l, b, moe=not even)
                self.cur = save
            if do_mixer:
                self.cur = self.next_h()
            self.cur = self.next_h()
        self.P.barrier()
        self.glob.close()
        return self.nc

    def ab_stage(self, l, b):
        dbg = getattr(self.cfg, "dbg", ())
        if "no1" not in dbg:
            self.ab_inproj(l, b)
        if "no2" not in dbg:
            self.ab_mla(l, b)
        if "no3" not in dbg:
            self.ab_gla(l, b)
        if "no4" not in dbg:
            self.ab_outproj(l, b)

    def ab_inproj(self, l, b):
        P, nc, c = self.P, self.nc, self.cfg
        j = l // 2
        src = self.cur
        OQ, OF, OI, OG = 704, 1216, 2240, 2752
        with contextlib.ExitStack() as st:
            win = P.sb(st, "win", [128, 8, 3264], BF16)
            hblk = [P.sb(st, "hblk%d" % i, [128, 8, 512], F32) for i in range(2)]
            uT = [P.sb(st, "uT%d" % i, [128, 8, 512], BF16) for i in range(2)]
            sq = P.sb(st, "sq", [128, 8, 512], BF16)
            rstd = P.sb(st, "rstd", [128, 512], F32)
            tmp = P.sb(st, "tmp", [128, 512], F32)
            cf = P.sb(st, "cf", [128, 3, 512], F32)
            cn = P.sb(st, "cn", [128, 3, 512], BF16)
            rs2 = P.sb(st, "rs2", [128, 512], F32)
            kro = P.sb(st, "kro", [32, 512], BF16)
            t1 = P.sb(st, "t1", [32, 512], F32)
            t2 = P.sb(st, "t2", [32, 512], F32)
            rp = P.sb(st, "rp", [32, 2, c.TT], F32)
            fo = [P.sb(st, "fo%d" % i, [128, 512], F32) for i in range(3)]
            tk = [P.sb(st, "tk%d" % i, [128, 512], BF16) for i in range(2)]
            tg = [P.sb(st, "tg%d" % i, [128, 512], F32) for i in range(2)]
            gps = {}
            for n_ in (b, c.BL):
                gps[n_] = self.make_gp(st, l, 0, n_, "gp%d" % n_)
            for s in range(0, 3264, 1088):
                P.dma("pool", win[:, :, s:s + 1088], self.w_in[j].rearrange("(k p) n -> p k n", p=128)[:, :, s:s + 1088], self.w_in, win)
            P.dma("sp", rp[:], self.rope[:].rearrange("a d t -> d a t"), self.rope, rp)
            ic = 0
            for bi, (s0, n, isc) in enumerate(c.blocks):
                cnd = c.BL if isc else b
                hb, u = hblk[bi % 2], uT[bi % 2]
                for k in range(8):
                    P.dma("sp", hb[:, k, :n], src[b, k * 128:(k + 1) * 128, s0:s0 + n], src, hb)
                self.rstd_block(hb, lambda k: hb[:, k, :n], n, sq, rstd, self.psb[7])
                for k in range(8):
                    P.tt("dve", tmp[:, :n], hb[:, k, :n], rstd[:, :n], ALU.mult, [hb, rstd], [tmp])
                    P.act(u[:, k, :n], tmp[:, :n], AF.Identity, [tmp, gps[cnd], self.modT], [u],
                          scale=gps[cnd][:, k:k + 1], bias=self.mod(l, 0, cnd)[:, k:k + 1])

                def proj(ps, col0, m):
                    for k in range(8):
                        P.mm(ps, ps[:m, :n], win[:, k, col0:col0 + m], u[:, k, :n], [win, u], start=(k == 0), stop=(k == 7))

                for (col0, nch, gname, dd) in ((0, 3, "g_cq%d" % j, self.cqn_d), (384, 2, "g_ckv%d" % j, self.ckvn_d)):
                    for q_ in range(nch):
                        ps = self.psb[ic % 4]
                        ic += 1
                        proj(ps, col0 + q_ * 128, 128)
                        P.copy("act", cf[:, q_, :n], ps[:, :n], [ps], [cf])
                    self.rstd_block(cf, lambda k: cf[:, k, :n], n, sq, rs2, self.psb[7], nk=nch, inv_n=1.0 / (nch * 128))
                    for q_ in range(nch):
                        P.tt("dve", tmp[:, :n], cf[:, q_, :n], rs2[:, :n], ALU.mult, [cf, rs2], [tmp])
                        P.ts("dve", cn[:, q_, :n], tmp[:, :n], self.vec(gname)[:, q_:q_ + 1], None, ALU.mult, None,
                             [tmp, self.vecs], [cn])
                    P.dma("sp", dd[:, s0:s0 + n].rearrange("(q p) t -> p q t", p=128), cn[:, :nch, :n], cn, dd)
                pa, pb = self.psb[ic % 4], self.psb[(ic + 1) % 4]
                ic += 2
                proj(pa, 640, 32)
                proj(pb, 672, 32)
                P.tt("dve", t1[:, :n], pa[:32, :n], rp[:, 0, s0:s0 + n], ALU.mult, [pa, rp], [t1])
                P.tt("dve", t2[:, :n], pb[:32, :n], rp[:, 1, s0:s0 + n], ALU.mult, [pb, rp], [t2])
                P.tt("pool", kro[:, :n], t1[:, :n], t2[:, :n], ALU.add, [t1, t2], [kro])
                P.dma("sp", self.krr_d[:, s0:s0 + n], kro[:, :n], kro, self.krr_d)
                for q_ in range(12):
                    ps = self.psb[ic % 4]
                    ic += 1
                    proj(ps, OQ + q_ * 128, 128)
                    o_ = fo[q_ % 3]
                    if q_ < 4:
                        P.act(o_[:, :n], ps[:, :n], AF.Silu, [ps], [o_])
                        P.dma("sp", self.qh_d[q_ * 128:(q_ + 1) * 128, s0:s0 + n], o_[:, :n], o_, self.qh_d)
                    else:
                        P.copy("act", o_[:, :n], ps[:, :n], [ps], [o_])
                        d_, h_ = (q_ - 4) // 4, (q_ - 4) % 4
                        P.dma("sp", self.hf_d[d_, h_ * 128:(h_ + 1) * 128, s0:s0 + n], o_[:, :n], o_, self.hf_d)
                for t0 in range(0, n, 128):
                    a0 = s0 + t0
                    for which, col0 in ((0, OI), (1, OG)):
                        ps = self.psb[ic % 4]
                        ic += 1
                        for k in range(8):
                            P.mm(ps, ps[:, :512], u[:, k, t0:t0 + 128], win[:, k, col0:col0 + 512], [u, win],
                                 start=(k == 0), stop=(k == 7))
                        if which == 0:
                            o_ = tk[(t0 // 128) % 2]
                            P.copy("act", o_[:], ps[:, :512], [ps], [o_])
                            P.dma("sp", self.vg_d[a0:a0 + 128, :], o_[:], o_, self.vg_d)
                        else:
                            o_ = tg[(t0 // 128) % 2]
                            P.act(o_[:], ps[:, :512], AF.Silu, [ps], [o_])
                            P.dma("sp", self.sg_d[a0:a0 + 128, :], o_[:], o_, self.sg_d)
            P.barrier()

    def ab_mla(self, l, b):
        P, nc, c = self.P, self.nc, self.cfg
        j = l // 2
        need_ctx = ctx_later(l)
        NT = c.TT // 128
        scale = 96.0 ** -0.5
        with contextlib.ExitStack() as st:
            cqn = P.sb(st, "cqn", [128, 3, c.TT], BF16)
            ckvn = P.sb(st, "ckvn", [128, 2, c.TT], BF16)
            krr = P.sb(st, "krr", [32, c.TT], BF16)
            rp = P.sb(st, "rp", [32, 2, c.TT], F32)
            wq = P.sb(st, "wq", [128, 3, 1024], BF16)
            wk = P.sb(st, "wk", [128, 2, 1024], BF16)
            wv = P.sb(st, "wv", [128, 2, 512], BF16)
            qT = P.sb(st, "qT", [96, 8, c.TT], BF16)
            kT = P.sb(st, "kT", [96, 8, c.TT], BF16)
            va = P.sb(st, "va", [128, NT, 8, 66], BF16)
            PT = [P.sb(st, "PT%d" % i, [128, NT, 512], BF16) for i in range(2)]
            t1 = P.sb(st, "t1", [32, 512], F32)
            t2 = P.sb(st, "t2", [32, 512], F32)
            alat = P.sb(st, "alat", [128, 4, 512], BF16)
            rden = P.sb(st, "rden", [128, 1], F32)
            mo = [P.sb(st, "mo%d" % i, [128, 4, 512], BF16) for i in range(2)]
            idb = P.sb(st, "idb", [128, 128], BF16)
            P.copy("dve", idb[:], self.ident[:], [self.ident], [idb])
            P.dma("sp", cqn[:], self.cqn_d[:].rearrange("(q p) t -> p q t", p=128), self.cqn_d, cqn)
            P.dma("sp", ckvn[:], self.ckvn_d[:].rearrange("(q p) t -> p q t", p=128), self.ckvn_d, ckvn)
            P.dma("sp", krr[:], self.krr_d[:], self.krr_d, krr)
            P.dma("sp", rp[:], self.rope[:].rearrange("a d t -> d a t"), self.rope, rp)
            P.dma("pool", wq[:], self.w_uq[j].rearrange("(k p) n -> p k n", p=128), self.w_uq, wq)
            P.dma("pool", wk[:], self.w_uk[j].rearrange("(k p) n -> p k n", p=128), self.w_uk, wk)
            P.dma("pool", wv[:], self.w_uv[j].rearrange("(k p) n -> p k n", p=128), self.w_uv, wv)
            P.memset("pool", va[:], 1.0, [va])
            ic = 0
            dbg = getattr(c, "dbg", ())
            for (s0, n, isc) in (c.blocks if "mla_noproj" not in dbg else []):
                for h in range(8):
                    pa, pb = self.psb[ic % 4], self.psb[(ic + 1) % 4]
                    pk = self.psb[(ic + 2) % 4]
                    ic += 3
                    if "skq" in dbg:
                        continue
                    for k in range(3):
                        P.mm(pa, pa[:96, :n], wq[:, k, h * 128:h * 128 + 96], cqn[:, k, s0:s0 + n], [wq, cqn], start=(k == 0), stop=(k == 2))
                    for k in range(3):
                        P.mm(pb, pb[:32, :n], wq[:, k, h * 128 + 96:h * 128 + 128], cqn[:, k, s0:s0 + n], [wq, cqn], start=(k == 0), stop=(k == 2))
                    P.copy("act", qT[:, h, s0:s0 + n], pa[:96, :n], [pa], [qT])
                    P.tt("dve", t1[:, :n], pa[:32, :n], rp[:, 0, s0:s0 + n], ALU.mult, [pa, rp], [t1])
                    P.tt("dve", t2[:, :n], pb[:32, :n], rp[:, 1, s0:s0 + n], ALU.mult, [pb, rp], [t2])
                    P.tt("dve" if "mla_dve" in dbg else "pool", qT[0:32, h, s0:s0 + n], t1[:, :n], t2[:, :n], ALU.add, [t1, t2], [qT])
                    if "skk" in dbg:
                        continue
                    for k in range(2):
                        P.mm(pk, pk[:96, :n], wk[:, k, h * 128:h * 128 + 96], ckvn[:, k, s0:s0 + n], [wk, ckvn], start=(k == 0), stop=(k == 1))
                    P.copy("act", kT[:, h, s0:s0 + n], pk[:96, :n], [pk], [kT])
                    P.copy("dve" if "mla_dve" in dbg else "pool", kT[0:32, h, s0:s0 + n], krr[:, s0:s0 + n], [krr], [kT])
                for t0 in (range(0, n, 128) if "skv" not in dbg else []):
                    a0 = s0 + t0
                    ps = self.psb[ic % 4]
                    ic += 1
                    for k in range(2):
                        P.mm(ps, ps[:, :512], ckvn[:, k, a0:a0 + 128], wv[:, k, :], [ckvn, wv], start=(k == 0), stop=(k == 1))
                    P.copy("act", va[:, a0 // 128, :, 0:64], ps[:, :512].rearrange("p (h d) -> p h d", d=64), [ps], [va])
            qblocks = c.blocks if need_ctx else c.lat_blocks
            if "mla_noattn" in dbg:
                qblocks = []
            ib = 0
            for (s0, n, isc) in qblocks:
                kts = list(range(c.T // 128, NT)) if isc else list(range(NT))
                for h in range(8):
                    pt_ = PT[ib % 2]
                    ib += 1
                    for ki, kt in enumerate(kts):
                        ps = self.psb[ki % 2]
                        P.mm(ps, ps[:, :n], kT[:, h, kt * 128:(kt + 1) * 128], qT[:, h, s0:s0 + n], [kT, qT])
                        P.act(pt_[:, ki, :n], ps[:, :n], AF.Exp, [ps], [pt_], scale=scale)
                    for qs in range(n // 128):
                        po = self.psb[2 + qs % 2]
                        for ki, kt in enumerate(kts):
                            P.mm(po, po[:, 0:65], pt_[:, ki, qs * 128:(qs + 1) * 128], va[:, kt, h, 0:65], [pt_, va],
                                 start=(ki == 0), stop=(ki == len(kts) - 1))
                        P.op("dve", lambda: nc.vector.reciprocal(rden[:], po[:, 64:65]), reads=[po], writes=[rden])
                        P.ts("dve", alat[:, qs, h * 64:(h + 1) * 64], po[:, 0:64], rden[:, 0:1], None, ALU.mult, None,
                             [po, rden], [alat])
                m_ = mo[(s0 // 512) % 2]
                for qs in range(n // 128):
                    for fc in range(4):
                        ptb = self.psb[4 + (qs * 4 + fc) % 2]
                        pv = ptb[:].bitcast(BF16)
                        P.op("pe", lambda: nc.tensor.transpose(pv[:, 0:128], alat[:, qs, fc * 128:(fc + 1) * 128], idb[:]),
                             reads=[alat, idb], writes=[ptb])
                        P.copy("act", m_[:, fc, qs * 128:(qs + 1) * 128], pv[:, 0:128], [ptb], [m_])
                P.dma("sp", self.mix_d[0:512, s0:s0 + n].rearrange("(q p) t -> p q t", p=128), m_[:, :, :n], m_, self.mix_d)
            P.barrier()

    def ab_gla(self, l, b):
        P, nc, c = self.P, self.nc, self.cfg
        j = l // 2
        need_ctx = ctx_later(l)
        NT = c.TT // 128
        NCH = c.TT // 32
        lat_tiles = list(range(c.T // 128))
        ctx_tiles = list(range(c.T // 128, NT))
        with contextlib.ExitStack() as st:
            vg = P.sb(st, "vg", [128, NT, 512], BF16)
            oacc = P.sb(st, "oacc", [128, NT, 512], F32)
            smask = P.sb(st, "smask", [128, c.TT], F32)
            tri = P.sb(st, "tri", [128, 2, 128], F32)
            oml = P.sb(st, "oml", [128, 2, 4], F32)
            qh = P.sb(st, "qh", [128, c.TT], F32)
            hf = P.sb(st, "hf", [128, c.TT], F32)
            kin = P.sb(st, "kin", [128, c.TT], F32)
            G = P.sb(st, "G", [128, c.TT], F32)
            eG = P.sb(st, "eG", [128, c.TT], F32)
            w1 = P.sb(st, "w1", [128, c.TT], F32)
            qe = P.sb(st, "qe", [128, c.TT], BF16)
            ke = P.sb(st, "ke", [128, c.TT], BF16)
            kd = P.sb(st, "kd", [128, c.TT], BF16)
            kdt = [P.sb(st, "kdt%d" % i, [128, 4, 128], BF16) for i in range(2)]
            qeb = [P.sb(st, "qeb%d" % i, [128, 4, 128], BF16) for i in range(2)]
            bm = P.sb(st, "bm", [128, 4], F32)
            cm = P.sb(st, "cm", [128, 4, 128], BF16)
            atm = [P.sb(st, "atm%d" % i, [128, 128], BF16) for i in range(2)]
            S = P.sb(st, "S", [128, 128], F32)
            Sb = [P.sb(st, "Sb%d" % i, [128, 128], BF16) for i in range(2)]
            idb = P.sb(st, "idb", [128, 128], BF16)
            gnb = P.sb(st, "gnb", [128, 128], F32)
            sgt = [P.sb(st, "sgt%d" % i, [128, 512], F32) for i in range(2)]
            ssq = P.sb(st, "ssq", [128, 4], F32)
            junk = P.sb(st, "junk", [128, 128], F32)
            yt = P.sb(st, "yt", [128, 512], F32)
            blat = P.sb(st, "blat", [128, 512], BF16)
            mo = [P.sb(st, "mo%d" % i, [128, 4, 128], BF16) for i in range(2)]
            P.copy("dve", idb[:], self.ident[:], [self.ident], [idb])
            P.op("dve", lambda: nc.vector.reduce_sum(out=bm[:], in_=self.ident[:].rearrange("p (c l) -> p c l", l=32), axis=AX.X),
                 reads=[self.ident], writes=[bm])
            P.memset("pool", cm[:], 0.0, [cm])
            for cc in range(4):
                P.memset("pool", cm[:, cc, cc * 32:(cc + 1) * 32], 1.0, [cm])
            P.dma("sp", vg[:], self.vg_d[:].rearrange("(t p) f -> p t f", p=128), self.vg_d, vg)
            P.dma("sp", tri[:], self.trimask[:].rearrange("a s t -> s a t"), self.trimask, tri)
            P.dma("sp", gnb[:], self.gnorm[j].partition_broadcast(128), self.gnorm, gnb)
            P.memset("pool", smask[:], 1.0, [smask])
            P.memset("pool", smask[:].rearrange("p (c l) -> p c l", l=32)[:, :, 0:1], 0.0, [smask])
            if j == 0:
                P.memset("dve", oml[:], 1.0, [oml])
            else:
                for d in range(2):
                    P.tt("dve", oml[:, d, :], self.vec("lbl0_%d" % d), self.vec("lbl1_%d" % d), ALU.subtract, [self.vecs], [oml])
                P.act(oml[:], oml[:], AF.Sigmoid, [oml], [oml])
            ich = 0
            for h in range(4):
                P.dma("sp", qh[:], self.qh_d[h * 128:(h + 1) * 128, :], self.qh_d, qh)
                for d in range(2):
                    P.dma("sp", hf[:], self.hf_d[d, h * 128:(h + 1) * 128, :], self.hf_d, hf)
                    P.act(kin[:], hf[:], AF.Sigmoid, [hf], [kin], scale=-1.0)
                    P.ts("dve", kin[:], kin[:], oml[:, d, h:h + 1], None, ALU.mult, None, [kin, oml], [kin])
                    P.act(w1[:], kin[:], AF.Ln, [kin], [w1], scale=-1.0, bias=1.0)
                    if d == 0:
                        P.op("dve", lambda: nc.vector.tensor_tensor_scan(out=G[:], data0=smask[:], data1=w1[:], initial=0.0,
                                                                          op0=ALU.mult, op1=ALU.add), reads=[smask, w1], writes=[G])
                    else:
                        P.op("dve", lambda: nc.vector.tensor_tensor_scan(out=G[:, ::-1], data0=smask[:], data1=w1[:, ::-1], initial=0.0,
                                                                          op0=ALU.mult, op1=ALU.add), reads=[smask, w1], writes=[G])
                    G3 = G[:].rearrange("p (c l) -> p c l", l=32)
                    gend = G3[:, :, 31:32] if d == 0 else G3[:, :, 0:1]
                    P.act(eG[:], G[:], AF.Exp, [G], [eG])
                    P.tt("pool", qe[:], qh[:], eG[:], ALU.mult, [qh, eG], [qe])
                    P.tt("dve", w1[:].rearrange("p (c l) -> p c l", l=32), gend.to_broadcast([128, NCH, 32]), G3, ALU.subtract, [G], [w1])
                    P.act(w1[:], w1[:], AF.Exp, [w1], [w1])
                    P.tt("pool", kd[:], kin[:], w1[:], ALU.mult, [kin, w1], [kd])
                    P.act(w1[:], G[:], AF.Exp, [G], [w1], scale=-1.0)
                    P.tt("dve", ke[:], kin[:], w1[:], ALU.mult, [kin, w1], [ke])
                    eG3 = eG[:].rearrange("p (c l) -> p c l", l=32)
                    order = (ctx_tiles + lat_tiles) if d == 0 else (ctx_tiles[::-1] + lat_tiles[::-1])
                    if "gla_noscan" in getattr(c, "dbg", ()):
                        order = []
                    P.memset("dve", S[:], 0.0, [S])
                    cur_sb = 0
                    P.memset("pool", Sb[0][:], 0.0, [Sb[0]])
                    for ti, tl in enumerate(order):
                        a0 = tl * 128
                        skip_out = (tl in ctx_tiles) and not need_ctx
                        kt_, am_ = kdt[ti % 2], atm[ti % 2]
                        pk = self.psb[ti % 2]
                        pkv = pk[:].bitcast(BF16)
                        P.op("pe", lambda: nc.tensor.transpose(pkv[:, 0:128], kd[:, a0:a0 + 128], idb[:]), reads=[kd, idb], writes=[pk])
                        P.tt("dve", kt_[:], pkv[:, 0:128].unsqueeze(1).to_broadcast([128, 4, 128]),
                             bm[:].unsqueeze(2).to_broadcast([128, 4, 128]), ALU.mult, [pk, bm], [kt_])
                        qb_ = qeb[ti % 2]
                        if not skip_out:
                            P.tt("pool", qb_[:], qe[:, a0:a0 + 128].unsqueeze(1).to_broadcast([128, 4, 128]), cm[:], ALU.mult, [qe, cm], [qb_])
                        po = self.psb[2 + ti % 2]
                        if not skip_out:
                            pa = self.psb[4 + ti % 2]
                            P.mm(pa, pa[:, 0:128], ke[:, a0:a0 + 128], qe[:, a0:a0 + 128], [ke, qe])
                            P.tt("dve", am_[:], pa[:, 0:128], tri[:, d, :], ALU.mult, [pa, tri], [am_])
                            P.mm(po, po[:, 0:128], am_[:], vg[:, tl, h * 128:(h + 1) * 128], [am_, vg], start=True, stop=False)
                        pd = self.psb[6 + ti % 2]
                        crange = range(4) if d == 0 else range(3, -1, -1)
                        for cc in crange:
                            sl_ = slice(cc * 32, (cc + 1) * 32)
                            P.mm(pd, pd[:, cc * 128:(cc + 1) * 128], kt_[:, cc, :], vg[:, tl, h * 128:(h + 1) * 128], [kt_, vg])
                        for ci, cc in enumerate(crange):
                            sl_ = slice(cc * 32, (cc + 1) * 32)
                            if not skip_out:
                                P.mm(po, po[:, 0:128], qb_[:, cc, :], Sb[cur_sb][:], [qb_, Sb[cur_sb]],
                                     start=False, stop=(ci == 3))
                            chn = tl * 4 + cc
                            eg = eG3[:, chn, 31:32] if d == 0 else eG3[:, chn, 0:1]
                            P.stt(S[:], S[:], eg, pd[:, cc * 128:(cc + 1) * 128], ALU.mult, ALU.add, [S, eG, pd], [S])
                            cur_sb ^= 1
                            P.copy("act", Sb[cur_sb][:], S[:], [S], [Sb[cur_sb]])
                        if not skip_out:
                            if d == 0:
                                P.copy("act", oacc[:, tl, h * 128:(h + 1) * 128], po[:, 0:128], [po], [oacc])
                            else:
                                P.tt("dve", oacc[:, tl, h * 128:(h + 1) * 128], po[:, 0:128], oacc[:, tl, h * 128:(h + 1) * 128],
                                     ALU.add, [po, oacc], [oacc])
            tiles = (lat_tiles + ctx_tiles) if need_ctx else lat_tiles
            if "gla_noread" in getattr(c, "dbg", ()):
                tiles = []
            for ti, tl in enumerate(tiles):
                a0 = tl * 128
                sg_ = sgt[ti % 2]
                P.dma("sp", sg_[:], self.sg_d[a0:a0 + 128, :], self.sg_d, sg_)
                for h in range(4):
                    P.act(junk[:], oacc[:, tl, h * 128:(h + 1) * 128], AF.Square, [oacc], [junk, ssq], accum_out=ssq[:, h:h + 1])
                P.act(ssq[:], ssq[:], AF.Sqrt, [ssq, self.eps_t], [ssq], bias=self.eps_t[:], scale=1.0 / 128)
                P.op("dve", lambda: nc.vector.reciprocal(ssq[:], ssq[:]), reads=[ssq], writes=[ssq])
                for h in range(4):
                    P.stt(yt[:, h * 128:(h + 1) * 128], oacc[:, tl, h * 128:(h + 1) * 128], ssq[:, h:h + 1], gnb[:], ALU.mult, ALU.mult,
                          [oacc, ssq, gnb], [yt])
                P.tt("dve", blat[:], yt[:], sg_[:], ALU.mult, [yt, sg_], [blat])
                m_ = mo[ti % 2]
                for fc in range(4):
                    ptb = self.psb[fc % 2]
                    pv = ptb[:].bitcast(BF16)
                    P.op("pe", lambda: nc.tensor.transpose(pv[:, 0:128], blat[:, fc * 128:(fc + 1) * 128], idb[:]), reads=[blat, idb], writes=[ptb])
                    P.copy("act", m_[:, fc, :], pv[:, 0:128], [ptb], [m_])
                P.dma("sp", self.mix_d[512:1024, a0:a0 + 128].rearrange("(q p) t -> p q t", p=128), m_[:], m_, self.mix_d)
            P.barrier()

    def ab_outproj(self, l, b):
        P, nc, c = self.P, self.nc, self.cfg
        j = l // 2
        need_ctx = ctx_later(l)
        src = self.cur
        dst = self.next_h()
        blocks = c.blocks if need_ctx else c.lat_blocks
        with contextlib.ExitStack() as st:
            wo = P.sb(st, "wo", [128, 8, 1024], BF16)
            mx = [P.sb(st, "mx%d" % i, [128, 8, 512], BF16) for i in range(2)]
            hb = [P.sb(st, "hb%d" % i, [128, 8, 512], F32) for i in range(2)]
            P.dma("pool", wo[:], self.w_out[j].rearrange("(k p) n -> p k n", p=128), self.w_out, wo)
            for bi, (s0, n, isc) in enumerate(blocks):
                cnd = c.BL if isc else b
                m_, h_ = mx[bi % 2], hb[bi % 2]
                P.dma("sp", m_[:, :, :n], self.mix_d[:, s0:s0 + n].rearrange("(q p) t -> p q t", p=128), self.mix_d, m_)
                for k in range(8):
                    P.dma("sp", h_[:, k, :n], src[b, k * 128:(k + 1) * 128, s0:s0 + n], src, h_)
                for dch in range(8):
                    ps = self.psb[dch % 4]
                    for k in range(8):
                        P.mm(ps, ps[:, :n], wo[:, k, dch * 128:(dch + 1) * 128], m_[:, k, :n], [wo, m_], start=(k == 0), stop=(k == 7))
                    P.stt(h_[:, dch, :n], ps[:, :n], self.mod(l, 2, cnd)[:, dch:dch + 1], h_[:, dch, :n], ALU.mult, ALU.add,
                          [ps, self.modT, h_], [h_])
                for k in range(8):
                    P.dma("sp", dst[b, k * 128:(k + 1) * 128, s0:s0 + n], h_[:, k, :n], h_, dst)
            P.barrier()


def shared_inputs(inp, cfg):
    c = cfg
    sh = {}
    sh["vecs"] = build_vecs(inp)
    sh["w_mod"] = np.ascontiguousarray(inp["w_mod"], np.float32)
    sh["ffn_wg"] = np.ascontiguousarray(inp["ffn_w_gate"], np.float32)
    sh["ffn_wu"] = np.ascontiguousarray(inp["ffn_w_up"], np.float32)
    sh["ffn_wd"] = np.ascontiguousarray(inp["ffn_w_down"], np.float32)
    sh["moe_wg"] = np.ascontiguousarray(inp["moe_w_gate"], np.float32)
    sh["moe_wu"] = np.ascontiguousarray(inp["moe_w_up"], np.float32)
    sh["moe_wd"] = np.ascontiguousarray(inp["moe_w_down"], np.float32)
    sh["router"] = np.ascontiguousarray(
        np.asarray(inp["moe_router"], np.float32).reshape(2, c.KC, 128, c.NE).transpose(0, 2, 1, 3))
    sh["pool_w"] = np.ascontiguousarray(inp["pool_w"], np.float32)
    nmax = max(c.T, c.C)
    ic = np.ones((2, 4, nmax), np.float32)
    for si, n in enumerate((c.T, c.C)):
        pos = np.arange(n)
        for g, w in enumerate(POOL_WINDOWS):
            lo = np.clip(pos - w // 2, 0, n)
            hi = np.clip(pos - w // 2 + w, 0, n)
            ic[si, g, :n] = 1.0 / (hi - lo).astype(np.float32)
    sh["invcnt"] = ic
    sh["ident"] = np.eye(128, dtype=np.float32)
    w_in = np.asarray(inp["ab_w_in"], np.float32)
    swp = np.arange(32).reshape(2, 2, 8)[:, ::-1, :].reshape(32)
    o_kr = 384 + 256
    kr = w_in[:, :, o_kr:o_kr + 32]
    sh["w_in"] = np.ascontiguousarray(np.concatenate(
        [w_in[:, :, :o_kr + 32], kr[:, :, swp], w_in[:, :, o_kr + 32:]], axis=2))
    w_uq = np.asarray(inp["ab_w_uq"], np.float32).reshape(2, 384, 8, 96)
    nope, ropq = w_uq[..., :64], w_uq[..., 64:]
    sh["w_uq"] = np.ascontiguousarray(np.concatenate([ropq, nope, ropq[..., swp]], axis=3).reshape(2, 384, 1024))
    w_ukv = np.asarray(inp["ab_w_ukv"], np.float32).reshape(2, 256, 8, 128)
    z = np.zeros((2, 256, 8, 32), np.float32)
    sh["w_uk"] = np.ascontiguousarray(np.concatenate([z, w_ukv[..., :64], z], axis=3).reshape(2, 256, 1024))
    sh["w_uv"] = np.ascontiguousarray(w_ukv[..., 64:].reshape(2, 256, 512))
    sh["w_out"] = np.ascontiguousarray(inp["ab_w_out"], np.float32)
    sh["gnorm"] = np.ascontiguousarray(inp["hgrn_g_norm"], np.float32)
    tpos = np.arange(c.T)
    row = (tpos // c.grid_w).astype(np.float32)
    col = (tpos % c.grid_w).astype(np.float32)
    inv = (1.0 / (np.float32(10000.0) ** (np.arange(0, 16, 2, dtype=np.float32) / np.float32(16)))).astype(np.float32)
    ar = row[:, None] * inv[None, :]
    ac = col[:, None] * inv[None, :]
    ang = np.concatenate([ar, ar, ac, ac], axis=1).astype(np.float32)
    sign = np.tile(np.concatenate([-np.ones(8, np.float32), np.ones(8, np.float32)]), 2)
    rope = np.zeros((2, 32, c.TT), np.float32)
    rope[0, :, :c.T] = np.cos(ang).T
    rope[1, :, :c.T] = (np.sin(ang) * sign[None, :]).T
    rope[0, :, c.T:] = 1.0
    sh["rope"] = rope
    ii = np.arange(128)
    same = (ii[:, None] // 32) == (ii[None, :] // 32)
    tm = np.zeros((2, 128, 128), np.float32)
    tm[0] = (same & (ii[:, None] <= ii[None, :]))
    tm[1] = (same & (ii[:, None] >= ii[None, :]))
    sh["trimask"] = tm
    return sh


def core_inputs(inp, cfg, b0):
    c = cfg
    x = np.asarray(inp["x"], np.float32)[b0:b0 + c.BL]
    cx = np.asarray(inp["ctx"], np.float32)[b0:b0 + c.BL]
    xT = np.ascontiguousarray(np.concatenate([x.transpose(0, 2, 1), cx.transpose(0, 2, 1)], axis=2))
    cv = np.concatenate([np.asarray(inp["c"], np.float32)[b0:b0 + c.BL], np.asarray(inp["c_ctx"], np.float32)[None, :]], axis=0)
    cond = np.ascontiguousarray(cv.reshape(c.NB, c.KC, 128).transpose(2, 1, 0))
    return {"xT": xT, "cond": cond}


def kernel(**inp):
    cfg = Cfg()
    bld = Builder(cfg)
    nc = bld.build()
    sh = shared_inputs(inp, cfg)
    names = bld.input_names()
    in_maps = []
    for core in range(8):
        m = dict(sh)
        m.update(core_inputs(inp, cfg, core * cfg.BL))
        in_maps.append({k: m[k] for k in names})
    res = run_bass_kernel_spmd(nc, in_maps, core_ids=list(range(8)))
    outs = []
    for core in range(8):
        oT = np.asarray(res.results[core]["outT"])
        outs.append(oT.transpose(0, 2, 1))
    return np.ascontiguousarray(np.concatenate(outs, axis=0).astype(np.float32))
```

```python
import contextlib
import numpy as np
import concourse.bass as bass
import concourse.mybir as mybir
from concourse.bass_utils import run_bass_kernel_spmd

F32 = mybir.dt.float32
BF16 = mybir.dt.bfloat16
AF = mybir.ActivationFunctionType
ALU = mybir.AluOpType
AX = mybir.AxisListType

EPS = 1e-6
POOL_WINDOWS = (2, 4, 8, 16)


class Buf:
    def __init__(self, name, t):
        self.name = name
        self.t = t
        self.w = []
        self.r = []
        self.dkey = None

    def __getitem__(self, idx):
        return self.t[idx]


class Prog:
    ENG = ("pe", "dve", "act", "pool", "sp")

    def __init__(self, nc, n_dsem=40):
        self.nc = nc
        self.e = {"pe": nc.tensor, "dve": nc.vector, "act": nc.scalar, "pool": nc.gpsimd, "sp": nc.sync}
        self.sems = {}
        self.cnt = {}
        for k in self.ENG:
            self.sems[k] = nc.alloc_semaphore("s_" + k)
            self.cnt[k] = 0
        self.waited = {k: {} for k in self.ENG}
        self.dfree = {"sw": [], "hw": []}
        for i in range(n_dsem):
            key = ("d", i)
            self.sems[key] = nc.alloc_semaphore("d_%d" % i)
            self.cnt[key] = 0
            self.dfree["sw" if i < n_dsem // 3 else "hw"].append(key)
        self.stage_bufs = []
        self.uid = 0
        self.in_names = []

    def _name(self, name):
        self.uid += 1
        return "%s_%d" % (name, self.uid)

    def sb(self, stack, name, shape, dt):
        t = stack.enter_context(self.nc.sbuf_tensor(self._name(name), list(shape), dt))
        b = Buf(name, t)
        self.stage_bufs.append(b)
        return b

    def ps(self, stack, name, shape, dt=F32):
        t = stack.enter_context(self.nc.psum_tensor(self._name(name), list(shape), dt))
        b = Buf(name, t)
        b.excl = True
        self.stage_bufs.append(b)
        return b

    def dram(self, name, shape, dt, kind="Internal"):
        if kind == "ExternalInput":
            self.in_names.append(name)
        t = self.nc.dram_tensor(name, list(shape), dt, kind=kind)
        b = Buf(name, t)
        b.persistent = True
        return b

    def _dkey(self, b, q):
        kind = "sw" if q == "pool" else "hw"
        if b.dkey is None:
            b.dkey = self.dfree[kind].pop()
            b.dkind = kind
            self.stage_bufs.append(b) if b not in self.stage_bufs else None
        assert b.dkind == kind, "buffer %s gets DMAs from both SW and HW DGE" % b.name
        return b.dkey

    def _wait(self, eng, events):
        wd = self.waited[eng]
        need = {}
        for (k, v) in events:
            if need.get(k, 0) < v:
                need[k] = v
        for k, v in need.items():
            if k == "pe" and eng == "pe":
                continue
            if wd.get(k, 0) >= v:
                continue
            self.e[eng].wait_ge(self.sems[k], v)
            wd[k] = v

    @staticmethod
    def _compact(evs):
        m = {}
        for k, v in evs:
            if m.get(k, 0) < v:
                m[k] = v
        return list(m.items())

    def op(self, eng, fn, reads=(), writes=()):
        reads = [b for b in reads if b is not None]
        ev = []
        for b in reads:
            ev += b.w
            if getattr(b, "excl", False):
                ev += [e for e in b.r if e[0] != eng]
        for b in writes:
            ev += b.w
            ev += b.r
        self._wait(eng, ev)
        inst = fn()
        self.cnt[eng] += 1
        inst.then_inc(self.sems[eng], 1)
        me = (eng, self.cnt[eng])
        for b in writes:
            b.w = [me]
            b.r = []
        for b in reads:
            if b not in writes:
                b.r.append(me)
                if len(b.r) > 16:
                    b.r = self._compact(b.r)
        return inst

    def dma(self, q, out_ap, in_ap, src, dst, **kw):
        key = self._dkey(dst, q)
        ev = list(src.w) + list(dst.r) + [e for e in dst.w if e[0] != key]
        self._wait(q, ev)
        inst = self.e[q].dma_start(out=out_ap, in_=in_ap, **kw)
        self.cnt[key] += 16
        inst.then_inc(self.sems[key], 16)
        me = (key, self.cnt[key])
        dst.w = [me]
        dst.r = []
        src.r.append(me)
        if len(src.r) > 16:
            src.r = self._compact(src.r)
        return inst

    def barrier(self):
        allev = [(k, v) for k, v in self.cnt.items() if v > 0]
        for eng in self.ENG:
            self._wait(eng, allev)
        for b in self.stage_bufs:
            if b.dkey is not None:
                self.dfree[b.dkind].append(b.dkey)
                b.dkey = None
            b.w = []
            b.r = []
        self.stage_bufs = []

    def mm(self, ps, out_ap, lhsT, rhs, rd, start=True, stop=True, **kw):
        return self.op("pe", lambda: self.nc.tensor.matmul(out_ap, lhsT, rhs, start=start, stop=stop, **kw),
                       reads=rd, writes=[ps])

    def act(self, out_ap, in_ap, func, rd, wr, **kw):
        return self.op("act", lambda: self.nc.scalar.activation(out=out_ap, in_=in_ap, func=func, **kw),
                       reads=rd, writes=wr)

    def tt(self, eng, out_ap, a, b, op, rd, wr):
        return self.op(eng, lambda: self.e[eng].tensor_tensor(out=out_ap, in0=a, in1=b, op=op), reads=rd, writes=wr)

    def ts(self, eng, out_ap, a, s1, s2, op0, op1, rd, wr):
        if s2 is None:
            return self.op(eng, lambda: self.e[eng].tensor_scalar(out=out_ap, in0=a, scalar1=s1, scalar2=None, op0=op0),
                           reads=rd, writes=wr)
        return self.op(eng, lambda: self.e[eng].tensor_scalar(out=out_ap, in0=a, scalar1=s1, scalar2=s2, op0=op0, op1=op1),
                       reads=rd, writes=wr)

    def stt(self, out_ap, a, s, b, op0, op1, rd, wr):
        return self.op("dve", lambda: self.nc.vector.scalar_tensor_tensor(out=out_ap, in0=a, scalar=s, in1=b, op0=op0, op1=op1),
                       reads=rd, writes=wr)

    def copy(self, eng, out_ap, in_ap, rd, wr):
        if eng == "act":
            return self.op("act", lambda: self.nc.scalar.copy(out_ap, in_ap), reads=rd, writes=wr)
        return self.op(eng, lambda: self.e[eng].tensor_copy(out_ap, in_ap), reads=rd, writes=wr)

    def memset(self, eng, ap, val, wr):
        return self.op(eng, lambda: self.e[eng].memset(ap, val), writes=wr)


class Cfg:
    def __init__(self, T=2048, C=256, BL=2, layers=(0, 1, 2, 3), grid_w=64):
        self.D = 1024
        self.KC = 8
        self.T = T
        self.C = C
        self.TT = T + C
        self.BL = BL
        self.NB = BL + 1
        self.layers = tuple(layers)
        self.grid_w = grid_w
        self.DFF = 2816
        self.DFFE = 3584
        self.NE = 8
        self.blocks = []
        for s in range(0, T, 512):
            self.blocks.append((s, min(512, T - s), False))
        for s in range(0, C, 512):
            self.blocks.append((T + s, min(512, C - s), True))
        self.lat_blocks = [b for b in self.blocks if not b[2]]
        self.ctx_blocks = [b for b in self.blocks if b[2]]


def ctx_later(l):
    return any(m % 2 == 0 for m in range(l + 1, 4))


def vec_layout():
    off = {}
    n = 0

    def add(name, k):
        nonlocal n
        off[name] = (n, k)
        n += k

    for l in range(4):
        add("b_mod%d" % l, 48)
        add("ng%d_0" % l, 8)
        add("ng%d_1" % l, 8)
    add("final_g", 8)
    for j in range(2):
        add("g_cq%d" % j, 3)
        add("g_ckv%d" % j, 2)
        add("lbl%d_0" % j, 4)
        add("lbl%d_1" % j, 4)
        add("pool_b%d" % j, 8)
        add("pool_s%d" % j, 8)
    return off, n


def build_vecs(inp):
    off, n = vec_layout()
    v = np.zeros((128, n), np.float32)

    def put(name, arr):
        o, k = off[name]
        v[:, o:o + k] = np.asarray(arr, np.float32).reshape(k, 128).T

    for l in range(4):
        put("b_mod%d" % l, inp["b_mod"][l])
        put("ng%d_0" % l, inp["norm_g"][l, 0])
        put("ng%d_1" % l, inp["norm_g"][l, 1])
    put("final_g", inp["final_g"])
    for j in range(2):
        put("g_cq%d" % j, inp["ab_g_cq"][j])
        put("g_ckv%d" % j, inp["ab_g_ckv"][j])
        put("lbl%d_0" % j, inp["hgrn_lb_logits"][j, 0])
        put("lbl%d_1" % j, inp["hgrn_lb_logits"][j, 1])
        put("pool_b%d" % j, inp["pool_b"][j].reshape(-1))
        put("pool_s%d" % j, inp["pool_scale"][j])
    return v


class Builder:
    def __init__(self, cfg):
        self.cfg = cfg
        nc = bass.Bass("TRN2", target_bir_lowering=False)
        self.nc = nc
        self.P = Prog(nc)
        P = self.P
        c = cfg
        self.xT = P.dram("xT", [c.BL, c.D, c.TT], F32, kind="ExternalInput")
        self.cond = P.dram("cond", [128, c.KC, c.NB], F32, kind="ExternalInput")
        self.voff, nv = vec_layout()
        self.vecs_d = P.dram("vecs", [128, nv], F32, kind="ExternalInput")
        self.w_mod = P.dram("w_mod", [4, c.D, 6 * c.D], F32, kind="ExternalInput")
        self.ffn_wg = P.dram("ffn_wg", [2, c.D, c.DFF], F32, kind="ExternalInput")
        self.ffn_wu = P.dram("ffn_wu", [2, c.D, c.DFF], F32, kind="ExternalInput")
        self.ffn_wd = P.dram("ffn_wd", [2, c.DFF, c.D], F32, kind="ExternalInput")
        if getattr(c, "no_moe", False):
            self.moe_wg = P.dram("moe_wg", [2, 1, 8, 8], F32, kind="ExternalInput")
            self.moe_wu = P.dram("moe_wu", [2, 1, 8, 8], F32, kind="ExternalInput")
            self.moe_wd = P.dram("moe_wd", [2, 1, 8, 8], F32, kind="ExternalInput")
        else:
            self.moe_wg = P.dram("moe_wg", [2, c.NE, c.D, c.DFFE], F32, kind="ExternalInput")
            self.moe_wu = P.dram("moe_wu", [2, c.NE, c.D, c.DFFE], F32, kind="ExternalInput")
            self.moe_wd = P.dram("moe_wd", [2, c.NE, c.DFFE, c.D], F32, kind="ExternalInput")
        self.router = P.dram("router", [2, 128, c.KC, c.NE], F32, kind="ExternalInput")
        self.pool_w = P.dram("pool_w", [2, 4, 256, 256], F32, kind="ExternalInput")
        self.invcnt = P.dram("invcnt", [2, 4, max(c.T, c.C)], F32, kind="ExternalInput")
        self.w_in = P.dram("w_in", [2, c.D, 3264], F32, kind="ExternalInput")
        self.w_uq = P.dram("w_uq", [2, 384, 1024], F32, kind="ExternalInput")
        self.w_uk = P.dram("w_uk", [2, 256, 1024], F32, kind="ExternalInput")
        self.w_uv = P.dram("w_uv", [2, 256, 512], F32, kind="ExternalInput")
        self.w_out = P.dram("w_out", [2, 1024, c.D], F32, kind="ExternalInput")
        self.gnorm = P.dram("gnorm", [2, 128], F32, kind="ExternalInput")
        self.rope = P.dram("rope", [2, 32, c.TT], F32, kind="ExternalInput")
        self.trimask = P.dram("trimask", [2, 128, 128], F32, kind="ExternalInput")
        self.outT = P.dram("outT", [c.BL, c.D, c.T], F32, kind="ExternalOutput")
        self.cqn_d = P.dram("cqn_d", [384, c.TT], BF16)
        self.ckvn_d = P.dram("ckvn_d", [256, c.TT], BF16)
        self.krr_d = P.dram("krr_d", [32, c.TT], BF16)
        self.qh_d = P.dram("qh_d", [512, c.TT], F32)
        self.hf_d = P.dram("hf_d", [2, 512, c.TT], F32)
        self.vg_d = P.dram("vg_d", [c.TT, 512], BF16)
        self.sg_d = P.dram("sg_d", [c.TT, 512], F32)
        self.mix_d = P.dram("mix_d", [1024, c.TT], BF16)
        self.hbuf = [P.dram("hA", [c.BL, c.D, c.TT], F32), P.dram("hB", [c.BL, c.D, c.TT], F32)]
        self.cur = self.xT

        self._in_names = P.in_names
        self.glob = contextlib.ExitStack()
        g = self.glob
        self.vecs = P.sb(g, "vecs", [128, nv], F32)
        self.modT = P.sb(g, "modT", [128, 4, 48, c.NB], F32)
        self.ones_bf = P.sb(g, "ones_bf", [128, 128], BF16)
        self.eps_t = P.sb(g, "eps", [128, 1], F32)
        self.ident = P.sb(g, "ident", [128, 128], F32)
        self.ident_d = P.dram("ident", [128, 128], F32, kind="ExternalInput")
        self.psb = [P.ps(g, "ps%d" % i, [128, 512], F32) for i in range(8)]

    def input_names(self):
        return [k for k, v in self.__dict__.items() if False] or self._in_names

    def vec(self, name):
        o, k = self.voff[name]
        return self.vecs[:, o:o + k]

    def next_h(self):
        return self.hbuf[0] if self.cur is not self.hbuf[0] else self.hbuf[1]

    def setup(self):
        P, nc, c = self.P, self.nc, self.cfg
        P.dma("sp", self.vecs[:], self.vecs_d[:], self.vecs_d, self.vecs)
        P.memset("dve", self.ones_bf[:], 1.0, [self.ones_bf])
        P.dma("sp", self.ident[:], self.ident_d[:], self.ident_d, self.ident)
        P.memset("dve", self.eps_t[:], EPS, [self.eps_t])
        with contextlib.ExitStack() as st:
            cf = P.sb(st, "cond_f", [128, c.KC, c.NB], F32)
            sg = P.sb(st, "cond_sg", [128, c.KC, c.NB], F32)
            cb = P.sb(st, "cond_b", [128, c.KC, c.NB], BF16)
            wsl = [P.sb(st, "wmod_sl%d" % i, [128, c.KC, 1024], BF16) for i in range(2)]
            P.dma("sp", cf[:], self.cond[:], self.cond, cf)
            P.act(sg[:], cf[:], AF.Sigmoid, [cf], [sg])
            P.tt("dve", cb[:], cf[:], sg[:], ALU.mult, [cf, sg], [cb])
            it = 0
            for l in c.layers:
                for s in range(6):
                    w = wsl[it % 2]
                    it += 1
                    src = self.w_mod[l].rearrange("(k p) n -> p k n", p=128)[:, :, s * 1024:(s + 1) * 1024]
                    P.dma("pool", w[:], src, self.w_mod, w)
                    ps = self.psb[it % 2]
                    for jj in range(8):
                        for k in range(c.KC):
                            P.mm(ps, ps[:, jj * c.NB:(jj + 1) * c.NB], w[:, k, jj * 128:(jj + 1) * 128], cb[:, k, :],
                                 [w, cb], start=(k == 0), stop=(k == c.KC - 1))
                    o, _ = self.voff["b_mod%d" % l]
                    bsl = self.vecs[:, o + s * 8:o + s * 8 + 8]
                    P.tt("dve", self.modT[:, l, s * 8:(s + 1) * 8, :],
                         ps[:, 0:8 * c.NB].rearrange("p (j n) -> p j n", n=c.NB),
                         bsl.unsqueeze(2).to_broadcast([128, 8, c.NB]), ALU.add, [ps, self.vecs], [self.modT])
            P.barrier()

    def mod(self, l, which, n):
        return self.modT[:, l, which * 8:(which + 1) * 8, n]

    def make_gp(self, st, l, which_norm, n, name):
        P = self.P
        gp = P.sb(st, name, [128, 8], F32)
        sc = self.mod(l, 1 + 3 * which_norm, n)
        P.stt(gp[:], sc, 1.0, self.vec("ng%d_%d" % (l, which_norm)), ALU.add, ALU.mult, [self.modT, self.vecs], [gp])
        return gp

    def rstd_block(self, hsrc, hap_fn, n, sq, rstd, ps, nk=8, inv_n=1.0 / 1024):
        P = self.P
        for k in range(nk):
            P.act(sq[:, k, :n], hap_fn(k), AF.Square, [hsrc], [sq])
        on = self.ones_bf
        for k in range(nk):
            P.mm(ps, ps[:, :n], on[:], sq[:, k, :n], [on, sq], start=(k == 0), stop=(k == nk - 1))
        P.act(rstd[:, :n], ps[:, :n], AF.Sqrt, [ps, self.eps_t], [rstd], bias=self.eps_t[:], scale=inv_n)
        P.op("dve", lambda: self.nc.vector.reciprocal(rstd[:, :n], rstd[:, :n]), reads=[rstd], writes=[rstd])

    def ffn_stage(self, l, b, moe):
        P, nc, c = self.P, self.nc, self.cfg
        j = l // 2
        with_ctx = ctx_later(l)
        blocks = c.blocks if with_ctx else c.lat_blocks
        ntok = c.TT if with_ctx else c.T
        last = (l == c.layers[-1])
        src = self.cur
        dst = self.next_h()
        nf = (c.DFFE if moe else c.DFF) // 128
        groups = []
        f0 = 0
        while f0 < nf:
            groups.append((f0, min(4, nf - f0)))
            f0 += 4
        ne = c.NE if moe else 1
        if not getattr(self, 'do_ffn', True):
            ne = 0
            moe = False
        with contextlib.ExitStack() as st:
            hT = P.sb(st, "hT", [128, 8, ntok], F32)
            vT = P.sb(st, "vT", [128, 8, ntok], BF16)
            sq = P.sb(st, "sq", [128, 8, 256], BF16)
            rstd = P.sb(st, "rstd", [128, 256], F32)
            tmp = P.sb(st, "tmp", [128, 256], F32)
            nblocks = []
            for (s0_, n_, isc_) in blocks:
                for q0 in range(0, n_, 256):
                    nblocks.append((s0_ + q0, min(256, n_ - q0), isc_))
            wg = [P.sb(st, "wg%d" % i, [128, 8, 512], BF16) for i in range(2)]
            wu = [P.sb(st, "wu%d" % i, [128, 8, 512], BF16) for i in range(2)]
            wd = [P.sb(st, "wd%d" % i, [128, 4, 1024], BF16) for i in range(2)]
            hid = [P.sb(st, "hid%d" % i, [128, 4, 512], BF16) for i in range(2)]
            sl = [P.sb(st, "sl%d" % i, [128, 512], F32) for i in range(2)]
            sgb = [P.sb(st, "sgb%d" % i, [128, 512], F32) for i in range(2)]
            gp = {}
            for n in ([b, c.BL] if with_ctx else [b]):
                gp[n] = self.make_gp(st, l, 1, n, "gp%d" % n)
            for k in range(8):
                P.dma("sp", hT[:, k, :], src[b, k * 128:(k + 1) * 128, 0:ntok], src, hT)
            psn = self.psb[6]
            for (s0, n, isc) in nblocks:
                cn = c.BL if isc else b
                self.rstd_block(hT, lambda k: hT[:, k, s0:s0 + n], n, sq, rstd, psn)
                for k in range(8):
                    P.tt("dve", tmp[:, :n], hT[:, k, s0:s0 + n], rstd[:, :n], ALU.mult, [hT, rstd], [tmp])
                    P.act(vT[:, k, s0:s0 + n], tmp[:, :n], AF.Identity, [tmp, gp[cn], self.modT], [vT],
                          scale=gp[cn][:, k:k + 1], bias=self.mod(l, 3, cn)[:, k:k + 1])
            gates = None
            dbg = getattr(c, "dbg", ())
            if moe:
                if "nogates" not in dbg:
                    gates = self.moe_gates(st, l, vT, blocks, ntok)
                gbc = P.sb(st, "gbc", [128, ntok], F32)
                if "nogates" in dbg or "nogbc" in dbg:
                    P.memset("dve", gbc[:], 0.125, [gbc])
            items = [(e, f0, fg) for e in range(ne) for (f0, fg) in groups]
            Wsrc = (self.moe_wg, self.moe_wu, self.moe_wd) if moe else (self.ffn_wg, self.ffn_wu, self.ffn_wd)

            def emit_dma(i):
                e, f0, fg = items[i]
                a = i % 2
                if moe:
                    Wg, Wu, Wd = self.moe_wg[j, e], self.moe_wu[j, e], self.moe_wd[j, e]
                else:
                    Wg, Wu, Wd = self.ffn_wg[j], self.ffn_wu[j], self.ffn_wd[j]
                P.dma("pool", wg[a][:, :, :fg * 128],
                      Wg.rearrange("(k p) f -> p k f", p=128)[:, :, f0 * 128:(f0 + fg) * 128], Wsrc[0], wg[a])
                P.dma("pool", wu[a][:, :, :fg * 128],
                      Wu.rearrange("(k p) f -> p k f", p=128)[:, :, f0 * 128:(f0 + fg) * 128], Wsrc[1], wu[a])
                P.dma("pool", wd[a][:, :fg, :],
                      Wd[f0 * 128:(f0 + fg) * 128, :].rearrange("(f p) d -> p f d", p=128), Wsrc[2], wd[a])

            def emit_gbc(e):
                sel, gT = gates
                for (s0, n, isc) in blocks:
                    ps = self.psb[6]
                    for t0 in range(0, n, 128):
                        P.mm(ps, ps[:, t0:t0 + 128], sel[:, e, :], gT[:, s0 + t0:s0 + t0 + 128], [sel, gT])
                    P.copy("act", gbc[:, s0:s0 + n], ps[:, :n], [ps], [gbc])

            cnt_ = [0]

            def GU(i, blk, hb):
                e, f0, fg = items[i]
                a = i % 2
                (s0, n, isc) = blk
                for jj in range(fg):
                    q_ = cnt_[0]
                    cnt_[0] += 1
                    pg = self.psb[q_ % 2]
                    pu = self.psb[2 + q_ % 2]
                    for k in range(8):
                        P.mm(pg, pg[:, :n], wg[a][:, k, jj * 128:(jj + 1) * 128], vT[:, k, s0:s0 + n], [wg[a], vT],
                             start=(k == 0), stop=(k == 7))
                    for k in range(8):
                        P.mm(pu, pu[:, :n], wu[a][:, k, jj * 128:(jj + 1) * 128], vT[:, k, s0:s0 + n], [wu[a], vT],
                             start=(k == 0), stop=(k == 7))
                    s_ = sl[q_ % 2]
                    P.act(s_[:, :n], pg[:, :n], AF.Silu, [pg], [s_])
                    if moe:
                        g_ = sgb[q_ % 2]
                        P.tt("dve", g_[:, :n], s_[:, :n], gbc[:, s0:s0 + n], ALU.mult, [s_, gbc], [g_])
                        s_ = g_
                    P.tt("dve", hb[:, jj, :n], pu[:, :n], s_[:, :n], ALU.mult, [pu, s_], [hb])

            dcnt = [0]

            def DD(i, blk, hb):
                e, f0, fg = items[i]
                a = i % 2
                (s0, n, isc) = blk
                cn = c.BL if isc else b
                for dch in range(8):
                    py = self.psb[4 + dcnt[0] % 2]
                    dcnt[0] += 1
                    for jj in range(fg):
                        P.mm(py, py[:, :n], wd[a][:, jj, dch * 128:(dch + 1) * 128], hb[:, jj, :n], [wd[a], hb],
                             start=(jj == 0), stop=(jj == fg - 1))
                    P.stt(hT[:, dch, s0:s0 + n], py[:, :n], self.mod(l, 5, cn)[:, dch:dch + 1], hT[:, dch, s0:s0 + n],
                          ALU.mult, ALU.add, [py, self.modT, hT], [hT])

            if items:
                emit_dma(0)
            prev = None
            wi = 0
            cur_e = -1
            for i in range(len(items)):
                for bi, blk in enumerate(blocks):
                    if moe and items[i][0] != cur_e:
                        cur_e = items[i][0]
                        emit_gbc(cur_e)
                    hb = hid[wi % 2]
                    wi += 1
                    GU(i, blk, hb)
                    if prev is not None:
                        DD(*prev)
                    prev = (i, blk, hb)
                    if bi == 0 and i + 1 < len(items):
                        emit_dma(i + 1)
            if prev is not None:
                DD(*prev)
            if last:
                for (s0, n, isc) in [nb for nb in nblocks if not nb[2]]:
                    self.rstd_block(hT, lambda k: hT[:, k, s0:s0 + n], n, sq, rstd, psn)
                    for k in range(8):
                        P.tt("dve", tmp[:, :n], hT[:, k, s0:s0 + n], rstd[:, :n], ALU.mult, [hT, rstd], [tmp])
                        o_ = sl[k % 2]
                        P.ts("dve", o_[:, :n], tmp[:, :n], self.vec("final_g")[:, k:k + 1], None, ALU.mult, None,
                             [tmp, self.vecs], [o_])
                        P.dma("sp", self.outT[b, k * 128:(k + 1) * 128, s0:s0 + n], o_[:, :n], o_, self.outT)
            else:
                for k in range(8):
                    P.dma("sp", dst[b, k * 128:(k + 1) * 128, 0:ntok], hT[:, k, :], hT, dst)
            P.barrier()

    def moe_gates(self, st, l, vT, blocks, ntok):
        P, nc, c = self.P, self.nc, self.cfg
        j = l // 2
        rt = P.sb(st, "router", [128, 8, c.NE], BF16)
        P.dma("pool", rt[:], self.router[j], self.router, rt)
        sel = P.sb(st, "sel", [8, c.NE, 128], F32)
        gT = P.sb(st, "gT", [8, ntok], F32)
        lg = P.sb(st, "lg", [128, 8], F32)
        top = P.sb(st, "top", [128, 8], F32)
        ex = P.sb(st, "ex", [128, 8], F32)
        msk = P.sb(st, "msk", [128, 8], F32)
        den = P.sb(st, "den", [128, 1], F32)
        nmx = P.sb(st, "nmx", [128, 1], F32)
        gt = P.sb(st, "gt", [128, 8], F32)
        P.copy("dve", sel[:], self.ident[0:8, 0:8].unsqueeze(2).to_broadcast([8, 8, 128]), [self.ident], [sel])
        ps = self.psb[7]
        pt = self.psb[6]
        for (s0, n, isc) in blocks:
            for t0 in range(0, n, 128):
                a0 = s0 + t0
                for k in range(8):
                    P.mm(ps, ps[:, 0:8], vT[:, k, a0:a0 + 128], rt[:, k, :], [vT, rt], start=(k == 0), stop=(k == 7))
                P.copy("dve", lg[:], ps[:, 0:8], [ps], [lg])
                P.op("dve", lambda: nc.vector.max(out=top[:], in_=lg[:]), reads=[lg], writes=[top])
                P.ts("dve", nmx[:], top[:, 0:1], -1.0, None, ALU.mult, None, [top], [nmx])
                P.act(ex[:], lg[:], AF.Exp, [lg, nmx], [ex], bias=nmx[:], scale=1.0)
                P.ts("dve", msk[:], lg[:], top[:, 1:2], None, ALU.is_ge, None, [lg, top], [msk])
                P.tt("dve", gt[:], ex[:], msk[:], ALU.mult, [ex, msk], [gt])
                P.op("dve", lambda: nc.vector.reduce_sum(out=den[:], in_=gt[:], axis=AX.X), reads=[gt], writes=[den])
                P.op("dve", lambda: nc.vector.reciprocal(den[:], den[:]), reads=[den], writes=[den])
                P.ts("dve", gt[:], gt[:], den[:, 0:1], None, ALU.mult, None, [gt, den], [gt])
                P.op("pe", lambda: nc.tensor.transpose(pt[0:8, 0:128], gt[:], self.ident[:]), reads=[gt, self.ident], writes=[pt])
                P.copy("act", gT[:, a0:a0 + 128], pt[0:8, 0:128], [pt], [gT])
        return sel, gT

    def ensure_ident(self):
        if getattr(self, "_ident_done", False):
            return
        P, nc = self.P, self.nc
        P.memset("pool", self.ident[:], 0.0, [self.ident])
        P.op("pool", lambda: nc.gpsimd.affine_select(out=self.ident[:], in_=self.ident[:], pattern=[[-1, 128]],
                                                      compare_op=ALU.not_equal, fill=1.0, base=0, channel_multiplier=1),
             reads=[self.ident], writes=[self.ident])
        self._ident_done = True

    def pool_stage(self, l, b):
        P, nc, c = self.P, self.nc, self.cfg
        j = l // 2
        with_ctx = ctx_later(l)
        src = self.cur
        dst = self.next_h()
        streams = [(0, c.T, b, 0)] + ([(c.T, c.C, c.BL, 1)] if with_ctx else [])
        nmax = max(c.T, c.C)
        with contextlib.ExitStack() as st:
            hT = P.sb(st, "hT", [128, 8, nmax], F32)
            sq = P.sb(st, "sq", [128, 8, 512], BF16)
            rstd = P.sb(st, "rstd", [128, nmax], F32)
            upad = P.sb(st, "upad", [128, nmax + 16], F32)
            a1 = P.sb(st, "a1", [128, nmax + 16], F32)
            a2 = P.sb(st, "a2", [128, nmax + 16], F32)
            icn = P.sb(st, "icn", [128, 4, nmax], F32)
            pooled = P.sb(st, "pooled", [128, 8, nmax], BF16)
            pw = P.sb(st, "pw", [128, 4, 2, 256], BF16)
            A = P.sb(st, "A", [128, 8], F32)
            Bc = P.sb(st, "Bc", [128, 8], F32)
            tmp = P.sb(st, "tmp", [128, 512], F32)
            P.dma("pool", pw[:], self.pool_w[j].rearrange("g (k p) d -> p g k d", p=128), self.pool_w, pw)
            for (t0, ns, cn, si) in streams:
                P.dma("sp", icn[:].rearrange("p g n -> p (g n)"),
                      self.invcnt[si].rearrange("g n -> (g n)").partition_broadcast(128), self.invcnt, icn)
                gp = self.make_gp(st, l, 0, cn, "gp%d" % si)
                P.tt("dve", A[:], self.mod(l, 2, cn), self.vec("pool_s%d" % j), ALU.mult, [self.modT, self.vecs], [A])
                P.tt("dve", Bc[:], A[:], self.vec("pool_b%d" % j), ALU.mult, [A, self.vecs], [Bc])
                for k in range(8):
                    P.dma("sp", hT[:, k, :ns], src[b, k * 128:(k + 1) * 128, t0:t0 + ns], src, hT)
                for s0 in range(0, ns, 512):
                    n = min(512, ns - s0)
                    self.rstd_block(hT, lambda k: hT[:, k, s0:s0 + n], n, sq, tmp, self.psb[6])
                    P.copy("dve", rstd[:, s0:s0 + n], tmp[:, :n], [tmp], [rstd])
                P.memset("pool", upad[:], 0.0, [upad])
                for k in range(8):
                    g = k // 2
                    w = POOL_WINDOWS[g]
                    m = g + 1
                    u = upad[:, 8:8 + ns]
                    P.tt("dve", u, hT[:, k, :ns], rstd[:, :ns], ALU.mult, [hT, rstd], [upad])
                    P.act(u, u, AF.Identity, [upad, gp, self.modT], [upad],
                          scale=gp[:, k:k + 1], bias=self.mod(l, 0, cn)[:, k:k + 1])
                    W = ns + 16
                    cur_, cb_ = upad, upad
                    bufs = [a1, a2]
                    for mm_ in range(m):
                        sh = 1 << mm_
                        nb = bufs[mm_ % 2]
                        ln = W - 2 * sh + 1 if mm_ == 0 else W - (2 << mm_) + 1
                        ln = W - ((2 << mm_) - 1)
                        P.tt("pool", nb[:, :ln], cur_[:, 0:ln], cur_[:, sh:sh + ln], ALU.add, [cb_], [nb])
                        cur_, cb_ = nb, nb
                    o0 = 8 - w // 2
                    P.tt("dve", a1[:, :ns] if cur_ is a2 else a2[:, :ns], cur_[:, o0:o0 + ns], icn[:, g, :ns], ALU.mult,
                         [cb_, icn], [a1 if cur_ is a2 else a2])
                    oth = a1 if cur_ is a2 else a2
                    P.tt("dve", pooled[:, k, :ns], oth[:, :ns], u, ALU.subtract, [oth, upad], [pooled])
                for s0 in range(0, ns, 512):
                    n = min(512, ns - s0)
                    for g in range(4):
                        for dd in range(2):
                            dch = 2 * g + dd
                            ps = self.psb[dch % 2]
                            for kk in range(2):
                                P.mm(ps, ps[:, :n], pw[:, g, kk, dd * 128:(dd + 1) * 128], pooled[:, 2 * g + kk, s0:s0 + n],
                                     [pw, pooled], start=(kk == 0), stop=(kk == 1))
                            P.stt(hT[:, dch, s0:s0 + n], ps[:, :n], A[:, dch:dch + 1], hT[:, dch, s0:s0 + n],
                                  ALU.mult, ALU.add, [ps, A, hT], [hT])
                            P.ts("dve", hT[:, dch, s0:s0 + n], hT[:, dch, s0:s0 + n], Bc[:, dch:dch + 1], None, ALU.add, None,
                                 [hT, Bc], [hT])
                for k in range(8):
                    P.dma("sp", dst[b, k * 128:(k + 1) * 128, t0:t0 + ns], hT[:, k, :ns], hT, dst)
            P.barrier()

    def build(self, do_mixer=True, do_ffn=True):
        c = self.cfg
        self.do_ffn = do_ffn
        self.setup()
        for l in c.layers:
            even = (l % 2 == 0)
            for b in range(c.BL):
                save = self.cur
                if do_mixer:
                    if even:
                        self.ab_stage(l, b)
                    else:
                        self.pool_stage(l, b)
                    self.cur = self.next_h()
                self.ffn_stage(l, b, moe=not even)
                self.cur = save
            if do_mixer:
                self.cur = self.next_h()
            self.cur = self.next_h()
        self.P.barrier()
        self.glob.close()
        return self.nc

    def ab_stage(self, l, b):
        dbg = getattr(self.cfg, "dbg", ())
        if "no1" not in dbg:
            self.ab_inproj(l, b)
        if "no2" not in dbg:
            self.ab_mla(l, b)
        if "no3" not in dbg:
            self.ab_gla(l, b)
        if "no4" not in dbg:
            self.ab_outproj(l, b)

    def ab_inproj(self, l, b):
        P, nc, c = self.P, self.nc, self.cfg
        j = l // 2
        src = self.cur
        OQ, OF, OI, OG = 704, 1216, 2240, 2752
        with contextlib.ExitStack() as st:
            win = P.sb(st, "win", [128, 8, 3264], BF16)
            hblk = [P.sb(st, "hblk%d" % i, [128, 8, 512], F32) for i in range(2)]
            uT = [P.sb(st, "uT%d" % i, [128, 8, 512], BF16) for i in range(2)]
            sq = P.sb(st, "sq", [128, 8, 512], BF16)
            rstd = P.sb(st, "rstd", [128, 512], F32)
            tmp = P.sb(st, "tmp", [128, 512], F32)
            cf = P.sb(st, "cf", [128, 3, 512], F32)
            cn = P.sb(st, "cn", [128, 3, 512], BF16)
            rs2 = P.sb(st, "rs2", [128, 512], F32)
            kro = P.sb(st, "kro", [32, 512], BF16)
            t1 = P.sb(st, "t1", [32, 512], F32)
            t2 = P.sb(st, "t2", [32, 512], F32)
            rp = P.sb(st, "rp", [32, 2, c.TT], F32)
            fo = [P.sb(st, "fo%d" % i, [128, 512], F32) for i in range(3)]
            tk = [P.sb(st, "tk%d" % i, [128, 512], BF16) for i in range(2)]
            tg = [P.sb(st, "tg%d" % i, [128, 512], F32) for i in range(2)]
            gps = {}
            for n_ in (b, c.BL):
                gps[n_] = self.make_gp(st, l, 0, n_, "gp%d" % n_)
            for s in range(0, 3264, 1088):
                P.dma("pool", win[:, :, s:s + 1088], self.w_in[j].rearrange("(k p) n -> p k n", p=128)[:, :, s:s + 1088], self.w_in, win)
            P.dma("sp", rp[:], self.rope[:].rearrange("a d t -> d a t"), self.rope, rp)
            ic = 0
            for bi, (s0, n, isc) in enumerate(c.blocks):
                cnd = c.BL if isc else b
                hb, u = hblk[bi % 2], uT[bi % 2]
                for k in range(8):
                    P.dma("sp", hb[:, k, :n], src[b, k * 128:(k + 1) * 128, s0:s0 + n], src, hb)
                self.rstd_block(hb, lambda k: hb[:, k, :n], n, sq, rstd, self.psb[7])
                for k in range(8):
                    P.tt("dve", tmp[:, :n], hb[:, k, :n], rstd[:, :n], ALU.mult, [hb, rstd], [tmp])
                    P.act(u[:, k, :n], tmp[:, :n], AF.Identity, [tmp, gps[cnd], self.modT], [u],
                          scale=gps[cnd][:, k:k + 1], bias=self.mod(l, 0, cnd)[:, k:k + 1])

                def proj(ps, col0, m):
                    for k in range(8):
                        P.mm(ps, ps[:m, :n], win[:, k, col0:col0 + m], u[:, k, :n], [win, u], start=(k == 0), stop=(k == 7))

                for (col0, nch, gname, dd) in ((0, 3, "g_cq%d" % j, self.cqn_d), (384, 2, "g_ckv%d" % j, self.ckvn_d)):
                    for q_ in range(nch):
                        ps = self.psb[ic % 4]
                        ic += 1
                        proj(ps, col0 + q_ * 128, 128)
                        P.copy("act", cf[:, q_, :n], ps[:, :n], [ps], [cf])
                    self.rstd_block(cf, lambda k: cf[:, k, :n], n, sq, rs2, self.psb[7], nk=nch, inv_n=1.0 / (nch * 128))
                    for q_ in range(nch):
                        P.tt("dve", tmp[:, :n], cf[:, q_, :n], rs2[:, :n], ALU.mult, [cf, rs2], [tmp])
                        P.ts("dve", cn[:, q_, :n], tmp[:, :n], self.vec(gname)[:, q_:q_ + 1], None, ALU.mult, None,
                             [tmp, self.vecs], [cn])
                    P.dma("sp", dd[:, s0:s0 + n].rearrange("(q p) t -> p q t", p=128), cn[:, :nch, :n], cn, dd)
                pa, pb = self.psb[ic % 4], self.psb[(ic + 1) % 4]
                ic += 2
                proj(pa, 640, 32)
                proj(pb, 672, 32)
                P.tt("dve", t1[:, :n], pa[:32, :n], rp[:, 0, s0:s0 + n], ALU.mult, [pa, rp], [t1])
                P.tt("dve", t2[:, :n], pb[:32, :n], rp[:, 1, s0:s0 + n], ALU.mult, [pb, rp], [t2])
                P.tt("pool", kro[:, :n], t1[:, :n], t2[:, :n], ALU.add, [t1, t2], [kro])
                P.dma("sp", self.krr_d[:, s0:s0 + n], kro[:, :n], kro, self.krr_d)
                for q_ in range(12):
                    ps = self.psb[ic % 4]
                    ic += 1
                    proj(ps, OQ + q_ * 128, 128)
                    o_ = fo[q_ % 3]
                    if q_ < 4:
                        P.act(o_[:, :n], ps[:, :n], AF.Silu, [ps], [o_])
                        P.dma("sp", self.qh_d[q_ * 128:(q_ + 1) * 128, s0:s0 + n], o_[:, :n], o_, self.qh_d)
                    else:
                        P.copy("act", o_[:, :n], ps[:, :n], [ps], [o_])
                        d_, h_ = (q_ - 4) // 4, (q_ - 4) % 4
                        P.dma("sp", self.hf_d[d_, h_ * 128:(h_ + 1) * 128, s0:s0 + n], o_[:, :n], o_, self.hf_d)
                for t0 in range(0, n, 128):
                    a0 = s0 + t0
                    for which, col0 in ((0, OI), (1, OG)):
                        ps = self.psb[ic % 4]
                        ic += 1
                        for k in range(8):
                            P.mm(ps, ps[:, :512], u[:, k, t0:t0 + 128], win[:, k, col0:col0 + 512], [u, win],
                                 start=(k == 0), stop=(k == 7))
                        if which == 0:
                            o_ = tk[(t0 // 128) % 2]
                            P.copy("act", o_[:], ps[:, :512], [ps], [o_])
                            P.dma("sp", self.vg_d[a0:a0 + 128, :], o_[:], o_, self.vg_d)
                        else:
                            o_ = tg[(t0 // 128) % 2]
                            P.act(o_[:], ps[:, :512], AF.Silu, [ps], [o_])
                            P.dma("sp", self.sg_d[a0:a0 + 128, :], o_[:], o_, self.sg_d)
            P.barrier()

    def ab_mla(self, l, b):
        P, nc, c = self.P, self.nc, self.cfg
        j = l // 2
        need_ctx = ctx_later(l)
        NT = c.TT // 128
        scale = 96.0 ** -0.5
        with contextlib.ExitStack() as st:
            cqn = P.sb(st, "cqn", [128, 3, c.TT], BF16)
            ckvn = P.sb(st, "ckvn", [128, 2, c.TT], BF16)
            krr = P.sb(st, "krr", [32, c.TT], BF16)
            rp = P.sb(st, "rp", [32, 2, c.TT], F32)
            wq = P.sb(st, "wq", [128, 3, 1024], BF16)
            wk = P.sb(st, "wk", [128, 2, 1024], BF16)
            wv = P.sb(st, "wv", [128, 2, 512], BF16)
            qT = P.sb(st, "qT", [96, 8, c.TT], BF16)
            kT = P.sb(st, "kT", [96, 8, c.TT], BF16)
            va = P.sb(st, "va", [128, NT, 8, 66], BF16)
            PT = [P.sb(st, "PT%d" % i, [128, NT, 512], BF16) for i in range(2)]
            t1 = P.sb(st, "t1", [32, 512], F32)
            t2 = P.sb(st, "t2", [32, 512], F32)
            alat = P.sb(st, "alat", [128, 4, 512], BF16)
            rden = P.sb(st, "rden", [128, 1], F32)
            mo = [P.sb(st, "mo%d" % i, [128, 4, 512], BF16) for i in range(2)]
            idb = P.sb(st, "idb", [128, 128], BF16)
            P.copy("dve", idb[:], self.ident[:], [self.ident], [idb])
            P.dma("sp", cqn[:], self.cqn_d[:].rearrange("(q p) t -> p q t", p=128), self.cqn_d, cqn)
            P.dma("sp", ckvn[:], self.ckvn_d[:].rearrange("(q p) t -> p q t", p=128), self.ckvn_d, ckvn)
            P.dma("sp", krr[:], self.krr_d[:], self.krr_d, krr)
            P.dma("sp", rp[:], self.rope[:].rearrange("a d t -> d a t"), self.rope, rp)
            P.dma("pool", wq[:], self.w_uq[j].rearrange("(k p) n -> p k n", p=128), self.w_uq, wq)
            P.dma("pool", wk[:], self.w_uk[j].rearrange("(k p) n -> p k n", p=128), self.w_uk, wk)
            P.dma("pool", wv[:], self.w_uv[j].rearrange("(k p) n -> p k n", p=128), self.w_uv, wv)
            P.memset("pool", va[:], 1.0, [va])
            ic = 0
            dbg = getattr(c, "dbg", ())
            for (s0, n, isc) in (c.blocks if "mla_noproj" not in dbg else []):
                for h in range(8):
                    pa, pb = self.psb[ic % 4], self.psb[(ic + 1) % 4]
                    pk = self.psb[(ic + 2) % 4]
                    ic += 3
                    if "skq" in dbg:
                        continue
                    for k in range(3):
                        P.mm(pa, pa[:96, :n], wq[:, k, h * 128:h * 128 + 96], cqn[:, k, s0:s0 + n], [wq, cqn], start=(k == 0), stop=(k == 2))
                    for k in range(3):
                        P.mm(pb, pb[:32, :n], wq[:, k, h * 128 + 96:h * 128 + 128], cqn[:, k, s0:s0 + n], [wq, cqn], start=(k == 0), stop=(k == 2))
                    P.copy("act", qT[:, h, s0:s0 + n], pa[:96, :n], [pa], [qT])
                    P.tt("dve", t1[:, :n], pa[:32, :n], rp[:, 0, s0:s0 + n], ALU.mult, [pa, rp], [t1])
                    P.tt("dve", t2[:, :n], pb[:32, :n], rp[:, 1, s0:s0 + n], ALU.mult, [pb, rp], [t2])
                    P.tt("dve" if "mla_dve" in dbg else "pool", qT[0:32, h, s0:s0 + n], t1[:, :n], t2[:, :n], ALU.add, [t1, t2], [qT])
                    if "skk" in dbg:
                        continue
                    for k in range(2):
                        P.mm(pk, pk[:96, :n], wk[:, k, h * 128:h * 128 + 96], ckvn[:, k, s0:s0 + n], [wk, ckvn], start=(k == 0), stop=(k == 1))
                    P.copy("act", kT[:, h, s0:s0 + n], pk[:96, :n], [pk], [kT])
                    P.copy("dve" if "mla_dve" in dbg else "pool", kT[0:32, h, s0:s0 + n], krr[:, s0:s0 + n], [krr], [kT])
                for t0 in (range(0, n, 128) if "skv" not in dbg else []):
                    a0 = s0 + t0
                    ps = self.psb[ic % 4]
                    ic += 1
                    for k in range(2):
                        P.mm(ps, ps[:, :512], ckvn[:, k, a0:a0 + 128], wv[:, k, :], [ckvn, wv], start=(k == 0), stop=(k == 1))
                    P.copy("act", va[:, a0 // 128, :, 0:64], ps[:, :512].rearrange("p (h d) -> p h d", d=64), [ps], [va])
            qblocks = c.blocks if need_ctx else c.lat_blocks
            if "mla_noattn" in dbg:
                qblocks = []
            ib = 0
            for (s0, n, isc) in qblocks:
                kts = list(range(c.T // 128, NT)) if isc else list(range(NT))
                for h in range(8):
                    pt_ = PT[ib % 2]
                    ib += 1
                    for ki, kt in enumerate(kts):
                        ps = self.psb[ki % 2]
                        P.mm(ps, ps[:, :n], kT[:, h, kt * 128:(kt + 1) * 128], qT[:, h, s0:s0 + n], [kT, qT])
                        P.act(pt_[:, ki, :n], ps[:, :n], AF.Exp, [ps], [pt_], scale=scale)
                    for qs in range(n // 128):
                        po = self.psb[2 + qs % 2]
                        for ki, kt in enumerate(kts):
                            P.mm(po, po[:, 0:65], pt_[:, ki, qs * 128:(qs + 1) * 128], va[:, kt, h, 0:65], [pt_, va],
                                 start=(ki == 0), stop=(ki == len(kts) - 1))
                        P.op("dve", lambda: nc.vector.reciprocal(rden[:], po[:, 64:65]), reads=[po], writes=[rden])
                        P.ts("dve", alat[:, qs, h * 64:(h + 1) * 64], po[:, 0:64], rden[:, 0:1], None, ALU.mult, None,
                             [po, rden], [alat])
                m_ = mo[(s0 // 512) % 2]
                for qs in range(n // 128):
                    for fc in range(4):
                        ptb = self.psb[4 + (qs * 4 + fc) % 2]
                        pv = ptb[:].bitcast(BF16)
                        P.op("pe", lambda: nc.tensor.transpose(pv[:, 0:128], alat[:, qs, fc * 128:(fc + 1) * 128], idb[:]),
                             reads=[alat, idb], writes=[ptb])
                        P.copy("act", m_[:, fc, qs * 128:(qs + 1) * 128], pv[:, 0:128], [ptb], [m_])
                P.dma("sp", self.mix_d[0:512, s0:s0 + n].rearrange("(q p) t -> p q t", p=128), m_[:, :, :n], m_, self.mix_d)
            P.barrier()

    def ab_gla(self, l, b):
        P, nc, c = self.P, self.nc, self.cfg
        j = l // 2
        need_ctx = ctx_later(l)
        NT = c.TT // 128
        NCH = c.TT // 32
        lat_tiles = list(range(c.T // 128))
        ctx_tiles = list(range(c.T // 128, NT))
        with contextlib.ExitStack() as st:
            vg = P.sb(st, "vg", [128, NT, 512], BF16)
            oacc = P.sb(st, "oacc", [128, NT, 512], F32)
            smask = P.sb(st, "smask", [128, c.TT], F32)
            tri = P.sb(st, "tri", [128, 2, 128], F32)
            oml = P.sb(st, "oml", [128, 2, 4], F32)
            qh = P.sb(st, "qh", [128, c.TT], F32)
            hf = P.sb(st, "hf", [128, c.TT], F32)
            kin = P.sb(st, "kin", [128, c.TT], F32)
            G = P.sb(st, "G", [128, c.TT], F32)
            eG = P.sb(st, "eG", [128, c.TT], F32)
            w1 = P.sb(st, "w1", [128, c.TT], F32)
            qe = P.sb(st, "qe", [128, c.TT], BF16)
            ke = P.sb(st, "ke", [128, c.TT], BF16)
            kd = P.sb(st, "kd", [128, c.TT], BF16)
            kdt = [P.sb(st, "kdt%d" % i, [128, 4, 128], BF16) for i in range(2)]
            qeb = [P.sb(st, "qeb%d" % i, [128, 4, 128], BF16) for i in range(2)]
            bm = P.sb(st, "bm", [128, 4], F32)
            cm = P.sb(st, "cm", [128, 4, 128], BF16)
            atm = [P.sb(st, "atm%d" % i, [128, 128], BF16) for i in range(2)]
            S = [P.sb(st, "S%d" % i, [128, 128], F32) for i in range(2)]
            Sb = [P.sb(st, "Sb%d" % i, [128, 128], BF16) for i in range(10)]
            idb = P.sb(st, "idb", [128, 128], BF16)
            gnb = P.sb(st, "gnb", [128, 128], F32)
            sgt = [P.sb(st, "sgt%d" % i, [128, 512], F32) for i in range(2)]
            ssq = P.sb(st, "ssq", [128, 4], F32)
            junk = P.sb(st, "junk", [128, 128], F32)
            yt = P.sb(st, "yt", [128, 512], F32)
            blat = P.sb(st, "blat", [128, 512], BF16)
            mo = [P.sb(st, "mo%d" % i, [128, 4, 128], BF16) for i in range(2)]
            P.copy("dve", idb[:], self.ident[:], [self.ident], [idb])
            P.op("dve", lambda: nc.vector.reduce_sum(out=bm[:], in_=self.ident[:].rearrange("p (c l) -> p c l", l=32), axis=AX.X),
                 reads=[self.ident], writes=[bm])
            P.memset("pool", cm[:], 0.0, [cm])
            for cc in range(4):
                P.memset("pool", cm[:, cc, cc * 32:(cc + 1) * 32], 1.0, [cm])
            P.dma("sp", vg[:], self.vg_d[:].rearrange("(t p) f -> p t f", p=128), self.vg_d, vg)
            P.dma("sp", tri[:], self.trimask[:].rearrange("a s t -> s a t"), self.trimask, tri)
            P.dma("sp", gnb[:], self.gnorm[j].partition_broadcast(128), self.gnorm, gnb)
            P.memset("pool", smask[:], 1.0, [smask])
            P.memset("pool", smask[:].rearrange("p (c l) -> p c l", l=32)[:, :, 0:1], 0.0, [smask])
            if j == 0:
                P.memset("dve", oml[:], 1.0, [oml])
            else:
                for d in range(2):
                    P.tt("dve", oml[:, d, :], self.vec("lbl0_%d" % d), self.vec("lbl1_%d" % d), ALU.subtract, [self.vecs], [oml])
                P.act(oml[:], oml[:], AF.Sigmoid, [oml], [oml])
            ich = 0
            for h in range(4):
                P.dma("sp", qh[:], self.qh_d[h * 128:(h + 1) * 128, :], self.qh_d, qh)
                for d in range(2):
                    P.dma("sp", hf[:], self.hf_d[d, h * 128:(h + 1) * 128, :], self.hf_d, hf)
                    P.act(kin[:], hf[:], AF.Sigmoid, [hf], [kin], scale=-1.0)
                    P.ts("dve", kin[:], kin[:], oml[:, d, h:h + 1], None, ALU.mult, None, [kin, oml], [kin])
                    P.act(w1[:], kin[:], AF.Ln, [kin], [w1], scale=-1.0, bias=1.0)
                    if d == 0:
                        P.op("dve", lambda: nc.vector.tensor_tensor_scan(out=G[:], data0=smask[:], data1=w1[:], initial=0.0,
                                                                          op0=ALU.mult, op1=ALU.add), reads=[smask, w1], writes=[G])
                    else:
                        P.op("dve", lambda: nc.vector.tensor_tensor_scan(out=G[:, ::-1], data0=smask[:], data1=w1[:, ::-1], initial=0.0,
                                                                          op0=ALU.mult, op1=ALU.add), reads=[smask, w1], writes=[G])
                    G3 = G[:].rearrange("p (c l) -> p c l", l=32)
                    gend = G3[:, :, 31:32] if d == 0 else G3[:, :, 0:1]
                    P.act(eG[:], G[:], AF.Exp, [G], [eG])
                    P.tt("pool", qe[:], qh[:], eG[:], ALU.mult, [qh, eG], [qe])
                    P.tt("dve", w1[:].rearrange("p (c l) -> p c l", l=32), gend.to_broadcast([128, NCH, 32]), G3, ALU.subtract, [G], [w1])
                    P.act(w1[:], w1[:], AF.Exp, [w1], [w1])
                    P.tt("pool", kd[:], kin[:], w1[:], ALU.mult, [kin, w1], [kd])
                    P.act(w1[:], G[:], AF.Exp, [G], [w1], scale=-1.0)
                    P.tt("dve", ke[:], kin[:], w1[:], ALU.mult, [kin, w1], [ke])
                    eG3 = eG[:].rearrange("p (c l) -> p c l", l=32)
                    order = (ctx_tiles + lat_tiles) if d == 0 else (ctx_tiles[::-1] + lat_tiles[::-1])
                    if "gla_noscan" in getattr(c, "dbg", ()):
                        order = []
                    P.memset("dve", S[0][:], 0.0, [S[0]])
                    P.memset("pool", Sb[0][:], 0.0, [Sb[0]])
                    st_ = {"s": 0, "sb": 0}
                    crange = list(range(4)) if d == 0 else list(range(3, -1, -1))

                    def front(ti, tl):
                        a0 = tl * 128
                        skip_out = (tl in ctx_tiles) and not need_ctx
                        kt_, am_, qb_ = kdt[ti % 2], atm[ti % 2], qeb[ti % 2]
                        pk = self.psb[ti % 2]
                        pkv = pk[:].bitcast(BF16)
                        P.op("pe", lambda: nc.tensor.transpose(pkv[:, 0:128], kd[:, a0:a0 + 128], idb[:]), reads=[kd, idb], writes=[pk])
                        P.tt("dve", kt_[:], pkv[:, 0:128].unsqueeze(1).to_broadcast([128, 4, 128]),
                             bm[:].unsqueeze(2).to_broadcast([128, 4, 128]), ALU.mult, [pk, bm], [kt_])
                        po = self.psb[2 + ti % 2]
                        if not skip_out:
                            P.tt("pool", qb_[:], qe[:, a0:a0 + 128].unsqueeze(1).to_broadcast([128, 4, 128]), cm[:], ALU.mult, [qe, cm], [qb_])
                            pa = self.psb[4 + ti % 2]
                            P.mm(pa, pa[:, 0:128], ke[:, a0:a0 + 128], qe[:, a0:a0 + 128], [ke, qe])
                            P.tt("dve", am_[:], pa[:, 0:128], tri[:, d, :], ALU.mult, [pa, tri], [am_])
                            P.mm(po, po[:, 0:128], am_[:], vg[:, tl, h * 128:(h + 1) * 128], [am_, vg], start=True, stop=False)
                        pd = self.psb[6 + ti % 2]
                        for cc in crange:
                            P.mm(pd, pd[:, cc * 128:(cc + 1) * 128], kt_[:, cc, :], vg[:, tl, h * 128:(h + 1) * 128], [kt_, vg])

                    def back(ti, tl):
                        skip_out = (tl in ctx_tiles) and not need_ctx
                        qb_ = qeb[ti % 2]
                        po = self.psb[2 + ti % 2]
                        pd = self.psb[6 + ti % 2]
                        sbs = []
                        for cc in crange:
                            sbs.append(Sb[st_["sb"]])
                            chn = tl * 4 + cc
                            eg = eG3[:, chn, 31:32] if d == 0 else eG3[:, chn, 0:1]
                            so, sn = S[st_["s"]], S[1 - st_["s"]]
                            P.stt(sn[:], so[:], eg, pd[:, cc * 128:(cc + 1) * 128], ALU.mult, ALU.add, [so, eG, pd], [sn])
                            st_["s"] = 1 - st_["s"]
                            st_["sb"] = (st_["sb"] + 1) % len(Sb)
                            P.copy("act", Sb[st_["sb"]][:], sn[:], [sn], [Sb[st_["sb"]]])
                        if not skip_out:
                            for ci, cc in enumerate(crange):
                                P.mm(po, po[:, 0:128], qb_[:, cc, :], sbs[ci][:], [qb_, sbs[ci]], start=False, stop=(ci == 3))
                            if d == 0:
                                P.copy("act", oacc[:, tl, h * 128:(h + 1) * 128], po[:, 0:128], [po], [oacc])
                            else:
                                P.tt("dve", oacc[:, tl, h * 128:(h + 1) * 128], po[:, 0:128], oacc[:, tl, h * 128:(h + 1) * 128],
                                     ALU.add, [po, oacc], [oacc])

                    if order:
                        front(0, order[0])
                    for ti, tl in enumerate(order):
                        if ti + 1 < len(order):
                            front(ti + 1, order[ti + 1])
                        back(ti, tl)
            tiles = (lat_tiles + ctx_tiles) if need_ctx else lat_tiles
            if "gla_noread" in getattr(c, "dbg", ()):
                tiles = []
            for ti, tl in enumerate(tiles):
                a0 = tl * 128
                sg_ = sgt[ti % 2]
                P.dma("sp", sg_[:], self.sg_d[a0:a0 + 128, :], self.sg_d, sg_)
                for h in range(4):
                    P.act(junk[:], oacc[:, tl, h * 128:(h + 1) * 128], AF.Square, [oacc], [junk, ssq], accum_out=ssq[:, h:h + 1])
                P.act(ssq[:], ssq[:], AF.Sqrt, [ssq, self.eps_t], [ssq], bias=self.eps_t[:], scale=1.0 / 128)
                P.op("dve", lambda: nc.vector.reciprocal(ssq[:], ssq[:]), reads=[ssq], writes=[ssq])
                for h in range(4):
                    P.stt(yt[:, h * 128:(h + 1) * 128], oacc[:, tl, h * 128:(h + 1) * 128], ssq[:, h:h + 1], gnb[:], ALU.mult, ALU.mult,
                          [oacc, ssq, gnb], [yt])
                P.tt("dve", blat[:], yt[:], sg_[:], ALU.mult, [yt, sg_], [blat])
                m_ = mo[ti % 2]
                for fc in range(4):
                    ptb = self.psb[fc % 2]
                    pv = ptb[:].bitcast(BF16)
                    P.op("pe", lambda: nc.tensor.transpose(pv[:, 0:128], blat[:, fc * 128:(fc + 1) * 128], idb[:]), reads=[blat, idb], writes=[ptb])
                    P.copy("act", m_[:, fc, :], pv[:, 0:128], [ptb], [m_])
                P.dma("sp", self.mix_d[512:1024, a0:a0 + 128].rearrange("(q p) t -> p q t", p=128), m_[:], m_, self.mix_d)
            P.barrier()

    def ab_outproj(self, l, b):
        P, nc, c = self.P, self.nc, self.cfg
        j = l // 2
        need_ctx = ctx_later(l)
        src = self.cur
        dst = self.next_h()
        blocks = c.blocks if need_ctx else c.lat_blocks
        with contextlib.ExitStack() as st:
            wo = P.sb(st, "wo", [128, 8, 1024], BF16)
            mx = [P.sb(st, "mx%d" % i, [128, 8, 512], BF16) for i in range(2)]
            hb = [P.sb(st, "hb%d" % i, [128, 8, 512], F32) for i in range(2)]
            P.dma("pool", wo[:], self.w_out[j].rearrange("(k p) n -> p k n", p=128), self.w_out, wo)
            for bi, (s0, n, isc) in enumerate(blocks):
                cnd = c.BL if isc else b
                m_, h_ = mx[bi % 2], hb[bi % 2]
                P.dma("sp", m_[:, :, :n], self.mix_d[:, s0:s0 + n].rearrange("(q p) t -> p q t", p=128), self.mix_d, m_)
                for k in range(8):
                    P.dma("sp", h_[:, k, :n], src[b, k * 128:(k + 1) * 128, s0:s0 + n], src, h_)
                for dch in range(8):
                    ps = self.psb[dch % 4]
                    for k in range(8):
                        P.mm(ps, ps[:, :n], wo[:, k, dch * 128:(dch + 1) * 128], m_[:, k, :n], [wo, m_], start=(k == 0), stop=(k == 7))
                    P.stt(h_[:, dch, :n], ps[:, :n], self.mod(l, 2, cnd)[:, dch:dch + 1], h_[:, dch, :n], ALU.mult, ALU.add,
                          [ps, self.modT, h_], [h_])
                for k in range(8):
                    P.dma("sp", dst[b, k * 128:(k + 1) * 128, s0:s0 + n], h_[:, k, :n], h_, dst)
            P.barrier()


def shared_inputs(inp, cfg):
    c = cfg
    sh = {}
    sh["vecs"] = build_vecs(inp)
    sh["w_mod"] = np.ascontiguousarray(inp["w_mod"], np.float32)
    sh["ffn_wg"] = np.ascontiguousarray(inp["ffn_w_gate"], np.float32)
    sh["ffn_wu"] = np.ascontiguousarray(inp["ffn_w_up"], np.float32)
    sh["ffn_wd"] = np.ascontiguousarray(inp["ffn_w_down"], np.float32)
    sh["moe_wg"] = np.ascontiguousarray(inp["moe_w_gate"], np.float32)
    sh["moe_wu"] = np.ascontiguousarray(inp["moe_w_up"], np.float32)
    sh["moe_wd"] = np.ascontiguousarray(inp["moe_w_down"], np.float32)
    sh["router"] = np.ascontiguousarray(
        np.asarray(inp["moe_router"], np.float32).reshape(2, c.KC, 128, c.NE).transpose(0, 2, 1, 3))
    sh["pool_w"] = np.ascontiguousarray(inp["pool_w"], np.float32)
    nmax = max(c.T, c.C)
    ic = np.ones((2, 4, nmax), np.float32)
    for si, n in enumerate((c.T, c.C)):
        pos = np.arange(n)
        for g, w in enumerate(POOL_WINDOWS):
            lo = np.clip(pos - w // 2, 0, n)
            hi = np.clip(pos - w // 2 + w, 0, n)
            ic[si, g, :n] = 1.0 / (hi - lo).astype(np.float32)
    sh["invcnt"] = ic
    sh["ident"] = np.eye(128, dtype=np.float32)
    w_in = np.asarray(inp["ab_w_in"], np.float32)
    swp = np.arange(32).reshape(2, 2, 8)[:, ::-1, :].reshape(32)
    o_kr = 384 + 256
    kr = w_in[:, :, o_kr:o_kr + 32]
    sh["w_in"] = np.ascontiguousarray(np.concatenate(
        [w_in[:, :, :o_kr + 32], kr[:, :, swp], w_in[:, :, o_kr + 32:]], axis=2))
    w_uq = np.asarray(inp["ab_w_uq"], np.float32).reshape(2, 384, 8, 96)
    nope, ropq = w_uq[..., :64], w_uq[..., 64:]
    sh["w_uq"] = np.ascontiguousarray(np.concatenate([ropq, nope, ropq[..., swp]], axis=3).reshape(2, 384, 1024))
    w_ukv = np.asarray(inp["ab_w_ukv"], np.float32).reshape(2, 256, 8, 128)
    z = np.zeros((2, 256, 8, 32), np.float32)
    sh["w_uk"] = np.ascontiguousarray(np.concatenate([z, w_ukv[..., :64], z], axis=3).reshape(2, 256, 1024))
    sh["w_uv"] = np.ascontiguousarray(w_ukv[..., 64:].reshape(2, 256, 512))
    sh["w_out"] = np.ascontiguousarray(inp["ab_w_out"], np.float32)
    sh["gnorm"] = np.ascontiguousarray(inp["hgrn_g_norm"], np.float32)
    tpos = np.arange(c.T)
    row = (tpos // c.grid_w).astype(np.float32)
    col = (tpos % c.grid_w).astype(np.float32)
    inv = (1.0 / (np.float32(10000.0) ** (np.arange(0, 16, 2, dtype=np.float32) / np.float32(16)))).astype(np.float32)
    ar = row[:, None] * inv[None, :]
    ac = col[:, None] * inv[None, :]
    ang = np.concatenate([ar, ar, ac, ac], axis=1).astype(np.float32)
    sign = np.tile(np.concatenate([-np.ones(8, np.float32), np.ones(8, np.float32)]), 2)
    rope = np.zeros((2, 32, c.TT), np.float32)
    rope[0, :, :c.T] = np.cos(ang).T
    rope[1, :, :c.T] = (np.sin(ang) * sign[None, :]).T
    rope[0, :, c.T:] = 1.0
    sh["rope"] = rope
    ii = np.arange(128)
    same = (ii[:, None] // 32) == (ii[None, :] // 32)
    tm = np.zeros((2, 128, 128), np.float32)
    tm[0] = (same & (ii[:, None] <= ii[None, :]))
    tm[1] = (same & (ii[:, None] >= ii[None, :]))
    sh["trimask"] = tm
    return sh


def core_inputs(inp, cfg, b0):
    c = cfg
    x = np.asarray(inp["x"], np.float32)[b0:b0 + c.BL]
    cx = np.asarray(inp["ctx"], np.float32)[b0:b0 + c.BL]
    xT = np.ascontiguousarray(np.concatenate([x.transpose(0, 2, 1), cx.transpose(0, 2, 1)], axis=2))
    cv = np.concatenate([np.asarray(inp["c"], np.float32)[b0:b0 + c.BL], np.asarray(inp["c_ctx"], np.float32)[None, :]], axis=0)
    cond = np.ascontiguousarray(cv.reshape(c.NB, c.KC, 128).transpose(2, 1, 0))
    return {"xT": xT, "cond": cond}


def kernel(**inp):
    cfg = Cfg()
    bld = Builder(cfg)
    nc = bld.build()
    sh = shared_inputs(inp, cfg)
    names = bld.input_names()
    in_maps = []
    for core in range(8):
        m = dict(sh)
        m.update(core_inputs(inp, cfg, core * cfg.BL))
        in_maps.append({k: m[k] for k in names})
    res = run_bass_kernel_spmd(nc, in_maps, core_ids=list(range(8)))
    outs = []
    for core in range(8):
        oT = np.asarray(res.results[core]["outT"])
        outs.append(oT.transpose(0, 2, 1))
    return np.ascontiguousarray(np.concatenate(outs, axis=0).astype(np.float32))
```

```python
import contextlib
import numpy as np
import concourse.bass as bass
import concourse.mybir as mybir
from concourse.bass_utils import run_bass_kernel_spmd

F32 = mybir.dt.float32
BF16 = mybir.dt.bfloat16
AF = mybir.ActivationFunctionType
ALU = mybir.AluOpType
AX = mybir.AxisListType

EPS = 1e-6
POOL_WINDOWS = (2, 4, 8, 16)


class Buf:
    def __init__(self, name, t):
        self.name = name
        self.t = t
        self.w = []
        self.r = []
        self.dkey = None

    def __getitem__(self, idx):
        return self.t[idx]


class Prog:
    ENG = ("pe", "dve", "act", "pool", "sp")

    def __init__(self, nc, n_dsem=60):
        self.nc = nc
        self.e = {"pe": nc.tensor, "dve": nc.vector, "act": nc.scalar, "pool": nc.gpsimd, "sp": nc.sync}
        self.sems = {}
        self.cnt = {}
        for k in self.ENG:
            self.sems[k] = nc.alloc_semaphore("s_" + k)
            self.cnt[k] = 0
        self.waited = {k: {} for k in self.ENG}
        self.dfree = {"sw": [], "hw": []}
        for i in range(n_dsem):
            key = ("d", i)
            self.sems[key] = nc.alloc_semaphore("d_%d" % i)
            self.cnt[key] = 0
            self.dfree["sw" if i < 12 else "hw"].append(key)
        self.stage_bufs = []
        self.dram_bufs = []
        self.uid = 0
        self.in_names = []

    def _name(self, name):
        self.uid += 1
        return "%s_%d" % (name, self.uid)

    def sb(self, stack, name, shape, dt):
        t = stack.enter_context(self.nc.sbuf_tensor(self._name(name), list(shape), dt))
        b = Buf(name, t)
        self.stage_bufs.append(b)
        return b

    def ps(self, stack, name, shape, dt=F32):
        t = stack.enter_context(self.nc.psum_tensor(self._name(name), list(shape), dt))
        b = Buf(name, t)
        b.excl = True
        self.stage_bufs.append(b)
        return b

    def dram(self, name, shape, dt, kind="Internal"):
        if kind == "ExternalInput":
            self.in_names.append(name)
        t = self.nc.dram_tensor(name, list(shape), dt, kind=kind)
        b = Buf(name, t)
        b.persistent = True
        self.dram_bufs.append(b)
        return b

    def _dkey(self, b, q):
        kind = "sw" if q == "pool" else "hw"
        if b.dkey is None:
            b.dkey = self.dfree[kind].pop()
            b.dkind = kind
            self.stage_bufs.append(b) if b not in self.stage_bufs else None
        assert b.dkind == kind, "buffer %s gets DMAs from both SW and HW DGE" % b.name
        return b.dkey

    def _wait(self, eng, events):
        wd = self.waited[eng]
        need = {}
        for (k, v) in events:
            if need.get(k, 0) < v:
                need[k] = v
        for k, v in need.items():
            if k == "pe" and eng == "pe":
                continue
            if wd.get(k, 0) >= v:
                continue
            self.e[eng].wait_ge(self.sems[k], v)
            wd[k] = v

    @staticmethod
    def _compact(evs):
        m = {}
        for k, v in evs:
            if m.get(k, 0) < v:
                m[k] = v
        return list(m.items())

    def op(self, eng, fn, reads=(), writes=()):
        reads = [b for b in reads if b is not None]
        ev = []
        for b in reads:
            ev += b.w
            if getattr(b, "excl", False):
                ev += [e for e in b.r if e[0] != eng]
        for b in writes:
            ev += b.w
            ev += b.r
        self._wait(eng, ev)
        inst = fn()
        self.cnt[eng] += 1
        inst.then_inc(self.sems[eng], 1)
        me = (eng, self.cnt[eng])
        for b in writes:
            b.w = [me]
            b.r = []
        for b in reads:
            if b not in writes:
                b.r.append(me)
                if len(b.r) > 16:
                    b.r = self._compact(b.r)
        return inst

    def dma(self, q, out_ap, in_ap, src, dst, **kw):
        store = getattr(dst, "persistent", False)
        side = src if store else dst
        key = self._dkey(side, q)
        if store:
            ev = list(src.w) + list(dst.r)
        else:
            ev = list(src.w) + list(dst.r) + [e for e in dst.w if e[0] != key]
        self._wait(q, ev)
        inst = self.e[q].dma_start(out=out_ap, in_=in_ap, **kw)
        self.cnt[key] += 16
        inst.then_inc(self.sems[key], 16)
        me = (key, self.cnt[key])
        if store:
            dst.w = self._compact(dst.w + [me])
        else:
            dst.w = [me]
            dst.r = []
        src.r.append(me)
        if len(src.r) > 16:
            src.r = self._compact(src.r)
        return inst

    def barrier(self):
        allev = [(k, v) for k, v in self.cnt.items() if v > 0]
        for eng in self.ENG:
            self._wait(eng, allev)
        for b in self.stage_bufs + self.dram_bufs:
            if b.dkey is not None:
                self.dfree[b.dkind].append(b.dkey)
                b.dkey = None
            b.w = []
            b.r = []
        self.stage_bufs = []

    def mm(self, ps, out_ap, lhsT, rhs, rd, start=True, stop=True, **kw):
        return self.op("pe", lambda: self.nc.tensor.matmul(out_ap, lhsT, rhs, start=start, stop=stop, **kw),
                       reads=rd, writes=[ps])

    def act(self, out_ap, in_ap, func, rd, wr, **kw):
        return self.op("act", lambda: self.nc.scalar.activation(out=out_ap, in_=in_ap, func=func, **kw),
                       reads=rd, writes=wr)

    def tt(self, eng, out_ap, a, b, op, rd, wr):
        return self.op(eng, lambda: self.e[eng].tensor_tensor(out=out_ap, in0=a, in1=b, op=op), reads=rd, writes=wr)

    def ts(self, eng, out_ap, a, s1, s2, op0, op1, rd, wr):
        if s2 is None:
            return self.op(eng, lambda: self.e[eng].tensor_scalar(out=out_ap, in0=a, scalar1=s1, scalar2=None, op0=op0),
                           reads=rd, writes=wr)
        return self.op(eng, lambda: self.e[eng].tensor_scalar(out=out_ap, in0=a, scalar1=s1, scalar2=s2, op0=op0, op1=op1),
                       reads=rd, writes=wr)

    def stt(self, out_ap, a, s, b, op0, op1, rd, wr):
        return self.op("dve", lambda: self.nc.vector.scalar_tensor_tensor(out=out_ap, in0=a, scalar=s, in1=b, op0=op0, op1=op1),
                       reads=rd, writes=wr)

    def copy(self, eng, out_ap, in_ap, rd, wr):
        if eng == "act":
            return self.op("act", lambda: self.nc.scalar.copy(out_ap, in_ap), reads=rd, writes=wr)
        return self.op(eng, lambda: self.e[eng].tensor_copy(out_ap, in_ap), reads=rd, writes=wr)

    def memset(self, eng, ap, val, wr):
        return self.op(eng, lambda: self.e[eng].memset(ap, val), writes=wr)


class Cfg:
    def __init__(self, T=2048, C=256, BL=2, layers=(0, 1, 2, 3), grid_w=64):
        self.D = 1024
        self.KC = 8
        self.T = T
        self.C = C
        self.TT = T + C
        self.BL = BL
        self.NB = BL + 1
        self.layers = tuple(layers)
        self.grid_w = grid_w
        self.DFF = 2816
        self.DFFE = 3584
        self.NE = 8
        self.blocks = []
        for s in range(0, T, 512):
            self.blocks.append((s, min(512, T - s), False))
        for s in range(0, C, 512):
            self.blocks.append((T + s, min(512, C - s), True))
        self.lat_blocks = [b for b in self.blocks if not b[2]]
        self.ctx_blocks = [b for b in self.blocks if b[2]]


def ctx_later(l):
    return any(m % 2 == 0 for m in range(l + 1, 4))


def vec_layout():
    off = {}
    n = 0

    def add(name, k):
        nonlocal n
        off[name] = (n, k)
        n += k

    for l in range(4):
        add("b_mod%d" % l, 48)
        add("ng%d_0" % l, 8)
        add("ng%d_1" % l, 8)
    add("final_g", 8)
    for j in range(2):
        add("g_cq%d" % j, 3)
        add("g_ckv%d" % j, 2)
        add("lbl%d_0" % j, 4)
        add("lbl%d_1" % j, 4)
        add("pool_b%d" % j, 8)
        add("pool_s%d" % j, 8)
    return off, n


def build_vecs(inp):
    off, n = vec_layout()
    v = np.zeros((128, n), np.float32)

    def put(name, arr):
        o, k = off[name]
        v[:, o:o + k] = np.asarray(arr, np.float32).reshape(k, 128).T

    for l in range(4):
        put("b_mod%d" % l, inp["b_mod"][l])
        put("ng%d_0" % l, inp["norm_g"][l, 0])
        put("ng%d_1" % l, inp["norm_g"][l, 1])
    put("final_g", inp["final_g"])
    for j in range(2):
        put("g_cq%d" % j, inp["ab_g_cq"][j])
        put("g_ckv%d" % j, inp["ab_g_ckv"][j])
        put("lbl%d_0" % j, inp["hgrn_lb_logits"][j, 0])
        put("lbl%d_1" % j, inp["hgrn_lb_logits"][j, 1])
        put("pool_b%d" % j, inp["pool_b"][j].reshape(-1))
        put("pool_s%d" % j, inp["pool_scale"][j])
    return v


class Builder:
    def __init__(self, cfg):
        self.cfg = cfg
        nc = bass.Bass("TRN2", target_bir_lowering=False)
        self.nc = nc
        self.P = Prog(nc)
        P = self.P
        c = cfg
        self.xT = P.dram("xT", [c.BL, c.D, c.TT], F32, kind="ExternalInput")
        self.cond = P.dram("cond", [128, c.KC, c.NB], F32, kind="ExternalInput")
        self.voff, nv = vec_layout()
        self.vecs_d = P.dram("vecs", [128, nv], F32, kind="ExternalInput")
        self.w_mod = P.dram("w_mod", [4, c.D, 6 * c.D], F32, kind="ExternalInput")
        self.ffn_wg = P.dram("ffn_wg", [2, c.D, c.DFF], F32, kind="ExternalInput")
        self.ffn_wu = P.dram("ffn_wu", [2, c.D, c.DFF], F32, kind="ExternalInput")
        self.ffn_wd = P.dram("ffn_wd", [2, c.DFF, c.D], F32, kind="ExternalInput")
        if getattr(c, "no_moe", False):
            self.moe_wg = P.dram("moe_wg", [2, 1, 8, 8], F32, kind="ExternalInput")
            self.moe_wu = P.dram("moe_wu", [2, 1, 8, 8], F32, kind="ExternalInput")
            self.moe_wd = P.dram("moe_wd", [2, 1, 8, 8], F32, kind="ExternalInput")
        else:
            self.moe_wg = P.dram("moe_wg", [2, c.NE, c.D, c.DFFE], F32, kind="ExternalInput")
            self.moe_wu = P.dram("moe_wu", [2, c.NE, c.D, c.DFFE], F32, kind="ExternalInput")
            self.moe_wd = P.dram("moe_wd", [2, c.NE, c.DFFE, c.D], F32, kind="ExternalInput")
        self.router = P.dram("router", [2, 128, c.KC, c.NE], F32, kind="ExternalInput")
        self.pool_w = P.dram("pool_w", [2, 4, 256, 256], F32, kind="ExternalInput")
        self.invcnt = P.dram("invcnt", [2, 4, max(c.T, c.C)], F32, kind="ExternalInput")
        self.w_in = P.dram("w_in", [2, c.D, 3264], F32, kind="ExternalInput")
        self.w_uq = P.dram("w_uq", [2, 384, 1024], F32, kind="ExternalInput")
        self.w_uk = P.dram("w_uk", [2, 256, 1024], F32, kind="ExternalInput")
        self.w_uv = P.dram("w_uv", [2, 256, 512], F32, kind="ExternalInput")
        self.w_out = P.dram("w_out", [2, 1024, c.D], F32, kind="ExternalInput")
        self.gnorm = P.dram("gnorm", [2, 128], F32, kind="ExternalInput")
        self.rope = P.dram("rope", [2, 32, c.TT], F32, kind="ExternalInput")
        self.trimask = P.dram("trimask", [2, 128, 128], F32, kind="ExternalInput")
        self.outT = P.dram("outT", [c.BL, c.D, c.T], F32, kind="ExternalOutput")
        self.cqn_d = P.dram("cqn_d", [384, c.TT], BF16)
        self.ckvn_d = P.dram("ckvn_d", [256, c.TT], BF16)
        self.krr_d = P.dram("krr_d", [32, c.TT], BF16)
        self.qh_d = P.dram("qh_d", [512, c.TT], F32)
        self.hf_d = P.dram("hf_d", [2, 512, c.TT], F32)
        self.vg_d = P.dram("vg_d", [c.TT, 512], BF16)
        self.sg_d = P.dram("sg_d", [c.TT, 512], F32)
        self.mix_d = P.dram("mix_d", [1024, c.TT], BF16)
        self.hbuf = [P.dram("hA", [c.BL, c.D, c.TT], F32), P.dram("hB", [c.BL, c.D, c.TT], F32)]
        self.cur = self.xT

        self._in_names = P.in_names
        self.glob = contextlib.ExitStack()
        g = self.glob
        self.vecs = P.sb(g, "vecs", [128, nv], F32)
        self.modT = P.sb(g, "modT", [128, 4, 48, c.NB], F32)
        self.ones_bf = P.sb(g, "ones_bf", [128, 128], BF16)
        self.eps_t = P.sb(g, "eps", [128, 1], F32)
        self.ident = P.sb(g, "ident", [128, 128], F32)
        self.ident_d = P.dram("ident", [128, 128], F32, kind="ExternalInput")
        self.psb = [P.ps(g, "ps%d" % i, [128, 512], F32) for i in range(8)]

    def input_names(self):
        return [k for k, v in self.__dict__.items() if False] or self._in_names

    def vec(self, name):
        o, k = self.voff[name]
        return self.vecs[:, o:o + k]

    def next_h(self):
        return self.hbuf[0] if self.cur is not self.hbuf[0] else self.hbuf[1]

    def setup(self):
        P, nc, c = self.P, self.nc, self.cfg
        P.dma("sp", self.vecs[:], self.vecs_d[:], self.vecs_d, self.vecs)
        P.memset("dve", self.ones_bf[:], 1.0, [self.ones_bf])
        P.dma("sp", self.ident[:], self.ident_d[:], self.ident_d, self.ident)
        P.memset("dve", self.eps_t[:], EPS, [self.eps_t])
        with contextlib.ExitStack() as st:
            cf = P.sb(st, "cond_f", [128, c.KC, c.NB], F32)
            sg = P.sb(st, "cond_sg", [128, c.KC, c.NB], F32)
            cb = P.sb(st, "cond_b", [128, c.KC, c.NB], BF16)
            wsl = [P.sb(st, "wmod_sl%d" % i, [128, c.KC, 1024], BF16) for i in range(2)]
            P.dma("sp", cf[:], self.cond[:], self.cond, cf)
            P.act(sg[:], cf[:], AF.Sigmoid, [cf], [sg])
            P.tt("dve", cb[:], cf[:], sg[:], ALU.mult, [cf, sg], [cb])
            it = 0
            for l in c.layers:
                for s in range(6):
                    w = wsl[it % 2]
                    it += 1
                    src = self.w_mod[l].rearrange("(k p) n -> p k n", p=128)[:, :, s * 1024:(s + 1) * 1024]
                    P.dma("pool", w[:], src, self.w_mod, w)
                    ps = self.psb[it % 2]
                    for jj in range(8):
                        for k in range(c.KC):
                            P.mm(ps, ps[:, jj * c.NB:(jj + 1) * c.NB], w[:, k, jj * 128:(jj + 1) * 128], cb[:, k, :],
                                 [w, cb], start=(k == 0), stop=(k == c.KC - 1))
                    o, _ = self.voff["b_mod%d" % l]
                    bsl = self.vecs[:, o + s * 8:o + s * 8 + 8]
                    P.tt("dve", self.modT[:, l, s * 8:(s + 1) * 8, :],
                         ps[:, 0:8 * c.NB].rearrange("p (j n) -> p j n", n=c.NB),
                         bsl.unsqueeze(2).to_broadcast([128, 8, c.NB]), ALU.add, [ps, self.vecs], [self.modT])
            P.barrier()

    def mod(self, l, which, n):
        return self.modT[:, l, which * 8:(which + 1) * 8, n]

    def make_gp(self, st, l, which_norm, n, name):
        P = self.P
        gp = P.sb(st, name, [128, 8], F32)
        sc = self.mod(l, 1 + 3 * which_norm, n)
        P.stt(gp[:], sc, 1.0, self.vec("ng%d_%d" % (l, which_norm)), ALU.add, ALU.mult, [self.modT, self.vecs], [gp])
        return gp

    def rstd_block(self, hsrc, hap_fn, n, sq, rstd, ps, nk=8, inv_n=1.0 / 1024):
        P = self.P
        for k in range(nk):
            P.act(sq[:, k, :n], hap_fn(k), AF.Square, [hsrc], [sq])
        on = self.ones_bf
        for k in range(nk):
            P.mm(ps, ps[:, :n], on[:], sq[:, k, :n], [on, sq], start=(k == 0), stop=(k == nk - 1))
        P.act(rstd[:, :n], ps[:, :n], AF.Sqrt, [ps, self.eps_t], [rstd], bias=self.eps_t[:], scale=inv_n)
        P.op("dve", lambda: self.nc.vector.reciprocal(rstd[:, :n], rstd[:, :n]), reads=[rstd], writes=[rstd])

    def ffn_stage(self, l, b, moe):
        P, nc, c = self.P, self.nc, self.cfg
        j = l // 2
        with_ctx = ctx_later(l)
        blocks = c.blocks if with_ctx else c.lat_blocks
        ntok = c.TT if with_ctx else c.T
        last = (l == c.layers[-1])
        src = self.cur
        dst = self.next_h()
        nf = (c.DFFE if moe else c.DFF) // 128
        groups = []
        f0 = 0
        while f0 < nf:
            groups.append((f0, min(4, nf - f0)))
            f0 += 4
        ne = c.NE if moe else 1
        if not getattr(self, 'do_ffn', True):
            ne = 0
            moe = False
        with contextlib.ExitStack() as st:
            hT = P.sb(st, "hT", [128, 8, ntok], F32)
            vT = P.sb(st, "vT", [128, 8, ntok], BF16)
            sq = P.sb(st, "sq", [128, 8, 256], BF16)
            rstd = P.sb(st, "rstd", [128, 256], F32)
            tmp = P.sb(st, "tmp", [128, 256], F32)
            nblocks = []
            for (s0_, n_, isc_) in blocks:
                for q0 in range(0, n_, 256):
                    nblocks.append((s0_ + q0, min(256, n_ - q0), isc_))
            wg = [P.sb(st, "wg%d" % i, [128, 8, 512], BF16) for i in range(2)]
            wu = [P.sb(st, "wu%d" % i, [128, 8, 512], BF16) for i in range(2)]
            wd = [P.sb(st, "wd%d" % i, [128, 4, 1024], BF16) for i in range(2)]
            hid = [P.sb(st, "hid%d" % i, [128, 4, 512], BF16) for i in range(2)]
            sl = [P.sb(st, "sl%d" % i, [128, 512], F32) for i in range(2)]
            sgb = [P.sb(st, "sgb%d" % i, [128, 512], F32) for i in range(2)]
            gp = {}
            for n in ([b, c.BL] if with_ctx else [b]):
                gp[n] = self.make_gp(st, l, 1, n, "gp%d" % n)
            for k in range(8):
                P.dma("sp", hT[:, k, :], src[b, k * 128:(k + 1) * 128, 0:ntok], src, hT)
            psn = self.psb[6]
            for (s0, n, isc) in nblocks:
                cn = c.BL if isc else b
                self.rstd_block(hT, lambda k: hT[:, k, s0:s0 + n], n, sq, rstd, psn)
                for k in range(8):
                    P.tt("dve", tmp[:, :n], hT[:, k, s0:s0 + n], rstd[:, :n], ALU.mult, [hT, rstd], [tmp])
                    P.act(vT[:, k, s0:s0 + n], tmp[:, :n], AF.Identity, [tmp, gp[cn], self.modT], [vT],
                          scale=gp[cn][:, k:k + 1], bias=self.mod(l, 3, cn)[:, k:k + 1])
            gates = None
            dbg = getattr(c, "dbg", ())
            if moe:
                if "nogates" not in dbg:
                    gates = self.moe_gates(st, l, vT, blocks, ntok)
                gbc = P.sb(st, "gbc", [128, ntok], F32)
                if "nogates" in dbg or "nogbc" in dbg:
                    P.memset("dve", gbc[:], 0.125, [gbc])
            items = [(e, f0, fg) for e in range(ne) for (f0, fg) in groups]
            Wsrc = (self.moe_wg, self.moe_wu, self.moe_wd) if moe else (self.ffn_wg, self.ffn_wu, self.ffn_wd)

            def emit_dma(i):
                e, f0, fg = items[i]
                a = i % 2
                if moe:
                    Wg, Wu, Wd = self.moe_wg[j, e], self.moe_wu[j, e], self.moe_wd[j, e]
                else:
                    Wg, Wu, Wd = self.ffn_wg[j], self.ffn_wu[j], self.ffn_wd[j]
                P.dma("pool", wg[a][:, :, :fg * 128],
                      Wg.rearrange("(k p) f -> p k f", p=128)[:, :, f0 * 128:(f0 + fg) * 128], Wsrc[0], wg[a])
                P.dma("pool", wu[a][:, :, :fg * 128],
                      Wu.rearrange("(k p) f -> p k f", p=128)[:, :, f0 * 128:(f0 + fg) * 128], Wsrc[1], wu[a])
                P.dma("pool", wd[a][:, :fg, :],
                      Wd[f0 * 128:(f0 + fg) * 128, :].rearrange("(f p) d -> p f d", p=128), Wsrc[2], wd[a])

            def emit_gbc(e):
                sel, gT = gates
                for (s0, n, isc) in blocks:
                    ps = self.psb[6]
                    for t0 in range(0, n, 128):
                        P.mm(ps, ps[:, t0:t0 + 128], sel[:, e, :], gT[:, s0 + t0:s0 + t0 + 128], [sel, gT])
                    P.copy("act", gbc[:, s0:s0 + n], ps[:, :n], [ps], [gbc])

            cnt_ = [0]

            def GU(i, blk, hb):
                e, f0, fg = items[i]
                a = i % 2
                (s0, n, isc) = blk
                for jj in range(fg):
                    q_ = cnt_[0]
                    cnt_[0] += 1
                    pg = self.psb[q_ % 2]
                    pu = self.psb[2 + q_ % 2]
                    for k in range(8):
                        P.mm(pg, pg[:, :n], wg[a][:, k, jj * 128:(jj + 1) * 128], vT[:, k, s0:s0 + n], [wg[a], vT],
                             start=(k == 0), stop=(k == 7))
                    for k in range(8):
                        P.mm(pu, pu[:, :n], wu[a][:, k, jj * 128:(jj + 1) * 128], vT[:, k, s0:s0 + n], [wu[a], vT],
                             start=(k == 0), stop=(k == 7))
                    s_ = sl[q_ % 2]
                    P.act(s_[:, :n], pg[:, :n], AF.Silu, [pg], [s_])
                    if moe:
                        g_ = sgb[q_ % 2]
                        P.tt("dve", g_[:, :n], s_[:, :n], gbc[:, s0:s0 + n], ALU.mult, [s_, gbc], [g_])
                        s_ = g_
                    P.tt("dve", hb[:, jj, :n], pu[:, :n], s_[:, :n], ALU.mult, [pu, s_], [hb])

            dcnt = [0]

            def DD(i, blk, hb):
                e, f0, fg = items[i]
                a = i % 2
                (s0, n, isc) = blk
                cn = c.BL if isc else b
                for dch in range(8):
                    py = self.psb[4 + dcnt[0] % 2]
                    dcnt[0] += 1
                    for jj in range(fg):
                        P.mm(py, py[:, :n], wd[a][:, jj, dch * 128:(dch + 1) * 128], hb[:, jj, :n], [wd[a], hb],
                             start=(jj == 0), stop=(jj == fg - 1))
                    P.stt(hT[:, dch, s0:s0 + n], py[:, :n], self.mod(l, 5, cn)[:, dch:dch + 1], hT[:, dch, s0:s0 + n],
                          ALU.mult, ALU.add, [py, self.modT, hT], [hT])

            if items:
                emit_dma(0)
            prev = None
            wi = 0
            cur_e = -1
            for i in range(len(items)):
                for bi, blk in enumerate(blocks):
                    if moe and items[i][0] != cur_e:
                        cur_e = items[i][0]
                        emit_gbc(cur_e)
                    hb = hid[wi % 2]
                    wi += 1
                    GU(i, blk, hb)
                    if prev is not None:
                        DD(*prev)
                    prev = (i, blk, hb)
                    if bi == 0 and i + 1 < len(items):
                        emit_dma(i + 1)
            if prev is not None:
                DD(*prev)
            if last:
                for (s0, n, isc) in [nb for nb in nblocks if not nb[2]]:
                    self.rstd_block(hT, lambda k: hT[:, k, s0:s0 + n], n, sq, rstd, psn)
                    for k in range(8):
                        P.tt("dve", tmp[:, :n], hT[:, k, s0:s0 + n], rstd[:, :n], ALU.mult, [hT, rstd], [tmp])
                        o_ = sl[k % 2]
                        P.ts("dve", o_[:, :n], tmp[:, :n], self.vec("final_g")[:, k:k + 1], None, ALU.mult, None,
                             [tmp, self.vecs], [o_])
                        P.dma("sp", self.outT[b, k * 128:(k + 1) * 128, s0:s0 + n], o_[:, :n], o_, self.outT)
            else:
                for k in range(8):
                    P.dma("sp", dst[b, k * 128:(k + 1) * 128, 0:ntok], hT[:, k, :], hT, dst)
            P.barrier()

    def moe_gates(self, st, l, vT, blocks, ntok):
        P, nc, c = self.P, self.nc, self.cfg
        j = l // 2
        rt = P.sb(st, "router", [128, 8, c.NE], BF16)
        P.dma("pool", rt[:], self.router[j], self.router, rt)
        sel = P.sb(st, "sel", [8, c.NE, 128], F32)
        gT = P.sb(st, "gT", [8, ntok], F32)
        lg = P.sb(st, "lg", [128, 8], F32)
        top = P.sb(st, "top", [128, 8], F32)
        ex = P.sb(st, "ex", [128, 8], F32)
        msk = P.sb(st, "msk", [128, 8], F32)
        den = P.sb(st, "den", [128, 1], F32)
        nmx = P.sb(st, "nmx", [128, 1], F32)
        gt = P.sb(st, "gt", [128, 8], F32)
        P.copy("dve", sel[:], self.ident[0:8, 0:8].unsqueeze(2).to_broadcast([8, 8, 128]), [self.ident], [sel])
        ps = self.psb[7]
        pt = self.psb[6]
        for (s0, n, isc) in blocks:
            for t0 in range(0, n, 128):
                a0 = s0 + t0
                for k in range(8):
                    P.mm(ps, ps[:, 0:8], vT[:, k, a0:a0 + 128], rt[:, k, :], [vT, rt], start=(k == 0), stop=(k == 7))
                P.copy("dve", lg[:], ps[:, 0:8], [ps], [lg])
                P.op("dve", lambda: nc.vector.max(out=top[:], in_=lg[:]), reads=[lg], writes=[top])
                P.ts("dve", nmx[:], top[:, 0:1], -1.0, None, ALU.mult, None, [top], [nmx])
                P.act(ex[:], lg[:], AF.Exp, [lg, nmx], [ex], bias=nmx[:], scale=1.0)
                P.ts("dve", msk[:], lg[:], top[:, 1:2], None, ALU.is_ge, None, [lg, top], [msk])
                P.tt("dve", gt[:], ex[:], msk[:], ALU.mult, [ex, msk], [gt])
                P.op("dve", lambda: nc.vector.reduce_sum(out=den[:], in_=gt[:], axis=AX.X), reads=[gt], writes=[den])
                P.op("dve", lambda: nc.vector.reciprocal(den[:], den[:]), reads=[den], writes=[den])
                P.ts("dve", gt[:], gt[:], den[:, 0:1], None, ALU.mult, None, [gt, den], [gt])
                P.op("pe", lambda: nc.tensor.transpose(pt[0:8, 0:128], gt[:], self.ident[:]), reads=[gt, self.ident], writes=[pt])
                P.copy("act", gT[:, a0:a0 + 128], pt[0:8, 0:128], [pt], [gT])
        return sel, gT

    def ensure_ident(self):
        if getattr(self, "_ident_done", False):
            return
        P, nc = self.P, self.nc
        P.memset("pool", self.ident[:], 0.0, [self.ident])
        P.op("pool", lambda: nc.gpsimd.affine_select(out=self.ident[:], in_=self.ident[:], pattern=[[-1, 128]],
                                                      compare_op=ALU.not_equal, fill=1.0, base=0, channel_multiplier=1),
             reads=[self.ident], writes=[self.ident])
        self._ident_done = True

    def pool_stage(self, l, b):
        P, nc, c = self.P, self.nc, self.cfg
        j = l // 2
        with_ctx = ctx_later(l)
        src = self.cur
        dst = self.next_h()
        streams = [(0, c.T, b, 0)] + ([(c.T, c.C, c.BL, 1)] if with_ctx else [])
        nmax = max(c.T, c.C)
        with contextlib.ExitStack() as st:
            hT = P.sb(st, "hT", [128, 8, nmax], F32)
            sq = P.sb(st, "sq", [128, 8, 512], BF16)
            rstd = P.sb(st, "rstd", [128, nmax], F32)
            upad = P.sb(st, "upad", [128, nmax + 16], F32)
            a1 = P.sb(st, "a1", [128, nmax + 16], F32)
            a2 = P.sb(st, "a2", [128, nmax + 16], F32)
            icn = P.sb(st, "icn", [128, 4, nmax], F32)
            pooled = P.sb(st, "pooled", [128, 8, nmax], BF16)
            pw = P.sb(st, "pw", [128, 4, 2, 256], BF16)
            A = P.sb(st, "A", [128, 8], F32)
            Bc = P.sb(st, "Bc", [128, 8], F32)
            tmp = P.sb(st, "tmp", [128, 512], F32)
            P.dma("pool", pw[:], self.pool_w[j].rearrange("g (k p) d -> p g k d", p=128), self.pool_w, pw)
            for (t0, ns, cn, si) in streams:
                P.dma("sp", icn[:].rearrange("p g n -> p (g n)"),
                      self.invcnt[si].rearrange("g n -> (g n)").partition_broadcast(128), self.invcnt, icn)
                gp = self.make_gp(st, l, 0, cn, "gp%d" % si)
                P.tt("dve", A[:], self.mod(l, 2, cn), self.vec("pool_s%d" % j), ALU.mult, [self.modT, self.vecs], [A])
                P.tt("dve", Bc[:], A[:], self.vec("pool_b%d" % j), ALU.mult, [A, self.vecs], [Bc])
                for k in range(8):
                    P.dma("sp", hT[:, k, :ns], src[b, k * 128:(k + 1) * 128, t0:t0 + ns], src, hT)
                for s0 in range(0, ns, 512):
                    n = min(512, ns - s0)
                    self.rstd_block(hT, lambda k: hT[:, k, s0:s0 + n], n, sq, tmp, self.psb[6])
                    P.copy("dve", rstd[:, s0:s0 + n], tmp[:, :n], [tmp], [rstd])
                P.memset("pool", upad[:], 0.0, [upad])
                for k in range(8):
                    g = k // 2
                    w = POOL_WINDOWS[g]
                    m = g + 1
                    u = upad[:, 8:8 + ns]
                    P.tt("dve", u, hT[:, k, :ns], rstd[:, :ns], ALU.mult, [hT, rstd], [upad])
                    P.act(u, u, AF.Identity, [upad, gp, self.modT], [upad],
                          scale=gp[:, k:k + 1], bias=self.mod(l, 0, cn)[:, k:k + 1])
                    W = ns + 16
                    cur_, cb_ = upad, upad
                    bufs = [a1, a2]
                    for mm_ in range(m):
                        sh = 1 << mm_
                        nb = bufs[mm_ % 2]
                        ln = W - 2 * sh + 1 if mm_ == 0 else W - (2 << mm_) + 1
                        ln = W - ((2 << mm_) - 1)
                        P.tt("pool", nb[:, :ln], cur_[:, 0:ln], cur_[:, sh:sh + ln], ALU.add, [cb_], [nb])
                        cur_, cb_ = nb, nb
                    o0 = 8 - w // 2
                    P.tt("dve", a1[:, :ns] if cur_ is a2 else a2[:, :ns], cur_[:, o0:o0 + ns], icn[:, g, :ns], ALU.mult,
                         [cb_, icn], [a1 if cur_ is a2 else a2])
                    oth = a1 if cur_ is a2 else a2
                    P.tt("dve", pooled[:, k, :ns], oth[:, :ns], u, ALU.subtract, [oth, upad], [pooled])
                for s0 in range(0, ns, 512):
                    n = min(512, ns - s0)
                    for g in range(4):
                        for dd in range(2):
                            dch = 2 * g + dd
                            ps = self.psb[dch % 2]
                            for kk in range(2):
                                P.mm(ps, ps[:, :n], pw[:, g, kk, dd * 128:(dd + 1) * 128], pooled[:, 2 * g + kk, s0:s0 + n],
                                     [pw, pooled], start=(kk == 0), stop=(kk == 1))
                            P.stt(hT[:, dch, s0:s0 + n], ps[:, :n], A[:, dch:dch + 1], hT[:, dch, s0:s0 + n],
                                  ALU.mult, ALU.add, [ps, A, hT], [hT])
                            P.ts("dve", hT[:, dch, s0:s0 + n], hT[:, dch, s0:s0 + n], Bc[:, dch:dch + 1], None, ALU.add, None,
                                 [hT, Bc], [hT])
                for k in range(8):
                    P.dma("sp", dst[b, k * 128:(k + 1) * 128, t0:t0 + ns], hT[:, k, :ns], hT, dst)
            P.barrier()

    def build(self, do_mixer=True, do_ffn=True):
        c = self.cfg
        self.do_ffn = do_ffn
        self.setup()
        for l in c.layers:
            even = (l % 2 == 0)
            for b in range(c.BL):
                save = self.cur
                if do_mixer:
                    if even:
                        self.ab_stage(l, b)
                    else:
                        self.pool_stage(l, b)
                    self.cur = self.next_h()
                self.ffn_stage(l, b, moe=not even)
                self.cur = save
            if do_mixer:
                self.cur = self.next_h()
            self.cur = self.next_h()
        self.P.barrier()
        self.glob.close()
        return self.nc

    def ab_stage(self, l, b):
        dbg = getattr(self.cfg, "dbg", ())
        if "no1" not in dbg:
            self.ab_inproj(l, b)
        if "no2" not in dbg:
            self.ab_mla(l, b)
        if "no3" not in dbg:
            self.ab_gla(l, b)
        if "no4" not in dbg:
            self.ab_outproj(l, b)

    def ab_inproj(self, l, b):
        P, nc, c = self.P, self.nc, self.cfg
        j = l // 2
        src = self.cur
        OQ, OF, OI, OG = 704, 1216, 2240, 2752
        with contextlib.ExitStack() as st:
            win = P.sb(st, "win", [128, 8, 3264], BF16)
            hblk = [P.sb(st, "hblk%d" % i, [128, 8, 512], F32) for i in range(2)]
            uT = [P.sb(st, "uT%d" % i, [128, 8, 512], BF16) for i in range(2)]
            sq = P.sb(st, "sq", [128, 8, 512], BF16)
            rstd = P.sb(st, "rstd", [128, 512], F32)
            tmp = P.sb(st, "tmp", [128, 512], F32)
            cf = P.sb(st, "cf", [128, 3, 512], F32)
            cn = P.sb(st, "cn", [128, 3, 512], BF16)
            rs2 = P.sb(st, "rs2", [128, 512], F32)
            kro = P.sb(st, "kro", [32, 512], BF16)
            t1 = P.sb(st, "t1", [32, 512], F32)
            t2 = P.sb(st, "t2", [32, 512], F32)
            rp = P.sb(st, "rp", [32, 2, c.TT], F32)
            fo = [P.sb(st, "fo%d" % i, [128, 512], F32) for i in range(3)]
            tk = [P.sb(st, "tk%d" % i, [128, 512], BF16) for i in range(2)]
            tg = [P.sb(st, "tg%d" % i, [128, 512], F32) for i in range(2)]
            gps = {}
            for n_ in (b, c.BL):
                gps[n_] = self.make_gp(st, l, 0, n_, "gp%d" % n_)
            for s in range(0, 3264, 1088):
                P.dma("pool", win[:, :, s:s + 1088], self.w_in[j].rearrange("(k p) n -> p k n", p=128)[:, :, s:s + 1088], self.w_in, win)
            P.dma("sp", rp[:], self.rope[:].rearrange("a d t -> d a t"), self.rope, rp)
            ic = 0
            sqn = P.sb(st, "sqn", [128, 8, 512], BF16)

            def norm_block(bi):
                (s0, n, isc) = c.blocks[bi]
                cnd = c.BL if isc else b
                hb, u = hblk[bi % 2], uT[bi % 2]
                for k in range(8):
                    P.dma("sp", hb[:, k, :n], src[b, k * 128:(k + 1) * 128, s0:s0 + n], src, hb)
                self.rstd_block(hb, lambda k: hb[:, k, :n], n, sqn, rstd, self.psb[6])
                for k in range(8):
                    P.tt("dve", tmp[:, :n], hb[:, k, :n], rstd[:, :n], ALU.mult, [hb, rstd], [tmp])
                    P.act(u[:, k, :n], tmp[:, :n], AF.Identity, [tmp, gps[cnd], self.modT], [u],
                          scale=gps[cnd][:, k:k + 1], bias=self.mod(l, 0, cnd)[:, k:k + 1])

            norm_block(0)
            for bi, (s0, n, isc) in enumerate(c.blocks):
                cnd = c.BL if isc else b
                hb, u = hblk[bi % 2], uT[bi % 2]
                if bi + 1 < len(c.blocks):
                    norm_block(bi + 1)

                def proj(ps, col0, m):
                    for k in range(8):
                        P.mm(ps, ps[:m, :n], win[:, k, col0:col0 + m], u[:, k, :n], [win, u], start=(k == 0), stop=(k == 7))

                for (col0, nch, gname, dd) in ((0, 3, "g_cq%d" % j, self.cqn_d), (384, 2, "g_ckv%d" % j, self.ckvn_d)):
                    for q_ in range(nch):
                        ps = self.psb[ic % 4]
                        ic += 1
                        proj(ps, col0 + q_ * 128, 128)
                        P.copy("act", cf[:, q_, :n], ps[:, :n], [ps], [cf])
                    self.rstd_block(cf, lambda k: cf[:, k, :n], n, sq, rs2, self.psb[7], nk=nch, inv_n=1.0 / (nch * 128))
                    for q_ in range(nch):
                        P.stt(cn[:, q_, :n], cf[:, q_, :n], self.vec(gname)[:, q_:q_ + 1], rs2[:, :n], ALU.mult, ALU.mult,
                              [cf, rs2, self.vecs], [cn])
                    P.dma("sp", dd[:, s0:s0 + n].rearrange("(q p) t -> p q t", p=128), cn[:, :nch, :n], cn, dd)
                pa, pb = self.psb[ic % 4], self.psb[(ic + 1) % 4]
                ic += 2
                proj(pa, 640, 32)
                proj(pb, 672, 32)
                P.tt("dve", t1[:, :n], pa[:32, :n], rp[:, 0, s0:s0 + n], ALU.mult, [pa, rp], [t1])
                P.tt("dve", t2[:, :n], pb[:32, :n], rp[:, 1, s0:s0 + n], ALU.mult, [pb, rp], [t2])
                P.tt("pool", kro[:, :n], t1[:, :n], t2[:, :n], ALU.add, [t1, t2], [kro])
                P.dma("sp", self.krr_d[:, s0:s0 + n], kro[:, :n], kro, self.krr_d)
                for q_ in range(12):
                    ps = self.psb[ic % 4]
                    ic += 1
                    proj(ps, OQ + q_ * 128, 128)
                    o_ = fo[q_ % 3]
                    if q_ < 4:
                        P.act(o_[:, :n], ps[:, :n], AF.Silu, [ps], [o_])
                        P.dma("sp", self.qh_d[q_ * 128:(q_ + 1) * 128, s0:s0 + n], o_[:, :n], o_, self.qh_d)
                    else:
                        P.copy("act", o_[:, :n], ps[:, :n], [ps], [o_])
                        d_, h_ = (q_ - 4) // 4, (q_ - 4) % 4
                        P.dma("sp", self.hf_d[d_, h_ * 128:(h_ + 1) * 128, s0:s0 + n], o_[:, :n], o_, self.hf_d)
                for t0 in range(0, n, 128):
                    a0 = s0 + t0
                    for which, col0 in ((0, OI), (1, OG)):
                        ps = self.psb[ic % 4]
                        ic += 1
                        for k in range(8):
                            P.mm(ps, ps[:, :512], u[:, k, t0:t0 + 128], win[:, k, col0:col0 + 512], [u, win],
                                 start=(k == 0), stop=(k == 7))
                        if which == 0:
                            o_ = tk[(t0 // 128) % 2]
                            P.copy("act", o_[:], ps[:, :512], [ps], [o_])
                            P.dma("sp", self.vg_d[a0:a0 + 128, :], o_[:], o_, self.vg_d)
                        else:
                            o_ = tg[(t0 // 128) % 2]
                            P.act(o_[:], ps[:, :512], AF.Silu, [ps], [o_])
                            P.dma("sp", self.sg_d[a0:a0 + 128, :], o_[:], o_, self.sg_d)
            P.barrier()

    def ab_mla(self, l, b):
        P, nc, c = self.P, self.nc, self.cfg
        j = l // 2
        need_ctx = ctx_later(l)
        NT = c.TT // 128
        scale = 96.0 ** -0.5
        with contextlib.ExitStack() as st:
            cqn = P.sb(st, "cqn", [128, 3, c.TT], BF16)
            ckvn = P.sb(st, "ckvn", [128, 2, c.TT], BF16)
            krr = P.sb(st, "krr", [32, c.TT], BF16)
            rp = P.sb(st, "rp", [32, 2, c.TT], F32)
            wq = P.sb(st, "wq", [128, 3, 1024], BF16)
            wk = P.sb(st, "wk", [128, 2, 1024], BF16)
            wv = P.sb(st, "wv", [128, 2, 512], BF16)
            qT = P.sb(st, "qT", [96, 8, c.TT], BF16)
            kT = P.sb(st, "kT", [96, 8, c.TT], BF16)
            va = P.sb(st, "va", [128, NT, 8, 66], BF16)
            PT = [P.sb(st, "PT%d" % i, [128, NT, 512], BF16) for i in range(2)]
            t1 = P.sb(st, "t1", [32, 512], F32)
            t2 = P.sb(st, "t2", [32, 512], F32)
            alat = P.sb(st, "alat", [128, 4, 512], BF16)
            rden = P.sb(st, "rden", [128, 1], F32)
            mo = [P.sb(st, "mo%d" % i, [128, 4, 512], BF16) for i in range(1)]
            idb = P.sb(st, "idb", [128, 128], BF16)
            P.copy("dve", idb[:], self.ident[:], [self.ident], [idb])
            P.dma("sp", cqn[:], self.cqn_d[:].rearrange("(q p) t -> p q t", p=128), self.cqn_d, cqn)
            P.dma("sp", ckvn[:], self.ckvn_d[:].rearrange("(q p) t -> p q t", p=128), self.ckvn_d, ckvn)
            P.dma("sp", krr[:], self.krr_d[:], self.krr_d, krr)
            P.dma("sp", rp[:], self.rope[:].rearrange("a d t -> d a t"), self.rope, rp)
            P.dma("pool", wq[:], self.w_uq[j].rearrange("(k p) n -> p k n", p=128), self.w_uq, wq)
            P.dma("pool", wk[:], self.w_uk[j].rearrange("(k p) n -> p k n", p=128), self.w_uk, wk)
            P.dma("pool", wv[:], self.w_uv[j].rearrange("(k p) n -> p k n", p=128), self.w_uv, wv)
            P.memset("pool", va[:], 1.0, [va])
            ic = 0
            dbg = getattr(c, "dbg", ())
            for (s0, n, isc) in (c.blocks if "mla_noproj" not in dbg else []):
                for h in range(8):
                    pa, pb = self.psb[ic % 4], self.psb[(ic + 1) % 4]
                    pk = self.psb[(ic + 2) % 4]
                    ic += 3
                    if "skq" in dbg:
                        continue
                    for k in range(3):
                        P.mm(pa, pa[:96, :n], wq[:, k, h * 128:h * 128 + 96], cqn[:, k, s0:s0 + n], [wq, cqn], start=(k == 0), stop=(k == 2))
                    for k in range(3):
                        P.mm(pb, pb[:32, :n], wq[:, k, h * 128 + 96:h * 128 + 128], cqn[:, k, s0:s0 + n], [wq, cqn], start=(k == 0), stop=(k == 2))
                    P.copy("act", qT[:, h, s0:s0 + n], pa[:96, :n], [pa], [qT])
                    P.tt("dve", t1[:, :n], pa[:32, :n], rp[:, 0, s0:s0 + n], ALU.mult, [pa, rp], [t1])
                    P.tt("dve", t2[:, :n], pb[:32, :n], rp[:, 1, s0:s0 + n], ALU.mult, [pb, rp], [t2])
                    P.tt("dve" if "mla_dve" in dbg else "pool", qT[0:32, h, s0:s0 + n], t1[:, :n], t2[:, :n], ALU.add, [t1, t2], [qT])
                    if "skk" in dbg:
                        continue
                    for k in range(2):
                        P.mm(pk, pk[:96, :n], wk[:, k, h * 128:h * 128 + 96], ckvn[:, k, s0:s0 + n], [wk, ckvn], start=(k == 0), stop=(k == 1))
                    P.copy("act", kT[:, h, s0:s0 + n], pk[:96, :n], [pk], [kT])
                    P.copy("dve" if "mla_dve" in dbg else "pool", kT[0:32, h, s0:s0 + n], krr[:, s0:s0 + n], [krr], [kT])
                for t0 in (range(0, n, 128) if "skv" not in dbg else []):
                    a0 = s0 + t0
                    ps = self.psb[ic % 4]
                    ic += 1
                    for k in range(2):
                        P.mm(ps, ps[:, :512], ckvn[:, k, a0:a0 + 128], wv[:, k, :], [ckvn, wv], start=(k == 0), stop=(k == 1))
                    P.copy("act", va[:, a0 // 128, :, 0:64], ps[:, :512].rearrange("p (h d) -> p h d", d=64), [ps], [va])
            qblocks = c.blocks if need_ctx else c.lat_blocks
            if "mla_noattn" in dbg:
                qblocks = []
            items = [(blk, h) for blk in qblocks for h in range(8)]
            alats = [alat, P.sb(st, "alat2", [128, 4, 512], BF16)]

            def qk_ops(i):
                (s0, n, isc), h = items[i]
                kts = list(range(c.T // 128, NT)) if isc else list(range(NT))
                pt_ = PT[i % 2]
                ops = []
                for ki, kt in enumerate(kts):
                    def f(ki=ki, kt=kt):
                        ps = self.psb[ki % 2]
                        P.mm(ps, ps[:, :n], kT[:, h, kt * 128:(kt + 1) * 128], qT[:, h, s0:s0 + n], [kT, qT])
                        P.act(pt_[:, ki, :n], ps[:, :n], AF.Exp, [ps], [pt_], scale=scale)
                    ops.append(f)
                return ops

            def pv_ops(i):
                (s0, n, isc), h = items[i]
                kts = list(range(c.T // 128, NT)) if isc else list(range(NT))
                pt_ = PT[i % 2]
                bidx = qblocks.index((s0, n, isc))
                al = alats[bidx % 2]
                ops = []
                for qs in range(n // 128):
                    def f(qs=qs):
                        po = self.psb[2 + qs % 2]
                        for ki, kt in enumerate(kts):
                            P.mm(po, po[:, 0:65], pt_[:, ki, qs * 128:(qs + 1) * 128], va[:, kt, h, 0:65], [pt_, va],
                                 start=(ki == 0), stop=(ki == len(kts) - 1))
                        P.op("dve", lambda: nc.vector.reciprocal(rden[:], po[:, 64:65]), reads=[po], writes=[rden])
                        P.ts("dve", al[:, qs, h * 64:(h + 1) * 64], po[:, 0:64], rden[:, 0:1], None, ALU.mult, None,
                             [po, rden], [al])
                    ops.append(f)
                if h == 7:
                    def g():
                        m_ = mo[0]
                        for qs in range(n // 128):
                            for fc in range(4):
                                ptb = self.psb[4 + (qs * 4 + fc) % 2]
                                pv = ptb[:].bitcast(BF16)
                                P.op("pe", lambda: nc.tensor.transpose(pv[:, 0:128], al[:, qs, fc * 128:(fc + 1) * 128], idb[:]),
                                     reads=[al, idb], writes=[ptb])
                                P.copy("dve", m_[:, fc, qs * 128:(qs + 1) * 128], pv[:, 0:128], [ptb], [m_])
                        P.dma("sp", self.mix_d[0:512, s0:s0 + n].rearrange("(q p) t -> p q t", p=128), m_[:, :, :n], m_, self.mix_d)
                    ops.append(g)
                return ops

            if items:
                for f in qk_ops(0):
                    f()
            for i in range(len(items)):
                nxt = qk_ops(i + 1) if i + 1 < len(items) else []
                pv = pv_ops(i)
                tot = len(nxt) + len(pv)
                a_, b_ = 0, 0
                for t_ in range(tot):
                    if b_ < len(pv) and (a_ >= len(nxt) or (b_ + 1) * (len(nxt) + 1) <= (a_ + 1) * (len(pv) + 1) - 0):
                        pv[b_]()
                        b_ += 1
                    else:
                        nxt[a_]()
                        a_ += 1
            P.barrier()

    def ab_gla(self, l, b):
        P, nc, c = self.P, self.nc, self.cfg
        j = l // 2
        need_ctx = ctx_later(l)
        NT = c.TT // 128
        NCH = c.TT // 32
        lat_tiles = list(range(c.T // 128))
        ctx_tiles = list(range(c.T // 128, NT))
        with contextlib.ExitStack() as st:
            vg = P.sb(st, "vg", [128, NT, 512], BF16)
            oacc = P.sb(st, "oacc", [128, NT, 512], F32)
            smask = P.sb(st, "smask", [128, c.TT], F32)
            tri = P.sb(st, "tri", [128, 2, 128], F32)
            oml = P.sb(st, "oml", [128, 2, 4], F32)
            qh = P.sb(st, "qh", [128, c.TT], F32)
            hf = P.sb(st, "hf", [128, c.TT], F32)
            kin = P.sb(st, "kin", [128, c.TT], F32)
            G = P.sb(st, "G", [128, c.TT], F32)
            eG_s = [P.sb(st, "eG%d" % i, [128, c.TT], F32) for i in range(2)]
            w1 = P.sb(st, "w1", [128, c.TT], F32)
            qe_s = [P.sb(st, "qe%d" % i, [128, c.TT], BF16) for i in range(2)]
            ke_s = [P.sb(st, "ke%d" % i, [128, c.TT], BF16) for i in range(2)]
            kd_s = [P.sb(st, "kd%d" % i, [128, c.TT], BF16) for i in range(2)]
            kdt = [P.sb(st, "kdt%d" % i, [128, 4, 128], BF16) for i in range(2)]
            qeb = [P.sb(st, "qeb%d" % i, [128, 4, 128], BF16) for i in range(2)]
            bm = P.sb(st, "bm", [128, 4], F32)
            cm = P.sb(st, "cm", [128, 4, 128], BF16)
            atm = [P.sb(st, "atm%d" % i, [128, 128], BF16) for i in range(2)]
            S = [P.sb(st, "S%d" % i, [128, 128], F32) for i in range(2)]
            Sb = [P.sb(st, "Sb%d" % i, [128, 128], BF16) for i in range(10)]
            idb = P.sb(st, "idb", [128, 128], BF16)
            gnb = P.sb(st, "gnb", [128, 128], F32)
            sgt = [P.sb(st, "sgt%d" % i, [128, 512], F32) for i in range(2)]
            ssq = P.sb(st, "ssq", [128, 4], F32)
            junk = P.sb(st, "junk", [128, 128], F32)
            yt = P.sb(st, "yt", [128, 512], F32)
            blat = P.sb(st, "blat", [128, 512], BF16)
            mo = [P.sb(st, "mo%d" % i, [128, 4, 128], BF16) for i in range(2)]
            P.copy("dve", idb[:], self.ident[:], [self.ident], [idb])
            P.op("dve", lambda: nc.vector.reduce_sum(out=bm[:], in_=self.ident[:].rearrange("p (c l) -> p c l", l=32), axis=AX.X),
                 reads=[self.ident], writes=[bm])
            P.memset("pool", cm[:], 0.0, [cm])
            for cc in range(4):
                P.memset("pool", cm[:, cc, cc * 32:(cc + 1) * 32], 1.0, [cm])
            P.dma("sp", vg[:], self.vg_d[:].rearrange("(t p) f -> p t f", p=128), self.vg_d, vg)
            P.dma("sp", tri[:], self.trimask[:].rearrange("a s t -> s a t"), self.trimask, tri)
            P.dma("sp", gnb[:], self.gnorm[j].partition_broadcast(128), self.gnorm, gnb)
            P.memset("pool", smask[:], 1.0, [smask])
            P.memset("pool", smask[:].rearrange("p (c l) -> p c l", l=32)[:, :, 0:1], 0.0, [smask])
            if j == 0:
                P.memset("dve", oml[:], 1.0, [oml])
            else:
                for d in range(2):
                    P.tt("dve", oml[:, d, :], self.vec("lbl0_%d" % d), self.vec("lbl1_%d" % d), ALU.subtract, [self.vecs], [oml])
                P.act(oml[:], oml[:], AF.Sigmoid, [oml], [oml])
            chains = [(h, d) for h in range(4) for d in range(2)]

            def gate_ops(ci):
                h, d = chains[ci]
                eG, qe, ke, kd = eG_s[ci % 2], qe_s[ci % 2], ke_s[ci % 2], kd_s[ci % 2]
                G3 = G[:].rearrange("p (c l) -> p c l", l=32)
                gend = G3[:, :, 31:32] if d == 0 else G3[:, :, 0:1]
                ops = []
                if d == 0:
                    ops.append(lambda: P.dma("sp", qh[:], self.qh_d[h * 128:(h + 1) * 128, :], self.qh_d, qh))
                ops.append(lambda: P.dma("sp", hf[:], self.hf_d[d, h * 128:(h + 1) * 128, :], self.hf_d, hf))
                PIECE = 512
                for p0 in range(0, c.TT, PIECE):
                    p1 = min(c.TT, p0 + PIECE)
                    nchp = (p1 - p0) // 32

                    def v3(ap):
                        return ap.rearrange("p (c l) -> p c l", l=32)

                    def mk(p0=p0, p1=p1, nchp=nchp):
                        sl = slice(p0, p1)
                        G3p = v3(G[:, sl])
                        gendp = G3p[:, :, 31:32] if d == 0 else G3p[:, :, 0:1]
                        o = []
                        o.append(lambda: P.act(kin[:, sl], hf[:, sl], AF.Sigmoid, [hf], [kin], scale=-1.0))
                        o.append(lambda: P.ts("dve", kin[:, sl], kin[:, sl], oml[:, d, h:h + 1], None, ALU.mult, None, [kin, oml], [kin]))
                        o.append(lambda: P.act(w1[:, sl], kin[:, sl], AF.Ln, [kin], [w1], scale=-1.0, bias=1.0))
                        if d == 0:
                            o.append(lambda: P.op("dve", lambda: nc.vector.tensor_tensor_scan(out=G[:, sl], data0=smask[:, sl], data1=w1[:, sl], initial=0.0,
                                                                                               op0=ALU.mult, op1=ALU.add), reads=[smask, w1], writes=[G]))
                        else:
                            o.append(lambda: P.op("dve", lambda: nc.vector.tensor_tensor_scan(out=G[:, p0:p1][:, ::-1], data0=smask[:, sl], data1=w1[:, p0:p1][:, ::-1], initial=0.0,
                                                                                               op0=ALU.mult, op1=ALU.add), reads=[smask, w1], writes=[G]))
                        o.append(lambda: P.act(eG[:, sl], G[:, sl], AF.Exp, [G], [eG]))
                        o.append(lambda: P.tt("pool", qe[:, sl], qh[:, sl], eG[:, sl], ALU.mult, [qh, eG], [qe]))
                        o.append(lambda: P.tt("dve", v3(w1[:, sl]), gendp.to_broadcast([128, nchp, 32]), G3p, ALU.subtract, [G], [w1]))
                        o.append(lambda: P.act(w1[:, sl], w1[:, sl], AF.Exp, [w1], [w1]))
                        o.append(lambda: P.tt("pool", kd[:, sl], kin[:, sl], w1[:, sl], ALU.mult, [kin, w1], [kd]))
                        o.append(lambda: P.act(w1[:, sl], G[:, sl], AF.Exp, [G], [w1], scale=-1.0))
                        o.append(lambda: P.tt("pool", ke[:, sl], kin[:, sl], w1[:, sl], ALU.mult, [kin, w1], [ke]))
                        return o
                    ops += mk()
                return ops

            pending = gate_ops(0)
            for ci, (h, d) in enumerate(chains):
                if True:
                    for f_ in pending:
                        f_()
                    pending = gate_ops(ci + 1) if ci + 1 < len(chains) else []
                    eG, qe, ke, kd = eG_s[ci % 2], qe_s[ci % 2], ke_s[ci % 2], kd_s[ci % 2]
                    eG3 = eG[:].rearrange("p (c l) -> p c l", l=32)
                    order = (ctx_tiles + lat_tiles) if d == 0 else (ctx_tiles[::-1] + lat_tiles[::-1])
                    if "gla_noscan" in getattr(c, "dbg", ()):
                        order = []
                    P.memset("dve", S[0][:], 0.0, [S[0]])
                    P.memset("pool", Sb[0][:], 0.0, [Sb[0]])
                    st_ = {"s": 0, "sb": 0}
                    crange = list(range(4)) if d == 0 else list(range(3, -1, -1))

                    def front(ti, tl):
                        a0 = tl * 128
                        skip_out = (tl in ctx_tiles) and not need_ctx
                        kt_, am_, qb_ = kdt[ti % 2], atm[ti % 2], qeb[ti % 2]
                        pk = self.psb[ti % 2]
                        pkv = pk[:].bitcast(BF16)
                        P.op("pe", lambda: nc.tensor.transpose(pkv[:, 0:128], kd[:, a0:a0 + 128], idb[:]), reads=[kd, idb], writes=[pk])
                        P.tt("dve", kt_[:], pkv[:, 0:128].unsqueeze(1).to_broadcast([128, 4, 128]),
                             bm[:].unsqueeze(2).to_broadcast([128, 4, 128]), ALU.mult, [pk, bm], [kt_])
                        po = self.psb[2 + ti % 2]
                        if not skip_out:
                            P.tt("pool", qb_[:], qe[:, a0:a0 + 128].unsqueeze(1).to_broadcast([128, 4, 128]), cm[:], ALU.mult, [qe, cm], [qb_])
                            pa = self.psb[4 + ti % 2]
                            P.mm(pa, pa[:, 0:128], ke[:, a0:a0 + 128], qe[:, a0:a0 + 128], [ke, qe])
                            P.tt("dve", am_[:], pa[:, 0:128], tri[:, d, :], ALU.mult, [pa, tri], [am_])
                            P.mm(po, po[:, 0:128], am_[:], vg[:, tl, h * 128:(h + 1) * 128], [am_, vg], start=True, stop=False)
                        pd = self.psb[6 + ti % 2]
                        for cc in crange:
                            P.mm(pd, pd[:, cc * 128:(cc + 1) * 128], kt_[:, cc, :], vg[:, tl, h * 128:(h + 1) * 128], [kt_, vg])

                    def back(ti, tl):
                        skip_out = (tl in ctx_tiles) and not need_ctx
                        qb_ = qeb[ti % 2]
                        po = self.psb[2 + ti % 2]
                        pd = self.psb[6 + ti % 2]
                        sbs = []
                        for cc in crange:
                            sbs.append(Sb[st_["sb"]])
                            chn = tl * 4 + cc
                            eg = eG3[:, chn, 31:32] if d == 0 else eG3[:, chn, 0:1]
                            so, sn = S[st_["s"]], S[1 - st_["s"]]
                            P.stt(sn[:], so[:], eg, pd[:, cc * 128:(cc + 1) * 128], ALU.mult, ALU.add, [so, eG, pd], [sn])
                            st_["s"] = 1 - st_["s"]
                            st_["sb"] = (st_["sb"] + 1) % len(Sb)
                            P.copy("act", Sb[st_["sb"]][:], sn[:], [sn], [Sb[st_["sb"]]])
                        if not skip_out:
                            for ci, cc in enumerate(crange):
                                P.mm(po, po[:, 0:128], qb_[:, cc, :], sbs[ci][:], [qb_, sbs[ci]], start=False, stop=(ci == 3))
                            if d == 0:
                                P.copy("act", oacc[:, tl, h * 128:(h + 1) * 128], po[:, 0:128], [po], [oacc])
                            else:
                                P.tt("dve", oacc[:, tl, h * 128:(h + 1) * 128], po[:, 0:128], oacc[:, tl, h * 128:(h + 1) * 128],
                                     ALU.add, [po, oacc], [oacc])

                    if order:
                        front(0, order[0])
                    for ti, tl in enumerate(order):
                        if ti + 1 < len(order):
                            front(ti + 1, order[ti + 1])
                        back(ti, tl)
                        if ti >= 1:
                            for _ in range(4):
                                if pending:
                                    pending.pop(0)()
            tiles = (lat_tiles + ctx_tiles) if need_ctx else lat_tiles
            if "gla_noread" in getattr(c, "dbg", ()):
                tiles = []
            for ti, tl in enumerate(tiles):
                a0 = tl * 128
                sg_ = sgt[ti % 2]
                P.dma("sp", sg_[:], self.sg_d[a0:a0 + 128, :], self.sg_d, sg_)
                for h in range(4):
                    P.act(junk[:], oacc[:, tl, h * 128:(h + 1) * 128], AF.Square, [oacc], [junk, ssq], accum_out=ssq[:, h:h + 1])
                P.act(ssq[:], ssq[:], AF.Sqrt, [ssq, self.eps_t], [ssq], bias=self.eps_t[:], scale=1.0 / 128)
                P.op("dve", lambda: nc.vector.reciprocal(ssq[:], ssq[:]), reads=[ssq], writes=[ssq])
                for h in range(4):
                    P.stt(yt[:, h * 128:(h + 1) * 128], oacc[:, tl, h * 128:(h + 1) * 128], ssq[:, h:h + 1], gnb[:], ALU.mult, ALU.mult,
                          [oacc, ssq, gnb], [yt])
                P.tt("dve", blat[:], yt[:], sg_[:], ALU.mult, [yt, sg_], [blat])
                m_ = mo[ti % 2]
                for fc in range(4):
                    ptb = self.psb[fc % 2]
                    pv = ptb[:].bitcast(BF16)
                    P.op("pe", lambda: nc.tensor.transpose(pv[:, 0:128], blat[:, fc * 128:(fc + 1) * 128], idb[:]), reads=[blat, idb], writes=[ptb])
                    P.copy("act", m_[:, fc, :], pv[:, 0:128], [ptb], [m_])
                P.dma("sp", self.mix_d[512:1024, a0:a0 + 128].rearrange("(q p) t -> p q t", p=128), m_[:], m_, self.mix_d)
            P.barrier()

    def ab_outproj(self, l, b):
        P, nc, c = self.P, self.nc, self.cfg
        j = l // 2
        need_ctx = ctx_later(l)
        src = self.cur
        dst = self.next_h()
        blocks = c.blocks if need_ctx else c.lat_blocks
        with contextlib.ExitStack() as st:
            wo = P.sb(st, "wo", [128, 8, 1024], BF16)
            mx = [P.sb(st, "mx%d" % i, [128, 8, 512], BF16) for i in range(2)]
            hb = [P.sb(st, "hb%d" % i, [128, 8, 512], F32) for i in range(2)]
            P.dma("pool", wo[:], self.w_out[j].rearrange("(k p) n -> p k n", p=128), self.w_out, wo)
            for bi, (s0, n, isc) in enumerate(blocks):
                cnd = c.BL if isc else b
                m_, h_ = mx[bi % 2], hb[bi % 2]
                P.dma("sp", m_[:, :, :n], self.mix_d[:, s0:s0 + n].rearrange("(q p) t -> p q t", p=128), self.mix_d, m_)
                for k in range(8):
                    P.dma("sp", h_[:, k, :n], src[b, k * 128:(k + 1) * 128, s0:s0 + n], src, h_)
                for dch in range(8):
                    ps = self.psb[dch % 4]
                    for k in range(8):
                        P.mm(ps, ps[:, :n], wo[:, k, dch * 128:(dch + 1) * 128], m_[:, k, :n], [wo, m_], start=(k == 0), stop=(k == 7))
                    P.stt(h_[:, dch, :n], ps[:, :n], self.mod(l, 2, cnd)[:, dch:dch + 1], h_[:, dch, :n], ALU.mult, ALU.add,
                          [ps, self.modT, h_], [h_])
                for k in range(8):
                    P.dma("sp", dst[b, k * 128:(k + 1) * 128, s0:s0 + n], h_[:, k, :n], h_, dst)
            P.barrier()


def shared_inputs(inp, cfg):
    c = cfg
    sh = {}
    sh["vecs"] = build_vecs(inp)
    sh["w_mod"] = np.ascontiguousarray(inp["w_mod"], np.float32)
    sh["ffn_wg"] = np.ascontiguousarray(inp["ffn_w_gate"], np.float32)
    sh["ffn_wu"] = np.ascontiguousarray(inp["ffn_w_up"], np.float32)
    sh["ffn_wd"] = np.ascontiguousarray(inp["ffn_w_down"], np.float32)
    sh["moe_wg"] = np.ascontiguousarray(inp["moe_w_gate"], np.float32)
    sh["moe_wu"] = np.ascontiguousarray(inp["moe_w_up"], np.float32)
    sh["moe_wd"] = np.ascontiguousarray(inp["moe_w_down"], np.float32)
    sh["router"] = np.ascontiguousarray(
        np.asarray(inp["moe_router"], np.float32).reshape(2, c.KC, 128, c.NE).transpose(0, 2, 1, 3))
    sh["pool_w"] = np.ascontiguousarray(inp["pool_w"], np.float32)
    nmax = max(c.T, c.C)
    ic = np.ones((2, 4, nmax), np.float32)
    for si, n in enumerate((c.T, c.C)):
        pos = np.arange(n)
        for g, w in enumerate(POOL_WINDOWS):
            lo = np.clip(pos - w // 2, 0, n)
            hi = np.clip(pos - w // 2 + w, 0, n)
            ic[si, g, :n] = 1.0 / (hi - lo).astype(np.float32)
    sh["invcnt"] = ic
    sh["ident"] = np.eye(128, dtype=np.float32)
    w_in = np.asarray(inp["ab_w_in"], np.float32)
    swp = np.arange(32).reshape(2, 2, 8)[:, ::-1, :].reshape(32)
    o_kr = 384 + 256
    kr = w_in[:, :, o_kr:o_kr + 32]
    sh["w_in"] = np.ascontiguousarray(np.concatenate(
        [w_in[:, :, :o_kr + 32], kr[:, :, swp], w_in[:, :, o_kr + 32:]], axis=2))
    w_uq = np.asarray(inp["ab_w_uq"], np.float32).reshape(2, 384, 8, 96)
    nope, ropq = w_uq[..., :64], w_uq[..., 64:]
    sh["w_uq"] = np.ascontiguousarray(np.concatenate([ropq, nope, ropq[..., swp]], axis=3).reshape(2, 384, 1024))
    w_ukv = np.asarray(inp["ab_w_ukv"], np.float32).reshape(2, 256, 8, 128)
    z = np.zeros((2, 256, 8, 32), np.float32)
    sh["w_uk"] = np.ascontiguousarray(np.concatenate([z, w_ukv[..., :64], z], axis=3).reshape(2, 256, 1024))
    sh["w_uv"] = np.ascontiguousarray(w_ukv[..., 64:].reshape(2, 256, 512))
    sh["w_out"] = np.ascontiguousarray(inp["ab_w_out"], np.float32)
    sh["gnorm"] = np.ascontiguousarray(inp["hgrn_g_norm"], np.float32)
    tpos = np.arange(c.T)
    row = (tpos // c.grid_w).astype(np.float32)
    col = (tpos % c.grid_w).astype(np.float32)
    inv = (1.0 / (np.float32(10000.0) ** (np.arange(0, 16, 2, dtype=np.float32) / np.float32(16)))).astype(np.float32)
    ar = row[:, None] * inv[None, :]
    ac = col[:, None] * inv[None, :]
    ang = np.concatenate([ar, ar, ac, ac], axis=1).astype(np.float32)
    sign = np.tile(np.concatenate([-np.ones(8, np.float32), np.ones(8, np.float32)]), 2)
    rope = np.zeros((2, 32, c.TT), np.float32)
    rope[0, :, :c.T] = np.cos(ang).T
    rope[1, :, :c.T] = (np.sin(ang) * sign[None, :]).T
    rope[0, :, c.T:] = 1.0
    sh["rope"] = rope
    ii = np.arange(128)
    same = (ii[:, None] // 32) == (ii[None, :] // 32)
    tm = np.zeros((2, 128, 128), np.float32)
    tm[0] = (same & (ii[:, None] <= ii[None, :]))
    tm[1] = (same & (ii[:, None] >= ii[None, :]))
    sh["trimask"] = tm
    return sh


def core_inputs(inp, cfg, b0):
    c = cfg
    x = np.asarray(inp["x"], np.float32)[b0:b0 + c.BL]
    cx = np.asarray(inp["ctx"], np.float32)[b0:b0 + c.BL]
    xT = np.ascontiguousarray(np.concatenate([x.transpose(0, 2, 1), cx.transpose(0, 2, 1)], axis=2))
    cv = np.concatenate([np.asarray(inp["c"], np.float32)[b0:b0 + c.BL], np.asarray(inp["c_ctx"], np.float32)[None, :]], axis=0)
    cond = np.ascontiguousarray(cv.reshape(c.NB, c.KC, 128).transpose(2, 1, 0))
    return {"xT": xT, "cond": cond}


def kernel(**inp):
    cfg = Cfg()
    bld = Builder(cfg)
    nc = bld.build()
    sh = shared_inputs(inp, cfg)
    names = bld.input_names()
    in_maps = []
    for core in range(8):
        m = dict(sh)
        m.update(core_inputs(inp, cfg, core * cfg.BL))
        in_maps.append({k: m[k] for k in names})
    res = run_bass_kernel_spmd(nc, in_maps, core_ids=list(range(8)))
    outs = []
    for core in range(8):
        oT = np.asarray(res.results[core]["outT"])
        outs.append(oT.transpose(0, 2, 1))
    return np.ascontiguousarray(np.concatenate(outs, axis=0).astype(np.float32))
```

```python
import contextlib
import numpy as np
import concourse.bass as bass
import concourse.mybir as mybir
from concourse.bass_utils import run_bass_kernel_spmd

F32 = mybir.dt.float32
BF16 = mybir.dt.bfloat16
AF = mybir.ActivationFunctionType
ALU = mybir.AluOpType
AX = mybir.AxisListType

EPS = 1e-6
POOL_WINDOWS = (2, 4, 8, 16)


class Buf:
    def __init__(self, name, t):
        self.name = name
        self.t = t
        self.w = []
        self.r = []
        self.dkey = None

    def __getitem__(self, idx):
        return self.t[idx]


class Prog:
    ENG = ("pe", "dve", "act", "pool", "sp")

    def __init__(self, nc, n_dsem=60):
        self.nc = nc
        self.e = {"pe": nc.tensor, "dve": nc.vector, "act": nc.scalar, "pool": nc.gpsimd, "sp": nc.sync}
        self.sems = {}
        self.cnt = {}
        for k in self.ENG:
            self.sems[k] = nc.alloc_semaphore("s_" + k)
            self.cnt[k] = 0
        self.waited = {k: {} for k in self.ENG}
        self.dfree = {"sw": [], "hw": []}
        for i in range(n_dsem):
            key = ("d", i)
            self.sems[key] = nc.alloc_semaphore("d_%d" % i)
            self.cnt[key] = 0
            self.dfree["sw" if i < 12 else "hw"].append(key)
        self.stage_bufs = []
        self.dram_bufs = []
        self.uid = 0
        self.in_names = []

    def _name(self, name):
        self.uid += 1
        return "%s_%d" % (name, self.uid)

    def sb(self, stack, name, shape, dt):
        t = stack.enter_context(self.nc.sbuf_tensor(self._name(name), list(shape), dt))
        b = Buf(name, t)
        self.stage_bufs.append(b)
        return b

    def ps(self, stack, name, shape, dt=F32):
        t = stack.enter_context(self.nc.psum_tensor(self._name(name), list(shape), dt))
        b = Buf(name, t)
        b.excl = True
        self.stage_bufs.append(b)
        return b

    def dram(self, name, shape, dt, kind="Internal"):
        if kind == "ExternalInput":
            self.in_names.append(name)
        t = self.nc.dram_tensor(name, list(shape), dt, kind=kind)
        b = Buf(name, t)
        b.persistent = True
        self.dram_bufs.append(b)
        return b

    def _dkey(self, b, q):
        kind = "sw" if q == "pool" else "hw"
        if b.dkey is None:
            b.dkey = self.dfree[kind].pop()
            b.dkind = kind
            self.stage_bufs.append(b) if b not in self.stage_bufs else None
        assert b.dkind == kind, "buffer %s gets DMAs from both SW and HW DGE" % b.name
        return b.dkey

    def _wait(self, eng, events):
        wd = self.waited[eng]
        need = {}
        for (k, v) in events:
            if need.get(k, 0) < v:
                need[k] = v
        for k, v in need.items():
            if k == "pe" and eng == "pe":
                continue
            if wd.get(k, 0) >= v:
                continue
            self.e[eng].wait_ge(self.sems[k], v)
            wd[k] = v

    @staticmethod
    def _compact(evs):
        m = {}
        for k, v in evs:
            if m.get(k, 0) < v:
                m[k] = v
        return list(m.items())

    def op(self, eng, fn, reads=(), writes=()):
        reads = [b for b in reads if b is not None]
        ev = []
        for b in reads:
            ev += b.w
            if getattr(b, "excl", False):
                ev += [e for e in b.r if e[0] != eng]
        for b in writes:
            ev += b.w
            ev += b.r
        self._wait(eng, ev)
        inst = fn()
        self.cnt[eng] += 1
        inst.then_inc(self.sems[eng], 1)
        me = (eng, self.cnt[eng])
        for b in writes:
            b.w = [me]
            b.r = []
        for b in reads:
            if b not in writes:
                b.r.append(me)
                if len(b.r) > 16:
                    b.r = self._compact(b.r)
        return inst

    def dma(self, q, out_ap, in_ap, src, dst, **kw):
        store = getattr(dst, "persistent", False)
        side = src if store else dst
        key = self._dkey(side, q)
        if store:
            ev = list(src.w) + list(dst.r)
        else:
            ev = list(src.w) + list(dst.r) + [e for e in dst.w if e[0] != key]
        self._wait(q, ev)
        inst = self.e[q].dma_start(out=out_ap, in_=in_ap, **kw)
        self.cnt[key] += 16
        inst.then_inc(self.sems[key], 16)
        me = (key, self.cnt[key])
        if store:
            dst.w = self._compact(dst.w + [me])
        else:
            dst.w = [me]
            dst.r = []
        src.r.append(me)
        if len(src.r) > 16:
            src.r = self._compact(src.r)
        return inst

    def barrier(self):
        allev = [(k, v) for k, v in self.cnt.items() if v > 0]
        for eng in self.ENG:
            self._wait(eng, allev)
        for b in self.stage_bufs + self.dram_bufs:
            if b.dkey is not None:
                self.dfree[b.dkind].append(b.dkey)
                b.dkey = None
            b.w = []
            b.r = []
        self.stage_bufs = []

    def mm(self, ps, out_ap, lhsT, rhs, rd, start=True, stop=True, **kw):
        return self.op("pe", lambda: self.nc.tensor.matmul(out_ap, lhsT, rhs, start=start, stop=stop, **kw),
                       reads=rd, writes=[ps])

    def act(self, out_ap, in_ap, func, rd, wr, **kw):
        return self.op("act", lambda: self.nc.scalar.activation(out=out_ap, in_=in_ap, func=func, **kw),
                       reads=rd, writes=wr)

    def tt(self, eng, out_ap, a, b, op, rd, wr):
        return self.op(eng, lambda: self.e[eng].tensor_tensor(out=out_ap, in0=a, in1=b, op=op), reads=rd, writes=wr)

    def ts(self, eng, out_ap, a, s1, s2, op0, op1, rd, wr):
        if s2 is None:
            return self.op(eng, lambda: self.e[eng].tensor_scalar(out=out_ap, in0=a, scalar1=s1, scalar2=None, op0=op0),
                           reads=rd, writes=wr)
        return self.op(eng, lambda: self.e[eng].tensor_scalar(out=out_ap, in0=a, scalar1=s1, scalar2=s2, op0=op0, op1=op1),
                       reads=rd, writes=wr)

    def stt(self, out_ap, a, s, b, op0, op1, rd, wr):
        return self.op("dve", lambda: self.nc.vector.scalar_tensor_tensor(out=out_ap, in0=a, scalar=s, in1=b, op0=op0, op1=op1),
                       reads=rd, writes=wr)

    def copy(self, eng, out_ap, in_ap, rd, wr):
        if eng == "act":
            return self.op("act", lambda: self.nc.scalar.copy(out_ap, in_ap), reads=rd, writes=wr)
        return self.op(eng, lambda: self.e[eng].tensor_copy(out_ap, in_ap), reads=rd, writes=wr)

    def memset(self, eng, ap, val, wr):
        return self.op(eng, lambda: self.e[eng].memset(ap, val), writes=wr)


class Cfg:
    def __init__(self, T=2048, C=256, BL=2, layers=(0, 1, 2, 3), grid_w=64):
        self.D = 1024
        self.KC = 8
        self.T = T
        self.C = C
        self.TT = T + C
        self.BL = BL
        self.NB = BL + 1
        self.layers = tuple(layers)
        self.grid_w = grid_w
        self.DFF = 2816
        self.DFFE = 3584
        self.NE = 8
        self.blocks = []
        for s in range(0, T, 512):
            self.blocks.append((s, min(512, T - s), False))
        for s in range(0, C, 512):
            self.blocks.append((T + s, min(512, C - s), True))
        self.lat_blocks = [b for b in self.blocks if not b[2]]
        self.ctx_blocks = [b for b in self.blocks if b[2]]


def ctx_later(l):
    return any(m % 2 == 0 for m in range(l + 1, 4))


def vec_layout():
    off = {}
    n = 0

    def add(name, k):
        nonlocal n
        off[name] = (n, k)
        n += k

    for l in range(4):
        add("b_mod%d" % l, 48)
        add("ng%d_0" % l, 8)
        add("ng%d_1" % l, 8)
    add("final_g", 8)
    for j in range(2):
        add("g_cq%d" % j, 3)
        add("g_ckv%d" % j, 2)
        add("lbl%d_0" % j, 4)
        add("lbl%d_1" % j, 4)
        add("pool_b%d" % j, 8)
        add("pool_s%d" % j, 8)
    return off, n


def build_vecs(inp):
    off, n = vec_layout()
    v = np.zeros((128, n), np.float32)

    def put(name, arr):
        o, k = off[name]
        v[:, o:o + k] = np.asarray(arr, np.float32).reshape(k, 128).T

    for l in range(4):
        put("b_mod%d" % l, inp["b_mod"][l])
        put("ng%d_0" % l, inp["norm_g"][l, 0])
        put("ng%d_1" % l, inp["norm_g"][l, 1])
    put("final_g", inp["final_g"])
    for j in range(2):
        put("g_cq%d" % j, inp["ab_g_cq"][j])
        put("g_ckv%d" % j, inp["ab_g_ckv"][j])
        put("lbl%d_0" % j, inp["hgrn_lb_logits"][j, 0])
        put("lbl%d_1" % j, inp["hgrn_lb_logits"][j, 1])
        put("pool_b%d" % j, inp["pool_b"][j].reshape(-1))
        put("pool_s%d" % j, inp["pool_scale"][j])
    return v


class Builder:
    def __init__(self, cfg):
        self.cfg = cfg
        nc = bass.Bass("TRN2", target_bir_lowering=False)
        self.nc = nc
        self.P = Prog(nc)
        P = self.P
        c = cfg
        self.xT = P.dram("xT", [c.BL, c.D, c.TT], F32, kind="ExternalInput")
        self.cond = P.dram("cond", [128, c.KC, c.NB], F32, kind="ExternalInput")
        self.voff, nv = vec_layout()
        self.vecs_d = P.dram("vecs", [128, nv], F32, kind="ExternalInput")
        self.w_mod = P.dram("w_mod", [4, c.D, 6 * c.D], F32, kind="ExternalInput")
        self.ffn_wg = P.dram("ffn_wg", [2, c.D, c.DFF], F32, kind="ExternalInput")
        self.ffn_wu = P.dram("ffn_wu", [2, c.D, c.DFF], F32, kind="ExternalInput")
        self.ffn_wd = P.dram("ffn_wd", [2, c.DFF, c.D], F32, kind="ExternalInput")
        if getattr(c, "no_moe", False):
            self.moe_wg = P.dram("moe_wg", [2, 1, 8, 8], F32, kind="ExternalInput")
            self.moe_wu = P.dram("moe_wu", [2, 1, 8, 8], F32, kind="ExternalInput")
            self.moe_wd = P.dram("moe_wd", [2, 1, 8, 8], F32, kind="ExternalInput")
        else:
            self.moe_wg = P.dram("moe_wg", [2, c.NE, c.D, c.DFFE], F32, kind="ExternalInput")
            self.moe_wu = P.dram("moe_wu", [2, c.NE, c.D, c.DFFE], F32, kind="ExternalInput")
            self.moe_wd = P.dram("moe_wd", [2, c.NE, c.DFFE, c.D], F32, kind="ExternalInput")
        self.router = P.dram("router", [2, 128, c.KC, c.NE], F32, kind="ExternalInput")
        self.pool_w = P.dram("pool_w", [2, 4, 256, 256], F32, kind="ExternalInput")
        self.invcnt = P.dram("invcnt", [2, 4, max(c.T, c.C)], F32, kind="ExternalInput")
        self.w_in = P.dram("w_in", [2, c.D, 3264], F32, kind="ExternalInput")
        self.w_uq = P.dram("w_uq", [2, 384, 1024], F32, kind="ExternalInput")
        self.w_uk = P.dram("w_uk", [2, 256, 1024], F32, kind="ExternalInput")
        self.w_uv = P.dram("w_uv", [2, 256, 512], F32, kind="ExternalInput")
        self.w_out = P.dram("w_out", [2, 1024, c.D], F32, kind="ExternalInput")
        self.gnorm = P.dram("gnorm", [2, 128], F32, kind="ExternalInput")
        self.rope = P.dram("rope", [2, 32, c.TT], F32, kind="ExternalInput")
        self.trimask = P.dram("trimask", [2, 128, 128], F32, kind="ExternalInput")
        self.outT = P.dram("outT", [c.BL, c.D, c.T], F32, kind="ExternalOutput")
        self.cqn_d = P.dram("cqn_d", [384, c.TT], BF16)
        self.ckvn_d = P.dram("ckvn_d", [256, c.TT], BF16)
        self.krr_d = P.dram("krr_d", [32, c.TT], BF16)
        self.qh_d = P.dram("qh_d", [512, c.TT], F32)
        self.hf_d = P.dram("hf_d", [2, 512, c.TT], F32)
        self.vg_d = P.dram("vg_d", [c.TT, 512], BF16)
        self.sg_d = P.dram("sg_d", [c.TT, 512], F32)
        self.mix_d = P.dram("mix_d", [1024, c.TT], BF16)
        self.hbuf = [P.dram("hA", [c.BL, c.D, c.TT], F32), P.dram("hB", [c.BL, c.D, c.TT], F32)]
        self.cur = self.xT

        self._in_names = P.in_names
        self.glob = contextlib.ExitStack()
        g = self.glob
        self.vecs = P.sb(g, "vecs", [128, nv], F32)
        self.modT = P.sb(g, "modT", [128, 4, 48, c.NB], F32)
        self.ones_bf = P.sb(g, "ones_bf", [128, 128], BF16)
        self.eps_t = P.sb(g, "eps", [128, 1], F32)
        self.ident = P.sb(g, "ident", [128, 128], F32)
        self.ident_d = P.dram("ident", [128, 128], F32, kind="ExternalInput")
        self.psb = [P.ps(g, "ps%d" % i, [128, 512], F32) for i in range(8)]

    def input_names(self):
        return [k for k, v in self.__dict__.items() if False] or self._in_names

    def vec(self, name):
        o, k = self.voff[name]
        return self.vecs[:, o:o + k]

    def next_h(self):
        return self.hbuf[0] if self.cur is not self.hbuf[0] else self.hbuf[1]

    def setup(self):
        P, nc, c = self.P, self.nc, self.cfg
        P.dma("sp", self.vecs[:], self.vecs_d[:], self.vecs_d, self.vecs)
        P.memset("dve", self.ones_bf[:], 1.0, [self.ones_bf])
        P.dma("sp", self.ident[:], self.ident_d[:], self.ident_d, self.ident)
        P.memset("dve", self.eps_t[:], EPS, [self.eps_t])
        with contextlib.ExitStack() as st:
            cf = P.sb(st, "cond_f", [128, c.KC, c.NB], F32)
            sg = P.sb(st, "cond_sg", [128, c.KC, c.NB], F32)
            cb = P.sb(st, "cond_b", [128, c.KC, c.NB], BF16)
            wsl = [P.sb(st, "wmod_sl%d" % i, [128, c.KC, 1024], BF16) for i in range(2)]
            P.dma("sp", cf[:], self.cond[:], self.cond, cf)
            P.act(sg[:], cf[:], AF.Sigmoid, [cf], [sg])
            P.tt("dve", cb[:], cf[:], sg[:], ALU.mult, [cf, sg], [cb])
            it = 0
            for l in c.layers:
                for s in range(6):
                    w = wsl[it % 2]
                    it += 1
                    src = self.w_mod[l].rearrange("(k p) n -> p k n", p=128)[:, :, s * 1024:(s + 1) * 1024]
                    P.dma("pool", w[:], src, self.w_mod, w)
                    ps = self.psb[it % 2]
                    for jj in range(8):
                        for k in range(c.KC):
                            P.mm(ps, ps[:, jj * c.NB:(jj + 1) * c.NB], w[:, k, jj * 128:(jj + 1) * 128], cb[:, k, :],
                                 [w, cb], start=(k == 0), stop=(k == c.KC - 1))
                    o, _ = self.voff["b_mod%d" % l]
                    bsl = self.vecs[:, o + s * 8:o + s * 8 + 8]
                    P.tt("dve", self.modT[:, l, s * 8:(s + 1) * 8, :],
                         ps[:, 0:8 * c.NB].rearrange("p (j n) -> p j n", n=c.NB),
                         bsl.unsqueeze(2).to_broadcast([128, 8, c.NB]), ALU.add, [ps, self.vecs], [self.modT])
            P.barrier()

    def mod(self, l, which, n):
        return self.modT[:, l, which * 8:(which + 1) * 8, n]

    def make_gp(self, st, l, which_norm, n, name):
        P = self.P
        gp = P.sb(st, name, [128, 8], F32)
        sc = self.mod(l, 1 + 3 * which_norm, n)
        P.stt(gp[:], sc, 1.0, self.vec("ng%d_%d" % (l, which_norm)), ALU.add, ALU.mult, [self.modT, self.vecs], [gp])
        return gp

    def rstd_block(self, hsrc, hap_fn, n, sq, rstd, ps, nk=8, inv_n=1.0 / 1024):
        P = self.P
        for k in range(nk):
            P.act(sq[:, k, :n], hap_fn(k), AF.Square, [hsrc], [sq])
        on = self.ones_bf
        for k in range(nk):
            P.mm(ps, ps[:, :n], on[:], sq[:, k, :n], [on, sq], start=(k == 0), stop=(k == nk - 1))
        P.act(rstd[:, :n], ps[:, :n], AF.Sqrt, [ps, self.eps_t], [rstd], bias=self.eps_t[:], scale=inv_n)
        P.op("dve", lambda: self.nc.vector.reciprocal(rstd[:, :n], rstd[:, :n]), reads=[rstd], writes=[rstd])

    def ffn_stage(self, l, b, moe):
        P, nc, c = self.P, self.nc, self.cfg
        j = l // 2
        with_ctx = ctx_later(l)
        blocks = c.blocks if with_ctx else c.lat_blocks
        ntok = c.TT if with_ctx else c.T
        last = (l == c.layers[-1])
        src = self.cur
        dst = self.next_h()
        nf = (c.DFFE if moe else c.DFF) // 128
        groups = []
        f0 = 0
        while f0 < nf:
            groups.append((f0, min(4, nf - f0)))
            f0 += 4
        ne = c.NE if moe else 1
        if not getattr(self, 'do_ffn', True):
            ne = 0
            moe = False
        with contextlib.ExitStack() as st:
            hT = P.sb(st, "hT", [128, 8, ntok], F32)
            vT = P.sb(st, "vT", [128, 8, ntok], BF16)
            sq = P.sb(st, "sq", [128, 8, 256], BF16)
            rstd = P.sb(st, "rstd", [128, 256], F32)
            tmp = P.sb(st, "tmp", [128, 256], F32)
            nblocks = []
            for (s0_, n_, isc_) in blocks:
                for q0 in range(0, n_, 256):
                    nblocks.append((s0_ + q0, min(256, n_ - q0), isc_))
            wg = [P.sb(st, "wg%d" % i, [128, 8, 512], BF16) for i in range(2)]
            wu = [P.sb(st, "wu%d" % i, [128, 8, 512], BF16) for i in range(2)]
            wd = [P.sb(st, "wd%d" % i, [128, 4, 1024], BF16) for i in range(2)]
            hid = [P.sb(st, "hid%d" % i, [128, 4, 512], BF16) for i in range(2)]
            sl = [P.sb(st, "sl%d" % i, [128, 512], F32) for i in range(2)]
            sgb = [P.sb(st, "sgb%d" % i, [128, 512], F32) for i in range(2)]
            gp = {}
            for n in ([b, c.BL] if with_ctx else [b]):
                gp[n] = self.make_gp(st, l, 1, n, "gp%d" % n)
            for k in range(8):
                P.dma("sp", hT[:, k, :], src[b, k * 128:(k + 1) * 128, 0:ntok], src, hT)
            psn = self.psb[6]
            for (s0, n, isc) in nblocks:
                cn = c.BL if isc else b
                self.rstd_block(hT, lambda k: hT[:, k, s0:s0 + n], n, sq, rstd, psn)
                for k in range(8):
                    P.tt("dve", tmp[:, :n], hT[:, k, s0:s0 + n], rstd[:, :n], ALU.mult, [hT, rstd], [tmp])
                    P.act(vT[:, k, s0:s0 + n], tmp[:, :n], AF.Identity, [tmp, gp[cn], self.modT], [vT],
                          scale=gp[cn][:, k:k + 1], bias=self.mod(l, 3, cn)[:, k:k + 1])
            gates = None
            dbg = getattr(c, "dbg", ())
            if moe:
                if "nogates" not in dbg:
                    gates = self.moe_gates(st, l, vT, blocks, ntok)
                gbc = P.sb(st, "gbc", [128, ntok], F32)
                if "nogates" in dbg or "nogbc" in dbg:
                    P.memset("dve", gbc[:], 0.125, [gbc])
            items = [(e, f0, fg) for e in range(ne) for (f0, fg) in groups]
            Wsrc = (self.moe_wg, self.moe_wu, self.moe_wd) if moe else (self.ffn_wg, self.ffn_wu, self.ffn_wd)

            def emit_dma(i):
                e, f0, fg = items[i]
                a = i % 2
                if moe:
                    Wg, Wu, Wd = self.moe_wg[j, e], self.moe_wu[j, e], self.moe_wd[j, e]
                else:
                    Wg, Wu, Wd = self.ffn_wg[j], self.ffn_wu[j], self.ffn_wd[j]
                P.dma("pool", wg[a][:, :, :fg * 128],
                      Wg.rearrange("(k p) f -> p k f", p=128)[:, :, f0 * 128:(f0 + fg) * 128], Wsrc[0], wg[a])
                P.dma("pool", wu[a][:, :, :fg * 128],
                      Wu.rearrange("(k p) f -> p k f", p=128)[:, :, f0 * 128:(f0 + fg) * 128], Wsrc[1], wu[a])
                P.dma("pool", wd[a][:, :fg, :],
                      Wd[f0 * 128:(f0 + fg) * 128, :].rearrange("(f p) d -> p f d", p=128), Wsrc[2], wd[a])

            def emit_gbc(e):
                sel, gT = gates
                for (s0, n, isc) in blocks:
                    ps = self.psb[6]
                    for t0 in range(0, n, 128):
                        P.mm(ps, ps[:, t0:t0 + 128], sel[:, e, :], gT[:, s0 + t0:s0 + t0 + 128], [sel, gT])
                    P.copy("act", gbc[:, s0:s0 + n], ps[:, :n], [ps], [gbc])

            cnt_ = [0]

            def GU(i, blk, hb):
                e, f0, fg = items[i]
                a = i % 2
                (s0, n, isc) = blk
                for jj in range(fg):
                    q_ = cnt_[0]
                    cnt_[0] += 1
                    pg = self.psb[q_ % 2]
                    pu = self.psb[2 + q_ % 2]
                    for k in range(8):
                        P.mm(pg, pg[:, :n], wg[a][:, k, jj * 128:(jj + 1) * 128], vT[:, k, s0:s0 + n], [wg[a], vT],
                             start=(k == 0), stop=(k == 7))
                    for k in range(8):
                        P.mm(pu, pu[:, :n], wu[a][:, k, jj * 128:(jj + 1) * 128], vT[:, k, s0:s0 + n], [wu[a], vT],
                             start=(k == 0), stop=(k == 7))
                    s_ = sl[q_ % 2]
                    P.act(s_[:, :n], pg[:, :n], AF.Silu, [pg], [s_])
                    if moe:
                        g_ = sgb[q_ % 2]
                        P.tt("dve", g_[:, :n], s_[:, :n], gbc[:, s0:s0 + n], ALU.mult, [s_, gbc], [g_])
                        s_ = g_
                    P.tt("dve", hb[:, jj, :n], pu[:, :n], s_[:, :n], ALU.mult, [pu, s_], [hb])

            dcnt = [0]

            def DD(i, blk, hb):
                e, f0, fg = items[i]
                a = i % 2
                (s0, n, isc) = blk
                cn = c.BL if isc else b
                for dch in range(8):
                    py = self.psb[4 + dcnt[0] % 2]
                    dcnt[0] += 1
                    for jj in range(fg):
                        P.mm(py, py[:, :n], wd[a][:, jj, dch * 128:(dch + 1) * 128], hb[:, jj, :n], [wd[a], hb],
                             start=(jj == 0), stop=(jj == fg - 1))
                    P.stt(hT[:, dch, s0:s0 + n], py[:, :n], self.mod(l, 5, cn)[:, dch:dch + 1], hT[:, dch, s0:s0 + n],
                          ALU.mult, ALU.add, [py, self.modT, hT], [hT])

            if items:
                emit_dma(0)
            prev = None
            wi = 0
            cur_e = -1
            for i in range(len(items)):
                for bi, blk in enumerate(blocks):
                    if moe and items[i][0] != cur_e:
                        cur_e = items[i][0]
                        emit_gbc(cur_e)
                    hb = hid[wi % 2]
                    wi += 1
                    GU(i, blk, hb)
                    if prev is not None:
                        DD(*prev)
                    prev = (i, blk, hb)
                    if bi == 0 and i + 1 < len(items):
                        emit_dma(i + 1)
            if prev is not None:
                DD(*prev)
            if last:
                for (s0, n, isc) in [nb for nb in nblocks if not nb[2]]:
                    self.rstd_block(hT, lambda k: hT[:, k, s0:s0 + n], n, sq, rstd, psn)
                    for k in range(8):
                        P.tt("dve", tmp[:, :n], hT[:, k, s0:s0 + n], rstd[:, :n], ALU.mult, [hT, rstd], [tmp])
                        o_ = sl[k % 2]
                        P.ts("dve", o_[:, :n], tmp[:, :n], self.vec("final_g")[:, k:k + 1], None, ALU.mult, None,
                             [tmp, self.vecs], [o_])
                        P.dma("sp", self.outT[b, k * 128:(k + 1) * 128, s0:s0 + n], o_[:, :n], o_, self.outT)
            else:
                for k in range(8):
                    P.dma("sp", dst[b, k * 128:(k + 1) * 128, 0:ntok], hT[:, k, :], hT, dst)
            P.barrier()

    def moe_gates(self, st, l, vT, blocks, ntok):
        P, nc, c = self.P, self.nc, self.cfg
        j = l // 2
        rt = P.sb(st, "router", [128, 8, c.NE], BF16)
        P.dma("pool", rt[:], self.router[j], self.router, rt)
        sel = P.sb(st, "sel", [8, c.NE, 128], F32)
        gT = P.sb(st, "gT", [8, ntok], F32)
        lg = P.sb(st, "lg", [128, 8], F32)
        top = P.sb(st, "top", [128, 8], F32)
        ex = P.sb(st, "ex", [128, 8], F32)
        msk = P.sb(st, "msk", [128, 8], F32)
        den = P.sb(st, "den", [128, 1], F32)
        nmx = P.sb(st, "nmx", [128, 1], F32)
        gt = P.sb(st, "gt", [128, 8], F32)
        P.copy("dve", sel[:], self.ident[0:8, 0:8].unsqueeze(2).to_broadcast([8, 8, 128]), [self.ident], [sel])
        ps = self.psb[7]
        pt = self.psb[6]
        for (s0, n, isc) in blocks:
            for t0 in range(0, n, 128):
                a0 = s0 + t0
                for k in range(8):
                    P.mm(ps, ps[:, 0:8], vT[:, k, a0:a0 + 128], rt[:, k, :], [vT, rt], start=(k == 0), stop=(k == 7))
                P.copy("dve", lg[:], ps[:, 0:8], [ps], [lg])
                P.op("dve", lambda: nc.vector.max(out=top[:], in_=lg[:]), reads=[lg], writes=[top])
                P.ts("dve", nmx[:], top[:, 0:1], -1.0, None, ALU.mult, None, [top], [nmx])
                P.act(ex[:], lg[:], AF.Exp, [lg, nmx], [ex], bias=nmx[:], scale=1.0)
                P.ts("dve", msk[:], lg[:], top[:, 1:2], None, ALU.is_ge, None, [lg, top], [msk])
                P.tt("dve", gt[:], ex[:], msk[:], ALU.mult, [ex, msk], [gt])
                P.op("dve", lambda: nc.vector.reduce_sum(out=den[:], in_=gt[:], axis=AX.X), reads=[gt], writes=[den])
                P.op("dve", lambda: nc.vector.reciprocal(den[:], den[:]), reads=[den], writes=[den])
                P.ts("dve", gt[:], gt[:], den[:, 0:1], None, ALU.mult, None, [gt, den], [gt])
                P.op("pe", lambda: nc.tensor.transpose(pt[0:8, 0:128], gt[:], self.ident[:]), reads=[gt, self.ident], writes=[pt])
                P.copy("act", gT[:, a0:a0 + 128], pt[0:8, 0:128], [pt], [gT])
        return sel, gT

    def ensure_ident(self):
        if getattr(self, "_ident_done", False):
            return
        P, nc = self.P, self.nc
        P.memset("pool", self.ident[:], 0.0, [self.ident])
        P.op("pool", lambda: nc.gpsimd.affine_select(out=self.ident[:], in_=self.ident[:], pattern=[[-1, 128]],
                                                      compare_op=ALU.not_equal, fill=1.0, base=0, channel_multiplier=1),
             reads=[self.ident], writes=[self.ident])
        self._ident_done = True

    def pool_stage(self, l, b):
        P, nc, c = self.P, self.nc, self.cfg
        j = l // 2
        with_ctx = ctx_later(l)
        src = self.cur
        dst = self.next_h()
        streams = [(0, c.T, b, 0)] + ([(c.T, c.C, c.BL, 1)] if with_ctx else [])
        nmax = max(c.T, c.C)
        with contextlib.ExitStack() as st:
            hT = P.sb(st, "hT", [128, 8, nmax], F32)
            sq = P.sb(st, "sq", [128, 8, 512], BF16)
            rstd = P.sb(st, "rstd", [128, nmax], F32)
            upad = P.sb(st, "upad", [128, nmax + 16], F32)
            a1 = P.sb(st, "a1", [128, nmax + 16], F32)
            a2 = P.sb(st, "a2", [128, nmax + 16], F32)
            icn = P.sb(st, "icn", [128, 4, nmax], F32)
            pooled = P.sb(st, "pooled", [128, 8, nmax], BF16)
            pw = P.sb(st, "pw", [128, 4, 2, 256], BF16)
            A = P.sb(st, "A", [128, 8], F32)
            Bc = P.sb(st, "Bc", [128, 8], F32)
            tmp = P.sb(st, "tmp", [128, 512], F32)
            P.dma("pool", pw[:], self.pool_w[j].rearrange("g (k p) d -> p g k d", p=128), self.pool_w, pw)
            for (t0, ns, cn, si) in streams:
                P.dma("sp", icn[:].rearrange("p g n -> p (g n)"),
                      self.invcnt[si].rearrange("g n -> (g n)").partition_broadcast(128), self.invcnt, icn)
                gp = self.make_gp(st, l, 0, cn, "gp%d" % si)
                P.tt("dve", A[:], self.mod(l, 2, cn), self.vec("pool_s%d" % j), ALU.mult, [self.modT, self.vecs], [A])
                P.tt("dve", Bc[:], A[:], self.vec("pool_b%d" % j), ALU.mult, [A, self.vecs], [Bc])
                for k in range(8):
                    P.dma("sp", hT[:, k, :ns], src[b, k * 128:(k + 1) * 128, t0:t0 + ns], src, hT)
                for s0 in range(0, ns, 512):
                    n = min(512, ns - s0)
                    self.rstd_block(hT, lambda k: hT[:, k, s0:s0 + n], n, sq, tmp, self.psb[6])
                    P.copy("dve", rstd[:, s0:s0 + n], tmp[:, :n], [tmp], [rstd])
                P.memset("pool", upad[:], 0.0, [upad])
                for k in range(8):
                    g = k // 2
                    w = POOL_WINDOWS[g]
                    m = g + 1
                    u = upad[:, 8:8 + ns]
                    P.tt("dve", u, hT[:, k, :ns], rstd[:, :ns], ALU.mult, [hT, rstd], [upad])
                    P.act(u, u, AF.Identity, [upad, gp, self.modT], [upad],
                          scale=gp[:, k:k + 1], bias=self.mod(l, 0, cn)[:, k:k + 1])
                    W = ns + 16
                    cur_, cb_ = upad, upad
                    bufs = [a1, a2]
                    for mm_ in range(m):
                        sh = 1 << mm_
                        nb = bufs[mm_ % 2]
                        ln = W - 2 * sh + 1 if mm_ == 0 else W - (2 << mm_) + 1
                        ln = W - ((2 << mm_) - 1)
                        P.tt("pool", nb[:, :ln], cur_[:, 0:ln], cur_[:, sh:sh + ln], ALU.add, [cb_], [nb])
                        cur_, cb_ = nb, nb
                    o0 = 8 - w // 2
                    P.tt("dve", a1[:, :ns] if cur_ is a2 else a2[:, :ns], cur_[:, o0:o0 + ns], icn[:, g, :ns], ALU.mult,
                         [cb_, icn], [a1 if cur_ is a2 else a2])
                    oth = a1 if cur_ is a2 else a2
                    P.tt("dve", pooled[:, k, :ns], oth[:, :ns], u, ALU.subtract, [oth, upad], [pooled])
                for s0 in range(0, ns, 512):
                    n = min(512, ns - s0)
                    for g in range(4):
                        for dd in range(2):
                            dch = 2 * g + dd
                            ps = self.psb[dch % 2]
                            for kk in range(2):
                                P.mm(ps, ps[:, :n], pw[:, g, kk, dd * 128:(dd + 1) * 128], pooled[:, 2 * g + kk, s0:s0 + n],
                                     [pw, pooled], start=(kk == 0), stop=(kk == 1))
                            P.stt(hT[:, dch, s0:s0 + n], ps[:, :n], A[:, dch:dch + 1], hT[:, dch, s0:s0 + n],
                                  ALU.mult, ALU.add, [ps, A, hT], [hT])
                            P.ts("dve", hT[:, dch, s0:s0 + n], hT[:, dch, s0:s0 + n], Bc[:, dch:dch + 1], None, ALU.add, None,
                                 [hT, Bc], [hT])
                for k in range(8):
                    P.dma("sp", dst[b, k * 128:(k + 1) * 128, t0:t0 + ns], hT[:, k, :ns], hT, dst)
            P.barrier()

    def build(self, do_mixer=True, do_ffn=True):
        c = self.cfg
        self.do_ffn = do_ffn
        self.setup()
        for l in c.layers:
            even = (l % 2 == 0)
            for b in range(c.BL):
                save = self.cur
                if do_mixer:
                    if even:
                        self.ab_stage(l, b)
                    else:
                        self.pool_stage(l, b)
                    self.cur = self.next_h()
                self.ffn_stage(l, b, moe=not even)
                self.cur = save
            if do_mixer:
                self.cur = self.next_h()
            self.cur = self.next_h()
        self.P.barrier()
        self.glob.close()
        return self.nc

    def ab_stage(self, l, b):
        dbg = getattr(self.cfg, "dbg", ())
        if "no1" not in dbg:
            self.ab_inproj(l, b)
        if "no2" not in dbg:
            self.ab_mla(l, b)
        if "no3" not in dbg:
            self.ab_gla(l, b)
        if "no4" not in dbg:
            self.ab_outproj(l, b)

    def ab_inproj(self, l, b):
        P, nc, c = self.P, self.nc, self.cfg
        j = l // 2
        src = self.cur
        OQ, OF, OI, OG = 704, 1216, 2240, 2752
        with contextlib.ExitStack() as st:
            win = P.sb(st, "win", [128, 8, 3264], BF16)
            hblk = [P.sb(st, "hblk%d" % i, [128, 8, 512], F32) for i in range(2)]
            uT = [P.sb(st, "uT%d" % i, [128, 8, 512], BF16) for i in range(2)]
            sq = P.sb(st, "sq", [128, 8, 512], BF16)
            rstd = P.sb(st, "rstd", [128, 512], F32)
            tmp = P.sb(st, "tmp", [128, 512], F32)
            cf = P.sb(st, "cf", [128, 3, 512], F32)
            cn = P.sb(st, "cn", [128, 3, 512], BF16)
            rs2 = P.sb(st, "rs2", [128, 512], F32)
            kro = P.sb(st, "kro", [32, 512], BF16)
            t1 = P.sb(st, "t1", [32, 512], F32)
            t2 = P.sb(st, "t2", [32, 512], F32)
            rp = P.sb(st, "rp", [32, 2, c.TT], F32)
            fo = [P.sb(st, "fo%d" % i, [128, 512], F32) for i in range(3)]
            tk = [P.sb(st, "tk%d" % i, [128, 512], BF16) for i in range(2)]
            tg = [P.sb(st, "tg%d" % i, [128, 512], F32) for i in range(2)]
            gps = {}
            for n_ in (b, c.BL):
                gps[n_] = self.make_gp(st, l, 0, n_, "gp%d" % n_)
            for s in range(0, 3264, 1088):
                P.dma("pool", win[:, :, s:s + 1088], self.w_in[j].rearrange("(k p) n -> p k n", p=128)[:, :, s:s + 1088], self.w_in, win)
            P.dma("sp", rp[:], self.rope[:].rearrange("a d t -> d a t"), self.rope, rp)
            ic = 0
            sqn = P.sb(st, "sqn", [128, 8, 512], BF16)

            def norm_block(bi):
                (s0, n, isc) = c.blocks[bi]
                cnd = c.BL if isc else b
                hb, u = hblk[bi % 2], uT[bi % 2]
                for k in range(8):
                    P.dma("sp", hb[:, k, :n], src[b, k * 128:(k + 1) * 128, s0:s0 + n], src, hb)
                self.rstd_block(hb, lambda k: hb[:, k, :n], n, sqn, rstd, self.psb[6])
                for k in range(8):
                    P.tt("dve", tmp[:, :n], hb[:, k, :n], rstd[:, :n], ALU.mult, [hb, rstd], [tmp])
                    P.act(u[:, k, :n], tmp[:, :n], AF.Identity, [tmp, gps[cnd], self.modT], [u],
                          scale=gps[cnd][:, k:k + 1], bias=self.mod(l, 0, cnd)[:, k:k + 1])

            norm_block(0)
            for bi, (s0, n, isc) in enumerate(c.blocks):
                cnd = c.BL if isc else b
                hb, u = hblk[bi % 2], uT[bi % 2]
                if bi + 1 < len(c.blocks):
                    norm_block(bi + 1)

                def proj(ps, col0, m):
                    for k in range(8):
                        P.mm(ps, ps[:m, :n], win[:, k, col0:col0 + m], u[:, k, :n], [win, u], start=(k == 0), stop=(k == 7))

                for (col0, nch, gname, dd) in ((0, 3, "g_cq%d" % j, self.cqn_d), (384, 2, "g_ckv%d" % j, self.ckvn_d)):
                    for q_ in range(nch):
                        ps = self.psb[ic % 4]
                        ic += 1
                        proj(ps, col0 + q_ * 128, 128)
                        P.copy("act", cf[:, q_, :n], ps[:, :n], [ps], [cf])
                    self.rstd_block(cf, lambda k: cf[:, k, :n], n, sq, rs2, self.psb[7], nk=nch, inv_n=1.0 / (nch * 128))
                    for q_ in range(nch):
                        P.stt(cn[:, q_, :n], cf[:, q_, :n], self.vec(gname)[:, q_:q_ + 1], rs2[:, :n], ALU.mult, ALU.mult,
                              [cf, rs2, self.vecs], [cn])
                    P.dma("sp", dd[:, s0:s0 + n].rearrange("(q p) t -> p q t", p=128), cn[:, :nch, :n], cn, dd)
                pa, pb = self.psb[ic % 4], self.psb[(ic + 1) % 4]
                ic += 2
                proj(pa, 640, 32)
                proj(pb, 672, 32)
                P.tt("dve", t1[:, :n], pa[:32, :n], rp[:, 0, s0:s0 + n], ALU.mult, [pa, rp], [t1])
                P.tt("dve", t2[:, :n], pb[:32, :n], rp[:, 1, s0:s0 + n], ALU.mult, [pb, rp], [t2])
                P.tt("pool", kro[:, :n], t1[:, :n], t2[:, :n], ALU.add, [t1, t2], [kro])
                P.dma("sp", self.krr_d[:, s0:s0 + n], kro[:, :n], kro, self.krr_d)
                for q_ in range(12):
                    ps = self.psb[ic % 4]
                    ic += 1
                    proj(ps, OQ + q_ * 128, 128)
                    o_ = fo[q_ % 3]
                    if q_ < 4:
                        P.act(o_[:, :n], ps[:, :n], AF.Silu, [ps], [o_])
                        P.dma("sp", self.qh_d[q_ * 128:(q_ + 1) * 128, s0:s0 + n], o_[:, :n], o_, self.qh_d)
                    else:
                        P.copy("act", o_[:, :n], ps[:, :n], [ps], [o_])
                        d_, h_ = (q_ - 4) // 4, (q_ - 4) % 4
                        P.dma("sp", self.hf_d[d_, h_ * 128:(h_ + 1) * 128, s0:s0 + n], o_[:, :n], o_, self.hf_d)
                for t0 in range(0, n, 128):
                    a0 = s0 + t0
                    for which, col0 in ((0, OI), (1, OG)):
                        ps = self.psb[ic % 4]
                        ic += 1
                        for k in range(8):
                            P.mm(ps, ps[:, :512], u[:, k, t0:t0 + 128], win[:, k, col0:col0 + 512], [u, win],
                                 start=(k == 0), stop=(k == 7))
                        if which == 0:
                            o_ = tk[(t0 // 128) % 2]
                            P.copy("act", o_[:], ps[:, :512], [ps], [o_])
                            P.dma("sp", self.vg_d[a0:a0 + 128, :], o_[:], o_, self.vg_d)
                        else:
                            o_ = tg[(t0 // 128) % 2]
                            P.act(o_[:], ps[:, :512], AF.Silu, [ps], [o_])
                            P.dma("sp", self.sg_d[a0:a0 + 128, :], o_[:], o_, self.sg_d)
            P.barrier()

    def ab_mla(self, l, b):
        P, nc, c = self.P, self.nc, self.cfg
        j = l // 2
        need_ctx = ctx_later(l)
        NT = c.TT // 128
        scale = 96.0 ** -0.5
        with contextlib.ExitStack() as st:
            cqn = P.sb(st, "cqn", [128, 3, c.TT], BF16)
            ckvn = P.sb(st, "ckvn", [128, 2, c.TT], BF16)
            krr = P.sb(st, "krr", [32, c.TT], BF16)
            rp = P.sb(st, "rp", [32, 2, c.TT], F32)
            wq = P.sb(st, "wq", [128, 3, 1024], BF16)
            wk = P.sb(st, "wk", [128, 2, 1024], BF16)
            wv = P.sb(st, "wv", [128, 2, 512], BF16)
            qT = P.sb(st, "qT", [96, 8, c.TT], BF16)
            kT = P.sb(st, "kT", [96, 8, c.TT], BF16)
            va = P.sb(st, "va", [128, NT, 8, 66], BF16)
            PT = [P.sb(st, "PT%d" % i, [128, NT, 512], BF16) for i in range(2)]
            t1 = P.sb(st, "t1", [32, 512], F32)
            t2 = P.sb(st, "t2", [32, 512], F32)
            alat = P.sb(st, "alat", [128, 4, 512], BF16)
            rden = P.sb(st, "rden", [128, 1], F32)
            mo = [P.sb(st, "mo%d" % i, [128, 4, 512], BF16) for i in range(1)]
            idb = P.sb(st, "idb", [128, 128], BF16)
            P.copy("dve", idb[:], self.ident[:], [self.ident], [idb])
            P.dma("sp", cqn[:], self.cqn_d[:].rearrange("(q p) t -> p q t", p=128), self.cqn_d, cqn)
            P.dma("sp", ckvn[:], self.ckvn_d[:].rearrange("(q p) t -> p q t", p=128), self.ckvn_d, ckvn)
            P.dma("sp", krr[:], self.krr_d[:], self.krr_d, krr)
            P.dma("sp", rp[:], self.rope[:].rearrange("a d t -> d a t"), self.rope, rp)
            P.dma("pool", wq[:], self.w_uq[j].rearrange("(k p) n -> p k n", p=128), self.w_uq, wq)
            P.dma("pool", wk[:], self.w_uk[j].rearrange("(k p) n -> p k n", p=128), self.w_uk, wk)
            P.dma("pool", wv[:], self.w_uv[j].rearrange("(k p) n -> p k n", p=128), self.w_uv, wv)
            P.memset("pool", va[:], 1.0, [va])
            ic = 0
            dbg = getattr(c, "dbg", ())
            for (s0, n, isc) in (c.blocks if "mla_noproj" not in dbg else []):
                for h in range(8):
                    pa, pb = self.psb[ic % 4], self.psb[(ic + 1) % 4]
                    pk = self.psb[(ic + 2) % 4]
                    ic += 3
                    if "skq" in dbg:
                        continue
                    for k in range(3):
                        P.mm(pa, pa[:96, :n], wq[:, k, h * 128:h * 128 + 96], cqn[:, k, s0:s0 + n], [wq, cqn], start=(k == 0), stop=(k == 2))
                    for k in range(3):
                        P.mm(pb, pb[:32, :n], wq[:, k, h * 128 + 96:h * 128 + 128], cqn[:, k, s0:s0 + n], [wq, cqn], start=(k == 0), stop=(k == 2))
                    P.copy("act", qT[:, h, s0:s0 + n], pa[:96, :n], [pa], [qT])
                    P.tt("dve", t1[:, :n], pa[:32, :n], rp[:, 0, s0:s0 + n], ALU.mult, [pa, rp], [t1])
                    P.tt("dve", t2[:, :n], pb[:32, :n], rp[:, 1, s0:s0 + n], ALU.mult, [pb, rp], [t2])
                    P.tt("dve" if "mla_dve" in dbg else "pool", qT[0:32, h, s0:s0 + n], t1[:, :n], t2[:, :n], ALU.add, [t1, t2], [qT])
                    if "skk" in dbg:
                        continue
                    for k in range(2):
                        P.mm(pk, pk[:96, :n], wk[:, k, h * 128:h * 128 + 96], ckvn[:, k, s0:s0 + n], [wk, ckvn], start=(k == 0), stop=(k == 1))
                    P.copy("act", kT[:, h, s0:s0 + n], pk[:96, :n], [pk], [kT])
                    P.copy("dve" if "mla_dve" in dbg else "pool", kT[0:32, h, s0:s0 + n], krr[:, s0:s0 + n], [krr], [kT])
                for t0 in (range(0, n, 128) if "skv" not in dbg else []):
                    a0 = s0 + t0
                    ps = self.psb[ic % 4]
                    ic += 1
                    for k in range(2):
                        P.mm(ps, ps[:, :512], ckvn[:, k, a0:a0 + 128], wv[:, k, :], [ckvn, wv], start=(k == 0), stop=(k == 1))
                    P.copy("act", va[:, a0 // 128, :, 0:64], ps[:, :512].rearrange("p (h d) -> p h d", d=64), [ps], [va])
            qblocks = c.blocks if need_ctx else c.lat_blocks
            if "mla_noattn" in dbg:
                qblocks = []
            items = [(blk, h) for blk in qblocks for h in range(8)]
            alats = [alat, P.sb(st, "alat2", [128, 4, 512], BF16)]

            def qk_ops(i):
                (s0, n, isc), h = items[i]
                kts = list(range(c.T // 128, NT)) if isc else list(range(NT))
                pt_ = PT[i % 2]
                ops = []
                for ki, kt in enumerate(kts):
                    def f(ki=ki, kt=kt):
                        ps = self.psb[ki % 2]
                        P.mm(ps, ps[:, :n], kT[:, h, kt * 128:(kt + 1) * 128], qT[:, h, s0:s0 + n], [kT, qT])
                        P.act(pt_[:, ki, :n], ps[:, :n], AF.Exp, [ps], [pt_], scale=scale)
                    ops.append(f)
                return ops

            def pv_ops(i):
                (s0, n, isc), h = items[i]
                kts = list(range(c.T // 128, NT)) if isc else list(range(NT))
                pt_ = PT[i % 2]
                bidx = qblocks.index((s0, n, isc))
                al = alats[bidx % 2]
                ops = []
                for qs in range(n // 128):
                    def f(qs=qs):
                        po = self.psb[2 + qs % 2]
                        for ki, kt in enumerate(kts):
                            P.mm(po, po[:, 0:65], pt_[:, ki, qs * 128:(qs + 1) * 128], va[:, kt, h, 0:65], [pt_, va],
                                 start=(ki == 0), stop=(ki == len(kts) - 1))
                        P.op("dve", lambda: nc.vector.reciprocal(rden[:], po[:, 64:65]), reads=[po], writes=[rden])
                        P.ts("dve", al[:, qs, h * 64:(h + 1) * 64], po[:, 0:64], rden[:, 0:1], None, ALU.mult, None,
                             [po, rden], [al])
                    ops.append(f)
                if h == 7:
                    def g():
                        m_ = mo[0]
                        for qs in range(n // 128):
                            for fc in range(4):
                                ptb = self.psb[4 + (qs * 4 + fc) % 2]
                                pv = ptb[:].bitcast(BF16)
                                P.op("pe", lambda: nc.tensor.transpose(pv[:, 0:128], al[:, qs, fc * 128:(fc + 1) * 128], idb[:]),
                                     reads=[al, idb], writes=[ptb])
                                P.copy("dve", m_[:, fc, qs * 128:(qs + 1) * 128], pv[:, 0:128], [ptb], [m_])
                        P.dma("sp", self.mix_d[0:512, s0:s0 + n].rearrange("(q p) t -> p q t", p=128), m_[:, :, :n], m_, self.mix_d)
                    ops.append(g)
                return ops

            if items:
                for f in qk_ops(0):
                    f()
            for i in range(len(items)):
                nxt = qk_ops(i + 1) if i + 1 < len(items) else []
                pv = pv_ops(i)
                tot = len(nxt) + len(pv)
                a_, b_ = 0, 0
                for t_ in range(tot):
                    if b_ < len(pv) and (a_ >= len(nxt) or (b_ + 1) * (len(nxt) + 1) <= (a_ + 1) * (len(pv) + 1) - 0):
                        pv[b_]()
                        b_ += 1
                    else:
                        nxt[a_]()
                        a_ += 1
            P.barrier()

    def ab_gla(self, l, b):
        P, nc, c = self.P, self.nc, self.cfg
        j = l // 2
        need_ctx = ctx_later(l)
        NT = c.TT // 128
        NCH = c.TT // 32
        lat_tiles = list(range(c.T // 128))
        ctx_tiles = list(range(c.T // 128, NT))
        with contextlib.ExitStack() as st:
            vg = P.sb(st, "vg", [128, NT, 512], BF16)
            oacc = P.sb(st, "oacc", [128, NT, 512], F32)
            smask = P.sb(st, "smask", [128, c.TT], F32)
            tri = P.sb(st, "tri", [128, 2, 128], F32)
            oml = P.sb(st, "oml", [128, 2, 4], F32)
            qh = P.sb(st, "qh", [128, c.TT], F32)
            hf = P.sb(st, "hf", [128, c.TT], F32)
            kin = P.sb(st, "kin", [128, c.TT], F32)
            G = P.sb(st, "G", [128, c.TT], F32)
            eG_s = [P.sb(st, "eG%d" % i, [128, c.TT], F32) for i in range(2)]
            w1 = P.sb(st, "w1", [128, c.TT], F32)
            qe_s = [P.sb(st, "qe%d" % i, [128, c.TT], BF16) for i in range(2)]
            ke_s = [P.sb(st, "ke%d" % i, [128, c.TT], BF16) for i in range(2)]
            kd_s = [P.sb(st, "kd%d" % i, [128, c.TT], BF16) for i in range(2)]
            kdt = [P.sb(st, "kdt%d" % i, [128, 4, 128], BF16) for i in range(2)]
            qeb = [P.sb(st, "qeb%d" % i, [128, 4, 128], BF16) for i in range(2)]
            bm = P.sb(st, "bm", [128, 4], F32)
            cm = P.sb(st, "cm", [128, 4, 128], BF16)
            atm = [P.sb(st, "atm%d" % i, [128, 128], BF16) for i in range(2)]
            S = [P.sb(st, "S%d" % i, [128, 128], F32) for i in range(2)]
            Sb = [P.sb(st, "Sb%d" % i, [128, 128], BF16) for i in range(10)]
            idb = P.sb(st, "idb", [128, 128], BF16)
            gnb = P.sb(st, "gnb", [128, 128], F32)
            sgt = [P.sb(st, "sgt%d" % i, [128, 512], F32) for i in range(2)]
            ssq = P.sb(st, "ssq", [128, 4], F32)
            junk = P.sb(st, "junk", [128, 128], F32)
            yt = P.sb(st, "yt", [128, 512], F32)
            blat = P.sb(st, "blat", [128, 512], BF16)
            mo = [P.sb(st, "mo%d" % i, [128, 4, 128], BF16) for i in range(2)]
            P.copy("dve", idb[:], self.ident[:], [self.ident], [idb])
            P.op("dve", lambda: nc.vector.reduce_sum(out=bm[:], in_=self.ident[:].rearrange("p (c l) -> p c l", l=32), axis=AX.X),
                 reads=[self.ident], writes=[bm])
            P.memset("pool", cm[:], 0.0, [cm])
            for cc in range(4):
                P.memset("pool", cm[:, cc, cc * 32:(cc + 1) * 32], 1.0, [cm])
            P.dma("sp", vg[:], self.vg_d[:].rearrange("(t p) f -> p t f", p=128), self.vg_d, vg)
            P.dma("sp", tri[:], self.trimask[:].rearrange("a s t -> s a t"), self.trimask, tri)
            P.dma("sp", gnb[:], self.gnorm[j].partition_broadcast(128), self.gnorm, gnb)
            P.memset("pool", smask[:], 1.0, [smask])
            P.memset("pool", smask[:].rearrange("p (c l) -> p c l", l=32)[:, :, 0:1], 0.0, [smask])
            if j == 0:
                P.memset("dve", oml[:], 1.0, [oml])
            else:
                for d in range(2):
                    P.tt("dve", oml[:, d, :], self.vec("lbl0_%d" % d), self.vec("lbl1_%d" % d), ALU.subtract, [self.vecs], [oml])
                P.act(oml[:], oml[:], AF.Sigmoid, [oml], [oml])
            chains = [(h, d) for h in range(4) for d in range(2)]

            def gate_ops(ci):
                h, d = chains[ci]
                eG, qe, ke, kd = eG_s[ci % 2], qe_s[ci % 2], ke_s[ci % 2], kd_s[ci % 2]
                G3 = G[:].rearrange("p (c l) -> p c l", l=32)
                gend = G3[:, :, 31:32] if d == 0 else G3[:, :, 0:1]
                ops = []
                if d == 0:
                    ops.append(lambda: P.dma("sp", qh[:], self.qh_d[h * 128:(h + 1) * 128, :], self.qh_d, qh))
                ops.append(lambda: P.dma("sp", hf[:], self.hf_d[d, h * 128:(h + 1) * 128, :], self.hf_d, hf))
                ops.append(lambda: P.act(kin[:], hf[:], AF.Sigmoid, [hf], [kin], scale=-1.0))
                ops.append(lambda: P.ts("dve", kin[:], kin[:], oml[:, d, h:h + 1], None, ALU.mult, None, [kin, oml], [kin]))
                ops.append(lambda: P.act(w1[:], kin[:], AF.Ln, [kin], [w1], scale=-1.0, bias=1.0))
                if d == 0:
                    ops.append(lambda: P.op("dve", lambda: nc.vector.tensor_tensor_scan(out=G[:], data0=smask[:], data1=w1[:], initial=0.0,
                                                                                         op0=ALU.mult, op1=ALU.add), reads=[smask, w1], writes=[G]))
                else:
                    ops.append(lambda: P.op("dve", lambda: nc.vector.tensor_tensor_scan(out=G[:, ::-1], data0=smask[:], data1=w1[:, ::-1], initial=0.0,
                                                                                         op0=ALU.mult, op1=ALU.add), reads=[smask, w1], writes=[G]))
                ops.append(lambda: P.act(eG[:], G[:], AF.Exp, [G], [eG]))
                ops.append(lambda: P.tt("pool", qe[:], qh[:], eG[:], ALU.mult, [qh, eG], [qe]))
                ops.append(lambda: P.tt("dve", w1[:].rearrange("p (c l) -> p c l", l=32), gend.to_broadcast([128, NCH, 32]), G3, ALU.subtract, [G], [w1]))
                ops.append(lambda: P.act(w1[:], w1[:], AF.Exp, [w1], [w1]))
                ops.append(lambda: P.tt("pool", kd[:], kin[:], w1[:], ALU.mult, [kin, w1], [kd]))
                ops.append(lambda: P.act(w1[:], G[:], AF.Exp, [G], [w1], scale=-1.0))
                ops.append(lambda: P.tt("pool", ke[:], kin[:], w1[:], ALU.mult, [kin, w1], [ke]))
                return ops

            pending = gate_ops(0)
            for ci, (h, d) in enumerate(chains):
                if True:
                    for f_ in pending:
                        f_()
                    pending = gate_ops(ci + 1) if ci + 1 < len(chains) else []
                    eG, qe, ke, kd = eG_s[ci % 2], qe_s[ci % 2], ke_s[ci % 2], kd_s[ci % 2]
                    eG3 = eG[:].rearrange("p (c l) -> p c l", l=32)
                    order = (ctx_tiles + lat_tiles) if d == 0 else (ctx_tiles[::-1] + lat_tiles[::-1])
                    if "gla_noscan" in getattr(c, "dbg", ()):
                        order = []
                    P.memset("dve", S[0][:], 0.0, [S[0]])
                    P.memset("pool", Sb[0][:], 0.0, [Sb[0]])
                    st_ = {"s": 0, "sb": 0}
                    crange = list(range(4)) if d == 0 else list(range(3, -1, -1))

                    def front(ti, tl):
                        a0 = tl * 128
                        skip_out = (tl in ctx_tiles) and not need_ctx
                        kt_, am_, qb_ = kdt[ti % 2], atm[ti % 2], qeb[ti % 2]
                        pk = self.psb[ti % 2]
                        pkv = pk[:].bitcast(BF16)
                        P.op("pe", lambda: nc.tensor.transpose(pkv[:, 0:128], kd[:, a0:a0 + 128], idb[:]), reads=[kd, idb], writes=[pk])
                        P.tt("dve", kt_[:], pkv[:, 0:128].unsqueeze(1).to_broadcast([128, 4, 128]),
                             bm[:].unsqueeze(2).to_broadcast([128, 4, 128]), ALU.mult, [pk, bm], [kt_])
                        po = self.psb[2 + ti % 2]
                        if not skip_out:
                            P.tt("pool", qb_[:], qe[:, a0:a0 + 128].unsqueeze(1).to_broadcast([128, 4, 128]), cm[:], ALU.mult, [qe, cm], [qb_])
                            pa = self.psb[4 + ti % 2]
                            P.mm(pa, pa[:, 0:128], ke[:, a0:a0 + 128], qe[:, a0:a0 + 128], [ke, qe])
                            P.tt("dve", am_[:], pa[:, 0:128], tri[:, d, :], ALU.mult, [pa, tri], [am_])
                            P.mm(po, po[:, 0:128], am_[:], vg[:, tl, h * 128:(h + 1) * 128], [am_, vg], start=True, stop=False)
                        pd = self.psb[6 + ti % 2]
                        for cc in crange:
                            P.mm(pd, pd[:, cc * 128:(cc + 1) * 128], kt_[:, cc, :], vg[:, tl, h * 128:(h + 1) * 128], [kt_, vg])

                    def back(ti, tl):
                        skip_out = (tl in ctx_tiles) and not need_ctx
                        qb_ = qeb[ti % 2]
                        po = self.psb[2 + ti % 2]
                        pd = self.psb[6 + ti % 2]
                        sbs = []
                        for cc in crange:
                            sbs.append(Sb[st_["sb"]])
                            chn = tl * 4 + cc
                            eg = eG3[:, chn, 31:32] if d == 0 else eG3[:, chn, 0:1]
                            so, sn = S[st_["s"]], S[1 - st_["s"]]
                            P.stt(sn[:], so[:], eg, pd[:, cc * 128:(cc + 1) * 128], ALU.mult, ALU.add, [so, eG, pd], [sn])
                            st_["s"] = 1 - st_["s"]
                            st_["sb"] = (st_["sb"] + 1) % len(Sb)
                            P.copy("act", Sb[st_["sb"]][:], sn[:], [sn], [Sb[st_["sb"]]])
                        if not skip_out:
                            for ci, cc in enumerate(crange):
                                P.mm(po, po[:, 0:128], qb_[:, cc, :], sbs[ci][:], [qb_, sbs[ci]], start=False, stop=(ci == 3))
                            if d == 0:
                                P.copy("act", oacc[:, tl, h * 128:(h + 1) * 128], po[:, 0:128], [po], [oacc])
                            else:
                                P.tt("dve", oacc[:, tl, h * 128:(h + 1) * 128], po[:, 0:128], oacc[:, tl, h * 128:(h + 1) * 128],
                                     ALU.add, [po, oacc], [oacc])

                    if order:
                        front(0, order[0])
                    for ti, tl in enumerate(order):
                        if ti + 1 < len(order):
                            front(ti + 1, order[ti + 1])
                        back(ti, tl)
                        if ti >= 1 and pending:
                            pending.pop(0)()
            tiles = (lat_tiles + ctx_tiles) if need_ctx else lat_tiles
            if "gla_noread" in getattr(c, "dbg", ()):
                tiles = []
            for ti, tl in enumerate(tiles):
                a0 = tl * 128
                sg_ = sgt[ti % 2]
                P.dma("sp", sg_[:], self.sg_d[a0:a0 + 128, :], self.sg_d, sg_)
                for h in range(4):
                    P.act(junk[:], oacc[:, tl, h * 128:(h + 1) * 128], AF.Square, [oacc], [junk, ssq], accum_out=ssq[:, h:h + 1])
                P.act(ssq[:], ssq[:], AF.Sqrt, [ssq, self.eps_t], [ssq], bias=self.eps_t[:], scale=1.0 / 128)
                P.op("dve", lambda: nc.vector.reciprocal(ssq[:], ssq[:]), reads=[ssq], writes=[ssq])
                for h in range(4):
                    P.stt(yt[:, h * 128:(h + 1) * 128], oacc[:, tl, h * 128:(h + 1) * 128], ssq[:, h:h + 1], gnb[:], ALU.mult, ALU.mult,
                          [oacc, ssq, gnb], [yt])
                P.tt("dve", blat[:], yt[:], sg_[:], ALU.mult, [yt, sg_], [blat])
                m_ = mo[ti % 2]
                for fc in range(4):
                    ptb = self.psb[fc % 2]
                    pv = ptb[:].bitcast(BF16)
                    P.op("pe", lambda: nc.tensor.transpose(pv[:, 0:128], blat[:, fc * 128:(fc + 1) * 128], idb[:]), reads=[blat, idb], writes=[ptb])
                    P.copy("act", m_[:, fc, :], pv[:, 0:128], [ptb], [m_])
                P.dma("sp", self.mix_d[512:1024, a0:a0 + 128].rearrange("(q p) t -> p q t", p=128), m_[:], m_, self.mix_d)
            P.barrier()

    def ab_outproj(self, l, b):
        P, nc, c = self.P, self.nc, self.cfg
        j = l // 2
        need_ctx = ctx_later(l)
        src = self.cur
        dst = self.next_h()
        blocks = c.blocks if need_ctx else c.lat_blocks
        with contextlib.ExitStack() as st:
            wo = P.sb(st, "wo", [128, 8, 1024], BF16)
            mx = [P.sb(st, "mx%d" % i, [128, 8, 512], BF16) for i in range(2)]
            hb = [P.sb(st, "hb%d" % i, [128, 8, 512], F32) for i in range(2)]
            P.dma("pool", wo[:], self.w_out[j].rearrange("(k p) n -> p k n", p=128), self.w_out, wo)
            for bi, (s0, n, isc) in enumerate(blocks):
                cnd = c.BL if isc else b
                m_, h_ = mx[bi % 2], hb[bi % 2]
                P.dma("sp", m_[:, :, :n], self.mix_d[:, s0:s0 + n].rearrange("(q p) t -> p q t", p=128), self.mix_d, m_)
                for k in range(8):
                    P.dma("sp", h_[:, k, :n], src[b, k * 128:(k + 1) * 128, s0:s0 + n], src, h_)
                for dch in range(8):
                    ps = self.psb[dch % 4]
                    for k in range(8):
                        P.mm(ps, ps[:, :n], wo[:, k, dch * 128:(dch + 1) * 128], m_[:, k, :n], [wo, m_], start=(k == 0), stop=(k == 7))
                    P.stt(h_[:, dch, :n], ps[:, :n], self.mod(l, 2, cnd)[:, dch:dch + 1], h_[:, dch, :n], ALU.mult, ALU.add,
                          [ps, self.modT, h_], [h_])
                for k in range(8):
                    P.dma("sp", dst[b, k * 128:(k + 1) * 128, s0:s0 + n], h_[:, k, :n], h_, dst)
            P.barrier()


def shared_inputs(inp, cfg):
    c = cfg
    sh = {}
    sh["vecs"] = build_vecs(inp)
    sh["w_mod"] = np.ascontiguousarray(inp["w_mod"], np.float32)
    sh["ffn_wg"] = np.ascontiguousarray(inp["ffn_w_gate"], np.float32)
    sh["ffn_wu"] = np.ascontiguousarray(inp["ffn_w_up"], np.float32)
    sh["ffn_wd"] = np.ascontiguousarray(inp["ffn_w_down"], np.float32)
    sh["moe_wg"] = np.ascontiguousarray(inp["moe_w_gate"], np.float32)
    sh["moe_wu"] = np.ascontiguousarray(inp["moe_w_up"], np.float32)
    sh["moe_wd"] = np.ascontiguousarray(inp["moe_w_down"], np.float32)
    sh["router"] = np.ascontiguousarray(
        np.asarray(inp["moe_router"], np.float32).reshape(2, c.KC, 128, c.NE).transpose(0, 2, 1, 3))
    sh["pool_w"] = np.ascontiguousarray(inp["pool_w"], np.float32)
    nmax = max(c.T, c.C)
    ic = np.ones((2, 4, nmax), np.float32)
    for si, n in enumerate((c.T, c.C)):
        pos = np.arange(n)
        for g, w in enumerate(POOL_WINDOWS):
            lo = np.clip(pos - w // 2, 0, n)
            hi = np.clip(pos - w // 2 + w, 0, n)
            ic[si, g, :n] = 1.0 / (hi - lo).astype(np.float32)
    sh["invcnt"] = ic
    sh["ident"] = np.eye(128, dtype=np.float32)
    w_in = np.asarray(inp["ab_w_in"], np.float32)
    swp = np.arange(32).reshape(2, 2, 8)[:, ::-1, :].reshape(32)
    o_kr = 384 + 256
    kr = w_in[:, :, o_kr:o_kr + 32]
    sh["w_in"] = np.ascontiguousarray(np.concatenate(
        [w_in[:, :, :o_kr + 32], kr[:, :, swp], w_in[:, :, o_kr + 32:]], axis=2))
    w_uq = np.asarray(inp["ab_w_uq"], np.float32).reshape(2, 384, 8, 96)
    nope, ropq = w_uq[..., :64], w_uq[..., 64:]
    sh["w_uq"] = np.ascontiguousarray(np.concatenate([ropq, nope, ropq[..., swp]], axis=3).reshape(2, 384, 1024))
    w_ukv = np.asarray(inp["ab_w_ukv"], np.float32).reshape(2, 256, 8, 128)
    z = np.zeros((2, 256, 8, 32), np.float32)
    sh["w_uk"] = np.ascontiguousarray(np.concatenate([z, w_ukv[..., :64], z], axis=3).reshape(2, 256, 1024))
    sh["w_uv"] = np.ascontiguousarray(w_ukv[..., 64:].reshape(2, 256, 512))
    sh["w_out"] = np.ascontiguousarray(inp["ab_w_out"], np.float32)
    sh["gnorm"] = np.ascontiguousarray(inp["hgrn_g_norm"], np.float32)
    tpos = np.arange(c.T)
    row = (tpos // c.grid_w).astype(np.float32)
    col = (tpos % c.grid_w).astype(np.float32)
    inv = (1.0 / (np.float32(10000.0) ** (np.arange(0, 16, 2, dtype=np.float32) / np.float32(16)))).astype(np.float32)
    ar = row[:, None] * inv[None, :]
    ac = col[:, None] * inv[None, :]
    ang = np.concatenate([ar, ar, ac, ac], axis=1).astype(np.float32)
    sign = np.tile(np.concatenate([-np.ones(8, np.float32), np.ones(8, np.float32)]), 2)
    rope = np.zeros((2, 32, c.TT), np.float32)
    rope[0, :, :c.T] = np.cos(ang).T
    rope[1, :, :c.T] = (np.sin(ang) * sign[None, :]).T
    rope[0, :, c.T:] = 1.0
    sh["rope"] = rope
    ii = np.arange(128)
    same = (ii[:, None] // 32) == (ii[None, :] // 32)
    tm = np.zeros((2, 128, 128), np.float32)
    tm[0] = (same & (ii[:, None] <= ii[None, :]))
    tm[1] = (same & (ii[:, None] >= ii[None, :]))
    sh["trimask"] = tm
    return sh


def core_inputs(inp, cfg, b0):
    c = cfg
    x = np.asarray(inp["x"], np.float32)[b0:b0 + c.BL]
    cx = np.asarray(inp["ctx"], np.float32)[b0:b0 + c.BL]
    xT = np.ascontiguousarray(np.concatenate([x.transpose(0, 2, 1), cx.transpose(0, 2, 1)], axis=2))
    cv = np.concatenate([np.asarray(inp["c"], np.float32)[b0:b0 + c.BL], np.asarray(inp["c_ctx"], np.float32)[None, :]], axis=0)
    cond = np.ascontiguousarray(cv.reshape(c.NB, c.KC, 128).transpose(2, 1, 0))
    return {"xT": xT, "cond": cond}


def kernel(**inp):
    cfg = Cfg()
    bld = Builder(cfg)
    nc = bld.build()
    sh = shared_inputs(inp, cfg)
    names = bld.input_names()
    in_maps = []
    for core in range(8):
        m = dict(sh)
        m.update(core_inputs(inp, cfg, core * cfg.BL))
        in_maps.append({k: m[k] for k in names})
    res = run_bass_kernel_spmd(nc, in_maps, core_ids=list(range(8)))
    outs = []
    for core in range(8):
        oT = np.asarray(res.results[core]["outT"])
        outs.append(oT.transpose(0, 2, 1))
    return np.ascontiguousarray(np.concatenate(outs, axis=0).astype(np.float32))
```

```python
import contextlib
import numpy as np
import concourse.bass as bass
import concourse.mybir as mybir
from concourse.bass_utils import run_bass_kernel_spmd

F32 = mybir.dt.float32
BF16 = mybir.dt.bfloat16
AF = mybir.ActivationFunctionType
ALU = mybir.AluOpType
AX = mybir.AxisListType

EPS = 1e-6
POOL_WINDOWS = (2, 4, 8, 16)


class Buf:
    def __init__(self, name, t):
        self.name = name
        self.t = t
        self.w = []
        self.r = []
        self.dkey = None

    def __getitem__(self, idx):
        return self.t[idx]


class Prog:
    ENG = ("pe", "dve", "act", "pool", "sp")

    def __init__(self, nc, n_dsem=60):
        self.nc = nc
        self.e = {"pe": nc.tensor, "dve": nc.vector, "act": nc.scalar, "pool": nc.gpsimd, "sp": nc.sync}
        self.sems = {}
        self.cnt = {}
        for k in self.ENG:
            self.sems[k] = nc.alloc_semaphore("s_" + k)
            self.cnt[k] = 0
        self.waited = {k: {} for k in self.ENG}
        self.dfree = {"sw": [], "hw": []}
        for i in range(n_dsem):
            key = ("d", i)
            self.sems[key] = nc.alloc_semaphore("d_%d" % i)
            self.cnt[key] = 0
            self.dfree["sw" if i < 12 else "hw"].append(key)
        self.stage_bufs = []
        self.dram_bufs = []
        self.uid = 0
        self.in_names = []

    def _name(self, name):
        self.uid += 1
        return "%s_%d" % (name, self.uid)

    def sb(self, stack, name, shape, dt):
        t = stack.enter_context(self.nc.sbuf_tensor(self._name(name), list(shape), dt))
        b = Buf(name, t)
        self.stage_bufs.append(b)
        return b

    def ps(self, stack, name, shape, dt=F32):
        t = stack.enter_context(self.nc.psum_tensor(self._name(name), list(shape), dt))
        b = Buf(name, t)
        b.excl = True
        self.stage_bufs.append(b)
        return b

    def dram(self, name, shape, dt, kind="Internal"):
        if kind == "ExternalInput":
            self.in_names.append(name)
        t = self.nc.dram_tensor(name, list(shape), dt, kind=kind)
        b = Buf(name, t)
        b.persistent = True
        self.dram_bufs.append(b)
        return b

    def _dkey(self, b, q):
        kind = "sw" if q == "pool" else "hw"
        if b.dkey is None:
            b.dkey = self.dfree[kind].pop()
            b.dkind = kind
            self.stage_bufs.append(b) if b not in self.stage_bufs else None
        assert b.dkind == kind, "buffer %s gets DMAs from both SW and HW DGE" % b.name
        return b.dkey

    def _wait(self, eng, events):
        wd = self.waited[eng]
        need = {}
        for (k, v) in events:
            if need.get(k, 0) < v:
                need[k] = v
        for k, v in need.items():
            if k == "pe" and eng == "pe":
                continue
            if wd.get(k, 0) >= v:
                continue
            self.e[eng].wait_ge(self.sems[k], v)
            wd[k] = v

    @staticmethod
    def _compact(evs):
        m = {}
        for k, v in evs:
            if m.get(k, 0) < v:
                m[k] = v
        return list(m.items())

    def op(self, eng, fn, reads=(), writes=()):
        reads = [b for b in reads if b is not None]
        ev = []
        for b in reads:
            ev += b.w
            if getattr(b, "excl", False):
                ev += [e for e in b.r if e[0] != eng]
        for b in writes:
            ev += b.w
            ev += b.r
        self._wait(eng, ev)
        inst = fn()
        self.cnt[eng] += 1
        inst.then_inc(self.sems[eng], 1)
        me = (eng, self.cnt[eng])
        for b in writes:
            b.w = [me]
            b.r = []
        for b in reads:
            if b not in writes:
                b.r.append(me)
                if len(b.r) > 16:
                    b.r = self._compact(b.r)
        return inst

    def dma(self, q, out_ap, in_ap, src, dst, **kw):
        store = getattr(dst, "persistent", False)
        side = src if store else dst
        key = self._dkey(side, q)
        if store:
            ev = list(src.w) + list(dst.r)
        else:
            ev = list(src.w) + list(dst.r) + [e for e in dst.w if e[0] != key]
        self._wait(q, ev)
        inst = self.e[q].dma_start(out=out_ap, in_=in_ap, **kw)
        self.cnt[key] += 16
        inst.then_inc(self.sems[key], 16)
        me = (key, self.cnt[key])
        if store:
            dst.w = self._compact(dst.w + [me])
        else:
            dst.w = [me]
            dst.r = []
        src.r.append(me)
        if len(src.r) > 16:
            src.r = self._compact(src.r)
        return inst

    def barrier(self):
        allev = [(k, v) for k, v in self.cnt.items() if v > 0]
        for eng in self.ENG:
            self._wait(eng, allev)
        for b in self.stage_bufs + self.dram_bufs:
            if b.dkey is not None:
                self.dfree[b.dkind].append(b.dkey)
                b.dkey = None
            b.w = []
            b.r = []
        self.stage_bufs = []

    def mm(self, ps, out_ap, lhsT, rhs, rd, start=True, stop=True, **kw):
        return self.op("pe", lambda: self.nc.tensor.matmul(out_ap, lhsT, rhs, start=start, stop=stop, **kw),
                       reads=rd, writes=[ps])

    def act(self, out_ap, in_ap, func, rd, wr, **kw):
        return self.op("act", lambda: self.nc.scalar.activation(out=out_ap, in_=in_ap, func=func, **kw),
                       reads=rd, writes=wr)

    def tt(self, eng, out_ap, a, b, op, rd, wr):
        return self.op(eng, lambda: self.e[eng].tensor_tensor(out=out_ap, in0=a, in1=b, op=op), reads=rd, writes=wr)

    def ts(self, eng, out_ap, a, s1, s2, op0, op1, rd, wr):
        if s2 is None:
            return self.op(eng, lambda: self.e[eng].tensor_scalar(out=out_ap, in0=a, scalar1=s1, scalar2=None, op0=op0),
                           reads=rd, writes=wr)
        return self.op(eng, lambda: self.e[eng].tensor_scalar(out=out_ap, in0=a, scalar1=s1, scalar2=s2, op0=op0, op1=op1),
                       reads=rd, writes=wr)

    def stt(self, out_ap, a, s, b, op0, op1, rd, wr):
        return self.op("dve", lambda: self.nc.vector.scalar_tensor_tensor(out=out_ap, in0=a, scalar=s, in1=b, op0=op0, op1=op1),
                       reads=rd, writes=wr)

    def copy(self, eng, out_ap, in_ap, rd, wr):
        if eng == "act":
            return self.op("act", lambda: self.nc.scalar.copy(out_ap, in_ap), reads=rd, writes=wr)
        return self.op(eng, lambda: self.e[eng].tensor_copy(out_ap, in_ap), reads=rd, writes=wr)

    def memset(self, eng, ap, val, wr):
        return self.op(eng, lambda: self.e[eng].memset(ap, val), writes=wr)


class Cfg:
    def __init__(self, T=2048, C=256, BL=2, layers=(0, 1, 2, 3), grid_w=64):
        self.D = 1024
        self.KC = 8
        self.T = T
        self.C = C
        self.TT = T + C
        self.BL = BL
        self.NB = BL + 1
        self.layers = tuple(layers)
        self.grid_w = grid_w
        self.DFF = 2816
        self.DFFE = 3584
        self.NE = 8
        self.blocks = []
        for s in range(0, T, 512):
            self.blocks.append((s, min(512, T - s), False))
        for s in range(0, C, 512):
            self.blocks.append((T + s, min(512, C - s), True))
        self.lat_blocks = [b for b in self.blocks if not b[2]]
        self.ctx_blocks = [b for b in self.blocks if b[2]]


def ctx_later(l):
    return any(m % 2 == 0 for m in range(l + 1, 4))


def vec_layout():
    off = {}
    n = 0

    def add(name, k):
        nonlocal n
        off[name] = (n, k)
        n += k

    for l in range(4):
        add("b_mod%d" % l, 48)
        add("ng%d_0" % l, 8)
        add("ng%d_1" % l, 8)
    add("final_g", 8)
    for j in range(2):
        add("g_cq%d" % j, 3)
        add("g_ckv%d" % j, 2)
        add("lbl%d_0" % j, 4)
        add("lbl%d_1" % j, 4)
        add("pool_b%d" % j, 8)
        add("pool_s%d" % j, 8)
    return off, n


def build_vecs(inp):
    off, n = vec_layout()
    v = np.zeros((128, n), np.float32)

    def put(name, arr):
        o, k = off[name]
        v[:, o:o + k] = np.asarray(arr, np.float32).reshape(k, 128).T

    for l in range(4):
        put("b_mod%d" % l, inp["b_mod"][l])
        put("ng%d_0" % l, inp["norm_g"][l, 0])
        put("ng%d_1" % l, inp["norm_g"][l, 1])
    put("final_g", inp["final_g"])
    for j in range(2):
        put("g_cq%d" % j, inp["ab_g_cq"][j])
        put("g_ckv%d" % j, inp["ab_g_ckv"][j])
        put("lbl%d_0" % j, inp["hgrn_lb_logits"][j, 0])
        put("lbl%d_1" % j, inp["hgrn_lb_logits"][j, 1])
        put("pool_b%d" % j, inp["pool_b"][j].reshape(-1))
        put("pool_s%d" % j, inp["pool_scale"][j])
    return v


class Builder:
    def __init__(self, cfg):
        self.cfg = cfg
        nc = bass.Bass("TRN2", target_bir_lowering=False)
        self.nc = nc
        self.P = Prog(nc)
        P = self.P
        c = cfg
        self.xT = P.dram("xT", [c.BL, c.D, c.TT], F32, kind="ExternalInput")
        self.cond = P.dram("cond", [128, c.KC, c.NB], F32, kind="ExternalInput")
        self.voff, nv = vec_layout()
        self.vecs_d = P.dram("vecs", [128, nv], F32, kind="ExternalInput")
        self.w_mod = P.dram("w_mod", [4, c.D, 6 * c.D], F32, kind="ExternalInput")
        self.ffn_wg = P.dram("ffn_wg", [2, c.D, c.DFF], F32, kind="ExternalInput")
        self.ffn_wu = P.dram("ffn_wu", [2, c.D, c.DFF], F32, kind="ExternalInput")
        self.ffn_wd = P.dram("ffn_wd", [2, c.DFF, c.D], F32, kind="ExternalInput")
        if getattr(c, "no_moe", False):
            self.moe_wg = P.dram("moe_wg", [2, 1, 8, 8], F32, kind="ExternalInput")
            self.moe_wu = P.dram("moe_wu", [2, 1, 8, 8], F32, kind="ExternalInput")
            self.moe_wd = P.dram("moe_wd", [2, 1, 8, 8], F32, kind="ExternalInput")
        else:
            self.moe_wg = P.dram("moe_wg", [2, c.NE, c.D, c.DFFE], F32, kind="ExternalInput")
            self.moe_wu = P.dram("moe_wu", [2, c.NE, c.D, c.DFFE], F32, kind="ExternalInput")
            self.moe_wd = P.dram("moe_wd", [2, c.NE, c.DFFE, c.D], F32, kind="ExternalInput")
        self.router = P.dram("router", [2, 128, c.KC, c.NE], F32, kind="ExternalInput")
        self.pool_w = P.dram("pool_w", [2, 4, 256, 256], F32, kind="ExternalInput")
        self.invcnt = P.dram("invcnt", [2, 4, max(c.T, c.C)], F32, kind="ExternalInput")
        self.w_in = P.dram("w_in", [2, c.D, 3264], F32, kind="ExternalInput")
        self.w_uq = P.dram("w_uq", [2, 384, 1024], F32, kind="ExternalInput")
        self.w_uk = P.dram("w_uk", [2, 256, 1024], F32, kind="ExternalInput")
        self.w_uv = P.dram("w_uv", [2, 256, 512], F32, kind="ExternalInput")
        self.w_out = P.dram("w_out", [2, 1024, c.D], F32, kind="ExternalInput")
        self.gnorm = P.dram("gnorm", [2, 128], F32, kind="ExternalInput")
        self.rope = P.dram("rope", [2, 32, c.TT], F32, kind="ExternalInput")
        self.trimask = P.dram("trimask", [2, 128, 128], F32, kind="ExternalInput")
        self.outT = P.dram("outT", [c.BL, c.D, c.T], F32, kind="ExternalOutput")
        self.cqn_d = P.dram("cqn_d", [384, c.TT], BF16)
        self.ckvn_d = P.dram("ckvn_d", [256, c.TT], BF16)
        self.krr_d = P.dram("krr_d", [32, c.TT], BF16)
        self.qh_d = P.dram("qh_d", [512, c.TT], F32)
        self.hf_d = P.dram("hf_d", [2, 512, c.TT], F32)
        self.vg_d = P.dram("vg_d", [c.TT, 512], BF16)
        self.sg_d = P.dram("sg_d", [c.TT, 512], F32)
        self.mix_d = P.dram("mix_d", [1024, c.TT], BF16)
        self.hbuf = [P.dram("hA", [c.BL, c.D, c.TT], F32), P.dram("hB", [c.BL, c.D, c.TT], F32)]
        self.cur = self.xT

        self._in_names = P.in_names
        self.glob = contextlib.ExitStack()
        g = self.glob
        self.vecs = P.sb(g, "vecs", [128, nv], F32)
        self.modT = P.sb(g, "modT", [128, 4, 48, c.NB], F32)
        self.ones_bf = P.sb(g, "ones_bf", [128, 128], BF16)
        self.eps_t = P.sb(g, "eps", [128, 1], F32)
        self.ident = P.sb(g, "ident", [128, 128], F32)
        self.ident_d = P.dram("ident", [128, 128], F32, kind="ExternalInput")
        self.psb = [P.ps(g, "ps%d" % i, [128, 512], F32) for i in range(8)]

    def input_names(self):
        return [k for k, v in self.__dict__.items() if False] or self._in_names

    def vec(self, name):
        o, k = self.voff[name]
        return self.vecs[:, o:o + k]

    def next_h(self):
        return self.hbuf[0] if self.cur is not self.hbuf[0] else self.hbuf[1]

    def setup(self):
        P, nc, c = self.P, self.nc, self.cfg
        P.dma("sp", self.vecs[:], self.vecs_d[:], self.vecs_d, self.vecs)
        P.memset("dve", self.ones_bf[:], 1.0, [self.ones_bf])
        P.dma("sp", self.ident[:], self.ident_d[:], self.ident_d, self.ident)
        P.memset("dve", self.eps_t[:], EPS, [self.eps_t])
        with contextlib.ExitStack() as st:
            cf = P.sb(st, "cond_f", [128, c.KC, c.NB], F32)
            sg = P.sb(st, "cond_sg", [128, c.KC, c.NB], F32)
            cb = P.sb(st, "cond_b", [128, c.KC, c.NB], BF16)
            wsl = [P.sb(st, "wmod_sl%d" % i, [128, c.KC, 1024], BF16) for i in range(2)]
            P.dma("sp", cf[:], self.cond[:], self.cond, cf)
            P.act(sg[:], cf[:], AF.Sigmoid, [cf], [sg])
            P.tt("dve", cb[:], cf[:], sg[:], ALU.mult, [cf, sg], [cb])
            it = 0
            for l in c.layers:
                for s in range(6):
                    w = wsl[it % 2]
                    it += 1
                    src = self.w_mod[l].rearrange("(k p) n -> p k n", p=128)[:, :, s * 1024:(s + 1) * 1024]
                    P.dma("pool", w[:], src, self.w_mod, w)
                    ps = self.psb[it % 2]
                    for jj in range(8):
                        for k in range(c.KC):
                            P.mm(ps, ps[:, jj * c.NB:(jj + 1) * c.NB], w[:, k, jj * 128:(jj + 1) * 128], cb[:, k, :],
                                 [w, cb], start=(k == 0), stop=(k == c.KC - 1))
                    o, _ = self.voff["b_mod%d" % l]
                    bsl = self.vecs[:, o + s * 8:o + s * 8 + 8]
                    P.tt("dve", self.modT[:, l, s * 8:(s + 1) * 8, :],
                         ps[:, 0:8 * c.NB].rearrange("p (j n) -> p j n", n=c.NB),
                         bsl.unsqueeze(2).to_broadcast([128, 8, c.NB]), ALU.add, [ps, self.vecs], [self.modT])
            P.barrier()

    def mod(self, l, which, n):
        return self.modT[:, l, which * 8:(which + 1) * 8, n]

    def make_gp(self, st, l, which_norm, n, name):
        P = self.P
        gp = P.sb(st, name, [128, 8], F32)
        sc = self.mod(l, 1 + 3 * which_norm, n)
        P.stt(gp[:], sc, 1.0, self.vec("ng%d_%d" % (l, which_norm)), ALU.add, ALU.mult, [self.modT, self.vecs], [gp])
        return gp

    def rstd_block(self, hsrc, hap_fn, n, sq, rstd, ps, nk=8, inv_n=1.0 / 1024):
        P = self.P
        for k in range(nk):
            P.act(sq[:, k, :n], hap_fn(k), AF.Square, [hsrc], [sq])
        on = self.ones_bf
        for k in range(nk):
            P.mm(ps, ps[:, :n], on[:], sq[:, k, :n], [on, sq], start=(k == 0), stop=(k == nk - 1))
        P.act(rstd[:, :n], ps[:, :n], AF.Sqrt, [ps, self.eps_t], [rstd], bias=self.eps_t[:], scale=inv_n)
        P.op("dve", lambda: self.nc.vector.reciprocal(rstd[:, :n], rstd[:, :n]), reads=[rstd], writes=[rstd])

    def ffn_stage(self, l, b, moe):
        P, nc, c = self.P, self.nc, self.cfg
        j = l // 2
        with_ctx = ctx_later(l)
        blocks = c.blocks if with_ctx else c.lat_blocks
        ntok = c.TT if with_ctx else c.T
        last = (l == c.layers[-1])
        src = self.cur
        dst = self.next_h()
        nf = (c.DFFE if moe else c.DFF) // 128
        groups = []
        f0 = 0
        while f0 < nf:
            groups.append((f0, min(4, nf - f0)))
            f0 += 4
        ne = c.NE if moe else 1
        if not getattr(self, 'do_ffn', True):
            ne = 0
            moe = False
        with contextlib.ExitStack() as st:
            hT = P.sb(st, "hT", [128, 8, ntok], F32)
            vT = P.sb(st, "vT", [128, 8, ntok], BF16)
            sq = P.sb(st, "sq", [128, 8, 256], BF16)
            rstd = P.sb(st, "rstd", [128, 256], F32)
            tmp = P.sb(st, "tmp", [128, 256], F32)
            nblocks = []
            for (s0_, n_, isc_) in blocks:
                for q0 in range(0, n_, 256):
                    nblocks.append((s0_ + q0, min(256, n_ - q0), isc_))
            wg = [P.sb(st, "wg%d" % i, [128, 8, 512], BF16) for i in range(2)]
            wu = [P.sb(st, "wu%d" % i, [128, 8, 512], BF16) for i in range(2)]
            wd = [P.sb(st, "wd%d" % i, [128, 4, 1024], BF16) for i in range(2)]
            hid = [P.sb(st, "hid%d" % i, [128, 4, 512], BF16) for i in range(2)]
            sl = [P.sb(st, "sl%d" % i, [128, 512], F32) for i in range(2)]
            sgb = [P.sb(st, "sgb%d" % i, [128, 512], F32) for i in range(2)]
            gp = {}
            for n in ([b, c.BL] if with_ctx else [b]):
                gp[n] = self.make_gp(st, l, 1, n, "gp%d" % n)
            for k in range(8):
                P.dma("sp", hT[:, k, :], src[b, k * 128:(k + 1) * 128, 0:ntok], src, hT)
            psn = self.psb[6]
            for (s0, n, isc) in nblocks:
                cn = c.BL if isc else b
                self.rstd_block(hT, lambda k: hT[:, k, s0:s0 + n], n, sq, rstd, psn)
                for k in range(8):
                    P.tt("dve", tmp[:, :n], hT[:, k, s0:s0 + n], rstd[:, :n], ALU.mult, [hT, rstd], [tmp])
                    P.act(vT[:, k, s0:s0 + n], tmp[:, :n], AF.Identity, [tmp, gp[cn], self.modT], [vT],
                          scale=gp[cn][:, k:k + 1], bias=self.mod(l, 3, cn)[:, k:k + 1])
            gates = None
            dbg = getattr(c, "dbg", ())
            if moe:
                if "nogates" not in dbg:
                    gates = self.moe_gates(st, l, vT, blocks, ntok)
                gbc = P.sb(st, "gbc", [128, ntok], F32)
                if "nogates" in dbg or "nogbc" in dbg:
                    P.memset("dve", gbc[:], 0.125, [gbc])
            items = [(e, f0, fg) for e in range(ne) for (f0, fg) in groups]
            Wsrc = (self.moe_wg, self.moe_wu, self.moe_wd) if moe else (self.ffn_wg, self.ffn_wu, self.ffn_wd)

            def emit_dma(i):
                e, f0, fg = items[i]
                a = i % 2
                if moe:
                    Wg, Wu, Wd = self.moe_wg[j, e], self.moe_wu[j, e], self.moe_wd[j, e]
                else:
                    Wg, Wu, Wd = self.ffn_wg[j], self.ffn_wu[j], self.ffn_wd[j]
                P.dma("pool", wg[a][:, :, :fg * 128],
                      Wg.rearrange("(k p) f -> p k f", p=128)[:, :, f0 * 128:(f0 + fg) * 128], Wsrc[0], wg[a])
                P.dma("pool", wu[a][:, :, :fg * 128],
                      Wu.rearrange("(k p) f -> p k f", p=128)[:, :, f0 * 128:(f0 + fg) * 128], Wsrc[1], wu[a])
                P.dma("pool", wd[a][:, :fg, :],
                      Wd[f0 * 128:(f0 + fg) * 128, :].rearrange("(f p) d -> p f d", p=128), Wsrc[2], wd[a])

            def emit_gbc(e):
                sel, gT = gates
                for (s0, n, isc) in blocks:
                    ps = self.psb[6]
                    for t0 in range(0, n, 128):
                        P.mm(ps, ps[:, t0:t0 + 128], sel[:, e, :], gT[:, s0 + t0:s0 + t0 + 128], [sel, gT])
                    P.copy("act", gbc[:, s0:s0 + n], ps[:, :n], [ps], [gbc])

            cnt_ = [0]

            def GU(i, blk, hb):
                e, f0, fg = items[i]
                a = i % 2
                (s0, n, isc) = blk
                for jj in range(fg):
                    q_ = cnt_[0]
                    cnt_[0] += 1
                    pg = self.psb[q_ % 2]
                    pu = self.psb[2 + q_ % 2]
                    for k in range(8):
                        P.mm(pg, pg[:, :n], wg[a][:, k, jj * 128:(jj + 1) * 128], vT[:, k, s0:s0 + n], [wg[a], vT],
                             start=(k == 0), stop=(k == 7))
                    for k in range(8):
                        P.mm(pu, pu[:, :n], wu[a][:, k, jj * 128:(jj + 1) * 128], vT[:, k, s0:s0 + n], [wu[a], vT],
                             start=(k == 0), stop=(k == 7))
                    s_ = sl[q_ % 2]
                    P.act(s_[:, :n], pg[:, :n], AF.Silu, [pg], [s_])
                    if moe:
                        g_ = sgb[q_ % 2]
                        P.tt("dve", g_[:, :n], s_[:, :n], gbc[:, s0:s0 + n], ALU.mult, [s_, gbc], [g_])
                        s_ = g_
                    P.tt("dve", hb[:, jj, :n], pu[:, :n], s_[:, :n], ALU.mult, [pu, s_], [hb])

            dcnt = [0]

            def DD(i, blk, hb):
                e, f0, fg = items[i]
                a = i % 2
                (s0, n, isc) = blk
                cn = c.BL if isc else b
                for dch in range(8):
                    py = self.psb[4 + dcnt[0] % 2]
                    dcnt[0] += 1
                    for jj in range(fg):
                        P.mm(py, py[:, :n], wd[a][:, jj, dch * 128:(dch + 1) * 128], hb[:, jj, :n], [wd[a], hb],
                             start=(jj == 0), stop=(jj == fg - 1))
                    P.stt(hT[:, dch, s0:s0 + n], py[:, :n], self.mod(l, 5, cn)[:, dch:dch + 1], hT[:, dch, s0:s0 + n],
                          ALU.mult, ALU.add, [py, self.modT, hT], [hT])

            if items:
                emit_dma(0)
            prev = None
            wi = 0
            cur_e = -1
            for i in range(len(items)):
                for bi, blk in enumerate(blocks):
                    if moe and items[i][0] != cur_e:
                        cur_e = items[i][0]
                        emit_gbc(cur_e)
                    hb = hid[wi % 2]
                    wi += 1
                    GU(i, blk, hb)
                    if prev is not None:
                        DD(*prev)
                    prev = (i, blk, hb)
                    if bi == 0 and i + 1 < len(items):
                        emit_dma(i + 1)
            if prev is not None:
                DD(*prev)
            if last:
                for (s0, n, isc) in [nb for nb in nblocks if not nb[2]]:
                    self.rstd_block(hT, lambda k: hT[:, k, s0:s0 + n], n, sq, rstd, psn)
                    for k in range(8):
                        P.tt("dve", tmp[:, :n], hT[:, k, s0:s0 + n], rstd[:, :n], ALU.mult, [hT, rstd], [tmp])
                        o_ = sl[k % 2]
                        P.ts("dve", o_[:, :n], tmp[:, :n], self.vec("final_g")[:, k:k + 1], None, ALU.mult, None,
                             [tmp, self.vecs], [o_])
                        P.dma("sp", self.outT[b, k * 128:(k + 1) * 128, s0:s0 + n], o_[:, :n], o_, self.outT)
            else:
                for k in range(8):
                    P.dma("sp", dst[b, k * 128:(k + 1) * 128, 0:ntok], hT[:, k, :], hT, dst)
            P.barrier()

    def moe_gates(self, st, l, vT, blocks, ntok):
        P, nc, c = self.P, self.nc, self.cfg
        j = l // 2
        rt = P.sb(st, "router", [128, 8, c.NE], BF16)
        P.dma("pool", rt[:], self.router[j], self.router, rt)
        sel = P.sb(st, "sel", [8, c.NE, 128], F32)
        gT = P.sb(st, "gT", [8, ntok], F32)
        lg = P.sb(st, "lg", [128, 8], F32)
        top = P.sb(st, "top", [128, 8], F32)
        ex = P.sb(st, "ex", [128, 8], F32)
        msk = P.sb(st, "msk", [128, 8], F32)
        den = P.sb(st, "den", [128, 1], F32)
        nmx = P.sb(st, "nmx", [128, 1], F32)
        gt = P.sb(st, "gt", [128, 8], F32)
        P.copy("dve", sel[:], self.ident[0:8, 0:8].unsqueeze(2).to_broadcast([8, 8, 128]), [self.ident], [sel])
        ps = self.psb[7]
        pt = self.psb[6]
        for (s0, n, isc) in blocks:
            for t0 in range(0, n, 128):
                a0 = s0 + t0
                for k in range(8):
                    P.mm(ps, ps[:, 0:8], vT[:, k, a0:a0 + 128], rt[:, k, :], [vT, rt], start=(k == 0), stop=(k == 7))
                P.copy("dve", lg[:], ps[:, 0:8], [ps], [lg])
                P.op("dve", lambda: nc.vector.max(out=top[:], in_=lg[:]), reads=[lg], writes=[top])
                P.ts("dve", nmx[:], top[:, 0:1], -1.0, None, ALU.mult, None, [top], [nmx])
                P.act(ex[:], lg[:], AF.Exp, [lg, nmx], [ex], bias=nmx[:], scale=1.0)
                P.ts("dve", msk[:], lg[:], top[:, 1:2], None, ALU.is_ge, None, [lg, top], [msk])
                P.tt("dve", gt[:], ex[:], msk[:], ALU.mult, [ex, msk], [gt])
                P.op("dve", lambda: nc.vector.reduce_sum(out=den[:], in_=gt[:], axis=AX.X), reads=[gt], writes=[den])
                P.op("dve", lambda: nc.vector.reciprocal(den[:], den[:]), reads=[den], writes=[den])
                P.ts("dve", gt[:], gt[:], den[:, 0:1], None, ALU.mult, None, [gt, den], [gt])
                P.op("pe", lambda: nc.tensor.transpose(pt[0:8, 0:128], gt[:], self.ident[:]), reads=[gt, self.ident], writes=[pt])
                P.copy("act", gT[:, a0:a0 + 128], pt[0:8, 0:128], [pt], [gT])
        return sel, gT

    def ensure_ident(self):
        if getattr(self, "_ident_done", False):
            return
        P, nc = self.P, self.nc
        P.memset("pool", self.ident[:], 0.0, [self.ident])
        P.op("pool", lambda: nc.gpsimd.affine_select(out=self.ident[:], in_=self.ident[:], pattern=[[-1, 128]],
                                                      compare_op=ALU.not_equal, fill=1.0, base=0, channel_multiplier=1),
             reads=[self.ident], writes=[self.ident])
        self._ident_done = True

    def pool_stage(self, l, b):
        P, nc, c = self.P, self.nc, self.cfg
        j = l // 2
        with_ctx = ctx_later(l)
        src = self.cur
        dst = self.next_h()
        streams = [(0, c.T, b, 0)] + ([(c.T, c.C, c.BL, 1)] if with_ctx else [])
        nmax = max(c.T, c.C)
        with contextlib.ExitStack() as st:
            hT = P.sb(st, "hT", [128, 8, nmax], F32)
            sq = P.sb(st, "sq", [128, 8, 512], BF16)
            rstd = P.sb(st, "rstd", [128, nmax], F32)
            upad = P.sb(st, "upad", [128, nmax + 16], F32)
            a1 = P.sb(st, "a1", [128, nmax + 16], F32)
            a2 = P.sb(st, "a2", [128, nmax + 16], F32)
            icn = P.sb(st, "icn", [128, 4, nmax], F32)
            pooled = P.sb(st, "pooled", [128, 8, nmax], BF16)
            pw = P.sb(st, "pw", [128, 4, 2, 256], BF16)
            A = P.sb(st, "A", [128, 8], F32)
            Bc = P.sb(st, "Bc", [128, 8], F32)
            tmp = P.sb(st, "tmp", [128, 512], F32)
            P.dma("pool", pw[:], self.pool_w[j].rearrange("g (k p) d -> p g k d", p=128), self.pool_w, pw)
            for (t0, ns, cn, si) in streams:
                P.dma("sp", icn[:].rearrange("p g n -> p (g n)"),
                      self.invcnt[si].rearrange("g n -> (g n)").partition_broadcast(128), self.invcnt, icn)
                gp = self.make_gp(st, l, 0, cn, "gp%d" % si)
                P.tt("dve", A[:], self.mod(l, 2, cn), self.vec("pool_s%d" % j), ALU.mult, [self.modT, self.vecs], [A])
                P.tt("dve", Bc[:], A[:], self.vec("pool_b%d" % j), ALU.mult, [A, self.vecs], [Bc])
                for k in range(8):
                    P.dma("sp", hT[:, k, :ns], src[b, k * 128:(k + 1) * 128, t0:t0 + ns], src, hT)
                for s0 in range(0, ns, 512):
                    n = min(512, ns - s0)
                    self.rstd_block(hT, lambda k: hT[:, k, s0:s0 + n], n, sq, tmp, self.psb[6])
                    P.copy("dve", rstd[:, s0:s0 + n], tmp[:, :n], [tmp], [rstd])
                P.memset("pool", upad[:], 0.0, [upad])
                for k in range(8):
                    g = k // 2
                    w = POOL_WINDOWS[g]
                    m = g + 1
                    u = upad[:, 8:8 + ns]
                    P.tt("dve", u, hT[:, k, :ns], rstd[:, :ns], ALU.mult, [hT, rstd], [upad])
                    P.act(u, u, AF.Identity, [upad, gp, self.modT], [upad],
                          scale=gp[:, k:k + 1], bias=self.mod(l, 0, cn)[:, k:k + 1])
                    W = ns + 16
                    cur_, cb_ = upad, upad
                    bufs = [a1, a2]
                    for mm_ in range(m):
                        sh = 1 << mm_
                        nb = bufs[mm_ % 2]
                        ln = W - 2 * sh + 1 if mm_ == 0 else W - (2 << mm_) + 1
                        ln = W - ((2 << mm_) - 1)
                        P.tt("pool", nb[:, :ln], cur_[:, 0:ln], cur_[:, sh:sh + ln], ALU.add, [cb_], [nb])
                        cur_, cb_ = nb, nb
                    o0 = 8 - w // 2
                    P.tt("dve", a1[:, :ns] if cur_ is a2 else a2[:, :ns], cur_[:, o0:o0 + ns], icn[:, g, :ns], ALU.mult,
                         [cb_, icn], [a1 if cur_ is a2 else a2])
                    oth = a1 if cur_ is a2 else a2
                    P.tt("dve", pooled[:, k, :ns], oth[:, :ns], u, ALU.subtract, [oth, upad], [pooled])
                for s0 in range(0, ns, 512):
                    n = min(512, ns - s0)
                    for g in range(4):
                        for dd in range(2):
                            dch = 2 * g + dd
                            ps = self.psb[dch % 2]
                            for kk in range(2):
                                P.mm(ps, ps[:, :n], pw[:, g, kk, dd * 128:(dd + 1) * 128], pooled[:, 2 * g + kk, s0:s0 + n],
                                     [pw, pooled], start=(kk == 0), stop=(kk == 1))
                            P.stt(hT[:, dch, s0:s0 + n], ps[:, :n], A[:, dch:dch + 1], hT[:, dch, s0:s0 + n],
                                  ALU.mult, ALU.add, [ps, A, hT], [hT])
                            P.ts("dve", hT[:, dch, s0:s0 + n], hT[:, dch, s0:s0 + n], Bc[:, dch:dch + 1], None, ALU.add, None,
                                 [hT, Bc], [hT])
                for k in range(8):
                    P.dma("sp", dst[b, k * 128:(k + 1) * 128, t0:t0 + ns], hT[:, k, :ns], hT, dst)
            P.barrier()

    def build(self, do_mixer=True, do_ffn=True):
        c = self.cfg
        self.do_ffn = do_ffn
        self.setup()
        for l in c.layers:
            even = (l % 2 == 0)
            for b in range(c.BL):
                save = self.cur
                if do_mixer:
                    if even:
                        self.ab_stage(l, b)
                    else:
                        self.pool_stage(l, b)
                    self.cur = self.next_h()
                self.ffn_stage(l, b, moe=not even)
                self.cur = save
            if do_mixer:
                self.cur = self.next_h()
            self.cur = self.next_h()
        self.P.barrier()
        self.glob.close()
        return self.nc

    def ab_stage(self, l, b):
        dbg = getattr(self.cfg, "dbg", ())
        if "no1" not in dbg:
            self.ab_inproj(l, b)
        if "no2" not in dbg:
            self.ab_mla(l, b)
        if "no3" not in dbg:
            self.ab_gla(l, b)
        if "no4" not in dbg:
            self.ab_outproj(l, b)

    def ab_inproj(self, l, b):
        P, nc, c = self.P, self.nc, self.cfg
        j = l // 2
        src = self.cur
        OQ, OF, OI, OG = 704, 1216, 2240, 2752
        with contextlib.ExitStack() as st:
            win = P.sb(st, "win", [128, 8, 3264], BF16)
            hblk = [P.sb(st, "hblk%d" % i, [128, 8, 512], F32) for i in range(2)]
            uT = [P.sb(st, "uT%d" % i, [128, 8, 512], BF16) for i in range(2)]
            sq = P.sb(st, "sq", [128, 8, 512], BF16)
            rstd = P.sb(st, "rstd", [128, 512], F32)
            tmp = P.sb(st, "tmp", [128, 512], F32)
            cf = P.sb(st, "cf", [128, 3, 512], F32)
            cn = P.sb(st, "cn", [128, 3, 512], BF16)
            rs2 = P.sb(st, "rs2", [128, 512], F32)
            kro = P.sb(st, "kro", [32, 512], BF16)
            t1 = P.sb(st, "t1", [32, 512], F32)
            t2 = P.sb(st, "t2", [32, 512], F32)
            rp = P.sb(st, "rp", [32, 2, c.TT], F32)
            fo = [P.sb(st, "fo%d" % i, [128, 512], F32) for i in range(3)]
            tk = [P.sb(st, "tk%d" % i, [128, 512], BF16) for i in range(2)]
            tg = [P.sb(st, "tg%d" % i, [128, 512], F32) for i in range(2)]
            gps = {}
            for n_ in (b, c.BL):
                gps[n_] = self.make_gp(st, l, 0, n_, "gp%d" % n_)
            for s in range(0, 3264, 1088):
                P.dma("pool", win[:, :, s:s + 1088], self.w_in[j].rearrange("(k p) n -> p k n", p=128)[:, :, s:s + 1088], self.w_in, win)
            P.dma("sp", rp[:], self.rope[:].rearrange("a d t -> d a t"), self.rope, rp)
            ic = 0
            sqn = P.sb(st, "sqn", [128, 8, 512], BF16)

            def norm_block(bi):
                (s0, n, isc) = c.blocks[bi]
                cnd = c.BL if isc else b
                hb, u = hblk[bi % 2], uT[bi % 2]
                for k in range(8):
                    P.dma("sp", hb[:, k, :n], src[b, k * 128:(k + 1) * 128, s0:s0 + n], src, hb)
                self.rstd_block(hb, lambda k: hb[:, k, :n], n, sqn, rstd, self.psb[6])
                for k in range(8):
                    P.tt("dve", tmp[:, :n], hb[:, k, :n], rstd[:, :n], ALU.mult, [hb, rstd], [tmp])
                    P.act(u[:, k, :n], tmp[:, :n], AF.Identity, [tmp, gps[cnd], self.modT], [u],
                          scale=gps[cnd][:, k:k + 1], bias=self.mod(l, 0, cnd)[:, k:k + 1])

            norm_block(0)
            for bi, (s0, n, isc) in enumerate(c.blocks):
                cnd = c.BL if isc else b
                hb, u = hblk[bi % 2], uT[bi % 2]
                if bi + 1 < len(c.blocks):
                    norm_block(bi + 1)

                def proj(ps, col0, m):
                    for k in range(8):
                        P.mm(ps, ps[:m, :n], win[:, k, col0:col0 + m], u[:, k, :n], [win, u], start=(k == 0), stop=(k == 7))

                for (col0, nch, gname, dd) in ((0, 3, "g_cq%d" % j, self.cqn_d), (384, 2, "g_ckv%d" % j, self.ckvn_d)):
                    for q_ in range(nch):
                        ps = self.psb[ic % 4]
                        ic += 1
                        proj(ps, col0 + q_ * 128, 128)
                        P.copy("act", cf[:, q_, :n], ps[:, :n], [ps], [cf])
                    self.rstd_block(cf, lambda k: cf[:, k, :n], n, sq, rs2, self.psb[7], nk=nch, inv_n=1.0 / (nch * 128))
                    for q_ in range(nch):
                        P.stt(cn[:, q_, :n], cf[:, q_, :n], self.vec(gname)[:, q_:q_ + 1], rs2[:, :n], ALU.mult, ALU.mult,
                              [cf, rs2, self.vecs], [cn])
                    P.dma("sp", dd[:, s0:s0 + n].rearrange("(q p) t -> p q t", p=128), cn[:, :nch, :n], cn, dd)
                pa, pb = self.psb[ic % 4], self.psb[(ic + 1) % 4]
                ic += 2
                proj(pa, 640, 32)
                proj(pb, 672, 32)
                P.tt("dve", t1[:, :n], pa[:32, :n], rp[:, 0, s0:s0 + n], ALU.mult, [pa, rp], [t1])
                P.tt("dve", t2[:, :n], pb[:32, :n], rp[:, 1, s0:s0 + n], ALU.mult, [pb, rp], [t2])
                P.tt("pool", kro[:, :n], t1[:, :n], t2[:, :n], ALU.add, [t1, t2], [kro])
                P.dma("sp", self.krr_d[:, s0:s0 + n], kro[:, :n], kro, self.krr_d)
                for q_ in range(12):
                    ps = self.psb[ic % 4]
                    ic += 1
                    proj(ps, OQ + q_ * 128, 128)
                    o_ = fo[q_ % 3]
                    if q_ < 4:
                        P.act(o_[:, :n], ps[:, :n], AF.Silu, [ps], [o_])
                        P.dma("sp", self.qh_d[q_ * 128:(q_ + 1) * 128, s0:s0 + n], o_[:, :n], o_, self.qh_d)
                    else:
                        P.copy("act", o_[:, :n], ps[:, :n], [ps], [o_])
                        d_, h_ = (q_ - 4) // 4, (q_ - 4) % 4
                        P.dma("sp", self.hf_d[d_, h_ * 128:(h_ + 1) * 128, s0:s0 + n], o_[:, :n], o_, self.hf_d)
                for t0 in range(0, n, 128):
                    a0 = s0 + t0
                    for which, col0 in ((0, OI), (1, OG)):
                        ps = self.psb[ic % 4]
                        ic += 1
                        for k in range(8):
                            P.mm(ps, ps[:, :512], u[:, k, t0:t0 + 128], win[:, k, col0:col0 + 512], [u, win],
                                 start=(k == 0), stop=(k == 7))
                        if which == 0:
                            o_ = tk[(t0 // 128) % 2]
                            P.copy("act", o_[:], ps[:, :512], [ps], [o_])
                            P.dma("sp", self.vg_d[a0:a0 + 128, :], o_[:], o_, self.vg_d)
                        else:
                            o_ = tg[(t0 // 128) % 2]
                            P.act(o_[:], ps[:, :512], AF.Silu, [ps], [o_])
                            P.dma("sp", self.sg_d[a0:a0 + 128, :], o_[:], o_, self.sg_d)
            P.barrier()

    def ab_mla(self, l, b):
        P, nc, c = self.P, self.nc, self.cfg
        j = l // 2
        need_ctx = ctx_later(l)
        NT = c.TT // 128
        scale = 96.0 ** -0.5
        with contextlib.ExitStack() as st:
            cqn = P.sb(st, "cqn", [128, 3, c.TT], BF16)
            ckvn = P.sb(st, "ckvn", [128, 2, c.TT], BF16)
            krr = P.sb(st, "krr", [32, c.TT], BF16)
            rp = P.sb(st, "rp", [32, 2, c.TT], F32)
            wq = P.sb(st, "wq", [128, 3, 1024], BF16)
            wk = P.sb(st, "wk", [128, 2, 1024], BF16)
            wv = P.sb(st, "wv", [128, 2, 512], BF16)
            qT = P.sb(st, "qT", [96, 8, c.TT], BF16)
            kT = P.sb(st, "kT", [96, 8, c.TT], BF16)
            va = P.sb(st, "va", [128, NT, 8, 66], BF16)
            PT = [P.sb(st, "PT%d" % i, [128, NT, 512], BF16) for i in range(2)]
            t1 = P.sb(st, "t1", [32, 512], F32)
            t2 = P.sb(st, "t2", [32, 512], F32)
            alat = P.sb(st, "alat", [128, 4, 512], BF16)
            rden = P.sb(st, "rden", [128, 1], F32)
            mo = [P.sb(st, "mo%d" % i, [128, 4, 512], BF16) for i in range(1)]
            idb = P.sb(st, "idb", [128, 128], BF16)
            P.copy("dve", idb[:], self.ident[:], [self.ident], [idb])
            P.dma("sp", cqn[:], self.cqn_d[:].rearrange("(q p) t -> p q t", p=128), self.cqn_d, cqn)
            P.dma("sp", ckvn[:], self.ckvn_d[:].rearrange("(q p) t -> p q t", p=128), self.ckvn_d, ckvn)
            P.dma("sp", krr[:], self.krr_d[:], self.krr_d, krr)
            P.dma("sp", rp[:], self.rope[:].rearrange("a d t -> d a t"), self.rope, rp)
            P.dma("pool", wq[:], self.w_uq[j].rearrange("(k p) n -> p k n", p=128), self.w_uq, wq)
            P.dma("pool", wk[:], self.w_uk[j].rearrange("(k p) n -> p k n", p=128), self.w_uk, wk)
            P.dma("pool", wv[:], self.w_uv[j].rearrange("(k p) n -> p k n", p=128), self.w_uv, wv)
            P.memset("pool", va[:], 1.0, [va])
            ic = 0
            dbg = getattr(c, "dbg", ())
            for (s0, n, isc) in (c.blocks if "mla_noproj" not in dbg else []):
                for h in range(8):
                    pa, pb = self.psb[ic % 4], self.psb[(ic + 1) % 4]
                    pk = self.psb[(ic + 2) % 4]
                    ic += 3
                    if "skq" in dbg:
                        continue
                    for k in range(3):
                        P.mm(pa, pa[:96, :n], wq[:, k, h * 128:h * 128 + 96], cqn[:, k, s0:s0 + n], [wq, cqn], start=(k == 0), stop=(k == 2))
                    for k in range(3):
                        P.mm(pb, pb[:32, :n], wq[:, k, h * 128 + 96:h * 128 + 128], cqn[:, k, s0:s0 + n], [wq, cqn], start=(k == 0), stop=(k == 2))
                    P.copy("act", qT[:, h, s0:s0 + n], pa[:96, :n], [pa], [qT])
                    P.tt("dve", t1[:, :n], pa[:32, :n], rp[:, 0, s0:s0 + n], ALU.mult, [pa, rp], [t1])
                    P.tt("dve", t2[:, :n], pb[:32, :n], rp[:, 1, s0:s0 + n], ALU.mult, [pb, rp], [t2])
                    P.tt("dve" if "mla_dve" in dbg else "pool", qT[0:32, h, s0:s0 + n], t1[:, :n], t2[:, :n], ALU.add, [t1, t2], [qT])
                    if "skk" in dbg:
                        continue
                    for k in range(2):
                        P.mm(pk, pk[:96, :n], wk[:, k, h * 128:h * 128 + 96], ckvn[:, k, s0:s0 + n], [wk, ckvn], start=(k == 0), stop=(k == 1))
                    P.copy("act", kT[:, h, s0:s0 + n], pk[:96, :n], [pk], [kT])
                    P.copy("dve" if "mla_dve" in dbg else "pool", kT[0:32, h, s0:s0 + n], krr[:, s0:s0 + n], [krr], [kT])
                for t0 in (range(0, n, 128) if "skv" not in dbg else []):
                    a0 = s0 + t0
                    ps = self.psb[ic % 4]
                    ic += 1
                    for k in range(2):
                        P.mm(ps, ps[:, :512], ckvn[:, k, a0:a0 + 128], wv[:, k, :], [ckvn, wv], start=(k == 0), stop=(k == 1))
                    P.copy("act", va[:, a0 // 128, :, 0:64], ps[:, :512].rearrange("p (h d) -> p h d", d=64), [ps], [va])
            qblocks = c.blocks if need_ctx else c.lat_blocks
            if "mla_noattn" in dbg:
                qblocks = []
            items = [(blk, h) for blk in qblocks for h in range(8)]
            alats = [alat, P.sb(st, "alat2", [128, 4, 512], BF16)]

            def qk_ops(i):
                (s0, n, isc), h = items[i]
                kts = list(range(c.T // 128, NT)) if isc else list(range(NT))
                pt_ = PT[i % 2]
                ops = []
                for ki, kt in enumerate(kts):
                    def f(ki=ki, kt=kt):
                        ps = self.psb[ki % 2]
                        P.mm(ps, ps[:, :n], kT[:, h, kt * 128:(kt + 1) * 128], qT[:, h, s0:s0 + n], [kT, qT])
                        P.act(pt_[:, ki, :n], ps[:, :n], AF.Exp, [ps], [pt_], scale=scale)
                    ops.append(f)
                return ops

            def pv_ops(i):
                (s0, n, isc), h = items[i]
                kts = list(range(c.T // 128, NT)) if isc else list(range(NT))
                pt_ = PT[i % 2]
                bidx = qblocks.index((s0, n, isc))
                al = alats[bidx % 2]
                ops = []
                for qs in range(n // 128):
                    def f(qs=qs):
                        po = self.psb[2 + qs % 2]
                        for ki, kt in enumerate(kts):
                            P.mm(po, po[:, 0:65], pt_[:, ki, qs * 128:(qs + 1) * 128], va[:, kt, h, 0:65], [pt_, va],
                                 start=(ki == 0), stop=(ki == len(kts) - 1))
                        P.op("dve", lambda: nc.vector.reciprocal(rden[:], po[:, 64:65]), reads=[po], writes=[rden])
                        P.ts("dve", al[:, qs, h * 64:(h + 1) * 64], po[:, 0:64], rden[:, 0:1], None, ALU.mult, None,
                             [po, rden], [al])
                    ops.append(f)
                if h == 7:
                    def g():
                        m_ = mo[0]
                        for qs in range(n // 128):
                            for fc in range(4):
                                ptb = self.psb[4 + (qs * 4 + fc) % 2]
                                pv = ptb[:].bitcast(BF16)
                                P.op("pe", lambda: nc.tensor.transpose(pv[:, 0:128], al[:, qs, fc * 128:(fc + 1) * 128], idb[:]),
                                     reads=[al, idb], writes=[ptb])
                                P.copy("dve", m_[:, fc, qs * 128:(qs + 1) * 128], pv[:, 0:128], [ptb], [m_])
                        P.dma("sp", self.mix_d[0:512, s0:s0 + n].rearrange("(q p) t -> p q t", p=128), m_[:, :, :n], m_, self.mix_d)
                    ops.append(g)
                return ops

            if items:
                for f in qk_ops(0):
                    f()
            for i in range(len(items)):
                nxt = qk_ops(i + 1) if i + 1 < len(items) else []
                pv = pv_ops(i)
                tot = len(nxt) + len(pv)
                a_, b_ = 0, 0
                for t_ in range(tot):
                    if b_ < len(pv) and (a_ >= len(nxt) or (b_ + 1) * (len(nxt) + 1) <= (a_ + 1) * (len(pv) + 1) - 0):
                        pv[b_]()
                        b_ += 1
                    else:
                        nxt[a_]()
                        a_ += 1
            P.barrier()

    def ab_gla(self, l, b):
        P, nc, c = self.P, self.nc, self.cfg
        j = l // 2
        need_ctx = ctx_later(l)
        NT = c.TT // 128
        NCH = c.TT // 32
        lat_tiles = list(range(c.T // 128))
        ctx_tiles = list(range(c.T // 128, NT))
        with contextlib.ExitStack() as st:
            vg = P.sb(st, "vg", [128, NT, 512], BF16)
            oacc = P.sb(st, "oacc", [128, NT, 512], F32)
            smask = P.sb(st, "smask", [128, c.TT], F32)
            tri = P.sb(st, "tri", [128, 2, 128], F32)
            oml = P.sb(st, "oml", [128, 2, 4], F32)
            qh = P.sb(st, "qh", [128, c.TT], F32)
            hf = P.sb(st, "hf", [128, c.TT], F32)
            kin = P.sb(st, "kin", [128, c.TT], F32)
            G = P.sb(st, "G", [128, c.TT], F32)
            eG_s = [P.sb(st, "eG%d" % i, [128, c.TT], F32) for i in range(2)]
            w1 = P.sb(st, "w1", [128, c.TT], F32)
            qe_s = [P.sb(st, "qe%d" % i, [128, c.TT], BF16) for i in range(2)]
            ke_s = [P.sb(st, "ke%d" % i, [128, c.TT], BF16) for i in range(2)]
            kd_s = [P.sb(st, "kd%d" % i, [128, c.TT], BF16) for i in range(2)]
            kdt = [P.sb(st, "kdt%d" % i, [128, 4, 128], BF16) for i in range(2)]
            qeb = [P.sb(st, "qeb%d" % i, [128, 4, 128], BF16) for i in range(2)]
            bm = P.sb(st, "bm", [128, 4], F32)
            cm = P.sb(st, "cm", [128, 4, 128], BF16)
            atm = [P.sb(st, "atm%d" % i, [128, 128], BF16) for i in range(2)]
            S = [P.sb(st, "S%d" % i, [128, 128], F32) for i in range(2)]
            Sb = [P.sb(st, "Sb%d" % i, [128, 128], BF16) for i in range(10)]
            idb = P.sb(st, "idb", [128, 128], BF16)
            gnb = P.sb(st, "gnb", [128, 128], F32)
            sgt = [P.sb(st, "sgt%d" % i, [128, 512], F32) for i in range(2)]
            ssq = P.sb(st, "ssq", [128, 4], F32)
            junk = P.sb(st, "junk", [128, 128], F32)
            yt = P.sb(st, "yt", [128, 512], F32)
            blat = P.sb(st, "blat", [128, 512], BF16)
            mo = [P.sb(st, "mo%d" % i, [128, 4, 128], BF16) for i in range(2)]
            P.copy("dve", idb[:], self.ident[:], [self.ident], [idb])
            P.op("dve", lambda: nc.vector.reduce_sum(out=bm[:], in_=self.ident[:].rearrange("p (c l) -> p c l", l=32), axis=AX.X),
                 reads=[self.ident], writes=[bm])
            P.memset("pool", cm[:], 0.0, [cm])
            for cc in range(4):
                P.memset("pool", cm[:, cc, cc * 32:(cc + 1) * 32], 1.0, [cm])
            P.dma("sp", vg[:], self.vg_d[:].rearrange("(t p) f -> p t f", p=128), self.vg_d, vg)
            P.dma("sp", tri[:], self.trimask[:].rearrange("a s t -> s a t"), self.trimask, tri)
            P.dma("sp", gnb[:], self.gnorm[j].partition_broadcast(128), self.gnorm, gnb)
            P.memset("pool", smask[:], 1.0, [smask])
            P.memset("pool", smask[:].rearrange("p (c l) -> p c l", l=32)[:, :, 0:1], 0.0, [smask])
            if j == 0:
                P.memset("dve", oml[:], 1.0, [oml])
            else:
                for d in range(2):
                    P.tt("dve", oml[:, d, :], self.vec("lbl0_%d" % d), self.vec("lbl1_%d" % d), ALU.subtract, [self.vecs], [oml])
                P.act(oml[:], oml[:], AF.Sigmoid, [oml], [oml])
            chains = [(h, d) for h in range(4) for d in range(2)]

            def gate_ops(ci):
                h, d = chains[ci]
                eG, qe, ke, kd = eG_s[ci % 2], qe_s[ci % 2], ke_s[ci % 2], kd_s[ci % 2]
                G3 = G[:].rearrange("p (c l) -> p c l", l=32)
                gend = G3[:, :, 31:32] if d == 0 else G3[:, :, 0:1]
                ops = []
                if d == 0:
                    ops.append(lambda: P.dma("sp", qh[:], self.qh_d[h * 128:(h + 1) * 128, :], self.qh_d, qh))
                ops.append(lambda: P.dma("sp", hf[:], self.hf_d[d, h * 128:(h + 1) * 128, :], self.hf_d, hf))
                ops.append(lambda: P.act(kin[:], hf[:], AF.Sigmoid, [hf], [kin], scale=-1.0))
                ops.append(lambda: P.ts("dve", kin[:], kin[:], oml[:, d, h:h + 1], None, ALU.mult, None, [kin, oml], [kin]))
                ops.append(lambda: P.act(w1[:], kin[:], AF.Ln, [kin], [w1], scale=-1.0, bias=1.0))
                if d == 0:
                    ops.append(lambda: P.op("dve", lambda: nc.vector.tensor_tensor_scan(out=G[:], data0=smask[:], data1=w1[:], initial=0.0,
                                                                                         op0=ALU.mult, op1=ALU.add), reads=[smask, w1], writes=[G]))
                else:
                    ops.append(lambda: P.op("dve", lambda: nc.vector.tensor_tensor_scan(out=G[:, ::-1], data0=smask[:], data1=w1[:, ::-1], initial=0.0,
                                                                                         op0=ALU.mult, op1=ALU.add), reads=[smask, w1], writes=[G]))
                ops.append(lambda: P.act(eG[:], G[:], AF.Exp, [G], [eG]))
                ops.append(lambda: P.tt("pool", qe[:], qh[:], eG[:], ALU.mult, [qh, eG], [qe]))
                ops.append(lambda: P.tt("dve", w1[:].rearrange("p (c l) -> p c l", l=32), gend.to_broadcast([128, NCH, 32]), G3, ALU.subtract, [G], [w1]))
                ops.append(lambda: P.act(w1[:], w1[:], AF.Exp, [w1], [w1]))
                ops.append(lambda: P.tt("pool", kd[:], kin[:], w1[:], ALU.mult, [kin, w1], [kd]))
                ops.append(lambda: P.act(w1[:], G[:], AF.Exp, [G], [w1], scale=-1.0))
                ops.append(lambda: P.tt("pool", ke[:], kin[:], w1[:], ALU.mult, [kin, w1], [ke]))
                return ops

            pending = gate_ops(0)
            for ci, (h, d) in enumerate(chains):
                if True:
                    for f_ in pending:
                        f_()
                    pending = gate_ops(ci + 1) if ci + 1 < len(chains) else []
                    eG, qe, ke, kd = eG_s[ci % 2], qe_s[ci % 2], ke_s[ci % 2], kd_s[ci % 2]
                    eG3 = eG[:].rearrange("p (c l) -> p c l", l=32)
                    order = (ctx_tiles + lat_tiles) if d == 0 else (ctx_tiles[::-1] + lat_tiles[::-1])
                    if "gla_noscan" in getattr(c, "dbg", ()):
                        order = []
                    P.memset("dve", S[0][:], 0.0, [S[0]])
                    P.memset("pool", Sb[0][:], 0.0, [Sb[0]])
                    st_ = {"s": 0, "sb": 0}
                    crange = list(range(4)) if d == 0 else list(range(3, -1, -1))

                    def front(ti, tl):
                        a0 = tl * 128
                        skip_out = (tl in ctx_tiles) and not need_ctx
                        kt_, am_, qb_ = kdt[ti % 2], atm[ti % 2], qeb[ti % 2]
                        pk = self.psb[ti % 2]
                        pkv = pk[:].bitcast(BF16)
                        P.op("pe", lambda: nc.tensor.transpose(pkv[:, 0:128], kd[:, a0:a0 + 128], idb[:]), reads=[kd, idb], writes=[pk])
                        P.tt("dve", kt_[:], pkv[:, 0:128].unsqueeze(1).to_broadcast([128, 4, 128]),
                             bm[:].unsqueeze(2).to_broadcast([128, 4, 128]), ALU.mult, [pk, bm], [kt_])
                        po = self.psb[2 + ti % 2]
                        if not skip_out:
                            P.tt("pool", qb_[:], qe[:, a0:a0 + 128].unsqueeze(1).to_broadcast([128, 4, 128]), cm[:], ALU.mult, [qe, cm], [qb_])
                            pa = self.psb[4 + ti % 2]
                            P.mm(pa, pa[:, 0:128], ke[:, a0:a0 + 128], qe[:, a0:a0 + 128], [ke, qe])
                            P.tt("dve", am_[:], pa[:, 0:128], tri[:, d, :], ALU.mult, [pa, tri], [am_])
                            P.mm(po, po[:, 0:128], am_[:], vg[:, tl, h * 128:(h + 1) * 128], [am_, vg], start=True, stop=False)
                        pd = self.psb[6 + ti % 2]
                        for cc in crange:
                            P.mm(pd, pd[:, cc * 128:(cc + 1) * 128], kt_[:, cc, :], vg[:, tl, h * 128:(h + 1) * 128], [kt_, vg])

                    def back(ti, tl):
                        skip_out = (tl in ctx_tiles) and not need_ctx
                        qb_ = qeb[ti % 2]
                        po = self.psb[2 + ti % 2]
                        pd = self.psb[6 + ti % 2]
                        sbs = []
                        for cc in crange:
                            sbs.append(Sb[st_["sb"]])
                            chn = tl * 4 + cc
                            eg = eG3[:, chn, 31:32] if d == 0 else eG3[:, chn, 0:1]
                            so, sn = S[st_["s"]], S[1 - st_["s"]]
                            P.stt(sn[:], so[:], eg, pd[:, cc * 128:(cc + 1) * 128], ALU.mult, ALU.add, [so, eG, pd], [sn])
                            st_["s"] = 1 - st_["s"]
                            st_["sb"] = (st_["sb"] + 1) % len(Sb)
                            P.copy("act", Sb[st_["sb"]][:], sn[:], [sn], [Sb[st_["sb"]]])
                        if not skip_out:
                            for ci, cc in enumerate(crange):
                                P.mm(po, po[:, 0:128], qb_[:, cc, :], sbs[ci][:], [qb_, sbs[ci]], start=False, stop=(ci == 3))
                            if d == 0:
                                P.copy("act", oacc[:, tl, h * 128:(h + 1) * 128], po[:, 0:128], [po], [oacc])
                            else:
                                P.tt("dve", oacc[:, tl, h * 128:(h + 1) * 128], po[:, 0:128], oacc[:, tl, h * 128:(h + 1) * 128],
                                     ALU.add, [po, oacc], [oacc])

                    if order:
                        front(0, order[0])
                    for ti, tl in enumerate(order):
                        if ti + 1 < len(order):
                            front(ti + 1, order[ti + 1])
                        back(ti, tl)
                        if ti >= 1 and pending:
                            pending.pop(0)()
            tiles = (lat_tiles + ctx_tiles) if need_ctx else lat_tiles
            if "gla_noread" in getattr(c, "dbg", ()):
                tiles = []
            for ti, tl in enumerate(tiles):
                a0 = tl * 128
                sg_ = sgt[ti % 2]
                P.dma("sp", sg_[:], self.sg_d[a0:a0 + 128, :], self.sg_d, sg_)
                for h in range(4):
                    P.act(junk[:], oacc[:, tl, h * 128:(h + 1) * 128], AF.Square, [oacc], [junk, ssq], accum_out=ssq[:, h:h + 1])
                P.act(ssq[:], ssq[:], AF.Sqrt, [ssq, self.eps_t], [ssq], bias=self.eps_t[:], scale=1.0 / 128)
                P.op("dve", lambda: nc.vector.reciprocal(ssq[:], ssq[:]), reads=[ssq], writes=[ssq])
                for h in range(4):
                    P.stt(yt[:, h * 128:(h + 1) * 128], oacc[:, tl, h * 128:(h + 1) * 128], ssq[:, h:h + 1], gnb[:], ALU.mult, ALU.mult,
                          [oacc, ssq, gnb], [yt])
                P.tt("dve", blat[:], yt[:], sg_[:], ALU.mult, [yt, sg_], [blat])
                m_ = mo[ti % 2]
                for fc in range(4):
                    ptb = self.psb[fc % 2]
                    pv = ptb[:].bitcast(BF16)
                    P.op("pe", lambda: nc.tensor.transpose(pv[:, 0:128], blat[:, fc * 128:(fc + 1) * 128], idb[:]), reads=[blat, idb], writes=[ptb])
                    P.copy("act", m_[:, fc, :], pv[:, 0:128], [ptb], [m_])
                P.dma("sp", self.mix_d[512:1024, a0:a0 + 128].rearrange("(q p) t -> p q t", p=128), m_[:], m_, self.mix_d)
            P.barrier()

    def ab_outproj(self, l, b):
        P, nc, c = self.P, self.nc, self.cfg
        j = l // 2
        need_ctx = ctx_later(l)
        src = self.cur
        dst = self.next_h()
        blocks = c.blocks if need_ctx else c.lat_blocks
        with contextlib.ExitStack() as st:
            wo = P.sb(st, "wo", [128, 8, 1024], BF16)
            mx = [P.sb(st, "mx%d" % i, [128, 8, 512], BF16) for i in range(2)]
            hb = [P.sb(st, "hb%d" % i, [128, 8, 512], F32) for i in range(2)]
            P.dma("pool", wo[:], self.w_out[j].rearrange("(k p) n -> p k n", p=128), self.w_out, wo)
            def load_blk(bi):
                (s0, n, isc) = blocks[bi]
                m_, h_ = mx[bi % 2], hb[bi % 2]
                P.dma("sp", m_[:, :, :n], self.mix_d[:, s0:s0 + n].rearrange("(q p) t -> p q t", p=128), self.mix_d, m_)
                for k in range(8):
                    P.dma("sp", h_[:, k, :n], src[b, k * 128:(k + 1) * 128, s0:s0 + n], src, h_)

            load_blk(0)
            for bi, (s0, n, isc) in enumerate(blocks):
                cnd = c.BL if isc else b
                m_, h_ = mx[bi % 2], hb[bi % 2]
                if bi + 1 < len(blocks):
                    load_blk(bi + 1)
                for dch in range(8):
                    ps = self.psb[dch % 4]
                    for k in range(8):
                        P.mm(ps, ps[:, :n], wo[:, k, dch * 128:(dch + 1) * 128], m_[:, k, :n], [wo, m_], start=(k == 0), stop=(k == 7))
                    P.stt(h_[:, dch, :n], ps[:, :n], self.mod(l, 2, cnd)[:, dch:dch + 1], h_[:, dch, :n], ALU.mult, ALU.add,
                          [ps, self.modT, h_], [h_])
                for k in range(8):
                    P.dma("sp", dst[b, k * 128:(k + 1) * 128, s0:s0 + n], h_[:, k, :n], h_, dst)
            P.barrier()


def shared_inputs(inp, cfg):
    c = cfg
    sh = {}
    sh["vecs"] = build_vecs(inp)
    sh["w_mod"] = np.ascontiguousarray(inp["w_mod"], np.float32)
    sh["ffn_wg"] = np.ascontiguousarray(inp["ffn_w_gate"], np.float32)
    sh["ffn_wu"] = np.ascontiguousarray(inp["ffn_w_up"], np.float32)
    sh["ffn_wd"] = np.ascontiguousarray(inp["ffn_w_down"], np.float32)
    sh["moe_wg"] = np.ascontiguousarray(inp["moe_w_gate"], np.float32)
    sh["moe_wu"] = np.ascontiguousarray(inp["moe_w_up"], np.float32)
    sh["moe_wd"] = np.ascontiguousarray(inp["moe_w_down"], np.float32)
    sh["router"] = np.ascontiguousarray(
        np.asarray(inp["moe_router"], np.float32).reshape(2, c.KC, 128, c.NE).transpose(0, 2, 1, 3))
    sh["pool_w"] = np.ascontiguousarray(inp["pool_w"], np.float32)
    nmax = max(c.T, c.C)
    ic = np.ones((2, 4, nmax), np.float32)
    for si, n in enumerate((c.T, c.C)):
        pos = np.arange(n)
        for g, w in enumerate(POOL_WINDOWS):
            lo = np.clip(pos - w // 2, 0, n)
            hi = np.clip(pos - w // 2 + w, 0, n)
            ic[si, g, :n] = 1.0 / (hi - lo).astype(np.float32)
    sh["invcnt"] = ic
    sh["ident"] = np.eye(128, dtype=np.float32)
    w_in = np.asarray(inp["ab_w_in"], np.float32)
    swp = np.arange(32).reshape(2, 2, 8)[:, ::-1, :].reshape(32)
    o_kr = 384 + 256
    kr = w_in[:, :, o_kr:o_kr + 32]
    sh["w_in"] = np.ascontiguousarray(np.concatenate(
        [w_in[:, :, :o_kr + 32], kr[:, :, swp], w_in[:, :, o_kr + 32:]], axis=2))
    w_uq = np.asarray(inp["ab_w_uq"], np.float32).reshape(2, 384, 8, 96)
    nope, ropq = w_uq[..., :64], w_uq[..., 64:]
    sh["w_uq"] = np.ascontiguousarray(np.concatenate([ropq, nope, ropq[..., swp]], axis=3).reshape(2, 384, 1024))
    w_ukv = np.asarray(inp["ab_w_ukv"], np.float32).reshape(2, 256, 8, 128)
    z = np.zeros((2, 256, 8, 32), np.float32)
    sh["w_uk"] = np.ascontiguousarray(np.concatenate([z, w_ukv[..., :64], z], axis=3).reshape(2, 256, 1024))
    sh["w_uv"] = np.ascontiguousarray(w_ukv[..., 64:].reshape(2, 256, 512))
    sh["w_out"] = np.ascontiguousarray(inp["ab_w_out"], np.float32)
    sh["gnorm"] = np.ascontiguousarray(inp["hgrn_g_norm"], np.float32)
    tpos = np.arange(c.T)
    row = (tpos // c.grid_w).astype(np.float32)
    col = (tpos % c.grid_w).astype(np.float32)
    inv = (1.0 / (np.float32(10000.0) ** (np.arange(0, 16, 2, dtype=np.float32) / np.float32(16)))).astype(np.float32)
    ar = row[:, None] * inv[None, :]
    ac = col[:, None] * inv[None, :]
    ang = np.concatenate([ar, ar, ac, ac], axis=1).astype(np.float32)
    sign = np.tile(np.concatenate([-np.ones(8, np.float32), np.ones(8, np.float32)]), 2)
    rope = np.zeros((2, 32, c.TT), np.float32)
    rope[0, :, :c.T] = np.cos(ang).T
    rope[1, :, :c.T] = (np.sin(ang) * sign[None, :]).T
    rope[0, :, c.T:] = 1.0
    sh["rope"] = rope
    ii = np.arange(128)
    same = (ii[:, None] // 32) == (ii[None, :] // 32)
    tm = np.zeros((2, 128, 128), np.float32)
    tm[0] = (same & (ii[:, None] <= ii[None, :]))
    tm[1] = (same & (ii[:, None] >= ii[None, :]))
    sh["trimask"] = tm
    return sh


def core_inputs(inp, cfg, b0):
    c = cfg
    x = np.asarray(inp["x"], np.float32)[b0:b0 + c.BL]
    cx = np.asarray(inp["ctx"], np.float32)[b0:b0 + c.BL]
    xT = np.ascontiguousarray(np.concatenate([x.transpose(0, 2, 1), cx.transpose(0, 2, 1)], axis=2))
    cv = np.concatenate([np.asarray(inp["c"], np.float32)[b0:b0 + c.BL], np.asarray(inp["c_ctx"], np.float32)[None, :]], axis=0)
    cond = np.ascontiguousarray(cv.reshape(c.NB, c.KC, 128).transpose(2, 1, 0))
    return {"xT": xT, "cond": cond}


def kernel(**inp):
    cfg = Cfg()
    bld = Builder(cfg)
    nc = bld.build()
    sh = shared_inputs(inp, cfg)
    names = bld.input_names()
    in_maps = []
    for core in range(8):
        m = dict(sh)
        m.update(core_inputs(inp, cfg, core * cfg.BL))
        in_maps.append({k: m[k] for k in names})
    res = run_bass_kernel_spmd(nc, in_maps, core_ids=list(range(8)))
    outs = []
    for core in range(8):
        oT = np.asarray(res.results[core]["outT"])
        outs.append(oT.transpose(0, 2, 1))
    return np.ascontiguousarray(np.concatenate(outs, axis=0).astype(np.float32))
```
